# Optimizing a Trainium2 kernel written in Bass

```python
import jax
import jax.numpy as jnp
from jax import lax

D_MODEL = 1024
BATCH = 8
SEQ = 4096
DEPTH = 2

CTX_LEN = 256
GRID_W = 64
D_MIX = D_MODEL

CONV_W = D_MIX // 4
ATT_HD = 64
ATT_Q_HEADS = (D_MIX // 2) // ATT_HD
ATT_KV_HEADS = ATT_Q_HEADS // 4
ATT_GROUP = ATT_Q_HEADS // ATT_KV_HEADS
ROPE_AXIS_DIM = ATT_HD // 2
ROPE_THETA = 10000.0
Q_BLOCK = 128
RW_HD = 64
RW = D_MIX // 4
RW_HEADS = RW // RW_HD
DECAY_LORA = 64
AAA_LORA = 64
GATE_LORA = 160
N_DIR = 2
GN_EPS = 64e-5
CONV_COLS = 3 * CONV_W
ATT_COLS = (ATT_Q_HEADS + 2 * ATT_KV_HEADS) * ATT_HD
RW_COLS = 3 * RW + N_DIR * (DECAY_LORA + AAA_LORA) + GATE_LORA
PROJ_COLS = CONV_COLS + ATT_COLS + RW_COLS
N_EXPERTS = 16
CAPACITY_FACTOR = 2
D_EXPERT = D_MODEL
N_MOD = 6
NORM_EPS = 1e-6

kernel_name = 'hybrid_conv_gqa_rwkv7_ec_moe_dit'


def rms_norm(x, g):
    xf = x.astype(jnp.float32)
    y = xf * lax.rsqrt(jnp.mean(xf * xf, axis=-1, keepdims=True) + NORM_EPS)
    return (y * g.astype(jnp.float32)).astype(x.dtype)


def adaln_params(cvec, w, b):
    m = (jax.nn.silu(cvec) @ w + b).reshape(cvec.shape[0], 1, N_MOD, D_MODEL)
    return [m[:, :, i] for i in range(N_MOD)]


def modulate(x, g, shift, scale):
    return rms_norm(x, g) * (1 + scale) + shift


def shift_prev(z):
    return jnp.pad(z, ((0, 0), (1, 0), (0, 0)))[:, :-1]


def shift_next(z):
    return jnp.pad(z, ((0, 0), (0, 1), (0, 0)))[:, 1:]


def short_conv_mixer(p_cv, conv_w):
    b_gate, c_gate, u = jnp.split(p_cv, 3, axis=-1)
    z = c_gate * u
    z = conv_w[0] * shift_prev(z) + conv_w[1] * z + conv_w[2] * shift_next(z)
    return b_gate * z


def attn_qkv(p_at, q_g, k_g):
    bsz, n, _ = p_at.shape
    q, k, v = jnp.split(p_at, [ATT_Q_HEADS * ATT_HD, (ATT_Q_HEADS + ATT_KV_HEADS) * ATT_HD], axis=-1)
    q = rms_norm(q.reshape(bsz, n, ATT_Q_HEADS, ATT_HD), q_g)
    k = rms_norm(k.reshape(bsz, n, ATT_KV_HEADS, ATT_HD), k_g)
    v = v.reshape(bsz, n, ATT_KV_HEADS, ATT_HD)
    return q, k, v


def axial_rope(n):
    rows = n // GRID_W
    row = jnp.repeat(jnp.arange(rows, dtype=jnp.float32), GRID_W)
    col = jnp.tile(jnp.arange(GRID_W, dtype=jnp.float32), rows)
    inv = ROPE_THETA ** (-jnp.arange(0, ROPE_AXIS_DIM, 2, dtype=jnp.float32) / ROPE_AXIS_DIM)
    ang = jnp.concatenate([row[:, None] * inv, col[:, None] * inv], axis=-1)
    return jnp.cos(ang)[:, None, :], jnp.sin(ang)[:, None, :]


def apply_rope(x, cos, sin):
    xf = x.astype(jnp.float32)
    x1, x2 = xf[..., :ATT_HD // 2], xf[..., ATT_HD // 2:]
    return jnp.concatenate([x1 * cos - x2 * sin, x1 * sin + x2 * cos], axis=-1).astype(x.dtype)


def gqa_attend(q, k, v):
    bsz, nq = q.shape[0], q.shape[1]
    qg = q.reshape(bsz, nq, ATT_KV_HEADS, ATT_GROUP, ATT_HD)
    s = jnp.einsum('bqkgd,bskd->bkgqs', qg, k).astype(jnp.float32) * (ATT_HD ** -0.5)
    p = jax.nn.softmax(s, axis=-1).astype(v.dtype)
    o = jnp.einsum('bkgqs,bskd->bqkgd', p, v)
    return o.reshape(bsz, nq, ATT_Q_HEADS * ATT_HD)


def latent_attention(q, k_lat, v_lat, k_ctx, v_ctx):
    k = jnp.concatenate([k_lat, k_ctx], axis=1)
    v = jnp.concatenate([v_lat, v_ctx], axis=1)
    bsz, n = q.shape[0], q.shape[1]
    nb = n // Q_BLOCK
    qb = jnp.moveaxis(q.reshape(bsz, nb, Q_BLOCK, ATT_Q_HEADS, ATT_HD), 1, 0)
    o = lax.map(lambda q_blk: gqa_attend(q_blk, k, v), qb)
    return jnp.moveaxis(o, 0, 1).reshape(bsz, n, ATT_Q_HEADS * ATT_HD)


def rwkv_features(p, mu, w0, w_b, a0, a_b, g_b, k_k, k_a):
    bsz, n, _ = p.shape
    p = p + mu * (0.5 * (shift_prev(p) + shift_next(p)) - p)
    splits = [RW, 2 * RW, 3 * RW, 3 * RW + N_DIR * DECAY_LORA, 3 * RW + N_DIR * (DECAY_LORA + AAA_LORA)]
    r, k, v, wl, al, gl = jnp.split(p, splits, axis=-1)
    wl = wl.reshape(bsz, n, N_DIR, DECAY_LORA)
    al = al.reshape(bsz, n, N_DIR, AAA_LORA)
    w_log = -jax.nn.softplus(-(w0 + jnp.einsum('bndr,drc->bndc', jnp.tanh(wl), w_b))) - 0.5
    decay = jnp.exp(-jnp.exp(w_log.astype(jnp.float32)))
    a = jax.nn.sigmoid(a0 + jnp.einsum('bndr,drc->bndc', al, a_b))
    kk = (k * k_k).reshape(bsz, n, RW_HEADS, RW_HD).astype(jnp.float32)
    kk = kk * lax.rsqrt(jnp.maximum(jnp.sum(kk * kk, axis=-1, keepdims=True), 1e-24))
    k_dir = k[:, :, None] * (1 + (a - 1) * k_a)
    b_dir = kk[:, :, None] * a.reshape(bsz, n, N_DIR, RW_HEADS, RW_HD)
    g = jax.nn.sigmoid(gl) @ g_b
    heads = lambda t: t.reshape(t.shape[:-1] + (RW_HEADS, RW_HD))
    return heads(r), heads(decay), heads(k_dir), heads(v), kk, b_dir, g


def wkv_scan(r, decay, k, v, a_vec, b_vec, s0, reverse):
    def step(s, inp):
        r_t, w_t, k_t, v_t, a_t, b_t = inp
        sa = jnp.einsum('bhvk,bhk->bhv', s, a_t)
        s = s * w_t[:, :, None, :] + sa[..., None] * b_t[:, :, None, :] + v_t[..., None] * k_t[:, :, None, :]
        return s, jnp.einsum('bhvk,bhk->bhv', s, r_t)
    xs = tuple(jnp.moveaxis(t.astype(jnp.float32), 1, 0) for t in (r, decay, k, v, a_vec, b_vec))
    s_final, ys = lax.scan(step, s0, xs, reverse=reverse)
    return s_final, jnp.moveaxis(ys, 0, 1)


def rwkv_out(y, r, k_dir, v, g, r_k, ln_w, ln_b):
    bsz, n = y.shape[0], y.shape[1]
    mean = jnp.mean(y, axis=-1, keepdims=True)
    var = jnp.mean(jnp.square(y - mean), axis=-1, keepdims=True)
    yn = (y - mean) * lax.rsqrt(var + GN_EPS)
    k_mean = 0.5 * (k_dir[:, :, 0] + k_dir[:, :, 1])
    bonus = jnp.sum(r * k_mean * r_k, axis=-1, keepdims=True) * v
    out = yn.reshape(bsz, n, RW) * ln_w + ln_b + bonus.reshape(bsz, n, RW)
    return (out * g).astype(g.dtype)


def rwkv_mixer(p_lat, p_ctx, mu, w0, w_b, a0, a_b, g_b, k_k, k_a, r_k, ln_w, ln_b, with_ctx_out):
    r_l, dec_l, k_l, v_l, kk_l, b_l, g_l = rwkv_features(p_lat, mu, w0, w_b, a0, a_b, g_b, k_k, k_a)
    r_c, dec_c, k_c, v_c, kk_c, b_c, g_c = rwkv_features(p_ctx, mu, w0, w_b, a0, a_b, g_b, k_k, k_a)
    s0 = jnp.zeros((p_lat.shape[0], RW_HEADS, RW_HD, RW_HD), jnp.float32)
    ys_l, ys_c = [], []
    for d in range(N_DIR):
        rev = d == 1
        s_c, y_c = wkv_scan(r_c, dec_c[:, :, d], k_c[:, :, d], v_c, -kk_c, b_c[:, :, d], s0, rev)
        _, y_l = wkv_scan(r_l, dec_l[:, :, d], k_l[:, :, d], v_l, -kk_l, b_l[:, :, d], s_c, rev)
        ys_l.append(y_l)
        ys_c.append(y_c)
    out_l = rwkv_out(ys_l[0] + ys_l[1], r_l, k_l, v_l, g_l, r_k, ln_w, ln_b)
    if not with_ctx_out:
        return out_l, None
    out_c = rwkv_out(ys_c[0] + ys_c[1], r_c, k_c, v_c, g_c, r_k, ln_w, ln_b)
    return out_l, out_c


def token_mixers(p_lat, p_ctx, conv_w, q_g, k_g, rw_mu, rw_w0, rw_w_b, rw_a0, rw_a_b, rw_g_b,
                 rw_k_k, rw_k_a, rw_r_k, rw_ln_w, rw_ln_b, with_ctx_out):
    split = [CONV_COLS, CONV_COLS + ATT_COLS]
    cv_l, at_l, rw_l = jnp.split(p_lat, split, axis=-1)
    cv_c, at_c, rw_c = jnp.split(p_ctx, split, axis=-1)
    conv_l = short_conv_mixer(cv_l, conv_w)
    q_l, k_l, v_l = attn_qkv(at_l, q_g, k_g)
    q_c, k_c, v_c = attn_qkv(at_c, q_g, k_g)
    cos, sin = axial_rope(p_lat.shape[1])
    att_l = latent_attention(apply_rope(q_l, cos, sin), apply_rope(k_l, cos, sin), v_l, k_c, v_c)
    rwkv_l, rwkv_c = rwkv_mixer(rw_l, rw_c, rw_mu, rw_w0, rw_w_b, rw_a0, rw_a_b, rw_g_b, rw_k_k, rw_k_a,
                                rw_r_k, rw_ln_w, rw_ln_b, with_ctx_out)
    mix_l = jnp.concatenate([conv_l, att_l, rwkv_l], axis=-1)
    if not with_ctx_out:
        return mix_l, None
    mix_c = jnp.concatenate([short_conv_mixer(cv_c, conv_w), gqa_attend(q_c, k_c, v_c), rwkv_c], axis=-1)
    return mix_l, mix_c


def expert_choice_ffn(h, router_w, w_gate, w_up, w_down):
    bsz, n, _ = h.shape
    cap = CAPACITY_FACTOR * n // N_EXPERTS
    aff = jax.nn.softmax((h @ router_w).astype(jnp.float32), axis=-1)
    gate, idx = lax.top_k(jnp.swapaxes(aff, 1, 2), cap)
    bidx = jnp.arange(bsz)[:, None, None]
    xs = h[bidx, idx]
    hid = jax.nn.silu(jnp.einsum('becd,edf->becf', xs, w_gate)) * jnp.einsum('becd,edf->becf', xs, w_up)
    y = jnp.einsum('becf,efd->becd', hid, w_down) * gate[..., None].astype(h.dtype)
    return jnp.zeros_like(h).at[bidx, idx].add(y)


def setup_inputs(seed: int = 0) -> dict:
    key = jax.random.key(seed)
    ks = jax.random.split(key, 28)
    f32 = jnp.float32
    nrm = lambda k, shape, s: jax.random.normal(k, shape, f32) * s
    uni = lambda k, shape: jax.random.uniform(k, shape, f32)
    L = DEPTH
    return {
        'x': nrm(ks[0], (BATCH, SEQ, D_MODEL), 1.0),
        'c': nrm(ks[1], (BATCH, D_MODEL), 1.0),
        'ctx': nrm(ks[2], (BATCH, CTX_LEN, D_MODEL), 1.0),
        'c_ctx': nrm(ks[3], (D_MODEL,), 1.0),
        'ada_w': nrm(ks[4], (L, D_MODEL, N_MOD * D_MODEL), 0.5 * D_MODEL ** -0.5),
        'ada_b': nrm(ks[5], (L, N_MOD * D_MODEL), 0.02),
        'norm1_g': 1.0 + nrm(ks[6], (L, D_MODEL), 0.02),
        'norm2_g': 1.0 + nrm(ks[7], (L, D_MODEL), 0.02),
        'w_in': nrm(ks[8], (L, D_MODEL, PROJ_COLS), D_MODEL ** -0.5),
        'w_out': nrm(ks[9], (L, D_MIX, D_MODEL), D_MIX ** -0.5),
        'conv_w': nrm(ks[10], (L, 3, CONV_W), 3 ** -0.5),
        'q_norm_g': 1.0 + nrm(ks[11], (L, ATT_HD), 0.02),
        'k_norm_g': 1.0 + nrm(ks[12], (L, ATT_HD), 0.02),
        'rw_mu': uni(ks[13], (L, RW_COLS)),
        'rw_w0': -6.0 + 5.0 * uni(ks[14], (L, N_DIR, RW)),
        'rw_w_b': nrm(ks[15], (L, N_DIR, DECAY_LORA, RW), 0.1),
        'rw_a0': nrm(ks[16], (L, N_DIR, RW), 0.1),
        'rw_a_b': nrm(ks[17], (L, N_DIR, AAA_LORA, RW), 0.1),
        'rw_g_b': nrm(ks[18], (L, GATE_LORA, RW), GATE_LORA ** -0.5),
        'rw_k_k': 0.85 + nrm(ks[19], (L, RW), 0.05),
        'rw_k_a': 1.0 + nrm(ks[20], (L, RW), 0.05),
        'rw_r_k': nrm(ks[21], (L, RW_HEADS, RW_HD), 0.1),
        'rw_ln_w': 1.0 + nrm(ks[22], (L, RW), 0.02),
        'rw_ln_b': nrm(ks[23], (L, RW), 0.02),
        'router_w': nrm(ks[24], (L, D_MODEL, N_EXPERTS), D_MODEL ** -0.5),
        'exp_w_gate': nrm(ks[25], (L, N_EXPERTS, D_MODEL, D_EXPERT), D_MODEL ** -0.5),
        'exp_w_up': nrm(ks[26], (L, N_EXPERTS, D_MODEL, D_EXPERT), D_MODEL ** -0.5),
        'exp_w_down': nrm(ks[27], (L, N_EXPERTS, D_EXPERT, D_MODEL), D_EXPERT ** -0.5),
    }


def reference(x, c, ctx, c_ctx, ada_w, ada_b, norm1_g, norm2_g, w_in, w_out, conv_w, q_norm_g, k_norm_g,
              rw_mu, rw_w0, rw_w_b, rw_a0, rw_a_b, rw_g_b, rw_k_k, rw_k_a, rw_r_k, rw_ln_w, rw_ln_b,
              router_w, exp_w_gate, exp_w_up, exp_w_down):
    xc = ctx
    for l in range(DEPTH):
        update_ctx = l < DEPTH - 1
        sh1, sc1, gt1, sh2, sc2, gt2 = adaln_params(c, ada_w[l], ada_b[l])
        csh1, csc1, cgt1, csh2, csc2, cgt2 = adaln_params(c_ctx[None], ada_w[l], ada_b[l])
        p_l = modulate(x, norm1_g[l], sh1, sc1) @ w_in[l]
        p_c = modulate(xc, norm1_g[l], csh1, csc1) @ w_in[l]
        mix_l, mix_c = token_mixers(p_l, p_c, conv_w[l], q_norm_g[l], k_norm_g[l], rw_mu[l], rw_w0[l],
                                    rw_w_b[l], rw_a0[l], rw_a_b[l], rw_g_b[l], rw_k_k[l], rw_k_a[l],
                                    rw_r_k[l], rw_ln_w[l], rw_ln_b[l], update_ctx)
        x = x + gt1 * (mix_l @ w_out[l])
        x = x + gt2 * expert_choice_ffn(modulate(x, norm2_g[l], sh2, sc2), router_w[l],
                                        exp_w_gate[l], exp_w_up[l], exp_w_down[l])
        if update_ctx:
            xc = xc + cgt1 * (mix_c @ w_out[l])
            xc = xc + cgt2 * expert_choice_ffn(modulate(xc, norm2_g[l], csh2, csc2), router_w[l],
                                               exp_w_gate[l], exp_w_up[l], exp_w_down[l])
    return x
```

```python
import math
from contextlib import ExitStack

import numpy as np
import concourse.bass as bass
import concourse.mybir as mybir
from concourse.bass_utils import run_bass_kernel_spmd

F32 = mybir.dt.float32
BF16 = mybir.dt.bfloat16
I32 = mybir.dt.int32
U32 = mybir.dt.uint32
AF = mybir.ActivationFunctionType
ALU = mybir.AluOpType
AX = mybir.AxisListType

ENGS = ("sync", "act", "dve", "pool", "pe")

D = 1024
SEQ = 4096
CTX = 256
NT = SEQ + CTX
NTILE = NT // 128
PROJ = 2720
NE = 16
CAP_L = 512
CAP_C = 32
LCH = 64


class Prog:
    SEMID = 0

    def __init__(self, nc):
        self.nc = nc
        self.streams = {e: [] for e in ENGS}
        self.ecount = {e: 0 for e in ENGS}
        self.seen = {e: {} for e in ENGS}
        self.bufs = {}
        self.dcount = {}
        self.sems = {}

    @staticmethod
    def _k(b):
        if isinstance(b, (str, tuple, int)):
            return b
        return b.name

    def _deps(self, eng, reads, writes):
        need = {}

        def add(ev):
            for k, v in ev.items():
                if need.get(k, 0) < v:
                    need[k] = v

        for b in reads:
            st = self.bufs.get(b)
            if st:
                add(st["w"])
        for b in writes:
            st = self.bufs.get(b)
            if st:
                add(st["w"])
                add(st["r"])
        waits = []
        seen = self.seen[eng]
        for k, v in need.items():
            if k[0] == "e" and k[1] == eng and eng == "pe":
                continue
            if seen.get(k, 0) >= v:
                continue
            seen[k] = v
            waits.append((k, v))
        return waits

    def _commit(self, reads, writes, ev):
        for b in reads:
            st = self.bufs.setdefault(b, {"w": {}, "r": {}})
            for k, v in ev.items():
                if st["r"].get(k, 0) < v:
                    st["r"][k] = v
        for b in writes:
            self.bufs[b] = {"w": dict(ev), "r": {}}

    EPOCH = 3000

    def op(self, eng, fn, r=(), w=(), rows=None):
        r = [self._k(b) for b in r]
        w = [self._k(b) for b in w]
        bk = [k for k in r if isinstance(k, tuple) and k[0] in ("pqb", "sqb")]
        if bk:
            r = [k for k in r if k not in bk]
            w = w + bk
        waits = self._deps(eng, r, w)
        self.ecount[eng] += 1
        ep = (self.ecount[eng] - 1) // self.EPOCH
        ek = ("e", eng, ep)
        ev = {ek: self.ecount[eng] - ep * self.EPOCH}
        if eng == "pe":
            if not hasattr(self, "pe_rows"):
                self.pe_rows = {}
            for bk_ in w:
                last = self.pe_rows.get(bk_)
                if last is not None and rows in (0, 64) and last[0] in (0, 64) and last[0] != rows:
                    for k_, v_ in last[1].items():
                        if self.seen[eng].get(k_, 0) < v_:
                            self.seen[eng][k_] = v_
                            waits.append((k_, v_))
                self.pe_rows[bk_] = (rows, ev)
        self.streams[eng].append((waits, fn, (ek, 1)))
        self._commit(r, w, ev)

    def dma(self, q, out, in_, r=(), w=(), sem=None, **kw):
        r = [self._k(b) for b in r]
        w = [self._k(b) for b in w]
        if sem is None:
            sem = (w[0] if w else r[0])
        waits = self._deps(q, r, w)
        k = ("d", sem)
        self.dcount[k] = self.dcount.get(k, 0) + 16
        ev = {k: self.dcount[k]}
        self.streams[q].append((waits, (lambda e: e.dma_start(out=out, in_=in_, **kw)), (k, 16)))
        self._commit(r, w, ev)

    def dma_fn(self, q, fn, r=(), w=(), sem=None):
        r = [self._k(b) for b in r]
        w = [self._k(b) for b in w]
        waits = self._deps(q, r, w)
        k = ("d", sem)
        self.dcount[k] = self.dcount.get(k, 0) + 16
        ev = {k: self.dcount[k]}
        self.streams[q].append((waits, fn, (k, 16)))
        self._commit(r, w, ev)

    def barrier(self):
        for eng in ENGS:
            waits = [(k, v) for k, v in self.dcount.items()]
            for e in ENGS:
                if e != eng and self.ecount[e] > 0:
                    ep = (self.ecount[e] - 1) // self.EPOCH
                    waits.append((("e", e, ep), self.ecount[e] - ep * self.EPOCH))
            self.streams[eng].append((waits, None, None))

    POOL = None

    def emit(self):
        nc = self.nc
        pool = Prog.POOL
        totals = {}
        keys = []
        for e in ENGS:
            for waits, fn, inc in self.streams[e]:
                for k, v in waits:
                    if k not in totals:
                        totals[k] = 0
                        keys.append(k)
                if inc:
                    if inc[0] not in totals:
                        totals[inc[0]] = 0
                        keys.append(inc[0])
                    totals[inc[0]] += inc[1]
        n = len(pool["h"])
        assert len(keys) <= n, len(keys)
        base = {}
        for i, k in enumerate(sorted(keys, key=str)):
            idx = (pool["next"] + i) % n
            self.sems[k] = pool["h"][idx]
            base[k] = pool["v"][idx]
            pool["v"][idx] += totals[k]
        pool["next"] = (pool["next"] + len(keys)) % n
        with ExitStack() as es:
            block = es.enter_context(nc.Block())
            handles = {"sync": block.sync, "act": block.scalar, "dve": block.vector,
                       "pool": block.gpsimd, "pe": block.tensor}
            for e in ENGS:
                stream = self.streams[e]

                def body(h, stream=stream):
                    for waits, fn, inc in stream:
                        for k, v in waits:
                            h.wait_ge(self.sems[k], base[k] + v)
                        if fn is not None:
                            ins = fn(h)
                            ins.then_inc(self.sems[inc[0]], inc[1])

                handles[e](body)


class Ctx:
    pass


def build(n_layers=2, dbg=None, upto=None, skip=(), small=False, rwsteps=None, zero_mix=False, layers=None):
    dbg = dbg or set()
    nc = bass.Bass("TRN2", target_bir_lowering=False)
    g = Ctx()
    g.nc = nc
    if layers is None:
        layers = list(range(n_layers))
    NL = len(layers)
    g.first = layers[0]
    g.last = layers[-1]
    g.wi = lambda l: l - layers[0]
    if layers[-1] == 0:
        dbg = set(dbg) | {"xcres"}

    def din(name, shape, dt=F32):
        return nc.dram_tensor(name, list(shape), dt, kind="ExternalInput").ap()

    def dscr(name, shape, dt=F32):
        kind = "ExternalOutput" if name in dbg else "Internal"
        return nc.dram_tensor(name, list(shape), dt, kind=kind).ap()

    I = Ctx()
    I.x = din("x", [SEQ, D])
    I.ctx = din("ctx", [CTX, D])
    I.c2T = din("c2T", [128, 8, 2])
    I.ada_w = din("ada_w", [NL, D, 6 * D])
    I.ada_b = din("ada_b", [NL, 1, 6 * D])
    I.n1g = din("norm1_g", [NL, 1, D])
    I.n2g = din("norm2_g", [NL, 1, D])
    I.w_in = din("w_in", [NL, D, PROJ])
    I.w_out = din("w_out", [NL, D, D])
    I.conv_wT = din("conv_wT", [NL, 256, 3])
    I.qg = din("q_norm_g", [NL, 1, 64])
    I.kg = din("k_norm_g", [NL, 1, 64])
    I.mu = din("rw_mu", [NL, 1184, 1])
    I.w0 = din("rw_w0", [NL, 512, 1])
    I.w_b = din("rw_w_b", [NL, 128, 256])
    I.a0 = din("rw_a0", [NL, 512, 1])
    I.a_b = din("rw_a_b", [NL, 128, 256])
    I.g_b = din("rw_g_b", [NL, 160, 256])
    I.k_k = din("rw_k_k", [NL, 256, 1])
    I.k_a = din("rw_k_a", [NL, 256, 1])
    I.r_k = din("rw_r_k", [NL, 256, 1])
    I.ln_w = din("rw_ln_w", [NL, 256, 1])
    I.ln_b = din("rw_ln_b", [NL, 256, 1])
    I.router = din("router_w", [NL, D, NE])
    esh = [NL, 1, 8, 8] if small else [NL, NE, D, D]
    I.wg = din("exp_w_gate", esh)
    I.wu = din("exp_w_up", esh)
    I.wd = din("exp_w_down", esh)
    g.rwsteps = rwsteps
    I.cs = din("cs_tab", [SEQ, 64])
    I.consts = din("consts", [128, 8 * 128])
    out = nc.dram_tensor("out", [SEQ, D], F32, kind="ExternalOutput").ap()

    S = Ctx()
    S.modv = dscr("modv", [NL, 2, 6, D])
    S.pfm = dscr("pfm", [1952, NT])
    S.qT = dscr("qT", [8, 64, NT], BF16)
    S.mixT = dscr("mixT", [D, NT], BF16)
    S.xres = dscr("xres", [SEQ, D])
    S.xcres = dscr("xcres", [CTX, D])
    S.h2l = dscr("h2l", [SEQ, D], BF16)
    S.h2c = dscr("h2c", [CTX, D], BF16)
    S.rwf = dscr("rwf", [10, 256, NT])
    g.I, g.S, g.out = I, S, out

    with ExitStack() as gs:
        def gsb(name, shape, dt=F32):
            return gs.enter_context(nc.sbuf_tensor("g_" + name, list(shape), dt))
        Prog.POOL = {"h": [gs.enter_context(nc.semaphore("gp%d" % i)) for i in range(72)], "v": [0] * 72, "next": 0}
        g.consts = gsb("consts", [128, 8, 128])
        g.identb = gsb("identb", [128, 128], BF16)
        g.kT = gsb("kT", [64, 2, NT], BF16)
        g.Vaug = gsb("Vaug", [128, NTILE, 2, 65], BF16)
        g.affT = gsb("affT", [NE, NT])
        phase_consts(g)
        phases = [phase_adaln, phase_proj, phase_conv, phase_attn, phase_rwfeat, phase_rwscan] + ([phase_zero_mix] if zero_mix else []) + [phase_wout, phase_moe]
        for l in layers:
            for ph in phases:
                if ph.__name__ in skip:
                    continue
                ph(g, l)
                if upto == (ph.__name__, l):
                    return nc
    return nc


C_ID, C_BONES, C_MS_IT, C_MI_IT, C_MS_TI, C_RESET, C_MS_IT_B, C_MI_IT_B = range(8)


def make_consts():
    c = np.zeros((8, 128, 128), np.float32)
    i = np.arange(128)[:, None]
    t = np.arange(128)[None, :]
    same = (i // 64) == (t // 64)
    c[C_ID] = np.eye(128)
    c[C_BONES] = same
    c[C_MS_IT] = same & (i < t)
    c[C_MI_IT] = same & (i <= t)
    c[C_MS_TI] = same & (t < i)
    c[C_RESET] = (t % 64 != 0) * np.ones((128, 1))
    c[C_MS_IT_B] = same & (i > t)
    c[C_MI_IT_B] = same & (i >= t)
    return np.ascontiguousarray(c.transpose(1, 0, 2).reshape(128, 8 * 128))


def phase_consts(g):
    nc = g.nc
    P = Prog(nc)
    P.dma("sync", g.consts[:], g.I.consts.rearrange("p (a b) -> p a b", a=8), w=[g.consts])
    P.op("dve", lambda e: e.tensor_copy(out=g.identb[:], in_=g.consts[:, C_ID, :]), r=[g.consts], w=[g.identb])
    P.op("pool", lambda e: e.memset(g.Vaug[:, :, :, 64:65], 1.0), w=[g.Vaug])
    P.barrier()
    P.emit()


def phase_adaln(g, l):
    nc, I, S = g.nc, g.I, g.S
    with ExitStack() as es:
        def sb(name, shape, dt=F32):
            return es.enter_context(nc.sbuf_tensor("a%d_" % l + name, list(shape), dt))
        c2 = sb("c2", [128, 8, 2])
        sc = sb("sc", [128, 8, 2])
        wt = [sb("wt%d" % i, [128, 8, 512]) for i in range(2)]
        bias = sb("bias", [2, 6 * D])
        mod = sb("mod", [2, 6 * D])
        gg = sb("gg", [2, 2, D])
        mv = sb("mv", [2, 6, D])
        ps = [es.enter_context(nc.psum_tensor("a%d_ps%d" % (l, i), [2, 512], F32)) for i in range(2)]
        P = Prog(nc)
        P.dma("sync", c2[:], I.c2T[:, :, :], w=[c2])
        P.dma("sync", bias[:], I.ada_b[g.wi(l), 0:1, :].to_broadcast([2, 6 * D]), w=[bias])
        P.dma("sync", gg[:, 0, :], I.n1g[g.wi(l), 0:1, :].to_broadcast([2, D]), w=[gg], sem="gg")
        P.dma("sync", gg[:, 1, :], I.n2g[g.wi(l), 0:1, :].to_broadcast([2, D]), w=[gg], sem="gg")
        P.op("act", lambda e: e.activation(out=sc[:], in_=c2[:], func=AF.Silu), r=[c2], w=[sc])
        wv = I.ada_w[g.wi(l)].rearrange("(kc p) n -> p kc n", p=128)
        for cc in range(12):
            b = cc % 2
            P.dma("sync" if b == 0 else "pool", wt[b][:], wv[:, :, cc * 512:(cc + 1) * 512], w=[wt[b]])
            for kc in range(8):
                P.op("pe", lambda e, kc=kc, b=b: e.matmul(out=ps[b][:], lhsT=sc[:, kc, :], rhs=wt[b][:, kc, :],
                                                           start=(kc == 0), stop=(kc == 7)),
                     r=[sc, wt[b]], w=[ps[b]])
            P.op("dve", lambda e, cc=cc, b=b: e.tensor_tensor(out=mod[:, cc * 512:(cc + 1) * 512], in0=ps[b][:],
                                                               in1=bias[:, cc * 512:(cc + 1) * 512], op=ALU.add),
                 r=[ps[b], bias], w=[mod])
        for j, (sci, shi, gti) in enumerate(((1, 0, 2), (4, 3, 5))):
            P.op("dve", lambda e, j=j, sci=sci: e.scalar_tensor_tensor(
                out=mv[:, 3 * j, :], in0=mod[:, sci * D:(sci + 1) * D], scalar=1.0, in1=gg[:, j, :],
                op0=ALU.add, op1=ALU.mult), r=[mod, gg], w=[mv])
            P.op("dve", lambda e, j=j, shi=shi: e.tensor_copy(out=mv[:, 3 * j + 1, :], in_=mod[:, shi * D:(shi + 1) * D]),
                 r=[mod], w=[mv])
            P.op("dve", lambda e, j=j, gti=gti: e.tensor_copy(out=mv[:, 3 * j + 2, :], in_=mod[:, gti * D:(gti + 1) * D]),
                 r=[mod], w=[mv])
        P.dma("sync", S.modv[g.wi(l)], mv[:], r=[mv], sem="mvst")
        P.barrier()
        P.emit()


def phase_proj(g, l):
    nc, I, S = g.nc, g.I, g.S
    with ExitStack() as es:
        def sb(name, shape, dt=F32):
            return es.enter_context(nc.sbuf_tensor("b%d_" % l + name, list(shape), dt))

        def psb(name, shape, dt=F32):
            return es.enter_context(nc.psum_tensor("b%d_" % l + name, list(shape), dt))
        wb = sb("wb", [128, 8, PROJ], BF16)
        m1 = [sb("m1_%d" % i, [128, D]) for i in range(2)]
        sh1 = [sb("sh1_%d" % i, [128, D]) for i in range(2)]
        qkg = sb("qkg", [128, 2, 64])
        cs = sb("cs", [128, 32, 64])
        xt = [sb("xt%d" % i, [128, D]) for i in range(2)]
        junk = sb("junk", [128, D])
        ss = [sb("ss%d" % i, [128, 1]) for i in range(2)]
        hb = [sb("hb%d" % i, [128, D], BF16) for i in range(2)]
        hT = [sb("hT%d" % i, [128, 8, 512], BF16) for i in range(2)]
        fm = [sb("fm%d" % i, [128, 512]) for i in range(3)]
        qkv = [sb("qkv%d" % i, [128, 768]) for i in range(2)]
        sq = sb("sq", [128, 640])
        ssq = sb("ssq", [128, 10])
        qn = sb("qn", [128, 10, 64])
        qr = sb("qr", [128, 10, 64])
        qrb = [sb("qrb%d" % i, [128, 10, 64], BF16) for i in range(2)]
        tmp = sb("tmp", [128, 10, 32])
        qTs = [sb("qTs%d" % i, [64, 8, 128], BF16) for i in range(2)]
        pT = [psb("pT%d" % i, [128, 8, 128], BF16) for i in range(2)]
        pF = [psb("pF%d" % i, [128, 512]) for i in range(2)]
        pA = psb("pA", [128, 512])
        pB = psb("pB", [128, 512])
        pQ = psb("pQ", [64, 8, 128], BF16)
        pQk = psb("pQk", [64, 8, 128], BF16)
        P = Prog(nc)
        wv = I.w_in[g.wi(l)].rearrange("(kc p) n -> p kc n", p=128)
        for (c0, c1) in ((0, 1024), (1024, 2048), (2048, PROJ)):
            P.dma("pool", wb[:, :, c0:c1], wv[:, :, c0:c1], w=[("wb", c0)], sem=("wb", c0))
        wbk = [("wb", 0), ("wb", 1024), ("wb", 2048)]
        for j in range(2):
            P.dma("sync", m1[j][:], S.modv[g.wi(l), j, 0:1, :].to_broadcast([128, D]), w=[m1[j]])
            P.dma("sync", sh1[j][:], S.modv[g.wi(l), j, 1:2, :].to_broadcast([128, D]), w=[sh1[j]])
        P.dma("sync", qkg[:, 0, :], I.qg[g.wi(l), 0:1, :].to_broadcast([128, 64]), w=[qkg], sem="qkg")
        P.dma("sync", qkg[:, 1, :], I.kg[g.wi(l), 0:1, :].to_broadcast([128, 64]), w=[qkg], sem="qkg")
        P.dma("sync", cs[:], I.cs.rearrange("(i p) c -> p i c", p=128), w=[cs])
        fchunks = [(c, 128, c) for c in range(0, 768, 128)]
        for j in range(10):
            c = 1536 + j * 128
            wdt = min(128, PROJ - c)
            fchunks.append((c, wdt, 768 + j * 128))
        sts = [(0, 2)] + [(2 + 4 * s, 4) for s in range(8)]
        fmi = 0
        for si, (t0, ntile) in enumerate(sts):
            hTs = hT[si % 2]
            ntok = ntile * 128
            for ti in range(ntile):
                i = t0 + ti
                b = i % 2
                j = 1 if i < 2 else 0
                if i < 2:
                    src = (I.ctx if l == g.first else S.xcres)[i * 128:(i + 1) * 128, :]
                else:
                    src = (I.x if l == g.first else S.xres)[(i - 2) * 128:(i - 1) * 128, :]
                P.dma("sync", xt[b][:], src, w=[xt[b]])
                P.op("act", lambda e, b=b: e.activation(out=junk[:], in_=xt[b][:], func=AF.Square, accum_out=ss[b][:]),
                     r=[xt[b]], w=[junk, ss[b]])
                P.op("dve", lambda e, b=b: e.tensor_scalar(out=ss[b][:], in0=ss[b][:], scalar1=1.0 / D, scalar2=1e-6,
                                                            op0=ALU.mult, op1=ALU.add), r=[ss[b]], w=[ss[b]])
                P.op("act", lambda e, b=b: e.activation(out=ss[b][:], in_=ss[b][:], func=AF.Sqrt), r=[ss[b]], w=[ss[b]])
                P.op("dve", lambda e, b=b: e.reciprocal(out=ss[b][:], in_=ss[b][:]), r=[ss[b]], w=[ss[b]])
                P.op("dve", lambda e, b=b, j=j: e.scalar_tensor_tensor(out=xt[b][:], in0=xt[b][:], scalar=ss[b][:, 0:1],
                                                                       in1=m1[j][:], op0=ALU.mult, op1=ALU.mult),
                     r=[xt[b], ss[b], m1[j]], w=[xt[b]])
                P.op("pool", lambda e, b=b, j=j: e.tensor_tensor(out=hb[b][:], in0=xt[b][:], in1=sh1[j][:], op=ALU.add),
                     r=[xt[b], sh1[j]], w=[hb[b]])
                for half in range(2):
                    for k4 in range(4):
                        kc = half * 4 + k4
                        P.op("pe", lambda e, b=b, kc=kc, k4=k4, half=half: e.transpose(
                            out=pT[half][:, k4, :], in_=hb[b][:, kc * 128:(kc + 1) * 128], identity=g.identb[:]),
                            r=[hb[b], g.identb], w=[pT[half]])
                    eng = "act" if half == 0 else "dve"
                    if eng == "act":
                        P.op("act", lambda e, half=half, ti=ti, hTs=hTs: e.activation(
                            out=hTs[:, half * 4:(half + 1) * 4, ti * 128:(ti + 1) * 128], in_=pT[half][:, 0:4, :], func=AF.Copy),
                            r=[pT[half]], w=[hTs])
                    else:
                        P.op("dve", lambda e, half=half, ti=ti, hTs=hTs: e.tensor_copy(
                            out=hTs[:, half * 4:(half + 1) * 4, ti * 128:(ti + 1) * 128], in_=pT[half][:, 0:4, :]),
                            r=[pT[half]], w=[hTs])
                for kc in range(8):
                    P.op("pe", lambda e, kc=kc, ti=ti, hTs=hTs: e.matmul(
                        out=pA[:], lhsT=hTs[:, kc, ti * 128:(ti + 1) * 128], rhs=wb[:, kc, 768:1280],
                        start=(kc == 0), stop=(kc == 7)), r=[hTs] + wbk, w=[pA])
                for kc in range(8):
                    P.op("pe", lambda e, kc=kc, ti=ti, hTs=hTs: e.matmul(
                        out=pB[:, 0:256], lhsT=hTs[:, kc, ti * 128:(ti + 1) * 128], rhs=wb[:, kc, 1280:1536],
                        start=(kc == 0), stop=(kc == 7)), r=[hTs] + wbk, w=[pB])
                qv = qkv[b]
                P.op("act", lambda e, qv=qv: e.activation(out=qv[:, 0:512], in_=pA[:], func=AF.Copy), r=[pA], w=[qv])
                P.op("act", lambda e, qv=qv: e.activation(out=qv[:, 512:768], in_=pB[:, 0:256], func=AF.Copy), r=[pB], w=[qv])
                P.op("pool", lambda e, qv=qv, i=i: e.tensor_copy(
                    out=g.Vaug[:, i, :, 0:64], in_=qv[:, 640:768].rearrange("p (g d) -> p g d", g=2)),
                    r=[qv], w=[("Vaug", i)])
                P.op("dve", lambda e, qv=qv: e.tensor_tensor(out=sq[:], in0=qv[:, 0:640], in1=qv[:, 0:640], op=ALU.mult),
                     r=[qv], w=[sq])
                P.op("dve", lambda e: e.tensor_reduce(out=ssq[:], in_=sq[:].rearrange("p (h d) -> p h d", h=10),
                                                       axis=AX.X, op=ALU.add), r=[sq], w=[ssq])
                P.op("dve", lambda e: e.tensor_scalar(out=ssq[:], in0=ssq[:], scalar1=1.0 / 64, scalar2=1e-6,
                                                       op0=ALU.mult, op1=ALU.add), r=[ssq], w=[ssq])
                P.op("act", lambda e: e.activation(out=ssq[:], in_=ssq[:], func=AF.Sqrt), r=[ssq], w=[ssq])
                P.op("dve", lambda e: e.reciprocal(out=ssq[:], in_=ssq[:]), r=[ssq], w=[ssq])
                P.op("dve", lambda e, qv=qv: e.tensor_tensor(
                    out=qn[:], in0=qv[:, 0:640].rearrange("p (h d) -> p h d", h=10),
                    in1=ssq[:].unsqueeze(2).to_broadcast([128, 10, 64]), op=ALU.mult), r=[qv, ssq], w=[qn])
                P.op("dve", lambda e: e.tensor_tensor(out=qn[:, 0:8, :], in0=qn[:, 0:8, :],
                                                       in1=qkg[:, 0:1, :].to_broadcast([128, 8, 64]), op=ALU.mult),
                     r=[qn, qkg], w=[qn])
                P.op("dve", lambda e: e.tensor_tensor(out=qn[:, 8:10, :], in0=qn[:, 8:10, :],
                                                       in1=qkg[:, 1:2, :].to_broadcast([128, 2, 64]), op=ALU.mult),
                     r=[qn, qkg], w=[qn])
                qb = qrb[b]
                if i < 2:
                    P.op("dve", lambda e, qb=qb: e.tensor_copy(out=qb[:], in_=qn[:]), r=[qn], w=[qb])
                else:
                    li = i - 2
                    cosb = cs[:, li:li + 1, 0:32].to_broadcast([128, 10, 32])
                    sinb = cs[:, li:li + 1, 32:64].to_broadcast([128, 10, 32])
                    x1 = qn[:, :, 0:32]
                    x2 = qn[:, :, 32:64]
                    P.op("dve", lambda e, cosb=cosb: e.tensor_tensor(out=qr[:, :, 0:32], in0=qn[:, :, 0:32], in1=cosb, op=ALU.mult),
                         r=[qn, cs], w=[qr])
                    P.op("dve", lambda e, sinb=sinb: e.tensor_tensor(out=tmp[:], in0=qn[:, :, 32:64], in1=sinb, op=ALU.mult),
                         r=[qn, cs], w=[tmp])
                    P.op("dve", lambda e, qb=qb: e.tensor_tensor(out=qb[:, :, 0:32], in0=qr[:, :, 0:32], in1=tmp[:], op=ALU.subtract),
                         r=[qr, tmp], w=[qb])
                    P.op("dve", lambda e, sinb=sinb: e.tensor_tensor(out=qr[:, :, 32:64], in0=qn[:, :, 0:32], in1=sinb, op=ALU.mult),
                         r=[qn, cs], w=[qr])
                    P.op("dve", lambda e, cosb=cosb: e.tensor_tensor(out=tmp[:], in0=qn[:, :, 32:64], in1=cosb, op=ALU.mult),
                         r=[qn, cs, qb], w=[tmp])
                    P.op("dve", lambda e, qb=qb: e.tensor_tensor(out=qb[:, :, 32:64], in0=qr[:, :, 32:64], in1=tmp[:], op=ALU.add),
                         r=[qr, tmp], w=[qb])
                for h in range(8):
                    P.op("pe", lambda e, h=h, qb=qb: e.transpose(out=pQ[:, h, :], in_=qb[:, h, :], identity=g.identb[:]),
                         r=[qb, g.identb], w=[pQ])
                for h in range(2):
                    P.op("pe", lambda e, h=h, qb=qb: e.transpose(out=pQk[:, h, :], in_=qb[:, 8 + h, :], identity=g.identb[:]),
                         r=[qb, g.identb], w=[pQk])
                qs = qTs[b]
                P.op("act", lambda e, qs=qs: e.activation(out=qs[:], in_=pQ[:], func=AF.Copy), r=[pQ], w=[qs])
                P.op("dve", lambda e, i=i: e.tensor_copy(out=g.kT[:, :, i * 128:(i + 1) * 128], in_=pQk[:, 0:2, :]),
                     r=[pQk], w=[("kT", i)])
                P.dma("pool", S.qT[:, :, i * 128:(i + 1) * 128].rearrange("h d t -> d h t"), qs[:], r=[qs], sem=("qs", b))
            for (c0, wdt, r0) in fchunks:
                pb = pF[fmi % 2]
                fb = fm[fmi % 3]
                for kc in range(8):
                    P.op("pe", lambda e, kc=kc, c0=c0, wdt=wdt, pb=pb, hTs=hTs, ntok=ntok: e.matmul(
                        out=pb[0:wdt, 0:ntok], lhsT=wb[:, kc, c0:c0 + wdt], rhs=hTs[:, kc, 0:ntok],
                        start=(kc == 0), stop=(kc == 7)), r=[hTs] + wbk, w=[pb])
                if fmi % 2 == 0:
                    P.op("act", lambda e, wdt=wdt, pb=pb, fb=fb, ntok=ntok: e.activation(
                        out=fb[0:wdt, 0:ntok], in_=pb[0:wdt, 0:ntok], func=AF.Copy), r=[pb], w=[fb])
                else:
                    P.op("dve", lambda e, wdt=wdt, pb=pb, fb=fb, ntok=ntok: e.tensor_copy(
                        out=fb[0:wdt, 0:ntok], in_=pb[0:wdt, 0:ntok]), r=[pb], w=[fb])
                P.dma("sync", S.pfm[r0:r0 + wdt, t0 * 128:t0 * 128 + ntok], fb[0:wdt, 0:ntok], r=[fb], sem=("fm", fmi % 3))
                fmi += 1
        P.barrier()
        P.emit()


def phase_conv(g, l):
    nc, I, S = g.nc, g.I, g.S
    with ExitStack() as es:
        def sb(name, shape, dt=F32):
            return es.enter_context(nc.sbuf_tensor("c%d_" % l + name, list(shape), dt))
        Bt = sb("Bt", [128, SEQ])
        Ct = sb("Ct", [128, SEQ])
        Ut = sb("Ut", [128, SEQ])
        zp = sb("zp", [128, SEQ + 2])
        acc = sb("acc", [128, SEQ])
        ob = sb("ob", [128, SEQ], BF16)
        cw = sb("cw", [128, 2, 3])
        P = Prog(nc)
        P.dma("sync", cw[:], I.conv_wT[g.wi(l)].rearrange("(c p) k -> p c k", p=128), w=[cw])
        seqs = [(CTX, SEQ)] + ([(0, CTX)] if l == 0 else [])
        for (t0, T) in seqs:
            for cc in range(2):
                P.dma("sync", Bt[:, 0:T], S.pfm[cc * 128:(cc + 1) * 128, t0:t0 + T], w=[Bt])
                P.dma("sync", Ct[:, 0:T], S.pfm[256 + cc * 128:256 + (cc + 1) * 128, t0:t0 + T], w=[Ct])
                P.dma("pool", Ut[:, 0:T], S.pfm[512 + cc * 128:512 + (cc + 1) * 128, t0:t0 + T], w=[Ut])
                P.op("pool", lambda e, T=T: e.memset(zp[:, 0:1], 0.0), w=[zp])
                P.op("pool", lambda e, T=T: e.memset(zp[:, T + 1:T + 2], 0.0), w=[zp])
                P.op("dve", lambda e, T=T: e.tensor_tensor(out=zp[:, 1:T + 1], in0=Ct[:, 0:T], in1=Ut[:, 0:T], op=ALU.mult),
                     r=[Ct, Ut], w=[zp])
                P.op("dve", lambda e, T=T, cc=cc: e.tensor_scalar(out=acc[:, 0:T], in0=zp[:, 0:T], scalar1=cw[:, cc, 0:1],
                                                                   scalar2=None, op0=ALU.mult), r=[zp, cw], w=[acc])
                P.op("dve", lambda e, T=T, cc=cc: e.scalar_tensor_tensor(out=acc[:, 0:T], in0=zp[:, 1:T + 1], scalar=cw[:, cc, 1:2],
                                                                         in1=acc[:, 0:T], op0=ALU.mult, op1=ALU.add),
                     r=[zp, cw, acc], w=[acc])
                P.op("dve", lambda e, T=T, cc=cc: e.scalar_tensor_tensor(out=acc[:, 0:T], in0=zp[:, 2:T + 2], scalar=cw[:, cc, 2:3],
                                                                         in1=acc[:, 0:T], op0=ALU.mult, op1=ALU.add),
                     r=[zp, cw, acc], w=[acc])
                P.op("pool", lambda e, T=T: e.tensor_tensor(out=ob[:, 0:T], in0=acc[:, 0:T], in1=Bt[:, 0:T], op=ALU.mult),
                     r=[acc, Bt], w=[ob])
                P.dma("sync", S.mixT[cc * 128:(cc + 1) * 128, t0:t0 + T], ob[:, 0:T], r=[ob], sem="obst")
        P.barrier()
        P.emit()


def phase_attn(g, l):
    nc, I, S = g.nc, g.I, g.S
    with ExitStack() as es:
        def sb(name, shape, dt=F32):
            return es.enter_context(nc.sbuf_tensor("d%d_" % l + name, list(shape), dt))

        def psb(name, shape, dt=F32):
            return es.enter_context(nc.psum_tensor("d%d_" % l + name, list(shape), dt))
        qc = [sb("qc%d" % i, [64, 512], BF16) for i in range(2)]
        eS = [sb("eS%d" % i, [128, 512], BF16) for i in range(3)]
        rs = sb("rs", [128, 512])
        rsb = sb("rsb", [64, 512])
        ob = [sb("ob%d" % i, [64, 512], BF16) for i in range(2)]
        nb = sb("nb", [128, 1])
        pS = [psb("pS%d" % i, [128, 512]) for i in range(3)]
        pO = [psb("pO%d" % i, [128, 512]) for i in range(2)]
        pR = psb("pR", [64, 512])
        P = Prog(nc)
        P.op("pool", lambda e: e.memset(nb[:], -8.0), w=[nb])
        jobs = []
        if l == 0:
            for h in range(8):
                jobs.append((h, 0, CTX, [0, 1]))
        for h in range(8):
            for qi in range(8):
                jobs.append((h, CTX + qi * 512, 512, list(range(NTILE))))
        cnt = 0
        for ji, (h, q0, nq, kts) in enumerate(jobs):
            gkv = h // 4
            qb = qc[ji % 2]
            po = pO[ji % 2]
            P.dma("sync", qb[:, 0:nq], S.qT[h, :, q0:q0 + nq], w=[qb])
            for ki, kt in enumerate(kts):
                ps = pS[cnt % 3]
                ee = eS[cnt % 3]
                cnt += 1
                P.op("pe", lambda e, ps=ps, kt=kt, gkv=gkv, qb=qb, nq=nq: e.matmul(
                    out=ps[:, 0:nq], lhsT=g.kT[:, gkv, kt * 128:(kt + 1) * 128], rhs=qb[:, 0:nq], start=True, stop=True),
                    r=[qb, ("kT", kt)], w=[ps])
                P.op("act", lambda e, ps=ps, ee=ee, nq=nq: e.activation(out=ee[:, 0:nq], in_=ps[:, 0:nq], func=AF.Exp,
                                                                       bias=nb[:, 0:1], scale=0.125),
                     r=[ps, nb], w=[ee])
                P.op("pe", lambda e, po=po, kt=kt, gkv=gkv, ee=ee, nq=nq, ki=ki, nk=len(kts): e.matmul(
                    out=po[0:65, 0:nq], lhsT=g.Vaug[:, kt, gkv, :], rhs=ee[:, 0:nq], start=(ki == 0), stop=(ki == nk - 1)),
                    r=[ee, ("Vaug", kt), g.Vaug], w=[po])
            P.op("dve", lambda e, po=po, nq=nq: e.reciprocal(out=rs[64:65, 0:nq], in_=po[64:65, 0:nq]), r=[po], w=[rs])
            P.op("pe", lambda e, nq=nq: e.matmul(out=pR[:, 0:nq], lhsT=g.consts[64:65, C_BONES, 64:128], rhs=rs[64:65, 0:nq],
                                                  start=True, stop=True), r=[rs, g.consts], w=[pR])
            P.op("act", lambda e, nq=nq: e.activation(out=rsb[:, 0:nq], in_=pR[:, 0:nq], func=AF.Copy), r=[pR], w=[rsb])
            o = ob[ji % 2]
            P.op("dve", lambda e, po=po, o=o, nq=nq: e.tensor_tensor(out=o[:, 0:nq], in0=po[0:64, 0:nq], in1=rsb[:, 0:nq], op=ALU.mult),
                 r=[po, rsb], w=[o])
            P.dma("pool", S.mixT[256 + h * 64:256 + (h + 1) * 64, q0:q0 + nq], o[:, 0:nq], r=[o], sem=("ob", ji % 2))
        P.barrier()
        P.emit()


NEG_EM05 = -math.exp(-0.5)


def phase_rwfeat(g, l):
    nc, I, S = g.nc, g.I, g.S
    with ExitStack() as es:
        def sb(name, shape, dt=F32):
            return es.enter_context(nc.sbuf_tensor("e%d_" % l + name, list(shape), dt))

        def psb(name, shape, dt=F32):
            return es.enter_context(nc.psum_tensor("e%d_" % l + name, list(shape), dt))
        SEG = 512
        rch = [("r0", 768, 128), ("r1", 896, 128), ("k0", 1024, 128), ("k1", 1152, 128), ("v0", 1280, 128),
               ("v1", 1408, 128), ("wl", 1536, 128), ("al", 1664, 128), ("g0", 1792, 128), ("g1", 1920, 32)]
        mu = sb("mu", [128, 10])
        omu = sb("omu", [128, 10])
        hmu = sb("hmu", [128, 10])
        pt = [sb("pt%d" % i, [128, SEG + 2]) for i in range(3)]
        s1 = [sb("s1%d" % i, [128, SEG]) for i in range(2)]
        sh = {nm: sb("sh_" + nm, [128, SEG]) for nm, _, _ in rch}
        w0c = sb("w0c", [128, 4])
        a0c = sb("a0c", [128, 4])
        kkc = sb("kkc", [128, 2])
        kac = sb("kac", [128, 2])
        omka = sb("omka", [128, 2])
        wbt = sb("wbt", [128, 256])
        abt = sb("abt", [128, 256])
        gb0 = sb("gb0", [128, 256])
        gb1 = sb("gb1", [32, 256])
        twl = sb("twl", [128, SEG])
        sg0 = sb("sg0", [128, SEG])
        sg1 = sb("sg1", [32, SEG])
        lw = [sb("lw%d" % i, [128, SEG]) for i in range(4)]
        asg = [sb("asg%d" % i, [128, SEG]) for i in range(4)]
        kk = [sb("kk%d" % i, [128, SEG]) for i in range(2)]
        sq = sb("sq", [128, SEG])
        rn = sb("rn", [128, SEG])
        kd = [sb("kd%d" % i, [128, SEG]) for i in range(4)]
        bd = [sb("bd%d" % i, [128, SEG]) for i in range(4)]
        gt = [sb("gt%d" % i, [128, SEG]) for i in range(2)]
        tq = sb("tq", [128, SEG])
        pp = [psb("pp%d" % i, [128, SEG]) for i in range(6)]
        P = Prog(nc)
        P.op("pool", lambda e: e.memset(mu[:, 9:10], 0.0), w=[mu])
        for ci, (nm, r0, nr) in enumerate(rch):
            P.dma("sync", mu[0:nr, ci:ci + 1], I.mu[g.wi(l), r0 - 768:r0 - 768 + nr, :], w=[mu], sem="mu")
        P.op("dve", lambda e: e.tensor_scalar(out=omu[:], in0=mu[:], scalar1=-1.0, scalar2=1.0, op0=ALU.mult, op1=ALU.add),
             r=[mu], w=[omu])
        P.op("dve", lambda e: e.tensor_scalar(out=hmu[:], in0=mu[:], scalar1=0.5, scalar2=None, op0=ALU.mult), r=[mu], w=[hmu])
        for d in range(2):
            for cc in range(2):
                P.dma("sync", w0c[:, d * 2 + cc:d * 2 + cc + 1], I.w0[g.wi(l), d * 256 + cc * 128:d * 256 + (cc + 1) * 128, :], w=[w0c], sem="w0c")
                P.dma("sync", a0c[:, d * 2 + cc:d * 2 + cc + 1], I.a0[g.wi(l), d * 256 + cc * 128:d * 256 + (cc + 1) * 128, :], w=[a0c], sem="a0c")
        for cc in range(2):
            P.dma("sync", kkc[:, cc:cc + 1], I.k_k[g.wi(l), cc * 128:(cc + 1) * 128, :], w=[kkc], sem="kkc")
            P.dma("sync", kac[:, cc:cc + 1], I.k_a[g.wi(l), cc * 128:(cc + 1) * 128, :], w=[kac], sem="kac")
        P.op("dve", lambda e: e.tensor_scalar(out=omka[:], in0=kac[:], scalar1=-1.0, scalar2=1.0, op0=ALU.mult, op1=ALU.add),
             r=[kac], w=[omka])
        P.dma("sync", wbt[:], I.w_b[g.wi(l)], w=[wbt])
        P.dma("sync", abt[:], I.a_b[g.wi(l)], w=[abt])
        P.dma("sync", gb0[:], I.g_b[g.wi(l), 0:128, :], w=[gb0])
        P.dma("sync", gb1[:], I.g_b[g.wi(l), 128:160, :], w=[gb1])
        segs = [(0, 0, CTX, CTX)] + [(CTX, CTX + i * SEG, SEG, SEQ) for i in range(8)]
        pti = 0
        sti = 0
        ppi = 0

        def store(idx, cc, src, n, t0):
            nonlocal sti
            q = "sync" if sti % 2 == 0 else "pool"
            sti += 1
            P.dma(q, S.rwf[idx, cc * 128:(cc + 1) * 128, t0:t0 + n], src[:, 0:n], r=[src], sem=("st", src.name))

        for (sq0, t0, n, slen) in segs:
            for ci, (nm, r0, nr) in enumerate(rch):
                p_ = pt[pti % 3]
                s_ = s1[pti % 2]
                pti += 1
                lo = t0 - 1
                hi = t0 + n + 1
                dlo, dhi = 0, n + 2
                if t0 == sq0:
                    lo += 1
                    dlo = 1
                    P.op("pool", lambda e, p_=p_, nr=nr: e.memset(p_[0:nr, 0:1], 0.0), w=[p_])
                if t0 + n == sq0 + slen:
                    hi -= 1
                    dhi = n + 1
                    P.op("pool", lambda e, p_=p_, nr=nr, n=n: e.memset(p_[0:nr, n + 1:n + 2], 0.0), w=[p_])
                P.dma("sync" if ci % 2 == 0 else "pool", p_[0:nr, dlo:dhi], S.pfm[r0:r0 + nr, lo:hi], w=[p_])
                P.op("pool", lambda e, p_=p_, s_=s_, nr=nr, n=n: e.tensor_tensor(out=s_[0:nr, 0:n], in0=p_[0:nr, 0:n], in1=p_[0:nr, 2:n + 2], op=ALU.add),
                     r=[p_], w=[s_])
                P.op("dve", lambda e, p_=p_, nr=nr, n=n, ci=ci, nm=nm: e.tensor_scalar(out=sh[nm][0:nr, 0:n], in0=p_[0:nr, 1:n + 1], scalar1=omu[0:nr, ci:ci + 1],
                                                                                scalar2=None, op0=ALU.mult), r=[p_, omu], w=[sh[nm]])
                P.op("dve", lambda e, s_=s_, nr=nr, n=n, ci=ci, nm=nm: e.scalar_tensor_tensor(out=sh[nm][0:nr, 0:n], in0=s_[0:nr, 0:n], scalar=hmu[0:nr, ci:ci + 1],
                                                                                       in1=sh[nm][0:nr, 0:n], op0=ALU.mult, op1=ALU.add),
                     r=[s_, hmu, sh[nm]], w=[sh[nm]])
            P.op("act", lambda e, n=n: e.activation(out=twl[:, 0:n], in_=sh["wl"][:, 0:n], func=AF.Tanh), r=[sh["wl"]], w=[twl])
            P.op("act", lambda e, n=n: e.activation(out=sg0[:, 0:n], in_=sh["g0"][:, 0:n], func=AF.Sigmoid), r=[sh["g0"]], w=[sg0])
            P.op("act", lambda e, n=n: e.activation(out=sg1[:, 0:n], in_=sh["g1"][0:32, 0:n], func=AF.Sigmoid), r=[sh["g1"]], w=[sg1])
            for d in range(2):
                for cc in range(2):
                    ix = d * 2 + cc
                    pw = pp[ppi % 6]
                    ppi += 1
                    P.op("pe", lambda e, pw=pw, d=d, cc=cc, n=n: e.matmul(out=pw[:, 0:n], lhsT=wbt[d * 64:(d + 1) * 64, cc * 128:(cc + 1) * 128],
                                                                       rhs=twl[d * 64:(d + 1) * 64, 0:n], start=True, stop=True),
                         r=[wbt, twl], w=[pw])
                    P.op("act", lambda e, pw=pw, ix=ix, n=n: e.activation(out=lw[ix][:, 0:n], in_=pw[:, 0:n], func=AF.Sigmoid, bias=w0c[:, ix:ix + 1]),
                         r=[pw, w0c], w=[lw[ix]])
                    P.op("pool", lambda e, ix=ix, n=n: e.tensor_scalar(out=lw[ix][:, 0:n], in0=lw[ix][:, 0:n], scalar1=NEG_EM05, scalar2=None, op0=ALU.mult),
                         r=[lw[ix]], w=[lw[ix]])
                    store(7 + d, cc, lw[ix], n, t0)
                    pa = pp[ppi % 6]
                    ppi += 1
                    P.op("pe", lambda e, pa=pa, d=d, cc=cc, n=n: e.matmul(out=pa[:, 0:n], lhsT=abt[d * 64:(d + 1) * 64, cc * 128:(cc + 1) * 128],
                                                                       rhs=sh["al"][d * 64:(d + 1) * 64, 0:n], start=True, stop=True),
                         r=[abt, sh["al"]], w=[pa])
                    P.op("act", lambda e, pa=pa, ix=ix, n=n: e.activation(out=asg[ix][:, 0:n], in_=pa[:, 0:n], func=AF.Sigmoid, bias=a0c[:, ix:ix + 1]),
                         r=[pa, a0c], w=[asg[ix]])
            for cc in range(2):
                kx = sh["k%d" % cc]
                P.op("dve", lambda e, cc=cc, kx=kx, n=n: e.tensor_scalar(out=kk[cc][:, 0:n], in0=kx[:, 0:n], scalar1=kkc[:, cc:cc + 1], scalar2=None, op0=ALU.mult),
                     r=[kx, kkc], w=[kk[cc]])
                P.op("pool", lambda e, cc=cc, n=n: e.tensor_tensor(out=sq[:, 0:n], in0=kk[cc][:, 0:n], in1=kk[cc][:, 0:n], op=ALU.mult),
                     r=[kk[cc]], w=[sq])
                pn = pp[ppi % 6]
                ppi += 1
                P.op("pe", lambda e, pn=pn, n=n: e.matmul(out=pn[:, 0:n], lhsT=g.consts[:, C_BONES, :], rhs=sq[:, 0:n], start=True, stop=True),
                     r=[sq, g.consts], w=[pn])
                P.op("act", lambda e, pn=pn, n=n: e.activation(out=rn[:, 0:n], in_=pn[:, 0:n], func=AF.Sqrt), r=[pn], w=[rn])
                P.op("dve", lambda e, n=n: e.tensor_scalar(out=rn[:, 0:n], in0=rn[:, 0:n], scalar1=1e-12, scalar2=None, op0=ALU.max), r=[rn], w=[rn])
                P.op("dve", lambda e, n=n: e.reciprocal(out=rn[:, 0:n], in_=rn[:, 0:n]), r=[rn], w=[rn])
                P.op("dve", lambda e, cc=cc, n=n: e.tensor_tensor(out=kk[cc][:, 0:n], in0=kk[cc][:, 0:n], in1=rn[:, 0:n], op=ALU.mult),
                     r=[kk[cc], rn], w=[kk[cc]])
                store(4, cc, kk[cc], n, t0)
                store(0, cc, sh["r%d" % cc], n, t0)
                store(3, cc, sh["v%d" % cc], n, t0)
                for d in range(2):
                    ix = d * 2 + cc
                    P.op("dve", lambda e, ix=ix, cc=cc, n=n: e.tensor_scalar(out=tq[:, 0:n], in0=asg[ix][:, 0:n], scalar1=kac[:, cc:cc + 1],
                                                                         scalar2=omka[:, cc:cc + 1], op0=ALU.mult, op1=ALU.add),
                         r=[asg[ix], kac, omka], w=[tq])
                    P.op("dve", lambda e, ix=ix, kx=kx, n=n: e.tensor_tensor(out=kd[ix][:, 0:n], in0=tq[:, 0:n], in1=kx[:, 0:n], op=ALU.mult),
                         r=[tq, kx], w=[kd[ix]])
                    store(1 + d, cc, kd[ix], n, t0)
                    P.op("pool", lambda e, ix=ix, cc=cc, n=n: e.tensor_tensor(out=bd[ix][:, 0:n], in0=kk[cc][:, 0:n], in1=asg[ix][:, 0:n], op=ALU.mult),
                         r=[kk[cc], asg[ix]], w=[bd[ix]])
                    store(5 + d, cc, bd[ix], n, t0)
                pg = pp[ppi % 6]
                ppi += 1
                P.op("pe", lambda e, pg=pg, cc=cc, n=n: e.matmul(out=pg[:, 0:n], lhsT=gb0[:, cc * 128:(cc + 1) * 128], rhs=sg0[:, 0:n], start=True, stop=False),
                     r=[gb0, sg0], w=[pg])
                P.op("pe", lambda e, pg=pg, cc=cc, n=n: e.matmul(out=pg[:, 0:n], lhsT=gb1[:, cc * 128:(cc + 1) * 128], rhs=sg1[:, 0:n], start=False, stop=True),
                     r=[gb1, sg1], w=[pg])
                P.op("act", lambda e, pg=pg, cc=cc, n=n: e.activation(out=gt[cc][:, 0:n], in_=pg[:, 0:n], func=AF.Copy), r=[pg], w=[gt[cc]])
                store(9, cc, gt[cc], n, t0)
        P.barrier()
        P.emit()


def phase_rwscan(g, l):
    nc, I, S = g.nc, g.I, g.S
    with ExitStack() as es:
        def sb(name, shape, dt=F32):
            return es.enter_context(nc.sbuf_tensor("f%d_" % l + name, list(shape), dt))

        def psb(name, shape, dt=F32):
            return es.enter_context(nc.psum_tensor("f%d_" % l + name, list(shape), dt))
        ident = g.consts[:, C_ID, :]
        ybuf = [[sb("y%d%d" % (p, d), [128, NT]) for d in range(2)] for p in range(2)]
        E64 = sb("E64", [128, 64])
        U = [[None, None], [None, None]]
        for d in range(2):
            for p in range(2):
                if p == 1:
                    U[d][1] = U[d][0]
                    continue
                u = Ctx()
                n = "%d%d" % (d, p)
                u.f = [sb("ld%d_" % i + n, [128, 128]) for i in range(6)]
                u.Lc = sb("Lc" + n, [128, 128])
                u.LC = sb("LC" + n, [128, 128])
                u.t1 = sb("t1" + n, [128, 128])
                u.tA = sb("tA" + n, [128, 128])
                u.tW = sb("tW" + n, [128, 128])
                u.eP = sb("eP" + n, [128, 128])
                u.eN = sb("eN" + n, [128, 128])
                u.eA = sb("eA" + n, [128, 128])
                u.eW = sb("eW" + n, [128, 128])
                u.WL = sb("WL" + n, [128, 2])
                u.ar = sb("ar" + n, [128, 256])
                u.bt = sb("bt" + n, [128, 128])
                u.kt = sb("kt" + n, [128, 128])
                u.bW = sb("bW" + n, [128, 128])
                u.kW = sb("kW" + n, [128, 128])
                u.Dg = sb("Dg" + n, [128, 2, 64])
                u.TM = sb("TM" + n, [128, 4, 128])
                u.q = []
                for hh in range(2):
                    q = Ctx()
                    m = n + "%d" % hh
                    q.XTR = sb("XTR" + m, [128, 256])
                    q.KTR = sb("KTR" + m, [128, 256])
                    q.X = [sb("X%d_" % i + m, [128, 128]) for i in range(2)]
                    q.XT = [sb("XT%d_" % i + m, [128, 128]) for i in range(2)]
                    q.PT = [sb("PT%d_" % i + m, [128, 128]) for i in range(2)]
                    q.Gs = sb("Gs" + m, [128, 64])
                    q.MAG = sb("MAG" + m, [128, 128])
                    q.Phi = sb("Phi" + m, [64, 2, 64])
                    q.Psi = sb("Psi" + m, [64, 2, 64])
                    q.RAT = sb("RAT" + m, [64, 128])
                    q.YCT = sb("YCT" + m, [64, 128])
                    u.q.append(q)
                U[d][p] = u
        ST = [[[sb("ST%d%d%d" % (h, d, i), [64, 64]) for i in range(2)] for d in range(2)] for h in range(4)]
        stpar = [[0, 0] for _ in range(4)]
        pq = [psb("pq%d" % i, [128, 512]) for i in range(4)]
        pTr = [psb("pTr%d" % i, [128, 4, 128]) for i in range(2)]
        pSq = [psb("pSq%d" % i, [64, 512]) for i in range(2)]
        P = Prog(nc)
        P.op("dve", lambda e: e.tensor_tensor(out=E64[:], in0=g.consts[:, C_ID, 0:64], in1=g.consts[:, C_ID, 64:128], op=ALU.add),
             r=[g.consts], w=[E64])
        for h in range(4):
            for d in range(2):
                P.op("pool", lambda e, h=h, d=d: e.memset(ST[h][d][0][:], 0.0), w=[ST[h][d][0]])
        order_f = list(range(NTILE))
        order_b = [1, 0] + list(range(NTILE - 1, 1, -1))
        fidx = [[0, 1, 3, 4, 5, 7], [0, 2, 3, 4, 6, 8]]
        slotc = [0, 0, 0, 0]

        def slot(qi):
            sl = slotc[qi] % 4
            slotc[qi] += 1
            return sl

        for step in range(NTILE if g.rwsteps is None else g.rwsteps):
            for p in range(2):
                units = [(0, order_f[step]), (1, order_b[step])]
                for (d, j) in units:
                    u = U[d][p]
                    for i6 in range(6):
                        P.dma("sync" if i6 % 2 == 0 else "pool", u.f[i6][:], S.rwf[fidx[d][i6], p * 128:(p + 1) * 128, j * 128:(j + 1) * 128],
                              w=[u.f[i6]])
                    fr, fkd, fv, fkk, fbd, flw = u.f
                    P.op("dve", lambda e, u=u, flw=flw: e.tensor_tensor_scan(out=u.Lc[:], data0=g.consts[:, C_RESET, :], data1=flw[:], initial=0.0,
                                                                           op0=ALU.mult, op1=ALU.add), r=[flw, g.consts], w=[u.Lc])
                    totv = u.Lc[:].rearrange("p (c l) -> p c l", c=2)[:, :, 63:64]
                    if d == 0:
                        LC = u.Lc
                    else:
                        LC = u.LC
                        P.op("pool", lambda e, u=u, flw=flw: e.tensor_tensor(out=u.t1[:], in0=flw[:], in1=u.Lc[:], op=ALU.subtract),
                             r=[flw, u.Lc], w=[u.t1])
                        P.op("pool", lambda e, u=u, totv=totv: e.tensor_tensor(out=u.LC[:].rearrange("p (c l) -> p c l", c=2),
                                                                            in0=u.t1[:].rearrange("p (c l) -> p c l", c=2),
                                                                            in1=totv.to_broadcast([128, 2, 64]), op=ALU.add),
                             r=[u.t1, u.Lc], w=[u.LC])
                    P.op("pool", lambda e, u=u, LC=LC, flw=flw: e.tensor_tensor(out=u.tA[:], in0=LC[:], in1=flw[:], op=ALU.subtract),
                         r=[LC, flw], w=[u.tA])
                    P.op("pool", lambda e, u=u, LC=LC, totv=totv: e.tensor_tensor(out=u.tW[:].rearrange("p (c l) -> p c l", c=2),
                                                                               in0=totv.to_broadcast([128, 2, 64]),
                                                                               in1=LC[:].rearrange("p (c l) -> p c l", c=2), op=ALU.subtract),
                         r=[LC, u.Lc], w=[u.tW])
                    P.op("act", lambda e, u=u, LC=LC: e.activation(out=u.eP[:], in_=LC[:], func=AF.Exp), r=[LC], w=[u.eP])
                    P.op("act", lambda e, u=u, LC=LC: e.activation(out=u.eN[:], in_=LC[:], func=AF.Exp, scale=-1.0), r=[LC], w=[u.eN])
                    P.op("act", lambda e, u=u: e.activation(out=u.eA[:], in_=u.tA[:], func=AF.Exp), r=[u.tA], w=[u.eA])
                    P.op("act", lambda e, u=u: e.activation(out=u.eW[:], in_=u.tW[:], func=AF.Exp), r=[u.tW], w=[u.eW])
                    P.op("act", lambda e, u=u, totv=totv: e.activation(out=u.WL[:].unsqueeze(2), in_=totv, func=AF.Exp), r=[u.Lc], w=[u.WL])
                    P.op("dve", lambda e, u=u, fkk=fkk: e.scalar_tensor_tensor(out=u.ar[:, 0:128], in0=fkk[:], scalar=-1.0, in1=u.eA[:],
                                                                             op0=ALU.mult, op1=ALU.mult), r=[fkk, u.eA], w=[(u.ar.name, 0)])
                    P.op("pool", lambda e, u=u, fr=fr: e.tensor_tensor(out=u.ar[:, 128:256], in0=fr[:], in1=u.eP[:], op=ALU.mult),
                         r=[fr, u.eP], w=[(u.ar.name, 1)])
                    P.op("dve", lambda e, u=u, fbd=fbd: e.tensor_tensor(out=u.bt[:], in0=fbd[:], in1=u.eN[:], op=ALU.mult), r=[fbd, u.eN], w=[u.bt])
                    P.op("pool", lambda e, u=u, fkd=fkd: e.tensor_tensor(out=u.kt[:], in0=fkd[:], in1=u.eN[:], op=ALU.mult), r=[fkd, u.eN], w=[u.kt])
                    P.op("dve", lambda e, u=u, fbd=fbd: e.tensor_tensor(out=u.bW[:], in0=fbd[:], in1=u.eW[:], op=ALU.mult), r=[fbd, u.eW], w=[u.bW])
                    P.op("pool", lambda e, u=u, fkd=fkd: e.tensor_tensor(out=u.kW[:], in0=fkd[:], in1=u.eW[:], op=ALU.mult), r=[fkd, u.eW], w=[u.kW])
                    for c in range(2):
                        P.op("pool", lambda e, u=u, c=c: e.tensor_scalar(out=u.Dg[:, c, :], in0=E64[:], scalar1=u.WL[:, c:c + 1], scalar2=None, op0=ALU.mult),
                             r=[E64, u.WL], w=[u.Dg])
                    srcs = [(u.ar, 0, (u.ar.name, 0)), (u.bW, None, u.bW.name), (u.kW, None, u.kW.name), (fv, None, fv.name)]
                    for k4, (src, off, key) in enumerate(srcs):
                        in_ap = src[:, 0:128]
                        P.op("pe", lambda e, d=d, k4=k4, in_ap=in_ap: e.transpose(out=pTr[d][:, k4, :], in_=in_ap, identity=ident),
                             r=[key, g.consts], w=[pTr[d]])
                    P.op("act", lambda e, u=u, d=d: e.activation(out=u.TM[:], in_=pTr[d][:], func=AF.Copy), r=[pTr[d]], w=[u.TM])
                probs = []
                for (d, j) in units:
                    for hh in range(2):
                        probs.append((d, j, hh, U[d][p], U[d][p].q[hh], d * 2 + hh))
                for (d, j, hh, u, q, qi) in probs:
                    pb = hh * 64
                    P.op("pe", lambda e, u=u, pb=pb, qi=qi: e.matmul(out=pq[qi][:, 0:256], lhsT=u.bt[pb:pb + 64, :], rhs=u.ar[pb:pb + 64, :], start=True, stop=True),
                         r=[u.bt, (u.ar.name, 0), (u.ar.name, 1)], w=[("pqb", qi), ("pqb", qi)], rows=pb)
                    P.op("pe", lambda e, u=u, pb=pb, qi=qi: e.matmul(out=pq[qi][:, 256:384], lhsT=u.ar[pb:pb + 64, 0:128], rhs=u.bt[pb:pb + 64, :], start=True, stop=True),
                         r=[u.bt, (u.ar.name, 0)], w=[("pqb", qi)], rows=pb)
                for (d, j, hh, u, q, qi) in probs:
                    m2 = (C_MS_IT if d == 0 else C_MS_IT_B)
                    mti = (C_MS_TI if d == 0 else C_MS_IT)
                    P.op("dve", lambda e, q=q, qi=qi, m2=m2: e.tensor_tensor(out=q.XTR[:], in0=pq[qi][:, 0:256],
                                                                          in1=g.consts[:, m2:m2 + 2, :].rearrange("p a b -> p (a b)"), op=ALU.mult),
                         r=[("pqb", qi), ("pqb", qi), g.consts], w=[q.XTR])
                    P.op("dve", lambda e, q=q, qi=qi, mti=mti: e.tensor_tensor(out=q.X[0][:], in0=pq[qi][:, 256:384], in1=g.consts[:, mti, :], op=ALU.mult),
                         r=[("pqb", qi), g.consts], w=[q.X[0]])
                    P.op("pool", lambda e, q=q: e.tensor_tensor(out=q.PT[0][:], in0=q.XTR[:, 0:128], in1=ident, op=ALU.add),
                         r=[q.XTR, g.consts], w=[q.PT[0]])
                for (d, j, hh, u, q, qi) in probs:
                    pb = hh * 64
                    P.op("pe", lambda e, u=u, pb=pb, qi=qi: e.matmul(out=pq[qi][:, 0:256], lhsT=u.kt[pb:pb + 64, :], rhs=u.ar[pb:pb + 64, :], start=True, stop=True),
                         r=[u.kt, (u.ar.name, 0), (u.ar.name, 1)], w=[("pqb", qi), ("pqb", qi)], rows=pb)
                for (d, j, hh, u, q, qi) in probs:
                    m2 = (C_MS_IT if d == 0 else C_MS_IT_B)
                    P.op("dve", lambda e, q=q, qi=qi, m2=m2: e.tensor_tensor(out=q.KTR[:], in0=pq[qi][:, 0:256],
                                                                          in1=g.consts[:, m2:m2 + 2, :].rearrange("p a b -> p (a b)"), op=ALU.mult),
                         r=[("pqb", qi), ("pqb", qi), g.consts], w=[q.KTR])
                curX = {qi: None for qi in range(4)}
                for lev in range(5):
                    last = (lev == 4)
                    for (d, j, hh, u, q, qi) in probs:
                        Xc = q.X[lev % 2]
                        XTc = q.XTR if lev == 0 else q.XT[lev % 2]
                        XTc_ap = XTc[:, 0:128]
                        P.op("pe", lambda e, qi=qi, Xc=Xc, XTc_ap=XTc_ap: e.matmul(out=pq[qi][:, 384:512], lhsT=XTc_ap, rhs=Xc[:], start=True, stop=True),
                             r=[Xc, XTc], w=[("pqb", qi)])
                        if not last:
                            P.op("pe", lambda e, qi=qi, Xc=Xc, XTc_ap=XTc_ap: e.matmul(out=pq[qi][:, 256:384], lhsT=Xc[:], rhs=XTc_ap, start=True, stop=True),
                                 r=[Xc, XTc], w=[("pqb", qi)])
                    for (d, j, hh, u, q, qi) in probs:
                        Xn = q.X[(lev + 1) % 2]
                        XTn = q.XT[(lev + 1) % 2]
                        P.op("act", lambda e, qi=qi, Xn=Xn: e.activation(out=Xn[:], in_=pq[qi][:, 384:512], func=AF.Copy), r=[("pqb", qi)], w=[Xn])
                        if not last:
                            P.op("act", lambda e, qi=qi, XTn=XTn: e.activation(out=XTn[:], in_=pq[qi][:, 256:384], func=AF.Copy), r=[("pqb", qi)], w=[XTn])
                    for (d, j, hh, u, q, qi) in probs:
                        Xn = q.X[(lev + 1) % 2]
                        PTc = q.PT[lev % 2]
                        P.op("pe", lambda e, qi=qi, Xn=Xn, PTc=PTc: e.matmul(out=pq[qi][:, 0:128], lhsT=Xn[:], rhs=PTc[:], start=True, stop=True),
                             r=[Xn, PTc], w=[("pqb", qi)])
                    for (d, j, hh, u, q, qi) in probs:
                        PTc = q.PT[lev % 2]
                        PTn = q.PT[(lev + 1) % 2]
                        P.op("dve", lambda e, qi=qi, PTc=PTc, PTn=PTn: e.tensor_tensor(out=PTn[:], in0=pq[qi][:, 0:128], in1=PTc[:], op=ALU.add),
                             r=[("pqb", qi), PTc], w=[PTn])
                for (d, j, hh, u, q, qi) in probs:
                    cb = hh * 64
                    P.op("pe", lambda e, qi=qi, q=q, u=u, cb=cb: e.matmul(out=pq[qi][:, 128:192], lhsT=q.KTR[:, 0:128], rhs=u.TM[:, 3, cb:cb + 64], start=True, stop=True),
                         r=[q.KTR, u.TM], w=[("pqb", qi)])
                for (d, j, hh, u, q, qi) in probs:
                    P.op("act", lambda e, qi=qi, q=q: e.activation(out=q.Gs[:], in_=pq[qi][:, 128:192], func=AF.Copy), r=[("pqb", qi)], w=[q.Gs])
                for (d, j, hh, u, q, qi) in probs:
                    cb = hh * 64
                    PTf = q.PT[1]
                    P.op("pe", lambda e, qi=qi, PTf=PTf, u=u, cb=cb: e.matmul(out=pq[qi][:, 256:320], lhsT=PTf[:], rhs=u.TM[:, 0, cb:cb + 64], start=True, stop=True),
                         r=[PTf, u.TM], w=[("pqb", qi)])
                    P.op("pe", lambda e, qi=qi, PTf=PTf, q=q: e.matmul(out=pq[qi][:, 320:384], lhsT=PTf[:], rhs=q.Gs[:], start=True, stop=True),
                         r=[PTf, q.Gs], w=[("pqb", qi)])
                for (d, j, hh, u, q, qi) in probs:
                    P.op("act", lambda e, qi=qi, q=q: e.activation(out=q.MAG[:], in_=pq[qi][:, 256:384], func=AF.Copy), r=[("pqb", qi)], w=[q.MAG])
                for (d, j, hh, u, q, qi) in probs:
                    cb = hh * 64
                    pb = hh * 64
                    for c in range(2):
                        rb = c * 64
                        P.op("pe", lambda e, qi=qi, q=q, u=u, rb=rb, cb=cb, c=c: e.matmul(out=pq[qi][0:64, 384 + c * 64:448 + c * 64], lhsT=q.MAG[rb:rb + 64, 0:64],
                                                                                       rhs=u.TM[rb:rb + 64, 1, cb:cb + 64], start=True, stop=False),
                             r=[q.MAG, u.TM], w=[("pqb", qi)], rows=rb)
                        P.op("pe", lambda e, qi=qi, u=u, pb=pb, c=c: e.matmul(out=pq[qi][0:64, 384 + c * 64:448 + c * 64], lhsT=E64[pb:pb + 64, :],
                                                                            rhs=u.Dg[pb:pb + 64, c, :], start=False, stop=True),
                             r=[E64, u.Dg], w=[("pqb", qi)], rows=pb)
                        P.op("pe", lambda e, qi=qi, q=q, u=u, rb=rb, cb=cb, c=c: e.matmul(out=pq[qi][0:64, c * 64:c * 64 + 64], lhsT=u.TM[rb:rb + 64, 1, cb:cb + 64],
                                                                                       rhs=q.MAG[rb:rb + 64, 64:128], start=True, stop=False),
                             r=[q.MAG, u.TM], w=[("pqb", qi)], rows=rb)
                        P.op("pe", lambda e, qi=qi, u=u, rb=rb, cb=cb, c=c: e.matmul(out=pq[qi][0:64, c * 64:c * 64 + 64], lhsT=u.TM[rb:rb + 64, 2, cb:cb + 64],
                                                                                  rhs=u.TM[rb:rb + 64, 3, cb:cb + 64], start=False, stop=True),
                             r=[u.TM], w=[("pqb", qi)], rows=rb)
                    P.op("pe", lambda e, qi=qi, q=q: e.matmul(out=pq[qi][0:64, 128:256], lhsT=q.MAG[:, 0:64], rhs=q.XTR[:, 128:256], start=True, stop=False),
                         r=[q.MAG, q.XTR], w=[("pqb", qi)])
                    P.op("pe", lambda e, qi=qi, u=u, pb=pb: e.matmul(out=pq[qi][0:64, 128:256], lhsT=E64[pb:pb + 64, :], rhs=u.ar[pb:pb + 64, 128:256], start=False, stop=True),
                         r=[E64, (u.ar.name, 1)], w=[("pqb", qi)], rows=pb)
                    P.op("pe", lambda e, qi=qi, q=q: e.matmul(out=pq[qi][0:64, 256:384], lhsT=q.MAG[:, 64:128], rhs=q.XTR[:, 128:256], start=True, stop=False),
                         r=[q.MAG, q.XTR], w=[("pqb", qi)])
                    P.op("pe", lambda e, qi=qi, q=q, u=u, cb=cb: e.matmul(out=pq[qi][0:64, 256:384], lhsT=u.TM[:, 3, cb:cb + 64], rhs=q.KTR[:, 128:256], start=False, stop=True),
                         r=[u.TM, q.KTR], w=[("pqb", qi)])
                for (d, j, hh, u, q, qi) in probs:
                    P.op("act", lambda e, qi=qi, q=q: e.activation(out=q.Phi[:].rearrange("p c k -> p (c k)"), in_=pq[qi][0:64, 384:512], func=AF.Copy),
                         r=[("pqb", qi)], w=[q.Phi])
                    P.op("dve", lambda e, qi=qi, q=q: e.tensor_copy(out=q.Psi[:].rearrange("p c k -> p (c k)"), in_=pq[qi][0:64, 0:128]),
                         r=[("pqb", qi)], w=[q.Psi])
                    P.op("act", lambda e, qi=qi, q=q: e.activation(out=q.RAT[:], in_=pq[qi][0:64, 128:256], func=AF.Copy), r=[("pqb", qi)], w=[q.RAT])
                    P.op("dve", lambda e, qi=qi, q=q: e.tensor_copy(out=q.YCT[:], in_=pq[qi][0:64, 256:384]), r=[("pqb", qi)], w=[q.YCT])
                for ci in range(2):
                    for (d, j, hh, u, q, qi) in probs:
                        c = ci if d == 0 else 1 - ci
                        h = p * 2 + hh
                        sp = stpar[h][d]
                        Sc = ST[h][d][sp]
                        Sn = ST[h][d][1 - sp]
                        stpar[h][d] = 1 - sp
                        psq = pSq[ci]
                        yc0 = qi * 128
                        P.op("pe", lambda e, psq=psq, yc0=yc0, Sc=Sc, q=q, c=c: e.matmul(out=psq[:, yc0:yc0 + 64], lhsT=Sc[:], rhs=q.RAT[:, c * 64:(c + 1) * 64], start=True, stop=False),
                             r=[Sc, q.RAT], w=[("sqb", ci)])
                        P.op("pe", lambda e, psq=psq, yc0=yc0, q=q, c=c: e.matmul(out=psq[:, yc0:yc0 + 64], lhsT=E64[0:64, :], rhs=q.YCT[:, c * 64:(c + 1) * 64], start=False, stop=True),
                             r=[E64, q.YCT], w=[("sqb", ci)])
                        P.op("pe", lambda e, psq=psq, yc0=yc0, Sc=Sc, q=q, c=c: e.matmul(out=psq[:, yc0 + 64:yc0 + 128], lhsT=q.Phi[:, c, :], rhs=Sc[:], start=True, stop=False),
                             r=[Sc, q.Phi], w=[("sqb", ci)])
                        P.op("pe", lambda e, psq=psq, yc0=yc0, q=q, c=c: e.matmul(out=psq[:, yc0 + 64:yc0 + 128], lhsT=E64[0:64, :], rhs=q.Psi[:, c, :], start=False, stop=True),
                             r=[E64, q.Psi], w=[("sqb", ci)])
                        tcol = j * 128 + c * 64
                        yb = ybuf[p][d]
                        P.op("act", lambda e, psq=psq, yc0=yc0, yb=yb, hh=hh, tcol=tcol: e.activation(out=yb[hh * 64:(hh + 1) * 64, tcol:tcol + 64], in_=psq[:, yc0:yc0 + 64], func=AF.Copy),
                             r=[("sqb", ci)], w=[(yb.name, j)])
                        P.op("dve", lambda e, psq=psq, yc0=yc0, Sn=Sn: e.tensor_copy(out=Sn[:], in_=psq[:, yc0 + 64:yc0 + 128]), r=[("sqb", ci)], w=[Sn])
        SEG = 512
        prm = sb("prm", [128, 2, 3])
        ld = [sb("o_ld%d" % i, [128, SEG]) for i in range(5)]
        ysum = sb("ysum", [128, SEG])
        yc_ = sb("yc_", [128, SEG])
        sq = sb("osq", [128, SEG])
        rstd = sb("rstd", [128, SEG])
        prod = sb("prod", [128, SEG])
        ob = [sb("oob%d" % i, [128, SEG], BF16) for i in range(2)]
        for p in range(2):
            for k3, src in enumerate((I.r_k, I.ln_w, I.ln_b)):
                P.dma("sync", prm[:, p, k3:k3 + 1], src[g.wi(l), p * 128:(p + 1) * 128, :], w=[prm], sem="prm")
        segs = ([(0, CTX)] if l == 0 else []) + [(CTX + i * SEG, SEG) for i in range(8)]
        oi = 0
        for (t0, n) in segs:
            for p in range(2):
                for k5, idx in enumerate((0, 1, 2, 3, 9)):
                    P.dma("sync" if k5 % 2 == 0 else "pool", ld[k5][:, 0:n], S.rwf[idx, p * 128:(p + 1) * 128, t0:t0 + n], w=[ld[k5]])
                ykeys = [(ybuf[p][dd].name, jj) for dd in range(2) for jj in range(t0 // 128, (t0 + n) // 128)]
                P.op("pool", lambda e, p=p, t0=t0, n=n: e.tensor_tensor(out=ysum[:, 0:n], in0=ybuf[p][0][:, t0:t0 + n], in1=ybuf[p][1][:, t0:t0 + n], op=ALU.add),
                     r=ykeys, w=[ysum])
                pm = pq[0]
                P.op("pe", lambda e, pm=pm, n=n: e.matmul(out=pm[:, 0:n], lhsT=g.consts[:, C_BONES, :], rhs=ysum[:, 0:n], start=True, stop=True),
                     r=[ysum, g.consts], w=[("pqb", 0), ("pqb", 0), ("pqb", 0), ("pqb", 0)])
                P.op("dve", lambda e, pm=pm, n=n: e.scalar_tensor_tensor(out=yc_[:, 0:n], in0=pm[:, 0:n], scalar=-1.0 / 64, in1=ysum[:, 0:n], op0=ALU.mult, op1=ALU.add),
                     r=[("pqb", 0), ("pqb", 0), ("pqb", 0), ("pqb", 0), ysum], w=[yc_])
                P.op("pool", lambda e, n=n: e.tensor_tensor(out=sq[:, 0:n], in0=yc_[:, 0:n], in1=yc_[:, 0:n], op=ALU.mult), r=[yc_], w=[sq])
                pv = pq[1]
                P.op("pe", lambda e, pv=pv, n=n: e.matmul(out=pv[:, 0:n], lhsT=g.consts[:, C_BONES, :], rhs=sq[:, 0:n], start=True, stop=True),
                     r=[sq, g.consts], w=[("pqb", 1), ("pqb", 1), ("pqb", 1), ("pqb", 1)])
                P.op("dve", lambda e, pv=pv, n=n: e.tensor_scalar(out=rstd[:, 0:n], in0=pv[:, 0:n], scalar1=1.0 / 64, scalar2=64e-5, op0=ALU.mult, op1=ALU.add),
                     r=[("pqb", 1), ("pqb", 1), ("pqb", 1), ("pqb", 1)], w=[rstd])
                P.op("act", lambda e, n=n: e.activation(out=rstd[:, 0:n], in_=rstd[:, 0:n], func=AF.Sqrt), r=[rstd], w=[rstd])
                P.op("dve", lambda e, n=n: e.reciprocal(out=rstd[:, 0:n], in_=rstd[:, 0:n]), r=[rstd], w=[rstd])
                P.op("dve", lambda e, n=n: e.tensor_tensor(out=yc_[:, 0:n], in0=yc_[:, 0:n], in1=rstd[:, 0:n], op=ALU.mult), r=[yc_, rstd], w=[yc_])
                P.op("dve", lambda e, n=n, p=p: e.tensor_scalar(out=yc_[:, 0:n], in0=yc_[:, 0:n], scalar1=prm[:, p, 1:2], scalar2=prm[:, p, 2:3], op0=ALU.mult, op1=ALU.add),
                     r=[yc_, prm], w=[yc_])
                P.op("pool", lambda e, n=n: e.tensor_tensor(out=prod[:, 0:n], in0=ld[1][:, 0:n], in1=ld[2][:, 0:n], op=ALU.add), r=[ld[1], ld[2]], w=[prod])
                P.op("pool", lambda e, n=n: e.tensor_tensor(out=prod[:, 0:n], in0=prod[:, 0:n], in1=ld[0][:, 0:n], op=ALU.mult), r=[prod, ld[0]], w=[prod])
                P.op("pool", lambda e, n=n, p=p: e.tensor_scalar(out=prod[:, 0:n], in0=prod[:, 0:n], scalar1=prm[:, p, 0:1], scalar2=0.5, op0=ALU.mult, op1=ALU.mult),
                     r=[prod, prm], w=[prod])
                pbn = pq[2]
                P.op("pe", lambda e, pbn=pbn, n=n: e.matmul(out=pbn[:, 0:n], lhsT=g.consts[:, C_BONES, :], rhs=prod[:, 0:n], start=True, stop=True),
                     r=[prod, g.consts], w=[("pqb", 2), ("pqb", 2), ("pqb", 2), ("pqb", 2)])
                P.op("dve", lambda e, pbn=pbn, n=n: e.tensor_tensor(out=sq[:, 0:n], in0=pbn[:, 0:n], in1=ld[3][:, 0:n], op=ALU.mult),
                     r=[("pqb", 2), ("pqb", 2), ("pqb", 2), ("pqb", 2), ld[3]], w=[sq])
                P.op("dve", lambda e, n=n: e.tensor_tensor(out=yc_[:, 0:n], in0=yc_[:, 0:n], in1=sq[:, 0:n], op=ALU.add), r=[yc_, sq], w=[yc_])
                o = ob[oi % 2]
                oi += 1
                P.op("dve", lambda e, n=n, o=o: e.tensor_tensor(out=o[:, 0:n], in0=yc_[:, 0:n], in1=ld[4][:, 0:n], op=ALU.mult), r=[yc_, ld[4]], w=[o])
                P.dma("sync", S.mixT[768 + p * 128:768 + (p + 1) * 128, t0:t0 + n], o[:, 0:n], r=[o], sem=("oob", oi % 2))
        P.barrier()
        P.emit()


def phase_wout(g, l):
    nc, I, S = g.nc, g.I, g.S
    with ExitStack() as es:
        def sb(name, shape, dt=F32):
            return es.enter_context(nc.sbuf_tensor("w%d_" % l + name, list(shape), dt))

        def psb(name, shape, dt=F32):
            return es.enter_context(nc.psum_tensor("w%d_" % l + name, list(shape), dt))
        wo = sb("wo", [128, 8, D], BF16)
        rw = sb("rw", [128, 8, NE])
        bc = [[sb("bc%d%d" % (j, k), [128, D]) for k in range(3)] for j in range(2)]
        mt = [sb("mt%d" % i, [128, 8, 128], BF16) for i in range(2)]
        xt = [sb("xt%d" % i, [128, D]) for i in range(2)]
        x1 = [sb("x1%d" % i, [128, D]) for i in range(2)]
        junk = sb("junk", [128, D])
        ss = [sb("ss%d" % i, [128, 1]) for i in range(2)]
        h2f = [sb("h2f%d" % i, [128, D]) for i in range(2)]
        h2b = [sb("h2b%d" % i, [128, D], BF16) for i in range(2)]
        h2T = sb("h2T", [128, 8, 128])
        lg = sb("lg", [128, NE])
        mx = sb("mx", [128, 1])
        sm = sb("sm", [128, 1])
        aff = sb("aff", [128, NE])
        pO = [psb("pO%d" % i, [128, 512]) for i in range(2)]
        pT = [psb("pT%d" % i, [128, 4, 128]) for i in range(2)]
        pL_ = psb("pL", [128, 512])
        pL = pL_[:, 0:NE]
        pA_ = psb("pA", [NE, 512])
        pA = pA_[:, 0:128]
        P = Prog(nc)
        P.dma("pool", wo[:], I.w_out[g.wi(l)].rearrange("(kc p) n -> p kc n", p=128), w=[wo])
        P.dma("sync", rw[:], I.router[g.wi(l)].rearrange("(kc p) n -> p kc n", p=128), w=[rw])
        for j in range(2):
            for k, mi in enumerate((2, 3, 4)):
                P.dma("sync", bc[j][k][:], S.modv[g.wi(l), j, mi:mi + 1, :].to_broadcast([128, D]), w=[bc[j][k]])
        tiles = list(range(NTILE)) if l == 0 else list(range(2, NTILE))
        for i in tiles:
            b = i % 2
            j = 1 if i < 2 else 0
            if i < 2:
                src = (I.ctx if l == g.first else S.xcres)[i * 128:(i + 1) * 128, :]
                dst = S.xcres[i * 128:(i + 1) * 128, :]
                h2dst = S.h2c[i * 128:(i + 1) * 128, :]
            else:
                src = (I.x if l == g.first else S.xres)[(i - 2) * 128:(i - 1) * 128, :]
                dst = (g.out if l == g.last else S.xres)[(i - 2) * 128:(i - 1) * 128, :]
                h2dst = S.h2l[(i - 2) * 128:(i - 1) * 128, :]
            P.dma("sync", mt[b][:], S.mixT[:, i * 128:(i + 1) * 128].rearrange("(kc p) t -> p kc t", p=128), w=[mt[b]])
            P.dma("sync", xt[b][:], src, w=[xt[b]])
            for half in range(2):
                for kc in range(8):
                    P.op("pe", lambda e, half=half, kc=kc, b=b: e.matmul(out=pO[half][:], lhsT=mt[b][:, kc, :], rhs=wo[:, kc, half * 512:(half + 1) * 512],
                                                                       start=(kc == 0), stop=(kc == 7)), r=[mt[b], wo], w=[pO[half]])
                P.op("dve", lambda e, half=half, b=b, j=j: e.tensor_tensor(out=x1[b][:, half * 512:(half + 1) * 512], in0=pO[half][:],
                                                                          in1=bc[j][0][:, half * 512:(half + 1) * 512], op=ALU.mult),
                     r=[pO[half], bc[j][0]], w=[(x1[b].name, half)])
            P.op("pool", lambda e, b=b: e.tensor_tensor(out=x1[b][:], in0=x1[b][:], in1=xt[b][:], op=ALU.add),
                 r=[(x1[b].name, 0), (x1[b].name, 1), xt[b]], w=[x1[b], (x1[b].name, 0), (x1[b].name, 1)])
            P.dma("pool", dst, x1[b][:], r=[x1[b]], sem=("x1st", b))
            P.op("act", lambda e, b=b: e.activation(out=junk[:], in_=x1[b][:], func=AF.Square, accum_out=ss[b][:]), r=[x1[b]], w=[junk, ss[b]])
            P.op("dve", lambda e, b=b: e.tensor_scalar(out=ss[b][:], in0=ss[b][:], scalar1=1.0 / D, scalar2=1e-6, op0=ALU.mult, op1=ALU.add),
                 r=[ss[b]], w=[ss[b]])
            P.op("act", lambda e, b=b: e.activation(out=ss[b][:], in_=ss[b][:], func=AF.Sqrt), r=[ss[b]], w=[ss[b]])
            P.op("dve", lambda e, b=b: e.reciprocal(out=ss[b][:], in_=ss[b][:]), r=[ss[b]], w=[ss[b]])
            P.op("dve", lambda e, b=b, j=j: e.scalar_tensor_tensor(out=h2f[b][:], in0=x1[b][:], scalar=ss[b][:, 0:1], in1=bc[j][1][:], op0=ALU.mult, op1=ALU.mult),
                 r=[x1[b], ss[b], bc[j][1]], w=[h2f[b]])
            P.op("pool", lambda e, b=b, j=j: e.tensor_tensor(out=h2f[b][:], in0=h2f[b][:], in1=bc[j][2][:], op=ALU.add), r=[h2f[b], bc[j][2]], w=[h2f[b]])
            P.op("act", lambda e, b=b: e.activation(out=h2b[b][:], in_=h2f[b][:], func=AF.Copy), r=[h2f[b]], w=[h2b[b]])
            P.dma("sync", h2dst, h2b[b][:], r=[h2b[b]], sem=("h2st", b))
            for half in range(2):
                for k4 in range(4):
                    kc = half * 4 + k4
                    P.op("pe", lambda e, half=half, k4=k4, kc=kc, b=b: e.transpose(out=pT[half][:, k4, :], in_=h2f[b][:, kc * 128:(kc + 1) * 128],
                                                                                identity=g.consts[:, C_ID, :]), r=[h2f[b], g.consts], w=[pT[half]])
                if half == 0:
                    P.op("act", lambda e, half=half: e.activation(out=h2T[:, 0:4, :], in_=pT[0][:], func=AF.Copy), r=[pT[0]], w=[("h2T", 0)])
                else:
                    P.op("dve", lambda e, half=half: e.tensor_copy(out=h2T[:, 4:8, :], in_=pT[1][:]), r=[pT[1]], w=[("h2T", 1)])
            for kc in range(8):
                P.op("pe", lambda e, kc=kc: e.matmul(out=pL, lhsT=h2T[:, kc, :], rhs=rw[:, kc, :], start=(kc == 0), stop=(kc == 7)),
                     r=[("h2T", 0), ("h2T", 1), rw], w=["pL"])
            P.op("dve", lambda e: e.tensor_copy(out=lg[:], in_=pL), r=["pL"], w=[lg])
            P.op("dve", lambda e: e.tensor_reduce(out=mx[:], in_=lg[:], axis=AX.X, op=ALU.max), r=[lg], w=[mx])
            P.op("dve", lambda e: e.tensor_scalar(out=mx[:], in0=mx[:], scalar1=-1.0, scalar2=None, op0=ALU.mult), r=[mx], w=[mx])
            P.op("act", lambda e: e.activation(out=aff[:], in_=lg[:], func=AF.Exp, bias=mx[:, 0:1], accum_out=sm[:]), r=[lg, mx], w=[aff, sm])
            P.op("dve", lambda e: e.reciprocal(out=sm[:], in_=sm[:]), r=[sm], w=[sm])
            P.op("dve", lambda e: e.tensor_scalar(out=aff[:], in0=aff[:], scalar1=sm[:, 0:1], scalar2=None, op0=ALU.mult), r=[aff, sm], w=[aff])
            P.op("pe", lambda e: e.transpose(out=pA, in_=aff[:], identity=g.consts[:, C_ID, :]), r=[aff, g.consts], w=["pA"])
            P.op("act", lambda e, i=i: e.activation(out=g.affT[:, i * 128:(i + 1) * 128], in_=pA, func=AF.Copy), r=["pA"], w=[("affT", i)])
        P.barrier()
        P.emit()


def phase_moe(g, l):
    nc, I, S = g.nc, g.I, g.S
    with ExitStack() as es:
        def sb(name, shape, dt=F32):
            return es.enter_context(nc.sbuf_tensor("m%d_" % l + name, list(shape), dt))

        def psb(name, shape, dt=F32):
            return es.enter_context(nc.psum_tensor("m%d_" % l + name, list(shape), dt))
        work = sb("work", [NE, SEQ])
        vals = sb("vals", [NE, CAP_L])
        idxu = sb("idxu", [NE, CAP_L], U32)
        idxf = sb("idxf", [NE, CAP_L])
        idxT = sb("idxT", [128, 4, NE], I32)
        gT = sb("gT", [128, 4, NE])
        gt2 = [sb("gt2_%d" % j, [128, D]) for j in range(2)]
        wgt = [sb("wg%d" % i, [128, 8, D], BF16) for i in range(2)]
        wut = [sb("wu%d" % i, [128, 8, D], BF16) for i in range(2)]
        wdt = [sb("wd%d" % i, [128, 8, D], BF16) for i in range(2)]
        xs = [sb("xs%d" % i, [128, D], BF16) for i in range(2)]
        xsT = sb("xsT", [128, 8, 512], BF16)
        hidT = sb("hidT", [128, 8, 512], BF16)
        sg = [sb("sg%d" % i, [128, 512]) for i in range(2)]
        y = [sb("y%d" % i, [128, D]) for i in range(2)]
        pTi = psb("pTi", [128, 32, NE])
        pX = [psb("pX%d" % i, [128, 8, 128], BF16) for i in range(2)]
        pG = psb("pG", [128, 512])
        pU = psb("pU", [128, 512])
        pY = [psb("pY%d" % i, [128, 512]) for i in range(2)]
        P = Prog(nc)
        for j in range(2):
            P.dma("sync", gt2[j][:], S.modv[g.wi(l), j, 5:6, :].to_broadcast([128, D]), w=[gt2[j]])
        sets = [(0, CTX, SEQ, CAP_L, S.h2l, (g.out if l == g.last else S.xres))]
        if l == 0:
            sets.append((1, 0, CTX, CAP_C, S.h2c, S.xcres))
        wi = 0
        xi = 0
        yi = 0
        for (j, a0, N, cap, h2src, dest) in sets:
            nch = (cap + 127) // 128
            npc = min(cap, 128)
            akeys = [("affT", i) for i in range(a0 // 128, (a0 + N) // 128)]
            P.op("pool", lambda e, a0=a0, N=N: e.tensor_copy(out=work[:, 0:N], in_=g.affT[:, a0:a0 + N]), r=akeys, w=[work])
            for r8 in range(cap // 8):
                P.op("dve", lambda e, r8=r8, N=N: e.max(out=vals[:, r8 * 8:(r8 + 1) * 8], in_=work[:, 0:N]), r=[work], w=[vals])
                P.op("dve", lambda e, r8=r8, N=N: e.max_index(out=idxu[:, r8 * 8:(r8 + 1) * 8], in_max=vals[:, r8 * 8:(r8 + 1) * 8], in_values=work[:, 0:N]),
                     r=[work, vals], w=[idxu])
                P.op("dve", lambda e, r8=r8, N=N: e.match_replace(out=work[:, 0:N], in_to_replace=vals[:, r8 * 8:(r8 + 1) * 8], in_values=work[:, 0:N], imm_value=-1.0),
                     r=[work, vals], w=[work])
            P.op("dve", lambda e, cap=cap: e.tensor_copy(out=idxf[:, 0:cap], in_=idxu[:, 0:cap]), r=[idxu], w=[idxf])
            for ch in range(nch):
                P.op("pe", lambda e, ch=ch, npc=npc: e.transpose(out=pTi[0:npc, 0, :], in_=idxf[:, ch * 128:ch * 128 + npc], identity=g.consts[0:NE, C_ID, 0:NE]),
                     r=[idxf, g.consts], w=[pTi])
                P.op("pe", lambda e, ch=ch, npc=npc: e.transpose(out=pTi[0:npc, 1, :], in_=vals[:, ch * 128:ch * 128 + npc], identity=g.consts[0:NE, C_ID, 0:NE]),
                     r=[vals, g.consts], w=[pTi])
                P.op("dve", lambda e, ch=ch, npc=npc: e.tensor_copy(out=idxT[0:npc, ch, :], in_=pTi[0:npc, 0, :]), r=[pTi], w=[idxT])
                P.op("dve", lambda e, ch=ch, npc=npc: e.tensor_copy(out=gT[0:npc, ch, :], in_=pTi[0:npc, 1, :]), r=[], w=[gT, pTi])
            ncol = nch * npc
            for ex in range(NE):
                wb_ = wi % 2
                wi += 1
                P.dma("pool", wgt[wb_][:], I.wg[g.wi(l), ex].rearrange("(kc p) n -> p kc n", p=128), w=[wgt[wb_]])
                P.dma("pool", wut[wb_][:], I.wu[g.wi(l), ex].rearrange("(kc p) n -> p kc n", p=128), w=[wut[wb_]])
                P.dma("pool", wdt[wb_][:], I.wd[g.wi(l), ex].rearrange("(kc p) n -> p kc n", p=128), w=[wdt[wb_]])
                for ch in range(nch):
                    xb = xs[xi % 2]
                    xi += 1
                    P.dma_fn("pool", lambda e, xb=xb, ch=ch, ex=ex, npc=npc, h2src=h2src: e.indirect_dma_start(
                        out=xb[0:npc, :], out_offset=None, in_=h2src[:, :],
                        in_offset=bass.IndirectOffsetOnAxis(ap=idxT[0:npc, ch, ex:ex + 1], axis=0)),
                        r=[idxT], w=[xb], sem=("xg", xb.name))
                    for half in range(2):
                        for k4 in range(4):
                            kc = half * 4 + k4
                            P.op("pe", lambda e, half=half, k4=k4, kc=kc, xb=xb, npc=npc: e.transpose(out=pX[half][:, k4, 0:npc], in_=xb[0:npc, kc * 128:(kc + 1) * 128],
                                                                                                 identity=g.identb[0:npc, 0:npc]), r=[xb, g.identb], w=[pX[half]])
                        if half == 0:
                            P.op("act", lambda e, ch=ch, npc=npc: e.activation(out=xsT[:, 0:4, ch * 128:ch * 128 + npc], in_=pX[0][:, 0:4, 0:npc], func=AF.Copy),
                                 r=[pX[0]], w=[("xsT", 0)])
                        else:
                            P.op("dve", lambda e, ch=ch, npc=npc: e.tensor_copy(out=xsT[:, 4:8, ch * 128:ch * 128 + npc], in_=pX[1][:, 0:4, 0:npc]),
                                 r=[pX[1]], w=[("xsT", 1)])
                for fc in range(8):
                    for kc in range(8):
                        P.op("pe", lambda e, fc=fc, kc=kc, wb_=wb_, ncol=ncol: e.matmul(out=pG[:, 0:ncol], lhsT=wgt[wb_][:, kc, fc * 128:(fc + 1) * 128], rhs=xsT[:, kc, 0:ncol],
                                                                                     start=(kc == 0), stop=(kc == 7)), r=[wgt[wb_], ("xsT", 0), ("xsT", 1)], w=[pG])
                    for kc in range(8):
                        P.op("pe", lambda e, fc=fc, kc=kc, wb_=wb_, ncol=ncol: e.matmul(out=pU[:, 0:ncol], lhsT=wut[wb_][:, kc, fc * 128:(fc + 1) * 128], rhs=xsT[:, kc, 0:ncol],
                                                                                     start=(kc == 0), stop=(kc == 7)), r=[wut[wb_], ("xsT", 0), ("xsT", 1)], w=[pU])
                    s_ = sg[fc % 2]
                    P.op("act", lambda e, s_=s_, ncol=ncol: e.activation(out=s_[:, 0:ncol], in_=pG[:, 0:ncol], func=AF.Silu), r=[pG], w=[s_])
                    P.op("dve", lambda e, s_=s_, fc=fc, ncol=ncol: e.tensor_tensor(out=hidT[:, fc, 0:ncol], in0=pU[:, 0:ncol], in1=s_[:, 0:ncol], op=ALU.mult),
                         r=[pU, s_], w=[("hidT", fc)])
                hk = [("hidT", fc) for fc in range(8)]
                for ch in range(nch):
                    yb = y[yi % 2]
                    yi += 1
                    for half in range(2):
                        for fc in range(8):
                            P.op("pe", lambda e, half=half, fc=fc, ch=ch, wb_=wb_, npc=npc: e.matmul(out=pY[half][0:npc, :], lhsT=hidT[:, fc, ch * 128:ch * 128 + npc],
                                                                                                 rhs=wdt[wb_][:, fc, half * 512:(half + 1) * 512], start=(fc == 0), stop=(fc == 7)),
                                 r=hk + [wdt[wb_]], w=[pY[half]])
                        P.op("dve", lambda e, half=half, yb=yb, ch=ch, ex=ex, npc=npc, j=j: e.scalar_tensor_tensor(
                            out=yb[0:npc, half * 512:(half + 1) * 512], in0=pY[half][0:npc, :], scalar=gT[0:npc, ch, ex:ex + 1],
                            in1=gt2[j][0:npc, half * 512:(half + 1) * 512], op0=ALU.mult, op1=ALU.mult), r=[pY[half], gT, gt2[j]], w=[(yb.name, half)])
                    P.dma_fn("pool", lambda e, yb=yb, ch=ch, ex=ex, npc=npc, dest=dest: e.indirect_dma_start(
                        out=dest[:, :], out_offset=bass.IndirectOffsetOnAxis(ap=idxT[0:npc, ch, ex:ex + 1], axis=0),
                        in_=yb[0:npc, :], in_offset=None, compute_op=ALU.add),
                        r=[(yb.name, 0), (yb.name, 1), idxT], w=[("dest", j)], sem=("ysc", j))
        P.barrier()
        P.emit()


def phase_zero_mix(g, l):
    nc, S = g.nc, g.S
    with ExitStack() as es:
        z = es.enter_context(nc.sbuf_tensor("z%d_z" % l, [128, NT], BF16))
        P = Prog(nc)
        P.op("pool", lambda e: e.memset(z[:], 0.0), w=[z])
        for r in range(2, 8):
            P.dma("sync", S.mixT[r * 128:(r + 1) * 128, :], z[:], r=[z], sem="zst")
        P.barrier()
        P.emit()


def prep_inputs(inputs):
    f = lambda a: np.ascontiguousarray(np.asarray(a, dtype=np.float32))
    x = f(inputs["x"])
    c = f(inputs["c"])
    ctx = f(inputs["ctx"])
    c_ctx = f(inputs["c_ctx"])
    shared = {
        "ada_w": f(inputs["ada_w"]),
        "ada_b": f(inputs["ada_b"]).reshape(2, 1, 6 * D),
        "norm1_g": f(inputs["norm1_g"]).reshape(2, 1, D),
        "norm2_g": f(inputs["norm2_g"]).reshape(2, 1, D),
        "w_in": f(inputs["w_in"]),
        "w_out": f(inputs["w_out"]),
        "conv_wT": f(np.transpose(f(inputs["conv_w"]), (0, 2, 1))),
        "q_norm_g": f(inputs["q_norm_g"]).reshape(2, 1, 64),
        "k_norm_g": f(inputs["k_norm_g"]).reshape(2, 1, 64),
        "rw_mu": f(inputs["rw_mu"]).reshape(2, 1184, 1),
        "rw_w0": f(inputs["rw_w0"]).reshape(2, 512, 1),
        "rw_w_b": f(inputs["rw_w_b"]).reshape(2, 128, 256),
        "rw_a0": f(inputs["rw_a0"]).reshape(2, 512, 1),
        "rw_a_b": f(inputs["rw_a_b"]).reshape(2, 128, 256),
        "rw_g_b": f(inputs["rw_g_b"]),
        "rw_k_k": f(inputs["rw_k_k"]).reshape(2, 256, 1),
        "rw_k_a": f(inputs["rw_k_a"]).reshape(2, 256, 1),
        "rw_r_k": f(inputs["rw_r_k"]).reshape(2, 256, 1),
        "rw_ln_w": f(inputs["rw_ln_w"]).reshape(2, 256, 1),
        "rw_ln_b": f(inputs["rw_ln_b"]).reshape(2, 256, 1),
        "router_w": f(inputs["router_w"]),
        "exp_w_gate": f(inputs["exp_w_gate"]),
        "exp_w_up": f(inputs["exp_w_up"]),
        "exp_w_down": f(inputs["exp_w_down"]),
        "consts": make_consts(),
    }
    t = np.arange(SEQ)
    row = (t // 64).astype(np.float32)
    col = (t % 64).astype(np.float32)
    inv = (10000.0 ** (-np.arange(0, 32, 2, dtype=np.float32) / 32)).astype(np.float32)
    ang = np.concatenate([row[:, None] * inv, col[:, None] * inv], axis=-1).astype(np.float32)
    shared["cs_tab"] = np.ascontiguousarray(np.concatenate([np.cos(ang), np.sin(ang)], axis=-1).astype(np.float32))
    maps = []
    for b in range(x.shape[0]):
        m = dict(shared)
        m["x"] = x[b]
        m["ctx"] = ctx[b]
        c2 = np.stack([c[b], c_ctx], axis=-1)
        m["c2T"] = np.ascontiguousarray(c2.reshape(8, 128, 2).transpose(1, 0, 2))
        maps.append(m)
    return maps


_NC_CACHE = {}

W_KEYS = ["ada_w", "ada_b", "norm1_g", "norm2_g", "w_in", "w_out", "conv_wT", "q_norm_g", "k_norm_g", "rw_mu", "rw_w0", "rw_w_b",
          "rw_a0", "rw_a_b", "rw_g_b", "rw_k_k", "rw_k_a", "rw_r_k", "rw_ln_w", "rw_ln_b", "router_w",
          "exp_w_gate", "exp_w_up", "exp_w_down"]


def kernel(**inputs):
    maps = prep_inputs(inputs)
    if "nc" not in _NC_CACHE:
        _NC_CACHE["nc"] = build(layers=[0, 1])
    nc = _NC_CACHE["nc"]
    res = run_bass_kernel_spmd(nc, maps, core_ids=list(range(8)))
    return np.stack([np.asarray(r["out"], dtype=np.float32) for r in res.results], axis=0)
```

```python
import math
from contextlib import ExitStack

import numpy as np
import concourse.bass as bass
import concourse.mybir as mybir
from concourse.bass_utils import run_bass_kernel_spmd

F32 = mybir.dt.float32
BF16 = mybir.dt.bfloat16
I32 = mybir.dt.int32
U32 = mybir.dt.uint32
AF = mybir.ActivationFunctionType
ALU = mybir.AluOpType
AX = mybir.AxisListType

ENGS = ("sync", "act", "dve", "pool", "pe")

D = 1024
SEQ = 4096
CTX = 256
NT = SEQ + CTX
NTILE = NT // 128
PROJ = 2720
NE = 16
CAP_L = 512
CAP_C = 32
LCH = 64


class Prog:
    SEMID = 0

    def __init__(self, nc):
        self.nc = nc
        self.streams = {e: [] for e in ENGS}
        self.ecount = {e: 0 for e in ENGS}
        self.seen = {e: {} for e in ENGS}
        self.bufs = {}
        self.dcount = {}
        self.sems = {}

    @staticmethod
    def _k(b):
        if isinstance(b, (str, tuple, int)):
            return b
        return b.name

    def _deps(self, eng, reads, writes):
        need = {}

        def add(ev):
            for k, v in ev.items():
                if need.get(k, 0) < v:
                    need[k] = v

        for b in reads:
            st = self.bufs.get(b)
            if st:
                add(st["w"])
        for b in writes:
            st = self.bufs.get(b)
            if st:
                add(st["w"])
                add(st["r"])
        waits = []
        seen = self.seen[eng]
        for k, v in need.items():
            if k[0] == "e" and k[1] == eng and eng == "pe":
                continue
            if seen.get(k, 0) >= v:
                continue
            seen[k] = v
            waits.append((k, v))
        return waits

    def _commit(self, reads, writes, ev):
        for b in reads:
            st = self.bufs.setdefault(b, {"w": {}, "r": {}})
            for k, v in ev.items():
                if st["r"].get(k, 0) < v:
                    st["r"][k] = v
        for b in writes:
            self.bufs[b] = {"w": dict(ev), "r": {}}

    EPOCH = 3000

    def op(self, eng, fn, r=(), w=(), rows=None):
        r = [self._k(b) for b in r]
        w = [self._k(b) for b in w]
        bk = [k for k in r if isinstance(k, tuple) and k[0] in ("pqb", "sqb")]
        if bk:
            r = [k for k in r if k not in bk]
            w = w + bk
        waits = self._deps(eng, r, w)
        self.ecount[eng] += 1
        ep = (self.ecount[eng] - 1) // self.EPOCH
        ek = ("e", eng, ep)
        ev = {ek: self.ecount[eng] - ep * self.EPOCH}
        if eng == "pe":
            if not hasattr(self, "pe_rows"):
                self.pe_rows = {}
            for bk_ in w:
                last = self.pe_rows.get(bk_)
                if last is not None and rows in (0, 64) and last[0] in (0, 64) and last[0] != rows:
                    for k_, v_ in last[1].items():
                        if self.seen[eng].get(k_, 0) < v_:
                            self.seen[eng][k_] = v_
                            waits.append((k_, v_))
                self.pe_rows[bk_] = (rows, ev)
        self.streams[eng].append((waits, fn, (ek, 1)))
        self._commit(r, w, ev)

    def dma(self, q, out, in_, r=(), w=(), sem=None, **kw):
        r = [self._k(b) for b in r]
        w = [self._k(b) for b in w]
        if sem is None:
            sem = (w[0] if w else r[0])
        waits = self._deps(q, r, w)
        k = ("d", sem)
        self.dcount[k] = self.dcount.get(k, 0) + 16
        ev = {k: self.dcount[k]}
        self.streams[q].append((waits, (lambda e: e.dma_start(out=out, in_=in_, **kw)), (k, 16)))
        self._commit(r, w, ev)

    def dma_fn(self, q, fn, r=(), w=(), sem=None):
        r = [self._k(b) for b in r]
        w = [self._k(b) for b in w]
        waits = self._deps(q, r, w)
        k = ("d", sem)
        self.dcount[k] = self.dcount.get(k, 0) + 16
        ev = {k: self.dcount[k]}
        self.streams[q].append((waits, fn, (k, 16)))
        self._commit(r, w, ev)

    def barrier(self):
        for eng in ENGS:
            waits = [(k, v) for k, v in self.dcount.items()]
            for e in ENGS:
                if e != eng and self.ecount[e] > 0:
                    ep = (self.ecount[e] - 1) // self.EPOCH
                    waits.append((("e", e, ep), self.ecount[e] - ep * self.EPOCH))
            self.streams[eng].append((waits, None, None))

    POOL = None

    def emit(self):
        nc = self.nc
        pool = Prog.POOL
        totals = {}
        keys = []
        for e in ENGS:
            for waits, fn, inc in self.streams[e]:
                for k, v in waits:
                    if k not in totals:
                        totals[k] = 0
                        keys.append(k)
                if inc:
                    if inc[0] not in totals:
                        totals[inc[0]] = 0
                        keys.append(inc[0])
                    totals[inc[0]] += inc[1]
        n = len(pool["h"])
        assert len(keys) <= n, len(keys)
        base = {}
        for i, k in enumerate(sorted(keys, key=str)):
            idx = (pool["next"] + i) % n
            self.sems[k] = pool["h"][idx]
            base[k] = pool["v"][idx]
            pool["v"][idx] += totals[k]
        pool["next"] = (pool["next"] + len(keys)) % n
        with ExitStack() as es:
            block = es.enter_context(nc.Block())
            handles = {"sync": block.sync, "act": block.scalar, "dve": block.vector,
                       "pool": block.gpsimd, "pe": block.tensor}
            for e in ENGS:
                stream = self.streams[e]

                def body(h, stream=stream):
                    for waits, fn, inc in stream:
                        for k, v in waits:
                            h.wait_ge(self.sems[k], base[k] + v)
                        if fn is not None:
                            ins = fn(h)
                            ins.then_inc(self.sems[inc[0]], inc[1])

                handles[e](body)


class Ctx:
    pass


def build(n_layers=2, dbg=None, upto=None, skip=(), small=False, rwsteps=None, zero_mix=False, layers=None):
    dbg = dbg or set()
    nc = bass.Bass("TRN2", target_bir_lowering=False)
    g = Ctx()
    g.nc = nc
    if layers is None:
        layers = list(range(n_layers))
    NL = len(layers)
    g.first = layers[0]
    g.last = layers[-1]
    g.wi = lambda l: l - layers[0]
    if layers[-1] == 0:
        dbg = set(dbg) | {"xcres"}

    def din(name, shape, dt=F32):
        return nc.dram_tensor(name, list(shape), dt, kind="ExternalInput").ap()

    def dscr(name, shape, dt=F32):
        kind = "ExternalOutput" if name in dbg else "Internal"
        return nc.dram_tensor(name, list(shape), dt, kind=kind).ap()

    I = Ctx()
    I.x = din("x", [SEQ, D])
    I.ctx = din("ctx", [CTX, D])
    I.c2T = din("c2T", [128, 8, 2])
    I.ada_w = din("ada_w", [NL, D, 6 * D])
    I.ada_b = din("ada_b", [NL, 1, 6 * D])
    I.n1g = din("norm1_g", [NL, 1, D])
    I.n2g = din("norm2_g", [NL, 1, D])
    I.w_in = din("w_in", [NL, D, PROJ])
    I.w_out = din("w_out", [NL, D, D])
    I.conv_wT = din("conv_wT", [NL, 256, 3])
    I.qg = din("q_norm_g", [NL, 1, 64])
    I.kg = din("k_norm_g", [NL, 1, 64])
    I.mu = din("rw_mu", [NL, 1184, 1])
    I.w0 = din("rw_w0", [NL, 512, 1])
    I.w_b = din("rw_w_b", [NL, 128, 256])
    I.a0 = din("rw_a0", [NL, 512, 1])
    I.a_b = din("rw_a_b", [NL, 128, 256])
    I.g_b = din("rw_g_b", [NL, 160, 256])
    I.k_k = din("rw_k_k", [NL, 256, 1])
    I.k_a = din("rw_k_a", [NL, 256, 1])
    I.r_k = din("rw_r_k", [NL, 256, 1])
    I.ln_w = din("rw_ln_w", [NL, 256, 1])
    I.ln_b = din("rw_ln_b", [NL, 256, 1])
    I.router = din("router_w", [NL, D, NE])
    esh = [NL, 1, 8, 8] if small else [NL, NE, D, D]
    I.wg = din("exp_w_gate", esh)
    I.wu = din("exp_w_up", esh)
    I.wd = din("exp_w_down", esh)
    g.rwsteps = rwsteps
    I.cs = din("cs_tab", [SEQ, 64])
    I.consts = din("consts", [128, 8 * 128])
    out = nc.dram_tensor("out", [SEQ, D], F32, kind="ExternalOutput").ap()

    S = Ctx()
    S.modv = dscr("modv", [NL, 2, 6, D])
    S.pfm = dscr("pfm", [1952, NT])
    S.qT = dscr("qT", [8, 64, NT], BF16)
    S.mixT = dscr("mixT", [D, NT], BF16)
    S.xres = dscr("xres", [SEQ, D])
    S.xcres = dscr("xcres", [CTX, D])
    S.h2l = dscr("h2l", [SEQ, D], BF16)
    S.h2c = dscr("h2c", [CTX, D], BF16)
    S.rwf = dscr("rwf", [10, 256, NT])
    g.I, g.S, g.out = I, S, out

    with ExitStack() as gs:
        def gsb(name, shape, dt=F32):
            return gs.enter_context(nc.sbuf_tensor("g_" + name, list(shape), dt))
        Prog.POOL = {"h": [gs.enter_context(nc.semaphore("gp%d" % i)) for i in range(72)], "v": [0] * 72, "next": 0}
        g.consts = gsb("consts", [128, 8, 128])
        g.identb = gsb("identb", [128, 128], BF16)
        g.kT = gsb("kT", [64, 2, NT], BF16)
        g.Vaug = gsb("Vaug", [128, NTILE, 2, 65], BF16)
        g.affT = gsb("affT", [NE, NT])
        phase_consts(g)
        phases = [phase_adaln, phase_proj, phase_conv, phase_attn, phase_rwfeat, phase_rwscan] + ([phase_zero_mix] if zero_mix else []) + [phase_wout, phase_moe]
        for l in layers:
            for ph in phases:
                if ph.__name__ in skip:
                    continue
                ph(g, l)
                if upto == (ph.__name__, l):
                    return nc
    return nc


C_ID, C_BONES, C_MS_IT, C_MI_IT, C_MS_TI, C_RESET, C_MS_IT_B, C_MI_IT_B = range(8)


def make_consts():
    c = np.zeros((8, 128, 128), np.float32)
    i = np.arange(128)[:, None]
    t = np.arange(128)[None, :]
    same = (i // 64) == (t // 64)
    c[C_ID] = np.eye(128)
    c[C_BONES] = same
    c[C_MS_IT] = same & (i < t)
    c[C_MI_IT] = same & (i <= t)
    c[C_MS_TI] = same & (t < i)
    c[C_RESET] = (t % 64 != 0) * np.ones((128, 1))
    c[C_MS_IT_B] = same & (i > t)
    c[C_MI_IT_B] = same & (i >= t)
    return np.ascontiguousarray(c.transpose(1, 0, 2).reshape(128, 8 * 128))


def phase_consts(g):
    nc = g.nc
    P = Prog(nc)
    P.dma("sync", g.consts[:], g.I.consts.rearrange("p (a b) -> p a b", a=8), w=[g.consts])
    P.op("dve", lambda e: e.tensor_copy(out=g.identb[:], in_=g.consts[:, C_ID, :]), r=[g.consts], w=[g.identb])
    P.op("pool", lambda e: e.memset(g.Vaug[:, :, :, 64:65], 1.0), w=[g.Vaug])
    P.barrier()
    P.emit()


def phase_adaln(g, l):
    nc, I, S = g.nc, g.I, g.S
    with ExitStack() as es:
        def sb(name, shape, dt=F32):
            return es.enter_context(nc.sbuf_tensor("a%d_" % l + name, list(shape), dt))
        c2 = sb("c2", [128, 8, 2])
        sc = sb("sc", [128, 8, 2])
        wt = [sb("wt%d" % i, [128, 8, 512]) for i in range(2)]
        bias = sb("bias", [2, 6 * D])
        mod = sb("mod", [2, 6 * D])
        gg = sb("gg", [2, 2, D])
        mv = sb("mv", [2, 6, D])
        ps = [es.enter_context(nc.psum_tensor("a%d_ps%d" % (l, i), [2, 512], F32)) for i in range(2)]
        P = Prog(nc)
        P.dma("sync", c2[:], I.c2T[:, :, :], w=[c2])
        P.dma("sync", bias[:], I.ada_b[g.wi(l), 0:1, :].to_broadcast([2, 6 * D]), w=[bias])
        P.dma("sync", gg[:, 0, :], I.n1g[g.wi(l), 0:1, :].to_broadcast([2, D]), w=[gg], sem="gg")
        P.dma("sync", gg[:, 1, :], I.n2g[g.wi(l), 0:1, :].to_broadcast([2, D]), w=[gg], sem="gg")
        P.op("act", lambda e: e.activation(out=sc[:], in_=c2[:], func=AF.Silu), r=[c2], w=[sc])
        wv = I.ada_w[g.wi(l)].rearrange("(kc p) n -> p kc n", p=128)
        for cc in range(12):
            b = cc % 2
            P.dma("sync" if b == 0 else "pool", wt[b][:], wv[:, :, cc * 512:(cc + 1) * 512], w=[wt[b]])
            for kc in range(8):
                P.op("pe", lambda e, kc=kc, b=b: e.matmul(out=ps[b][:], lhsT=sc[:, kc, :], rhs=wt[b][:, kc, :],
                                                           start=(kc == 0), stop=(kc == 7)),
                     r=[sc, wt[b]], w=[ps[b]])
            P.op("dve", lambda e, cc=cc, b=b: e.tensor_tensor(out=mod[:, cc * 512:(cc + 1) * 512], in0=ps[b][:],
                                                               in1=bias[:, cc * 512:(cc + 1) * 512], op=ALU.add),
                 r=[ps[b], bias], w=[mod])
        for j, (sci, shi, gti) in enumerate(((1, 0, 2), (4, 3, 5))):
            P.op("dve", lambda e, j=j, sci=sci: e.scalar_tensor_tensor(
                out=mv[:, 3 * j, :], in0=mod[:, sci * D:(sci + 1) * D], scalar=1.0, in1=gg[:, j, :],
                op0=ALU.add, op1=ALU.mult), r=[mod, gg], w=[mv])
            P.op("dve", lambda e, j=j, shi=shi: e.tensor_copy(out=mv[:, 3 * j + 1, :], in_=mod[:, shi * D:(shi + 1) * D]),
                 r=[mod], w=[mv])
            P.op("dve", lambda e, j=j, gti=gti: e.tensor_copy(out=mv[:, 3 * j + 2, :], in_=mod[:, gti * D:(gti + 1) * D]),
                 r=[mod], w=[mv])
        P.dma("sync", S.modv[g.wi(l)], mv[:], r=[mv], sem="mvst")
        P.barrier()
        P.emit()


def phase_proj(g, l):
    nc, I, S = g.nc, g.I, g.S
    with ExitStack() as es:
        def sb(name, shape, dt=F32):
            return es.enter_context(nc.sbuf_tensor("b%d_" % l + name, list(shape), dt))

        def psb(name, shape, dt=F32):
            return es.enter_context(nc.psum_tensor("b%d_" % l + name, list(shape), dt))
        wb = sb("wb", [128, 8, PROJ], BF16)
        m1 = [sb("m1_%d" % i, [128, D]) for i in range(2)]
        sh1 = [sb("sh1_%d" % i, [128, D]) for i in range(2)]
        qkg = sb("qkg", [128, 2, 64])
        cs = sb("cs", [128, 32, 64])
        xt = [sb("xt%d" % i, [128, D]) for i in range(2)]
        junk = sb("junk", [128, D])
        ss = [sb("ss%d" % i, [128, 1]) for i in range(2)]
        hb = [sb("hb%d" % i, [128, D], BF16) for i in range(2)]
        hT = [sb("hT%d" % i, [128, 8, 512], BF16) for i in range(2)]
        fm = [sb("fm%d" % i, [128, 512]) for i in range(3)]
        qkv = [sb("qkv%d" % i, [128, 768]) for i in range(2)]
        sq = sb("sq", [128, 640])
        ssq = sb("ssq", [128, 10])
        qn = sb("qn", [128, 10, 64])
        qr = sb("qr", [128, 10, 64])
        qrb = [sb("qrb%d" % i, [128, 10, 64], BF16) for i in range(2)]
        tmp = sb("tmp", [128, 10, 32])
        qTs = [sb("qTs%d" % i, [64, 8, 128], BF16) for i in range(2)]
        pT = [psb("pT%d" % i, [128, 8, 128], BF16) for i in range(2)]
        pF = [psb("pF%d" % i, [128, 512]) for i in range(2)]
        pA = psb("pA", [128, 512])
        pB = psb("pB", [128, 512])
        pQ = psb("pQ", [64, 8, 128], BF16)
        pQk = psb("pQk", [64, 8, 128], BF16)
        P = Prog(nc)
        wv = I.w_in[g.wi(l)].rearrange("(kc p) n -> p kc n", p=128)
        for (c0, c1) in ((0, 1024), (1024, 2048), (2048, PROJ)):
            P.dma("pool", wb[:, :, c0:c1], wv[:, :, c0:c1], w=[("wb", c0)], sem=("wb", c0))
        wbk = [("wb", 0), ("wb", 1024), ("wb", 2048)]
        for j in range(2):
            P.dma("sync", m1[j][:], S.modv[g.wi(l), j, 0:1, :].to_broadcast([128, D]), w=[m1[j]])
            P.dma("sync", sh1[j][:], S.modv[g.wi(l), j, 1:2, :].to_broadcast([128, D]), w=[sh1[j]])
        P.dma("sync", qkg[:, 0, :], I.qg[g.wi(l), 0:1, :].to_broadcast([128, 64]), w=[qkg], sem="qkg")
        P.dma("sync", qkg[:, 1, :], I.kg[g.wi(l), 0:1, :].to_broadcast([128, 64]), w=[qkg], sem="qkg")
        P.dma("sync", cs[:], I.cs.rearrange("(i p) c -> p i c", p=128), w=[cs])
        fchunks = [(c, 128, c) for c in range(0, 768, 128)]
        for j in range(10):
            c = 1536 + j * 128
            wdt = min(128, PROJ - c)
            fchunks.append((c, wdt, 768 + j * 128))
        sts = [(0, 2)] + [(2 + 4 * s, 4) for s in range(8)]
        fmi = 0
        for si, (t0, ntile) in enumerate(sts):
            hTs = hT[si % 2]
            ntok = ntile * 128
            for ti in range(ntile):
                i = t0 + ti
                b = i % 2
                j = 1 if i < 2 else 0
                if i < 2:
                    src = (I.ctx if l == g.first else S.xcres)[i * 128:(i + 1) * 128, :]
                else:
                    src = (I.x if l == g.first else S.xres)[(i - 2) * 128:(i - 1) * 128, :]
                P.dma("sync", xt[b][:], src, w=[xt[b]])
                P.op("act", lambda e, b=b: e.activation(out=junk[:], in_=xt[b][:], func=AF.Square, accum_out=ss[b][:]),
                     r=[xt[b]], w=[junk, ss[b]])
                P.op("dve", lambda e, b=b: e.tensor_scalar(out=ss[b][:], in0=ss[b][:], scalar1=1.0 / D, scalar2=1e-6,
                                                            op0=ALU.mult, op1=ALU.add), r=[ss[b]], w=[ss[b]])
                P.op("act", lambda e, b=b: e.activation(out=ss[b][:], in_=ss[b][:], func=AF.Sqrt), r=[ss[b]], w=[ss[b]])
                P.op("dve", lambda e, b=b: e.reciprocal(out=ss[b][:], in_=ss[b][:]), r=[ss[b]], w=[ss[b]])
                P.op("dve", lambda e, b=b, j=j: e.scalar_tensor_tensor(out=xt[b][:], in0=xt[b][:], scalar=ss[b][:, 0:1],
                                                                       in1=m1[j][:], op0=ALU.mult, op1=ALU.mult),
                     r=[xt[b], ss[b], m1[j]], w=[xt[b]])
                P.op("pool", lambda e, b=b, j=j: e.tensor_tensor(out=hb[b][:], in0=xt[b][:], in1=sh1[j][:], op=ALU.add),
                     r=[xt[b], sh1[j]], w=[hb[b]])
                for half in range(2):
                    for k4 in range(4):
                        kc = half * 4 + k4
                        P.op("pe", lambda e, b=b, kc=kc, k4=k4, half=half: e.transpose(
                            out=pT[half][:, k4, :], in_=hb[b][:, kc * 128:(kc + 1) * 128], identity=g.identb[:]),
                            r=[hb[b], g.identb], w=[pT[half]])
                    eng = "act" if half == 0 else "dve"
                    if eng == "act":
                        P.op("act", lambda e, half=half, ti=ti, hTs=hTs: e.activation(
                            out=hTs[:, half * 4:(half + 1) * 4, ti * 128:(ti + 1) * 128], in_=pT[half][:, 0:4, :], func=AF.Copy),
                            r=[pT[half]], w=[hTs])
                    else:
                        P.op("dve", lambda e, half=half, ti=ti, hTs=hTs: e.tensor_copy(
                            out=hTs[:, half * 4:(half + 1) * 4, ti * 128:(ti + 1) * 128], in_=pT[half][:, 0:4, :]),
                            r=[pT[half]], w=[hTs])
                for kc in range(8):
                    P.op("pe", lambda e, kc=kc, ti=ti, hTs=hTs: e.matmul(
                        out=pA[:], lhsT=hTs[:, kc, ti * 128:(ti + 1) * 128], rhs=wb[:, kc, 768:1280],
                        start=(kc == 0), stop=(kc == 7)), r=[hTs] + wbk, w=[pA])
                for kc in range(8):
                    P.op("pe", lambda e, kc=kc, ti=ti, hTs=hTs: e.matmul(
                        out=pB[:, 0:256], lhsT=hTs[:, kc, ti * 128:(ti + 1) * 128], rhs=wb[:, kc, 1280:1536],
                        start=(kc == 0), stop=(kc == 7)), r=[hTs] + wbk, w=[pB])
                qv = qkv[b]
                P.op("act", lambda e, qv=qv: e.activation(out=qv[:, 0:512], in_=pA[:], func=AF.Copy), r=[pA], w=[qv])
                P.op("act", lambda e, qv=qv: e.activation(out=qv[:, 512:768], in_=pB[:, 0:256], func=AF.Copy), r=[pB], w=[qv])
                P.op("pool", lambda e, qv=qv, i=i: e.tensor_copy(
                    out=g.Vaug[:, i, :, 0:64], in_=qv[:, 640:768].rearrange("p (g d) -> p g d", g=2)),
                    r=[qv], w=[("Vaug", i)])
                P.op("dve", lambda e, qv=qv: e.tensor_tensor(out=sq[:], in0=qv[:, 0:640], in1=qv[:, 0:640], op=ALU.mult),
                     r=[qv], w=[sq])
                P.op("dve", lambda e: e.tensor_reduce(out=ssq[:], in_=sq[:].rearrange("p (h d) -> p h d", h=10),
                                                       axis=AX.X, op=ALU.add), r=[sq], w=[ssq])
                P.op("dve", lambda e: e.tensor_scalar(out=ssq[:], in0=ssq[:], scalar1=1.0 / 64, scalar2=1e-6,
                                                       op0=ALU.mult, op1=ALU.add), r=[ssq], w=[ssq])
                P.op("act", lambda e: e.activation(out=ssq[:], in_=ssq[:], func=AF.Sqrt), r=[ssq], w=[ssq])
                P.op("dve", lambda e: e.reciprocal(out=ssq[:], in_=ssq[:]), r=[ssq], w=[ssq])
                P.op("dve", lambda e, qv=qv: e.tensor_tensor(
                    out=qn[:], in0=qv[:, 0:640].rearrange("p (h d) -> p h d", h=10),
                    in1=ssq[:].unsqueeze(2).to_broadcast([128, 10, 64]), op=ALU.mult), r=[qv, ssq], w=[qn])
                P.op("dve", lambda e: e.tensor_tensor(out=qn[:, 0:8, :], in0=qn[:, 0:8, :],
                                                       in1=qkg[:, 0:1, :].to_broadcast([128, 8, 64]), op=ALU.mult),
                     r=[qn, qkg], w=[qn])
                P.op("dve", lambda e: e.tensor_tensor(out=qn[:, 8:10, :], in0=qn[:, 8:10, :],
                                                       in1=qkg[:, 1:2, :].to_broadcast([128, 2, 64]), op=ALU.mult),
                     r=[qn, qkg], w=[qn])
                qb = qrb[b]
                if i < 2:
                    P.op("dve", lambda e, qb=qb: e.tensor_copy(out=qb[:], in_=qn[:]), r=[qn], w=[qb])
                else:
                    li = i - 2
                    cosb = cs[:, li:li + 1, 0:32].to_broadcast([128, 10, 32])
                    sinb = cs[:, li:li + 1, 32:64].to_broadcast([128, 10, 32])
                    x1 = qn[:, :, 0:32]
                    x2 = qn[:, :, 32:64]
                    P.op("dve", lambda e, cosb=cosb: e.tensor_tensor(out=qr[:, :, 0:32], in0=qn[:, :, 0:32], in1=cosb, op=ALU.mult),
                         r=[qn, cs], w=[qr])
                    P.op("dve", lambda e, sinb=sinb: e.tensor_tensor(out=tmp[:], in0=qn[:, :, 32:64], in1=sinb, op=ALU.mult),
                         r=[qn, cs], w=[tmp])
                    P.op("dve", lambda e, qb=qb: e.tensor_tensor(out=qb[:, :, 0:32], in0=qr[:, :, 0:32], in1=tmp[:], op=ALU.subtract),
                         r=[qr, tmp], w=[qb])
                    P.op("dve", lambda e, sinb=sinb: e.tensor_tensor(out=qr[:, :, 32:64], in0=qn[:, :, 0:32], in1=sinb, op=ALU.mult),
                         r=[qn, cs], w=[qr])
                    P.op("dve", lambda e, cosb=cosb: e.tensor_tensor(out=tmp[:], in0=qn[:, :, 32:64], in1=cosb, op=ALU.mult),
                         r=[qn, cs, qb], w=[tmp])
                    P.op("dve", lambda e, qb=qb: e.tensor_tensor(out=qb[:, :, 32:64], in0=qr[:, :, 32:64], in1=tmp[:], op=ALU.add),
                         r=[qr, tmp], w=[qb])
                for h in range(8):
                    P.op("pe", lambda e, h=h, qb=qb: e.transpose(out=pQ[:, h, :], in_=qb[:, h, :], identity=g.identb[:]),
                         r=[qb, g.identb], w=[pQ])
                for h in range(2):
                    P.op("pe", lambda e, h=h, qb=qb: e.transpose(out=pQk[:, h, :], in_=qb[:, 8 + h, :], identity=g.identb[:]),
                         r=[qb, g.identb], w=[pQk])
                qs = qTs[b]
                P.op("act", lambda e, qs=qs: e.activation(out=qs[:], in_=pQ[:], func=AF.Copy), r=[pQ], w=[qs])
                P.op("dve", lambda e, i=i: e.tensor_copy(out=g.kT[:, :, i * 128:(i + 1) * 128], in_=pQk[:, 0:2, :]),
                     r=[pQk], w=[("kT", i)])
                P.dma("pool", S.qT[:, :, i * 128:(i + 1) * 128].rearrange("h d t -> d h t"), qs[:], r=[qs], sem=("qs", b))
            for (c0, wdt, r0) in fchunks:
                pb = pF[fmi % 2]
                fb = fm[fmi % 3]
                for kc in range(8):
                    P.op("pe", lambda e, kc=kc, c0=c0, wdt=wdt, pb=pb, hTs=hTs, ntok=ntok: e.matmul(
                        out=pb[0:wdt, 0:ntok], lhsT=wb[:, kc, c0:c0 + wdt], rhs=hTs[:, kc, 0:ntok],
                        start=(kc == 0), stop=(kc == 7)), r=[hTs] + wbk, w=[pb])
                if fmi % 2 == 0:
                    P.op("act", lambda e, wdt=wdt, pb=pb, fb=fb, ntok=ntok: e.activation(
                        out=fb[0:wdt, 0:ntok], in_=pb[0:wdt, 0:ntok], func=AF.Copy), r=[pb], w=[fb])
                else:
                    P.op("dve", lambda e, wdt=wdt, pb=pb, fb=fb, ntok=ntok: e.tensor_copy(
                        out=fb[0:wdt, 0:ntok], in_=pb[0:wdt, 0:ntok]), r=[pb], w=[fb])
                P.dma("sync", S.pfm[r0:r0 + wdt, t0 * 128:t0 * 128 + ntok], fb[0:wdt, 0:ntok], r=[fb], sem=("fm", fmi % 3))
                fmi += 1
        P.barrier()
        P.emit()


def phase_conv(g, l):
    nc, I, S = g.nc, g.I, g.S
    with ExitStack() as es:
        def sb(name, shape, dt=F32):
            return es.enter_context(nc.sbuf_tensor("c%d_" % l + name, list(shape), dt))
        Bt = sb("Bt", [128, SEQ])
        Ct = sb("Ct", [128, SEQ])
        Ut = sb("Ut", [128, SEQ])
        zp = sb("zp", [128, SEQ + 2])
        acc = sb("acc", [128, SEQ])
        ob = sb("ob", [128, SEQ], BF16)
        cw = sb("cw", [128, 2, 3])
        P = Prog(nc)
        P.dma("sync", cw[:], I.conv_wT[g.wi(l)].rearrange("(c p) k -> p c k", p=128), w=[cw])
        seqs = [(CTX, SEQ)] + ([(0, CTX)] if l == 0 else [])
        for (t0, T) in seqs:
            for cc in range(2):
                P.dma("sync", Bt[:, 0:T], S.pfm[cc * 128:(cc + 1) * 128, t0:t0 + T], w=[Bt])
                P.dma("sync", Ct[:, 0:T], S.pfm[256 + cc * 128:256 + (cc + 1) * 128, t0:t0 + T], w=[Ct])
                P.dma("pool", Ut[:, 0:T], S.pfm[512 + cc * 128:512 + (cc + 1) * 128, t0:t0 + T], w=[Ut])
                P.op("pool", lambda e, T=T: e.memset(zp[:, 0:1], 0.0), w=[zp])
                P.op("pool", lambda e, T=T: e.memset(zp[:, T + 1:T + 2], 0.0), w=[zp])
                P.op("dve", lambda e, T=T: e.tensor_tensor(out=zp[:, 1:T + 1], in0=Ct[:, 0:T], in1=Ut[:, 0:T], op=ALU.mult),
                     r=[Ct, Ut], w=[zp])
                P.op("dve", lambda e, T=T, cc=cc: e.tensor_scalar(out=acc[:, 0:T], in0=zp[:, 0:T], scalar1=cw[:, cc, 0:1],
                                                                   scalar2=None, op0=ALU.mult), r=[zp, cw], w=[acc])
                P.op("dve", lambda e, T=T, cc=cc: e.scalar_tensor_tensor(out=acc[:, 0:T], in0=zp[:, 1:T + 1], scalar=cw[:, cc, 1:2],
                                                                         in1=acc[:, 0:T], op0=ALU.mult, op1=ALU.add),
                     r=[zp, cw, acc], w=[acc])
                P.op("dve", lambda e, T=T, cc=cc: e.scalar_tensor_tensor(out=acc[:, 0:T], in0=zp[:, 2:T + 2], scalar=cw[:, cc, 2:3],
                                                                         in1=acc[:, 0:T], op0=ALU.mult, op1=ALU.add),
                     r=[zp, cw, acc], w=[acc])
                P.op("pool", lambda e, T=T: e.tensor_tensor(out=ob[:, 0:T], in0=acc[:, 0:T], in1=Bt[:, 0:T], op=ALU.mult),
                     r=[acc, Bt], w=[ob])
                P.dma("sync", S.mixT[cc * 128:(cc + 1) * 128, t0:t0 + T], ob[:, 0:T], r=[ob], sem="obst")
        P.barrier()
        P.emit()


def phase_attn(g, l):
    nc, I, S = g.nc, g.I, g.S
    with ExitStack() as es:
        def sb(name, shape, dt=F32):
            return es.enter_context(nc.sbuf_tensor("d%d_" % l + name, list(shape), dt))

        def psb(name, shape, dt=F32):
            return es.enter_context(nc.psum_tensor("d%d_" % l + name, list(shape), dt))
        qc = [sb("qc%d" % i, [64, 512], BF16) for i in range(2)]
        eS = [sb("eS%d" % i, [128, 512], BF16) for i in range(3)]
        rs = sb("rs", [128, 512])
        rsb = sb("rsb", [64, 512])
        ob = [sb("ob%d" % i, [64, 512], BF16) for i in range(2)]
        nb = sb("nb", [128, 1])
        pS = [psb("pS%d" % i, [128, 512]) for i in range(3)]
        pO = [psb("pO%d" % i, [128, 512]) for i in range(2)]
        pR = psb("pR", [64, 512])
        P = Prog(nc)
        P.op("pool", lambda e: e.memset(nb[:], -8.0), w=[nb])
        jobs = []
        if l == 0:
            for h in range(8):
                jobs.append((h, 0, CTX, [0, 1]))
        for h in range(8):
            for qi in range(8):
                jobs.append((h, CTX + qi * 512, 512, list(range(NTILE))))
        cnt = 0
        for ji, (h, q0, nq, kts) in enumerate(jobs):
            gkv = h // 4
            qb = qc[ji % 2]
            po = pO[ji % 2]
            P.dma("sync", qb[:, 0:nq], S.qT[h, :, q0:q0 + nq], w=[qb])
            for ki, kt in enumerate(kts):
                ps = pS[cnt % 3]
                ee = eS[cnt % 3]
                cnt += 1
                P.op("pe", lambda e, ps=ps, kt=kt, gkv=gkv, qb=qb, nq=nq: e.matmul(
                    out=ps[:, 0:nq], lhsT=g.kT[:, gkv, kt * 128:(kt + 1) * 128], rhs=qb[:, 0:nq], start=True, stop=True),
                    r=[qb, ("kT", kt)], w=[ps])
                P.op("act", lambda e, ps=ps, ee=ee, nq=nq: e.activation(out=ee[:, 0:nq], in_=ps[:, 0:nq], func=AF.Exp,
                                                                       bias=nb[:, 0:1], scale=0.125),
                     r=[ps, nb], w=[ee])
                P.op("pe", lambda e, po=po, kt=kt, gkv=gkv, ee=ee, nq=nq, ki=ki, nk=len(kts): e.matmul(
                    out=po[0:65, 0:nq], lhsT=g.Vaug[:, kt, gkv, :], rhs=ee[:, 0:nq], start=(ki == 0), stop=(ki == nk - 1)),
                    r=[ee, ("Vaug", kt), g.Vaug], w=[po])
            P.op("dve", lambda e, po=po, nq=nq: e.reciprocal(out=rs[64:65, 0:nq], in_=po[64:65, 0:nq]), r=[po], w=[rs])
            P.op("pe", lambda e, nq=nq: e.matmul(out=pR[:, 0:nq], lhsT=g.consts[64:65, C_BONES, 64:128], rhs=rs[64:65, 0:nq],
                                                  start=True, stop=True), r=[rs, g.consts], w=[pR])
            P.op("act", lambda e, nq=nq: e.activation(out=rsb[:, 0:nq], in_=pR[:, 0:nq], func=AF.Copy), r=[pR], w=[rsb])
            o = ob[ji % 2]
            P.op("dve", lambda e, po=po, o=o, nq=nq: e.tensor_tensor(out=o[:, 0:nq], in0=po[0:64, 0:nq], in1=rsb[:, 0:nq], op=ALU.mult),
                 r=[po, rsb], w=[o])
            P.dma("pool", S.mixT[256 + h * 64:256 + (h + 1) * 64, q0:q0 + nq], o[:, 0:nq], r=[o], sem=("ob", ji % 2))
        P.barrier()
        P.emit()


NEG_EM05 = -math.exp(-0.5)


def phase_rwfeat(g, l):
    nc, I, S = g.nc, g.I, g.S
    with ExitStack() as es:
        def sb(name, shape, dt=F32):
            return es.enter_context(nc.sbuf_tensor("e%d_" % l + name, list(shape), dt))

        def psb(name, shape, dt=F32):
            return es.enter_context(nc.psum_tensor("e%d_" % l + name, list(shape), dt))
        SEG = 512
        rch = [("r0", 768, 128), ("r1", 896, 128), ("k0", 1024, 128), ("k1", 1152, 128), ("v0", 1280, 128),
               ("v1", 1408, 128), ("wl", 1536, 128), ("al", 1664, 128), ("g0", 1792, 128), ("g1", 1920, 32)]
        mu = sb("mu", [128, 10])
        omu = sb("omu", [128, 10])
        hmu = sb("hmu", [128, 10])
        pt = [sb("pt%d" % i, [128, SEG + 2]) for i in range(3)]
        s1 = [sb("s1%d" % i, [128, SEG]) for i in range(2)]
        sh = {nm: sb("sh_" + nm, [128, SEG]) for nm, _, _ in rch}
        w0c = sb("w0c", [128, 4])
        a0c = sb("a0c", [128, 4])
        kkc = sb("kkc", [128, 2])
        kac = sb("kac", [128, 2])
        omka = sb("omka", [128, 2])
        wbt = sb("wbt", [128, 256])
        abt = sb("abt", [128, 256])
        gb0 = sb("gb0", [128, 256])
        gb1 = sb("gb1", [32, 256])
        twl = sb("twl", [128, SEG])
        sg0 = sb("sg0", [128, SEG])
        sg1 = sb("sg1", [32, SEG])
        lw = [sb("lw%d" % i, [128, SEG]) for i in range(4)]
        asg = [sb("asg%d" % i, [128, SEG]) for i in range(4)]
        kk = [sb("kk%d" % i, [128, SEG]) for i in range(2)]
        sq = sb("sq", [128, SEG])
        rn = sb("rn", [128, SEG])
        kd = [sb("kd%d" % i, [128, SEG]) for i in range(4)]
        bd = [sb("bd%d" % i, [128, SEG]) for i in range(4)]
        gt = [sb("gt%d" % i, [128, SEG]) for i in range(2)]
        tq = sb("tq", [128, SEG])
        pp = [psb("pp%d" % i, [128, SEG]) for i in range(6)]
        P = Prog(nc)
        P.op("pool", lambda e: e.memset(mu[:, 9:10], 0.0), w=[mu])
        for ci, (nm, r0, nr) in enumerate(rch):
            P.dma("sync", mu[0:nr, ci:ci + 1], I.mu[g.wi(l), r0 - 768:r0 - 768 + nr, :], w=[mu], sem="mu")
        P.op("dve", lambda e: e.tensor_scalar(out=omu[:], in0=mu[:], scalar1=-1.0, scalar2=1.0, op0=ALU.mult, op1=ALU.add),
             r=[mu], w=[omu])
        P.op("dve", lambda e: e.tensor_scalar(out=hmu[:], in0=mu[:], scalar1=0.5, scalar2=None, op0=ALU.mult), r=[mu], w=[hmu])
        for d in range(2):
            for cc in range(2):
                P.dma("sync", w0c[:, d * 2 + cc:d * 2 + cc + 1], I.w0[g.wi(l), d * 256 + cc * 128:d * 256 + (cc + 1) * 128, :], w=[w0c], sem="w0c")
                P.dma("sync", a0c[:, d * 2 + cc:d * 2 + cc + 1], I.a0[g.wi(l), d * 256 + cc * 128:d * 256 + (cc + 1) * 128, :], w=[a0c], sem="a0c")
        for cc in range(2):
            P.dma("sync", kkc[:, cc:cc + 1], I.k_k[g.wi(l), cc * 128:(cc + 1) * 128, :], w=[kkc], sem="kkc")
            P.dma("sync", kac[:, cc:cc + 1], I.k_a[g.wi(l), cc * 128:(cc + 1) * 128, :], w=[kac], sem="kac")
        P.op("dve", lambda e: e.tensor_scalar(out=omka[:], in0=kac[:], scalar1=-1.0, scalar2=1.0, op0=ALU.mult, op1=ALU.add),
             r=[kac], w=[omka])
        P.dma("sync", wbt[:], I.w_b[g.wi(l)], w=[wbt])
        P.dma("sync", abt[:], I.a_b[g.wi(l)], w=[abt])
        P.dma("sync", gb0[:], I.g_b[g.wi(l), 0:128, :], w=[gb0])
        P.dma("sync", gb1[:], I.g_b[g.wi(l), 128:160, :], w=[gb1])
        segs = [(0, 0, CTX, CTX)] + [(CTX, CTX + i * SEG, SEG, SEQ) for i in range(8)]
        pti = 0
        sti = 0
        ppi = 0

        def store(idx, cc, src, n, t0):
            nonlocal sti
            q = "sync" if sti % 2 == 0 else "pool"
            sti += 1
            P.dma(q, S.rwf[idx, cc * 128:(cc + 1) * 128, t0:t0 + n], src[:, 0:n], r=[src], sem=("st", src.name))

        for (sq0, t0, n, slen) in segs:
            for ci, (nm, r0, nr) in enumerate(rch):
                p_ = pt[pti % 3]
                s_ = s1[pti % 2]
                pti += 1
                lo = t0 - 1
                hi = t0 + n + 1
                dlo, dhi = 0, n + 2
                if t0 == sq0:
                    lo += 1
                    dlo = 1
                    P.op("pool", lambda e, p_=p_, nr=nr: e.memset(p_[0:nr, 0:1], 0.0), w=[p_])
                if t0 + n == sq0 + slen:
                    hi -= 1
                    dhi = n + 1
                    P.op("pool", lambda e, p_=p_, nr=nr, n=n: e.memset(p_[0:nr, n + 1:n + 2], 0.0), w=[p_])
                P.dma("sync" if ci % 2 == 0 else "pool", p_[0:nr, dlo:dhi], S.pfm[r0:r0 + nr, lo:hi], w=[p_])
                P.op("pool", lambda e, p_=p_, s_=s_, nr=nr, n=n: e.tensor_tensor(out=s_[0:nr, 0:n], in0=p_[0:nr, 0:n], in1=p_[0:nr, 2:n + 2], op=ALU.add),
                     r=[p_], w=[s_])
                P.op("dve", lambda e, p_=p_, nr=nr, n=n, ci=ci, nm=nm: e.tensor_scalar(out=sh[nm][0:nr, 0:n], in0=p_[0:nr, 1:n + 1], scalar1=omu[0:nr, ci:ci + 1],
                                                                                scalar2=None, op0=ALU.mult), r=[p_, omu], w=[sh[nm]])
                P.op("dve", lambda e, s_=s_, nr=nr, n=n, ci=ci, nm=nm: e.scalar_tensor_tensor(out=sh[nm][0:nr, 0:n], in0=s_[0:nr, 0:n], scalar=hmu[0:nr, ci:ci + 1],
                                                                                       in1=sh[nm][0:nr, 0:n], op0=ALU.mult, op1=ALU.add),
                     r=[s_, hmu, sh[nm]], w=[sh[nm]])
            P.op("act", lambda e, n=n: e.activation(out=twl[:, 0:n], in_=sh["wl"][:, 0:n], func=AF.Tanh), r=[sh["wl"]], w=[twl])
            P.op("act", lambda e, n=n: e.activation(out=sg0[:, 0:n], in_=sh["g0"][:, 0:n], func=AF.Sigmoid), r=[sh["g0"]], w=[sg0])
            P.op("act", lambda e, n=n: e.activation(out=sg1[:, 0:n], in_=sh["g1"][0:32, 0:n], func=AF.Sigmoid), r=[sh["g1"]], w=[sg1])
            for d in range(2):
                for cc in range(2):
                    ix = d * 2 + cc
                    pw = pp[ppi % 6]
                    ppi += 1
                    P.op("pe", lambda e, pw=pw, d=d, cc=cc, n=n: e.matmul(out=pw[:, 0:n], lhsT=wbt[d * 64:(d + 1) * 64, cc * 128:(cc + 1) * 128],
                                                                       rhs=twl[d * 64:(d + 1) * 64, 0:n], start=True, stop=True),
                         r=[wbt, twl], w=[pw])
                    P.op("act", lambda e, pw=pw, ix=ix, n=n: e.activation(out=lw[ix][:, 0:n], in_=pw[:, 0:n], func=AF.Sigmoid, bias=w0c[:, ix:ix + 1]),
                         r=[pw, w0c], w=[lw[ix]])
                    P.op("pool", lambda e, ix=ix, n=n: e.tensor_scalar(out=lw[ix][:, 0:n], in0=lw[ix][:, 0:n], scalar1=NEG_EM05, scalar2=None, op0=ALU.mult),
                         r=[lw[ix]], w=[lw[ix]])
                    store(7 + d, cc, lw[ix], n, t0)
                    pa = pp[ppi % 6]
                    ppi += 1
                    P.op("pe", lambda e, pa=pa, d=d, cc=cc, n=n: e.matmul(out=pa[:, 0:n], lhsT=abt[d * 64:(d + 1) * 64, cc * 128:(cc + 1) * 128],
                                                                       rhs=sh["al"][d * 64:(d + 1) * 64, 0:n], start=True, stop=True),
                         r=[abt, sh["al"]], w=[pa])
                    P.op("act", lambda e, pa=pa, ix=ix, n=n: e.activation(out=asg[ix][:, 0:n], in_=pa[:, 0:n], func=AF.Sigmoid, bias=a0c[:, ix:ix + 1]),
                         r=[pa, a0c], w=[asg[ix]])
            for cc in range(2):
                kx = sh["k%d" % cc]
                P.op("dve", lambda e, cc=cc, kx=kx, n=n: e.tensor_scalar(out=kk[cc][:, 0:n], in0=kx[:, 0:n], scalar1=kkc[:, cc:cc + 1], scalar2=None, op0=ALU.mult),
                     r=[kx, kkc], w=[kk[cc]])
                P.op("pool", lambda e, cc=cc, n=n: e.tensor_tensor(out=sq[:, 0:n], in0=kk[cc][:, 0:n], in1=kk[cc][:, 0:n], op=ALU.mult),
                     r=[kk[cc]], w=[sq])
                pn = pp[ppi % 6]
                ppi += 1
                P.op("pe", lambda e, pn=pn, n=n: e.matmul(out=pn[:, 0:n], lhsT=g.consts[:, C_BONES, :], rhs=sq[:, 0:n], start=True, stop=True),
                     r=[sq, g.consts], w=[pn])
                P.op("act", lambda e, pn=pn, n=n: e.activation(out=rn[:, 0:n], in_=pn[:, 0:n], func=AF.Sqrt), r=[pn], w=[rn])
                P.op("dve", lambda e, n=n: e.tensor_scalar(out=rn[:, 0:n], in0=rn[:, 0:n], scalar1=1e-12, scalar2=None, op0=ALU.max), r=[rn], w=[rn])
                P.op("dve", lambda e, n=n: e.reciprocal(out=rn[:, 0:n], in_=rn[:, 0:n]), r=[rn], w=[rn])
                P.op("dve", lambda e, cc=cc, n=n: e.tensor_tensor(out=kk[cc][:, 0:n], in0=kk[cc][:, 0:n], in1=rn[:, 0:n], op=ALU.mult),
                     r=[kk[cc], rn], w=[kk[cc]])
                store(4, cc, kk[cc], n, t0)
                store(0, cc, sh["r%d" % cc], n, t0)
                store(3, cc, sh["v%d" % cc], n, t0)
                for d in range(2):
                    ix = d * 2 + cc
                    P.op("dve", lambda e, ix=ix, cc=cc, n=n: e.tensor_scalar(out=tq[:, 0:n], in0=asg[ix][:, 0:n], scalar1=kac[:, cc:cc + 1],
                                                                         scalar2=omka[:, cc:cc + 1], op0=ALU.mult, op1=ALU.add),
                         r=[asg[ix], kac, omka], w=[tq])
                    P.op("dve", lambda e, ix=ix, kx=kx, n=n: e.tensor_tensor(out=kd[ix][:, 0:n], in0=tq[:, 0:n], in1=kx[:, 0:n], op=ALU.mult),
                         r=[tq, kx], w=[kd[ix]])
                    store(1 + d, cc, kd[ix], n, t0)
                    P.op("pool", lambda e, ix=ix, cc=cc, n=n: e.tensor_tensor(out=bd[ix][:, 0:n], in0=kk[cc][:, 0:n], in1=asg[ix][:, 0:n], op=ALU.mult),
                         r=[kk[cc], asg[ix]], w=[bd[ix]])
                    store(5 + d, cc, bd[ix], n, t0)
                pg = pp[ppi % 6]
                ppi += 1
                P.op("pe", lambda e, pg=pg, cc=cc, n=n: e.matmul(out=pg[:, 0:n], lhsT=gb0[:, cc * 128:(cc + 1) * 128], rhs=sg0[:, 0:n], start=True, stop=False),
                     r=[gb0, sg0], w=[pg])
                P.op("pe", lambda e, pg=pg, cc=cc, n=n: e.matmul(out=pg[:, 0:n], lhsT=gb1[:, cc * 128:(cc + 1) * 128], rhs=sg1[:, 0:n], start=False, stop=True),
                     r=[gb1, sg1], w=[pg])
                P.op("act", lambda e, pg=pg, cc=cc, n=n: e.activation(out=gt[cc][:, 0:n], in_=pg[:, 0:n], func=AF.Copy), r=[pg], w=[gt[cc]])
                store(9, cc, gt[cc], n, t0)
        P.barrier()
        P.emit()


def phase_rwscan(g, l):
    nc, I, S = g.nc, g.I, g.S
    with ExitStack() as es:
        def sb(name, shape, dt=F32):
            return es.enter_context(nc.sbuf_tensor("f%d_" % l + name, list(shape), dt))

        def psb(name, shape, dt=F32):
            return es.enter_context(nc.psum_tensor("f%d_" % l + name, list(shape), dt))
        ident = g.consts[:, C_ID, :]
        ybuf = [[sb("y%d%d" % (p, d), [128, NT]) for d in range(2)] for p in range(2)]
        E64 = sb("E64", [128, 64])
        U = [[None, None], [None, None]]
        for d in range(2):
            for p in range(2):
                if p == 1:
                    U[d][1] = U[d][0]
                    continue
                u = Ctx()
                n = "%d%d" % (d, p)
                u.f = [sb("ld%d_" % i + n, [128, 128]) for i in range(6)]
                u.Lc = sb("Lc" + n, [128, 128])
                u.LC = sb("LC" + n, [128, 128])
                u.t1 = sb("t1" + n, [128, 128])
                u.tA = sb("tA" + n, [128, 128])
                u.tW = sb("tW" + n, [128, 128])
                u.eP = sb("eP" + n, [128, 128])
                u.eN = sb("eN" + n, [128, 128])
                u.eA = sb("eA" + n, [128, 128])
                u.eW = sb("eW" + n, [128, 128])
                u.WL = sb("WL" + n, [128, 2])
                u.ar = sb("ar" + n, [128, 256])
                u.bt = sb("bt" + n, [128, 128])
                u.kt = sb("kt" + n, [128, 128])
                u.bW = sb("bW" + n, [128, 128])
                u.kW = sb("kW" + n, [128, 128])
                u.Dg = sb("Dg" + n, [128, 2, 64])
                u.TM = sb("TM" + n, [128, 4, 128])
                u.q = []
                for hh in range(2):
                    q = Ctx()
                    m = n + "%d" % hh
                    q.XTR = sb("XTR" + m, [128, 256])
                    q.KTR = sb("KTR" + m, [128, 256])
                    q.X = [sb("X%d_" % i + m, [128, 128]) for i in range(2)]
                    q.XT = [sb("XT%d_" % i + m, [128, 128]) for i in range(2)]
                    q.PT = [sb("PT%d_" % i + m, [128, 128]) for i in range(2)]
                    q.Gs = sb("Gs" + m, [128, 64])
                    q.MAG = sb("MAG" + m, [128, 128])
                    q.Phi = sb("Phi" + m, [64, 2, 64])
                    q.Psi = sb("Psi" + m, [64, 2, 64])
                    q.RAT = sb("RAT" + m, [64, 128])
                    q.YCT = sb("YCT" + m, [64, 128])
                    u.q.append(q)
                U[d][p] = u
        ST = [[[sb("ST%d%d%d" % (h, d, i), [64, 64]) for i in range(2)] for d in range(2)] for h in range(4)]
        stpar = [[0, 0] for _ in range(4)]
        pq = [psb("pq%d" % i, [128, 512]) for i in range(4)]
        pTr = [psb("pTr%d" % i, [128, 4, 128]) for i in range(2)]
        pSq = [psb("pSq%d" % i, [64, 512]) for i in range(2)]
        P = Prog(nc)
        P.op("dve", lambda e: e.tensor_tensor(out=E64[:], in0=g.consts[:, C_ID, 0:64], in1=g.consts[:, C_ID, 64:128], op=ALU.add),
             r=[g.consts], w=[E64])
        for h in range(4):
            for d in range(2):
                P.op("pool", lambda e, h=h, d=d: e.memset(ST[h][d][0][:], 0.0), w=[ST[h][d][0]])
        order_f = list(range(NTILE))
        order_b = [1, 0] + list(range(NTILE - 1, 1, -1))
        fidx = [[0, 1, 3, 4, 5, 7], [0, 2, 3, 4, 6, 8]]
        slotc = [0, 0, 0, 0]

        def slot(qi):
            sl = slotc[qi] % 4
            slotc[qi] += 1
            return sl

        for step in range(NTILE if g.rwsteps is None else g.rwsteps):
            for p in range(2):
                units = [(0, order_f[step]), (1, order_b[step])]
                for (d, j) in units:
                    u = U[d][p]
                    for i6 in range(6):
                        P.dma("sync" if i6 % 2 == 0 else "pool", u.f[i6][:], S.rwf[fidx[d][i6], p * 128:(p + 1) * 128, j * 128:(j + 1) * 128],
                              w=[u.f[i6]])
                    fr, fkd, fv, fkk, fbd, flw = u.f
                    P.op("dve", lambda e, u=u, flw=flw: e.tensor_tensor_scan(out=u.Lc[:], data0=g.consts[:, C_RESET, :], data1=flw[:], initial=0.0,
                                                                           op0=ALU.mult, op1=ALU.add), r=[flw, g.consts], w=[u.Lc])
                    totv = u.Lc[:].rearrange("p (c l) -> p c l", c=2)[:, :, 63:64]
                    if d == 0:
                        LC = u.Lc
                    else:
                        LC = u.LC
                        P.op("pool", lambda e, u=u, flw=flw: e.tensor_tensor(out=u.t1[:], in0=flw[:], in1=u.Lc[:], op=ALU.subtract),
                             r=[flw, u.Lc], w=[u.t1])
                        P.op("pool", lambda e, u=u, totv=totv: e.tensor_tensor(out=u.LC[:].rearrange("p (c l) -> p c l", c=2),
                                                                            in0=u.t1[:].rearrange("p (c l) -> p c l", c=2),
                                                                            in1=totv.to_broadcast([128, 2, 64]), op=ALU.add),
                             r=[u.t1, u.Lc], w=[u.LC])
                    P.op("pool", lambda e, u=u, LC=LC, flw=flw: e.tensor_tensor(out=u.tA[:], in0=LC[:], in1=flw[:], op=ALU.subtract),
                         r=[LC, flw], w=[u.tA])
                    P.op("pool", lambda e, u=u, LC=LC, totv=totv: e.tensor_tensor(out=u.tW[:].rearrange("p (c l) -> p c l", c=2),
                                                                               in0=totv.to_broadcast([128, 2, 64]),
                                                                               in1=LC[:].rearrange("p (c l) -> p c l", c=2), op=ALU.subtract),
                         r=[LC, u.Lc], w=[u.tW])
                    P.op("act", lambda e, u=u, LC=LC: e.activation(out=u.eP[:], in_=LC[:], func=AF.Exp), r=[LC], w=[u.eP])
                    P.op("act", lambda e, u=u, LC=LC: e.activation(out=u.eN[:], in_=LC[:], func=AF.Exp, scale=-1.0), r=[LC], w=[u.eN])
                    P.op("act", lambda e, u=u: e.activation(out=u.eA[:], in_=u.tA[:], func=AF.Exp), r=[u.tA], w=[u.eA])
                    P.op("act", lambda e, u=u: e.activation(out=u.eW[:], in_=u.tW[:], func=AF.Exp), r=[u.tW], w=[u.eW])
                    P.op("act", lambda e, u=u, totv=totv: e.activation(out=u.WL[:].unsqueeze(2), in_=totv, func=AF.Exp), r=[u.Lc], w=[u.WL])
                    P.op("dve", lambda e, u=u, fkk=fkk: e.scalar_tensor_tensor(out=u.ar[:, 0:128], in0=fkk[:], scalar=-1.0, in1=u.eA[:],
                                                                             op0=ALU.mult, op1=ALU.mult), r=[fkk, u.eA], w=[(u.ar.name, 0)])
                    P.op("pool", lambda e, u=u, fr=fr: e.tensor_tensor(out=u.ar[:, 128:256], in0=fr[:], in1=u.eP[:], op=ALU.mult),
                         r=[fr, u.eP], w=[(u.ar.name, 1)])
                    P.op("dve", lambda e, u=u, fbd=fbd: e.tensor_tensor(out=u.bt[:], in0=fbd[:], in1=u.eN[:], op=ALU.mult), r=[fbd, u.eN], w=[u.bt])
                    P.op("pool", lambda e, u=u, fkd=fkd: e.tensor_tensor(out=u.kt[:], in0=fkd[:], in1=u.eN[:], op=ALU.mult), r=[fkd, u.eN], w=[u.kt])
                    P.op("dve", lambda e, u=u, fbd=fbd: e.tensor_tensor(out=u.bW[:], in0=fbd[:], in1=u.eW[:], op=ALU.mult), r=[fbd, u.eW], w=[u.bW])
                    P.op("pool", lambda e, u=u, fkd=fkd: e.tensor_tensor(out=u.kW[:], in0=fkd[:], in1=u.eW[:], op=ALU.mult), r=[fkd, u.eW], w=[u.kW])
                    for c in range(2):
                        P.op("pool", lambda e, u=u, c=c: e.tensor_scalar(out=u.Dg[:, c, :], in0=E64[:], scalar1=u.WL[:, c:c + 1], scalar2=None, op0=ALU.mult),
                             r=[E64, u.WL], w=[u.Dg])
                    srcs = [(u.ar, 0, (u.ar.name, 0)), (u.bW, None, u.bW.name), (u.kW, None, u.kW.name), (fv, None, fv.name)]
                    for k4, (src, off, key) in enumerate(srcs):
                        in_ap = src[:, 0:128]
                        P.op("pe", lambda e, d=d, k4=k4, in_ap=in_ap: e.transpose(out=pTr[d][:, k4, :], in_=in_ap, identity=ident),
                             r=[key, g.consts], w=[pTr[d]])
                    P.op("act", lambda e, u=u, d=d: e.activation(out=u.TM[:], in_=pTr[d][:], func=AF.Copy), r=[pTr[d]], w=[u.TM])
                probs = []
                for (d, j) in units:
                    for hh in range(2):
                        probs.append((d, j, hh, U[d][p], U[d][p].q[hh], d * 2 + hh))
                for (d, j, hh, u, q, qi) in probs:
                    pb = hh * 64
                    P.op("pe", lambda e, u=u, pb=pb, qi=qi: e.matmul(out=pq[qi][:, 0:256], lhsT=u.bt[pb:pb + 64, :], rhs=u.ar[pb:pb + 64, :], start=True, stop=True),
                         r=[u.bt, (u.ar.name, 0), (u.ar.name, 1)], w=[("pqb", qi), ("pqb", qi)], rows=pb)
                    P.op("pe", lambda e, u=u, pb=pb, qi=qi: e.matmul(out=pq[qi][:, 256:384], lhsT=u.ar[pb:pb + 64, 0:128], rhs=u.bt[pb:pb + 64, :], start=True, stop=True),
                         r=[u.bt, (u.ar.name, 0)], w=[("pqb", qi)], rows=pb)
                for (d, j, hh, u, q, qi) in probs:
                    m2 = (C_MS_IT if d == 0 else C_MS_IT_B)
                    mti = (C_MS_TI if d == 0 else C_MS_IT)
                    P.op("dve", lambda e, q=q, qi=qi, m2=m2: e.tensor_tensor(out=q.XTR[:], in0=pq[qi][:, 0:256],
                                                                          in1=g.consts[:, m2:m2 + 2, :].rearrange("p a b -> p (a b)"), op=ALU.mult),
                         r=[("pqb", qi), ("pqb", qi), g.consts], w=[q.XTR])
                    P.op("dve", lambda e, q=q, qi=qi, mti=mti: e.tensor_tensor(out=q.X[0][:], in0=pq[qi][:, 256:384], in1=g.consts[:, mti, :], op=ALU.mult),
                         r=[("pqb", qi), g.consts], w=[q.X[0]])
                    P.op("pool", lambda e, q=q: e.tensor_tensor(out=q.PT[0][:], in0=q.XTR[:, 0:128], in1=ident, op=ALU.add),
                         r=[q.XTR, g.consts], w=[q.PT[0]])
                for (d, j, hh, u, q, qi) in probs:
                    pb = hh * 64
                    P.op("pe", lambda e, u=u, pb=pb, qi=qi: e.matmul(out=pq[qi][:, 0:256], lhsT=u.kt[pb:pb + 64, :], rhs=u.ar[pb:pb + 64, :], start=True, stop=True),
                         r=[u.kt, (u.ar.name, 0), (u.ar.name, 1)], w=[("pqb", qi), ("pqb", qi)], rows=pb)
                for (d, j, hh, u, q, qi) in probs:
                    m2 = (C_MS_IT if d == 0 else C_MS_IT_B)
                    P.op("dve", lambda e, q=q, qi=qi, m2=m2: e.tensor_tensor(out=q.KTR[:], in0=pq[qi][:, 0:256],
                                                                          in1=g.consts[:, m2:m2 + 2, :].rearrange("p a b -> p (a b)"), op=ALU.mult),
                         r=[("pqb", qi), ("pqb", qi), g.consts], w=[q.KTR])
                curX = {qi: None for qi in range(4)}
                for lev in range(5):
                    last = (lev == 4)
                    for (d, j, hh, u, q, qi) in probs:
                        Xc = q.X[lev % 2]
                        XTc = q.XTR if lev == 0 else q.XT[lev % 2]
                        XTc_ap = XTc[:, 0:128]
                        P.op("pe", lambda e, qi=qi, Xc=Xc, XTc_ap=XTc_ap: e.matmul(out=pq[qi][:, 384:512], lhsT=XTc_ap, rhs=Xc[:], start=True, stop=True),
                             r=[Xc, XTc], w=[("pqb", qi)])
                        if not last:
                            P.op("pe", lambda e, qi=qi, Xc=Xc, XTc_ap=XTc_ap: e.matmul(out=pq[qi][:, 256:384], lhsT=Xc[:], rhs=XTc_ap, start=True, stop=True),
                                 r=[Xc, XTc], w=[("pqb", qi)])
                    for (d, j, hh, u, q, qi) in probs:
                        Xn = q.X[(lev + 1) % 2]
                        XTn = q.XT[(lev + 1) % 2]
                        P.op("act", lambda e, qi=qi, Xn=Xn: e.activation(out=Xn[:], in_=pq[qi][:, 384:512], func=AF.Copy), r=[("pqb", qi)], w=[Xn])
                        if not last:
                            P.op("act", lambda e, qi=qi, XTn=XTn: e.activation(out=XTn[:], in_=pq[qi][:, 256:384], func=AF.Copy), r=[("pqb", qi)], w=[XTn])
                    for (d, j, hh, u, q, qi) in probs:
                        Xn = q.X[(lev + 1) % 2]
                        PTc = q.PT[lev % 2]
                        P.op("pe", lambda e, qi=qi, Xn=Xn, PTc=PTc: e.matmul(out=pq[qi][:, 0:128], lhsT=Xn[:], rhs=PTc[:], start=True, stop=True),
                             r=[Xn, PTc], w=[("pqb", qi)])
                    for (d, j, hh, u, q, qi) in probs:
                        PTc = q.PT[lev % 2]
                        PTn = q.PT[(lev + 1) % 2]
                        P.op("dve", lambda e, qi=qi, PTc=PTc, PTn=PTn: e.tensor_tensor(out=PTn[:], in0=pq[qi][:, 0:128], in1=PTc[:], op=ALU.add),
                             r=[("pqb", qi), PTc], w=[PTn])
                for (d, j, hh, u, q, qi) in probs:
                    cb = hh * 64
                    P.op("pe", lambda e, qi=qi, q=q, u=u, cb=cb: e.matmul(out=pq[qi][:, 128:192], lhsT=q.KTR[:, 0:128], rhs=u.TM[:, 3, cb:cb + 64], start=True, stop=True),
                         r=[q.KTR, u.TM], w=[("pqb", qi)])
                for (d, j, hh, u, q, qi) in probs:
                    P.op("act", lambda e, qi=qi, q=q: e.activation(out=q.Gs[:], in_=pq[qi][:, 128:192], func=AF.Copy), r=[("pqb", qi)], w=[q.Gs])
                for (d, j, hh, u, q, qi) in probs:
                    cb = hh * 64
                    PTf = q.PT[1]
                    P.op("pe", lambda e, qi=qi, PTf=PTf, u=u, cb=cb: e.matmul(out=pq[qi][:, 256:320], lhsT=PTf[:], rhs=u.TM[:, 0, cb:cb + 64], start=True, stop=True),
                         r=[PTf, u.TM], w=[("pqb", qi)])
                    P.op("pe", lambda e, qi=qi, PTf=PTf, q=q: e.matmul(out=pq[qi][:, 320:384], lhsT=PTf[:], rhs=q.Gs[:], start=True, stop=True),
                         r=[PTf, q.Gs], w=[("pqb", qi)])
                for (d, j, hh, u, q, qi) in probs:
                    P.op("act", lambda e, qi=qi, q=q: e.activation(out=q.MAG[:], in_=pq[qi][:, 256:384], func=AF.Copy), r=[("pqb", qi)], w=[q.MAG])
                for (d, j, hh, u, q, qi) in probs:
                    cb = hh * 64
                    pb = hh * 64
                    for c in range(2):
                        rb = c * 64
                        P.op("pe", lambda e, qi=qi, q=q, u=u, rb=rb, cb=cb, c=c: e.matmul(out=pq[qi][0:64, 384 + c * 64:448 + c * 64], lhsT=q.MAG[rb:rb + 64, 0:64],
                                                                                       rhs=u.TM[rb:rb + 64, 1, cb:cb + 64], start=True, stop=False),
                             r=[q.MAG, u.TM], w=[("pqb", qi)], rows=rb)
                        P.op("pe", lambda e, qi=qi, u=u, pb=pb, c=c: e.matmul(out=pq[qi][0:64, 384 + c * 64:448 + c * 64], lhsT=E64[pb:pb + 64, :],
                                                                            rhs=u.Dg[pb:pb + 64, c, :], start=False, stop=True),
                             r=[E64, u.Dg], w=[("pqb", qi)], rows=pb)
                        P.op("pe", lambda e, qi=qi, q=q, u=u, rb=rb, cb=cb, c=c: e.matmul(out=pq[qi][0:64, c * 64:c * 64 + 64], lhsT=u.TM[rb:rb + 64, 1, cb:cb + 64],
                                                                                       rhs=q.MAG[rb:rb + 64, 64:128], start=True, stop=False),
                             r=[q.MAG, u.TM], w=[("pqb", qi)], rows=rb)
                        P.op("pe", lambda e, qi=qi, u=u, rb=rb, cb=cb, c=c: e.matmul(out=pq[qi][0:64, c * 64:c * 64 + 64], lhsT=u.TM[rb:rb + 64, 2, cb:cb + 64],
                                                                                  rhs=u.TM[rb:rb + 64, 3, cb:cb + 64], start=False, stop=True),
                             r=[u.TM], w=[("pqb", qi)], rows=rb)
                    P.op("pe", lambda e, qi=qi, q=q: e.matmul(out=pq[qi][0:64, 128:256], lhsT=q.MAG[:, 0:64], rhs=q.XTR[:, 128:256], start=True, stop=False),
                         r=[q.MAG, q.XTR], w=[("pqb", qi)])
                    P.op("pe", lambda e, qi=qi, u=u, pb=pb: e.matmul(out=pq[qi][0:64, 128:256], lhsT=E64[pb:pb + 64, :], rhs=u.ar[pb:pb + 64, 128:256], start=False, stop=True),
                         r=[E64, (u.ar.name, 1)], w=[("pqb", qi)], rows=pb)
                    P.op("pe", lambda e, qi=qi, q=q: e.matmul(out=pq[qi][0:64, 256:384], lhsT=q.MAG[:, 64:128], rhs=q.XTR[:, 128:256], start=True, stop=False),
                         r=[q.MAG, q.XTR], w=[("pqb", qi)])
                    P.op("pe", lambda e, qi=qi, q=q, u=u, cb=cb: e.matmul(out=pq[qi][0:64, 256:384], lhsT=u.TM[:, 3, cb:cb + 64], rhs=q.KTR[:, 128:256], start=False, stop=True),
                         r=[u.TM, q.KTR], w=[("pqb", qi)])
                for (d, j, hh, u, q, qi) in probs:
                    P.op("act", lambda e, qi=qi, q=q: e.activation(out=q.Phi[:].rearrange("p c k -> p (c k)"), in_=pq[qi][0:64, 384:512], func=AF.Copy),
                         r=[("pqb", qi)], w=[q.Phi])
                    P.op("dve", lambda e, qi=qi, q=q: e.tensor_copy(out=q.Psi[:].rearrange("p c k -> p (c k)"), in_=pq[qi][0:64, 0:128]),
                         r=[("pqb", qi)], w=[q.Psi])
                    P.op("act", lambda e, qi=qi, q=q: e.activation(out=q.RAT[:], in_=pq[qi][0:64, 128:256], func=AF.Copy), r=[("pqb", qi)], w=[q.RAT])
                    P.op("dve", lambda e, qi=qi, q=q: e.tensor_copy(out=q.YCT[:], in_=pq[qi][0:64, 256:384]), r=[("pqb", qi)], w=[q.YCT])
                for ci in range(2):
                    for (d, j, hh, u, q, qi) in probs:
                        c = ci if d == 0 else 1 - ci
                        h = p * 2 + hh
                        sp = stpar[h][d]
                        Sc = ST[h][d][sp]
                        Sn = ST[h][d][1 - sp]
                        stpar[h][d] = 1 - sp
                        psq = pSq[ci]
                        yc0 = qi * 128
                        P.op("pe", lambda e, psq=psq, yc0=yc0, Sc=Sc, q=q, c=c: e.matmul(out=psq[:, yc0:yc0 + 64], lhsT=Sc[:], rhs=q.RAT[:, c * 64:(c + 1) * 64], start=True, stop=False),
                             r=[Sc, q.RAT], w=[("sqb", ci)])
                        P.op("pe", lambda e, psq=psq, yc0=yc0, q=q, c=c: e.matmul(out=psq[:, yc0:yc0 + 64], lhsT=E64[0:64, :], rhs=q.YCT[:, c * 64:(c + 1) * 64], start=False, stop=True),
                             r=[E64, q.YCT], w=[("sqb", ci)])
                        P.op("pe", lambda e, psq=psq, yc0=yc0, Sc=Sc, q=q, c=c: e.matmul(out=psq[:, yc0 + 64:yc0 + 128], lhsT=q.Phi[:, c, :], rhs=Sc[:], start=True, stop=False),
                             r=[Sc, q.Phi], w=[("sqb", ci)])
                        P.op("pe", lambda e, psq=psq, yc0=yc0, q=q, c=c: e.matmul(out=psq[:, yc0 + 64:yc0 + 128], lhsT=E64[0:64, :], rhs=q.Psi[:, c, :], start=False, stop=True),
                             r=[E64, q.Psi], w=[("sqb", ci)])
                        tcol = j * 128 + c * 64
                        yb = ybuf[p][d]
                        P.op("act", lambda e, psq=psq, yc0=yc0, yb=yb, hh=hh, tcol=tcol: e.activation(out=yb[hh * 64:(hh + 1) * 64, tcol:tcol + 64], in_=psq[:, yc0:yc0 + 64], func=AF.Copy),
                             r=[("sqb", ci)], w=[(yb.name, j)])
                        P.op("dve", lambda e, psq=psq, yc0=yc0, Sn=Sn: e.tensor_copy(out=Sn[:], in_=psq[:, yc0 + 64:yc0 + 128]), r=[("sqb", ci)], w=[Sn])
        SEG = 512
        prm = sb("prm", [128, 2, 3])
        ld = [sb("o_ld%d" % i, [128, SEG]) for i in range(5)]
        ysum = sb("ysum", [128, SEG])
        yc_ = sb("yc_", [128, SEG])
        sq = sb("osq", [128, SEG])
        rstd = sb("rstd", [128, SEG])
        prod = sb("prod", [128, SEG])
        ob = [sb("oob%d" % i, [128, SEG], BF16) for i in range(2)]
        for p in range(2):
            for k3, src in enumerate((I.r_k, I.ln_w, I.ln_b)):
                P.dma("sync", prm[:, p, k3:k3 + 1], src[g.wi(l), p * 128:(p + 1) * 128, :], w=[prm], sem="prm")
        segs = ([(0, CTX)] if l == 0 else []) + [(CTX + i * SEG, SEG) for i in range(8)]
        oi = 0
        for (t0, n) in segs:
            for p in range(2):
                for k5, idx in enumerate((0, 1, 2, 3, 9)):
                    P.dma("sync" if k5 % 2 == 0 else "pool", ld[k5][:, 0:n], S.rwf[idx, p * 128:(p + 1) * 128, t0:t0 + n], w=[ld[k5]])
                ykeys = [(ybuf[p][dd].name, jj) for dd in range(2) for jj in range(t0 // 128, (t0 + n) // 128)]
                P.op("pool", lambda e, p=p, t0=t0, n=n: e.tensor_tensor(out=ysum[:, 0:n], in0=ybuf[p][0][:, t0:t0 + n], in1=ybuf[p][1][:, t0:t0 + n], op=ALU.add),
                     r=ykeys, w=[ysum])
                pm = pq[0]
                P.op("pe", lambda e, pm=pm, n=n: e.matmul(out=pm[:, 0:n], lhsT=g.consts[:, C_BONES, :], rhs=ysum[:, 0:n], start=True, stop=True),
                     r=[ysum, g.consts], w=[("pqb", 0), ("pqb", 0), ("pqb", 0), ("pqb", 0)])
                P.op("dve", lambda e, pm=pm, n=n: e.scalar_tensor_tensor(out=yc_[:, 0:n], in0=pm[:, 0:n], scalar=-1.0 / 64, in1=ysum[:, 0:n], op0=ALU.mult, op1=ALU.add),
                     r=[("pqb", 0), ("pqb", 0), ("pqb", 0), ("pqb", 0), ysum], w=[yc_])
                P.op("pool", lambda e, n=n: e.tensor_tensor(out=sq[:, 0:n], in0=yc_[:, 0:n], in1=yc_[:, 0:n], op=ALU.mult), r=[yc_], w=[sq])
                pv = pq[1]
                P.op("pe", lambda e, pv=pv, n=n: e.matmul(out=pv[:, 0:n], lhsT=g.consts[:, C_BONES, :], rhs=sq[:, 0:n], start=True, stop=True),
                     r=[sq, g.consts], w=[("pqb", 1), ("pqb", 1), ("pqb", 1), ("pqb", 1)])
                P.op("dve", lambda e, pv=pv, n=n: e.tensor_scalar(out=rstd[:, 0:n], in0=pv[:, 0:n], scalar1=1.0 / 64, scalar2=64e-5, op0=ALU.mult, op1=ALU.add),
                     r=[("pqb", 1), ("pqb", 1), ("pqb", 1), ("pqb", 1)], w=[rstd])
                P.op("act", lambda e, n=n: e.activation(out=rstd[:, 0:n], in_=rstd[:, 0:n], func=AF.Sqrt), r=[rstd], w=[rstd])
                P.op("dve", lambda e, n=n: e.reciprocal(out=rstd[:, 0:n], in_=rstd[:, 0:n]), r=[rstd], w=[rstd])
                P.op("dve", lambda e, n=n: e.tensor_tensor(out=yc_[:, 0:n], in0=yc_[:, 0:n], in1=rstd[:, 0:n], op=ALU.mult), r=[yc_, rstd], w=[yc_])
                P.op("dve", lambda e, n=n, p=p: e.tensor_scalar(out=yc_[:, 0:n], in0=yc_[:, 0:n], scalar1=prm[:, p, 1:2], scalar2=prm[:, p, 2:3], op0=ALU.mult, op1=ALU.add),
                     r=[yc_, prm], w=[yc_])
                P.op("pool", lambda e, n=n: e.tensor_tensor(out=prod[:, 0:n], in0=ld[1][:, 0:n], in1=ld[2][:, 0:n], op=ALU.add), r=[ld[1], ld[2]], w=[prod])
                P.op("pool", lambda e, n=n: e.tensor_tensor(out=prod[:, 0:n], in0=prod[:, 0:n], in1=ld[0][:, 0:n], op=ALU.mult), r=[prod, ld[0]], w=[prod])
                P.op("pool", lambda e, n=n, p=p: e.tensor_scalar(out=prod[:, 0:n], in0=prod[:, 0:n], scalar1=prm[:, p, 0:1], scalar2=0.5, op0=ALU.mult, op1=ALU.mult),
                     r=[prod, prm], w=[prod])
                pbn = pq[2]
                P.op("pe", lambda e, pbn=pbn, n=n: e.matmul(out=pbn[:, 0:n], lhsT=g.consts[:, C_BONES, :], rhs=prod[:, 0:n], start=True, stop=True),
                     r=[prod, g.consts], w=[("pqb", 2), ("pqb", 2), ("pqb", 2), ("pqb", 2)])
                P.op("dve", lambda e, pbn=pbn, n=n: e.tensor_tensor(out=sq[:, 0:n], in0=pbn[:, 0:n], in1=ld[3][:, 0:n], op=ALU.mult),
                     r=[("pqb", 2), ("pqb", 2), ("pqb", 2), ("pqb", 2), ld[3]], w=[sq])
                P.op("dve", lambda e, n=n: e.tensor_tensor(out=yc_[:, 0:n], in0=yc_[:, 0:n], in1=sq[:, 0:n], op=ALU.add), r=[yc_, sq], w=[yc_])
                o = ob[oi % 2]
                oi += 1
                P.op("dve", lambda e, n=n, o=o: e.tensor_tensor(out=o[:, 0:n], in0=yc_[:, 0:n], in1=ld[4][:, 0:n], op=ALU.mult), r=[yc_, ld[4]], w=[o])
                P.dma("sync", S.mixT[768 + p * 128:768 + (p + 1) * 128, t0:t0 + n], o[:, 0:n], r=[o], sem=("oob", oi % 2))
        P.barrier()
        P.emit()


def phase_wout(g, l):
    nc, I, S = g.nc, g.I, g.S
    with ExitStack() as es:
        def sb(name, shape, dt=F32):
            return es.enter_context(nc.sbuf_tensor("w%d_" % l + name, list(shape), dt))

        def psb(name, shape, dt=F32):
            return es.enter_context(nc.psum_tensor("w%d_" % l + name, list(shape), dt))
        wo = sb("wo", [128, 8, D], BF16)
        rw = sb("rw", [128, 8, NE])
        bc = [[sb("bc%d%d" % (j, k), [128, D]) for k in range(3)] for j in range(2)]
        mt = [sb("mt%d" % i, [128, 8, 128], BF16) for i in range(2)]
        xt = [sb("xt%d" % i, [128, D]) for i in range(2)]
        x1 = [sb("x1%d" % i, [128, D]) for i in range(2)]
        junk = sb("junk", [128, D])
        ss = [sb("ss%d" % i, [128, 1]) for i in range(2)]
        h2f = [sb("h2f%d" % i, [128, D]) for i in range(2)]
        h2b = [sb("h2b%d" % i, [128, D], BF16) for i in range(2)]
        h2T = sb("h2T", [128, 8, 128])
        lg = sb("lg", [128, NE])
        mx = sb("mx", [128, 1])
        sm = sb("sm", [128, 1])
        aff = sb("aff", [128, NE])
        pO = [psb("pO%d" % i, [128, 512]) for i in range(2)]
        pT = [psb("pT%d" % i, [128, 4, 128]) for i in range(2)]
        pL_ = psb("pL", [128, 512])
        pL = pL_[:, 0:NE]
        pA_ = psb("pA", [NE, 512])
        pA = pA_[:, 0:128]
        P = Prog(nc)
        P.dma("pool", wo[:], I.w_out[g.wi(l)].rearrange("(kc p) n -> p kc n", p=128), w=[wo])
        P.dma("sync", rw[:], I.router[g.wi(l)].rearrange("(kc p) n -> p kc n", p=128), w=[rw])
        for j in range(2):
            for k, mi in enumerate((2, 3, 4)):
                P.dma("sync", bc[j][k][:], S.modv[g.wi(l), j, mi:mi + 1, :].to_broadcast([128, D]), w=[bc[j][k]])
        tiles = list(range(NTILE)) if l == 0 else list(range(2, NTILE))
        for i in tiles:
            b = i % 2
            j = 1 if i < 2 else 0
            if i < 2:
                src = (I.ctx if l == g.first else S.xcres)[i * 128:(i + 1) * 128, :]
                dst = S.xcres[i * 128:(i + 1) * 128, :]
                h2dst = S.h2c[i * 128:(i + 1) * 128, :]
            else:
                src = (I.x if l == g.first else S.xres)[(i - 2) * 128:(i - 1) * 128, :]
                dst = (g.out if l == g.last else S.xres)[(i - 2) * 128:(i - 1) * 128, :]
                h2dst = S.h2l[(i - 2) * 128:(i - 1) * 128, :]
            P.dma("sync", mt[b][:], S.mixT[:, i * 128:(i + 1) * 128].rearrange("(kc p) t -> p kc t", p=128), w=[mt[b]])
            P.dma("sync", xt[b][:], src, w=[xt[b]])
            for half in range(2):
                for kc in range(8):
                    P.op("pe", lambda e, half=half, kc=kc, b=b: e.matmul(out=pO[half][:], lhsT=mt[b][:, kc, :], rhs=wo[:, kc, half * 512:(half + 1) * 512],
                                                                       start=(kc == 0), stop=(kc == 7)), r=[mt[b], wo], w=[pO[half]])
                P.op("dve", lambda e, half=half, b=b, j=j: e.tensor_tensor(out=x1[b][:, half * 512:(half + 1) * 512], in0=pO[half][:],
                                                                          in1=bc[j][0][:, half * 512:(half + 1) * 512], op=ALU.mult),
                     r=[pO[half], bc[j][0]], w=[(x1[b].name, half)])
            P.op("pool", lambda e, b=b: e.tensor_tensor(out=x1[b][:], in0=x1[b][:], in1=xt[b][:], op=ALU.add),
                 r=[xt[b]], w=[(x1[b].name, 0), (x1[b].name, 1)])
            P.dma("pool", dst, x1[b][:], r=[(x1[b].name, 0), (x1[b].name, 1)], sem=("x1st", b))
            P.op("act", lambda e, b=b: e.activation(out=junk[:], in_=x1[b][:], func=AF.Square, accum_out=ss[b][:]), r=[(x1[b].name, 0), (x1[b].name, 1)], w=[junk, ss[b]])
            P.op("dve", lambda e, b=b: e.tensor_scalar(out=ss[b][:], in0=ss[b][:], scalar1=1.0 / D, scalar2=1e-6, op0=ALU.mult, op1=ALU.add),
                 r=[ss[b]], w=[ss[b]])
            P.op("act", lambda e, b=b: e.activation(out=ss[b][:], in_=ss[b][:], func=AF.Sqrt), r=[ss[b]], w=[ss[b]])
            P.op("dve", lambda e, b=b: e.reciprocal(out=ss[b][:], in_=ss[b][:]), r=[ss[b]], w=[ss[b]])
            P.op("dve", lambda e, b=b, j=j: e.scalar_tensor_tensor(out=h2f[b][:], in0=x1[b][:], scalar=ss[b][:, 0:1], in1=bc[j][1][:], op0=ALU.mult, op1=ALU.mult),
                 r=[(x1[b].name, 0), (x1[b].name, 1), ss[b], bc[j][1]], w=[h2f[b]])
            P.op("pool", lambda e, b=b, j=j: e.tensor_tensor(out=h2f[b][:], in0=h2f[b][:], in1=bc[j][2][:], op=ALU.add), r=[h2f[b], bc[j][2]], w=[h2f[b]])
            P.op("act", lambda e, b=b: e.activation(out=h2b[b][:], in_=h2f[b][:], func=AF.Copy), r=[h2f[b]], w=[h2b[b]])
            P.dma("sync", h2dst, h2b[b][:], r=[h2b[b]], sem=("h2st", b))
            for half in range(2):
                for k4 in range(4):
                    kc = half * 4 + k4
                    P.op("pe", lambda e, half=half, k4=k4, kc=kc, b=b: e.transpose(out=pT[half][:, k4, :], in_=h2f[b][:, kc * 128:(kc + 1) * 128],
                                                                                identity=g.consts[:, C_ID, :]), r=[h2f[b], g.consts], w=[pT[half]])
                if half == 0:
                    P.op("act", lambda e, half=half: e.activation(out=h2T[:, 0:4, :], in_=pT[0][:], func=AF.Copy), r=[pT[0]], w=[("h2T", 0)])
                else:
                    P.op("dve", lambda e, half=half: e.tensor_copy(out=h2T[:, 4:8, :], in_=pT[1][:]), r=[pT[1]], w=[("h2T", 1)])
            for kc in range(8):
                P.op("pe", lambda e, kc=kc: e.matmul(out=pL, lhsT=h2T[:, kc, :], rhs=rw[:, kc, :], start=(kc == 0), stop=(kc == 7)),
                     r=[("h2T", 0), ("h2T", 1), rw], w=["pL"])
            P.op("dve", lambda e: e.tensor_copy(out=lg[:], in_=pL), r=["pL"], w=[lg])
            P.op("dve", lambda e: e.tensor_reduce(out=mx[:], in_=lg[:], axis=AX.X, op=ALU.max), r=[lg], w=[mx])
            P.op("dve", lambda e: e.tensor_scalar(out=mx[:], in0=mx[:], scalar1=-1.0, scalar2=None, op0=ALU.mult), r=[mx], w=[mx])
            P.op("act", lambda e: e.activation(out=aff[:], in_=lg[:], func=AF.Exp, bias=mx[:, 0:1], accum_out=sm[:]), r=[lg, mx], w=[aff, sm])
            P.op("dve", lambda e: e.reciprocal(out=sm[:], in_=sm[:]), r=[sm], w=[sm])
            P.op("dve", lambda e: e.tensor_scalar(out=aff[:], in0=aff[:], scalar1=sm[:, 0:1], scalar2=None, op0=ALU.mult), r=[aff, sm], w=[aff])
            P.op("pe", lambda e: e.transpose(out=pA, in_=aff[:], identity=g.consts[:, C_ID, :]), r=[aff, g.consts], w=["pA"])
            P.op("act", lambda e, i=i: e.activation(out=g.affT[:, i * 128:(i + 1) * 128], in_=pA, func=AF.Copy), r=["pA"], w=[("affT", i)])
        P.barrier()
        P.emit()


def phase_moe(g, l):
    nc, I, S = g.nc, g.I, g.S
    with ExitStack() as es:
        def sb(name, shape, dt=F32):
            return es.enter_context(nc.sbuf_tensor("m%d_" % l + name, list(shape), dt))

        def psb(name, shape, dt=F32):
            return es.enter_context(nc.psum_tensor("m%d_" % l + name, list(shape), dt))
        work = sb("work", [NE, SEQ])
        vals = sb("vals", [NE, CAP_L])
        idxu = sb("idxu", [NE, CAP_L], U32)
        idxf = sb("idxf", [NE, CAP_L])
        idxT = sb("idxT", [128, 4, NE], I32)
        gT = sb("gT", [128, 4, NE])
        gt2 = [sb("gt2_%d" % j, [128, D]) for j in range(2)]
        wgt = [sb("wg%d" % i, [128, 8, D], BF16) for i in range(2)]
        wut = [sb("wu%d" % i, [128, 8, D], BF16) for i in range(2)]
        wdt = [sb("wd%d" % i, [128, 8, D], BF16) for i in range(2)]
        xs = [sb("xs%d" % i, [128, D], BF16) for i in range(2)]
        xsT = sb("xsT", [128, 8, 512], BF16)
        hidT = sb("hidT", [128, 8, 512], BF16)
        sg = [sb("sg%d" % i, [128, 512]) for i in range(2)]
        y = [sb("y%d" % i, [128, D]) for i in range(2)]
        pTi = psb("pTi", [128, 32, NE])
        pX = [psb("pX%d" % i, [128, 8, 128], BF16) for i in range(2)]
        pG = psb("pG", [128, 512])
        pU = psb("pU", [128, 512])
        pY = [psb("pY%d" % i, [128, 512]) for i in range(2)]
        P = Prog(nc)
        for j in range(2):
            P.dma("sync", gt2[j][:], S.modv[g.wi(l), j, 5:6, :].to_broadcast([128, D]), w=[gt2[j]])
        sets = [(0, CTX, SEQ, CAP_L, S.h2l, (g.out if l == g.last else S.xres))]
        if l == 0:
            sets.append((1, 0, CTX, CAP_C, S.h2c, S.xcres))
        wi = 0
        xi = 0
        yi = 0
        for (j, a0, N, cap, h2src, dest) in sets:
            nch = (cap + 127) // 128
            npc = min(cap, 128)
            akeys = [("affT", i) for i in range(a0 // 128, (a0 + N) // 128)]
            P.op("pool", lambda e, a0=a0, N=N: e.tensor_copy(out=work[:, 0:N], in_=g.affT[:, a0:a0 + N]), r=akeys, w=[work])
            for r8 in range(cap // 8):
                P.op("dve", lambda e, r8=r8, N=N: e.max(out=vals[:, r8 * 8:(r8 + 1) * 8], in_=work[:, 0:N]), r=[work], w=[vals])
                P.op("dve", lambda e, r8=r8, N=N: e.max_index(out=idxu[:, r8 * 8:(r8 + 1) * 8], in_max=vals[:, r8 * 8:(r8 + 1) * 8], in_values=work[:, 0:N]),
                     r=[work, vals], w=[idxu])
                P.op("dve", lambda e, r8=r8, N=N: e.match_replace(out=work[:, 0:N], in_to_replace=vals[:, r8 * 8:(r8 + 1) * 8], in_values=work[:, 0:N], imm_value=-1.0),
                     r=[work, vals], w=[work])
            P.op("dve", lambda e, cap=cap: e.tensor_copy(out=idxf[:, 0:cap], in_=idxu[:, 0:cap]), r=[idxu], w=[idxf])
            for ch in range(nch):
                P.op("pe", lambda e, ch=ch, npc=npc: e.transpose(out=pTi[0:npc, 0, :], in_=idxf[:, ch * 128:ch * 128 + npc], identity=g.consts[0:NE, C_ID, 0:NE]),
                     r=[idxf, g.consts], w=[pTi])
                P.op("pe", lambda e, ch=ch, npc=npc: e.transpose(out=pTi[0:npc, 1, :], in_=vals[:, ch * 128:ch * 128 + npc], identity=g.consts[0:NE, C_ID, 0:NE]),
                     r=[vals, g.consts], w=[pTi])
                P.op("dve", lambda e, ch=ch, npc=npc: e.tensor_copy(out=idxT[0:npc, ch, :], in_=pTi[0:npc, 0, :]), r=[pTi], w=[idxT])
                P.op("dve", lambda e, ch=ch, npc=npc: e.tensor_copy(out=gT[0:npc, ch, :], in_=pTi[0:npc, 1, :]), r=[], w=[gT, pTi])
            ncol = nch * npc
            for ex in range(NE):
                wb_ = wi % 2
                wi += 1
                P.dma("pool", wgt[wb_][:], I.wg[g.wi(l), ex].rearrange("(kc p) n -> p kc n", p=128), w=[wgt[wb_]])
                P.dma("pool", wut[wb_][:], I.wu[g.wi(l), ex].rearrange("(kc p) n -> p kc n", p=128), w=[wut[wb_]])
                P.dma("pool", wdt[wb_][:], I.wd[g.wi(l), ex].rearrange("(kc p) n -> p kc n", p=128), w=[wdt[wb_]])
                for ch in range(nch):
                    xb = xs[xi % 2]
                    xi += 1
                    P.dma_fn("pool", lambda e, xb=xb, ch=ch, ex=ex, npc=npc, h2src=h2src: e.indirect_dma_start(
                        out=xb[0:npc, :], out_offset=None, in_=h2src[:, :],
                        in_offset=bass.IndirectOffsetOnAxis(ap=idxT[0:npc, ch, ex:ex + 1], axis=0)),
                        r=[idxT], w=[xb], sem=("xg", xb.name))
                    for half in range(2):
                        for k4 in range(4):
                            kc = half * 4 + k4
                            P.op("pe", lambda e, half=half, k4=k4, kc=kc, xb=xb, npc=npc: e.transpose(out=pX[half][:, k4, 0:npc], in_=xb[0:npc, kc * 128:(kc + 1) * 128],
                                                                                                 identity=g.identb[0:npc, 0:npc]), r=[xb, g.identb], w=[pX[half]])
                        if half == 0:
                            P.op("act", lambda e, ch=ch, npc=npc: e.activation(out=xsT[:, 0:4, ch * 128:ch * 128 + npc], in_=pX[0][:, 0:4, 0:npc], func=AF.Copy),
                                 r=[pX[0]], w=[("xsT", 0)])
                        else:
                            P.op("dve", lambda e, ch=ch, npc=npc: e.tensor_copy(out=xsT[:, 4:8, ch * 128:ch * 128 + npc], in_=pX[1][:, 0:4, 0:npc]),
                                 r=[pX[1]], w=[("xsT", 1)])
                for fc in range(8):
                    for kc in range(8):
                        P.op("pe", lambda e, fc=fc, kc=kc, wb_=wb_, ncol=ncol: e.matmul(out=pG[:, 0:ncol], lhsT=wgt[wb_][:, kc, fc * 128:(fc + 1) * 128], rhs=xsT[:, kc, 0:ncol],
                                                                                     start=(kc == 0), stop=(kc == 7)), r=[wgt[wb_], ("xsT", 0), ("xsT", 1)], w=[pG])
                    for kc in range(8):
                        P.op("pe", lambda e, fc=fc, kc=kc, wb_=wb_, ncol=ncol: e.matmul(out=pU[:, 0:ncol], lhsT=wut[wb_][:, kc, fc * 128:(fc + 1) * 128], rhs=xsT[:, kc, 0:ncol],
                                                                                     start=(kc == 0), stop=(kc == 7)), r=[wut[wb_], ("xsT", 0), ("xsT", 1)], w=[pU])
                    s_ = sg[fc % 2]
                    P.op("act", lambda e, s_=s_, ncol=ncol: e.activation(out=s_[:, 0:ncol], in_=pG[:, 0:ncol], func=AF.Silu), r=[pG], w=[s_])
                    P.op("dve", lambda e, s_=s_, fc=fc, ncol=ncol: e.tensor_tensor(out=hidT[:, fc, 0:ncol], in0=pU[:, 0:ncol], in1=s_[:, 0:ncol], op=ALU.mult),
                         r=[pU, s_], w=[("hidT", fc)])
                hk = [("hidT", fc) for fc in range(8)]
                for ch in range(nch):
                    yb = y[yi % 2]
                    yi += 1
                    for half in range(2):
                        for fc in range(8):
                            P.op("pe", lambda e, half=half, fc=fc, ch=ch, wb_=wb_, npc=npc: e.matmul(out=pY[half][0:npc, :], lhsT=hidT[:, fc, ch * 128:ch * 128 + npc],
                                                                                                 rhs=wdt[wb_][:, fc, half * 512:(half + 1) * 512], start=(fc == 0), stop=(fc == 7)),
                                 r=hk + [wdt[wb_]], w=[pY[half]])
                        P.op("dve", lambda e, half=half, yb=yb, ch=ch, ex=ex, npc=npc, j=j: e.scalar_tensor_tensor(
                            out=yb[0:npc, half * 512:(half + 1) * 512], in0=pY[half][0:npc, :], scalar=gT[0:npc, ch, ex:ex + 1],
                            in1=gt2[j][0:npc, half * 512:(half + 1) * 512], op0=ALU.mult, op1=ALU.mult), r=[pY[half], gT, gt2[j]], w=[(yb.name, half)])
                    P.dma_fn("pool", lambda e, yb=yb, ch=ch, ex=ex, npc=npc, dest=dest: e.indirect_dma_start(
                        out=dest[:, :], out_offset=bass.IndirectOffsetOnAxis(ap=idxT[0:npc, ch, ex:ex + 1], axis=0),
                        in_=yb[0:npc, :], in_offset=None, compute_op=ALU.add),
                        r=[(yb.name, 0), (yb.name, 1), idxT], w=[("dest", j)], sem=("ysc", j))
        P.barrier()
        P.emit()


def phase_zero_mix(g, l):
    nc, S = g.nc, g.S
    with ExitStack() as es:
        z = es.enter_context(nc.sbuf_tensor("z%d_z" % l, [128, NT], BF16))
        P = Prog(nc)
        P.op("pool", lambda e: e.memset(z[:], 0.0), w=[z])
        for r in range(2, 8):
            P.dma("sync", S.mixT[r * 128:(r + 1) * 128, :], z[:], r=[z], sem="zst")
        P.barrier()
        P.emit()


def prep_inputs(inputs):
    f = lambda a: np.ascontiguousarray(np.asarray(a, dtype=np.float32))
    x = f(inputs["x"])
    c = f(inputs["c"])
    ctx = f(inputs["ctx"])
    c_ctx = f(inputs["c_ctx"])
    shared = {
        "ada_w": f(inputs["ada_w"]),
        "ada_b": f(inputs["ada_b"]).reshape(2, 1, 6 * D),
        "norm1_g": f(inputs["norm1_g"]).reshape(2, 1, D),
        "norm2_g": f(inputs["norm2_g"]).reshape(2, 1, D),
        "w_in": f(inputs["w_in"]),
        "w_out": f(inputs["w_out"]),
        "conv_wT": f(np.transpose(f(inputs["conv_w"]), (0, 2, 1))),
        "q_norm_g": f(inputs["q_norm_g"]).reshape(2, 1, 64),
        "k_norm_g": f(inputs["k_norm_g"]).reshape(2, 1, 64),
        "rw_mu": f(inputs["rw_mu"]).reshape(2, 1184, 1),
        "rw_w0": f(inputs["rw_w0"]).reshape(2, 512, 1),
        "rw_w_b": f(inputs["rw_w_b"]).reshape(2, 128, 256),
        "rw_a0": f(inputs["rw_a0"]).reshape(2, 512, 1),
        "rw_a_b": f(inputs["rw_a_b"]).reshape(2, 128, 256),
        "rw_g_b": f(inputs["rw_g_b"]),
        "rw_k_k": f(inputs["rw_k_k"]).reshape(2, 256, 1),
        "rw_k_a": f(inputs["rw_k_a"]).reshape(2, 256, 1),
        "rw_r_k": f(inputs["rw_r_k"]).reshape(2, 256, 1),
        "rw_ln_w": f(inputs["rw_ln_w"]).reshape(2, 256, 1),
        "rw_ln_b": f(inputs["rw_ln_b"]).reshape(2, 256, 1),
        "router_w": f(inputs["router_w"]),
        "exp_w_gate": f(inputs["exp_w_gate"]),
        "exp_w_up": f(inputs["exp_w_up"]),
        "exp_w_down": f(inputs["exp_w_down"]),
        "consts": make_consts(),
    }
    t = np.arange(SEQ)
    row = (t // 64).astype(np.float32)
    col = (t % 64).astype(np.float32)
    inv = (10000.0 ** (-np.arange(0, 32, 2, dtype=np.float32) / 32)).astype(np.float32)
    ang = np.concatenate([row[:, None] * inv, col[:, None] * inv], axis=-1).astype(np.float32)
    shared["cs_tab"] = np.ascontiguousarray(np.concatenate([np.cos(ang), np.sin(ang)], axis=-1).astype(np.float32))
    maps = []
    for b in range(x.shape[0]):
        m = dict(shared)
        m["x"] = x[b]
        m["ctx"] = ctx[b]
        c2 = np.stack([c[b], c_ctx], axis=-1)
        m["c2T"] = np.ascontiguousarray(c2.reshape(8, 128, 2).transpose(1, 0, 2))
        maps.append(m)
    return maps


_NC_CACHE = {}

W_KEYS = ["ada_w", "ada_b", "norm1_g", "norm2_g", "w_in", "w_out", "conv_wT", "q_norm_g", "k_norm_g", "rw_mu", "rw_w0", "rw_w_b",
          "rw_a0", "rw_a_b", "rw_g_b", "rw_k_k", "rw_k_a", "rw_r_k", "rw_ln_w", "rw_ln_b", "router_w",
          "exp_w_gate", "exp_w_up", "exp_w_down"]


def kernel(**inputs):
    maps = prep_inputs(inputs)
    if "nc" not in _NC_CACHE:
        _NC_CACHE["nc"] = build(layers=[0, 1])
    nc = _NC_CACHE["nc"]
    res = run_bass_kernel_spmd(nc, maps, core_ids=list(range(8)))
    return np.stack([np.asarray(r["out"], dtype=np.float32) for r in res.results], axis=0)
```

```python
import math
from contextlib import ExitStack

import numpy as np
import concourse.bass as bass
import concourse.mybir as mybir
from concourse.bass_utils import run_bass_kernel_spmd

F32 = mybir.dt.float32
BF16 = mybir.dt.bfloat16
I32 = mybir.dt.int32
U32 = mybir.dt.uint32
AF = mybir.ActivationFunctionType
ALU = mybir.AluOpType
AX = mybir.AxisListType

ENGS = ("sync", "act", "dve", "pool", "pe")

D = 1024
SEQ = 4096
CTX = 256
NT = SEQ + CTX
NTILE = NT // 128
PROJ = 2720
NE = 16
CAP_L = 512
CAP_C = 32
LCH = 64


class Prog:
    SEMID = 0

    def __init__(self, nc):
        self.nc = nc
        self.streams = {e: [] for e in ENGS}
        self.ecount = {e: 0 for e in ENGS}
        self.seen = {e: {} for e in ENGS}
        self.bufs = {}
        self.dcount = {}
        self.sems = {}

    @staticmethod
    def _k(b):
        if isinstance(b, (str, tuple, int)):
            return b
        return b.name

    def _deps(self, eng, reads, writes):
        need = {}

        def add(ev):
            for k, v in ev.items():
                if need.get(k, 0) < v:
                    need[k] = v

        for b in reads:
            st = self.bufs.get(b)
            if st:
                add(st["w"])
        for b in writes:
            st = self.bufs.get(b)
            if st:
                add(st["w"])
                add(st["r"])
        waits = []
        seen = self.seen[eng]
        for k, v in need.items():
            if k[0] == "e" and k[1] == eng and eng == "pe":
                continue
            if seen.get(k, 0) >= v:
                continue
            seen[k] = v
            waits.append((k, v))
        return waits

    def _commit(self, reads, writes, ev):
        for b in reads:
            st = self.bufs.setdefault(b, {"w": {}, "r": {}})
            for k, v in ev.items():
                if st["r"].get(k, 0) < v:
                    st["r"][k] = v
        for b in writes:
            self.bufs[b] = {"w": dict(ev), "r": {}}

    EPOCH = 3000

    def op(self, eng, fn, r=(), w=(), rows=None):
        r = [self._k(b) for b in r]
        w = [self._k(b) for b in w]
        bk = [k for k in r if isinstance(k, tuple) and k[0] in ("pqb", "sqb")]
        if bk:
            r = [k for k in r if k not in bk]
            w = w + bk
        waits = self._deps(eng, r, w)
        self.ecount[eng] += 1
        ep = (self.ecount[eng] - 1) // self.EPOCH
        ek = ("e", eng, ep)
        ev = {ek: self.ecount[eng] - ep * self.EPOCH}
        if eng == "pe":
            if not hasattr(self, "pe_rows"):
                self.pe_rows = {}
            for bk_ in w:
                last = self.pe_rows.get(bk_)
                if last is not None and rows in (0, 64) and last[0] in (0, 64) and last[0] != rows:
                    for k_, v_ in last[1].items():
                        if self.seen[eng].get(k_, 0) < v_:
                            self.seen[eng][k_] = v_
                            waits.append((k_, v_))
                self.pe_rows[bk_] = (rows, ev)
        self.streams[eng].append((waits, fn, (ek, 1)))
        self._commit(r, w, ev)

    def dma(self, q, out, in_, r=(), w=(), sem=None, **kw):
        r = [self._k(b) for b in r]
        w = [self._k(b) for b in w]
        if sem is None:
            sem = (w[0] if w else r[0])
        waits = self._deps(q, r, w)
        k = ("d", sem)
        self.dcount[k] = self.dcount.get(k, 0) + 16
        ev = {k: self.dcount[k]}
        self.streams[q].append((waits, (lambda e: e.dma_start(out=out, in_=in_, **kw)), (k, 16)))
        self._commit(r, w, ev)

    def dma_fn(self, q, fn, r=(), w=(), sem=None):
        r = [self._k(b) for b in r]
        w = [self._k(b) for b in w]
        waits = self._deps(q, r, w)
        k = ("d", sem)
        self.dcount[k] = self.dcount.get(k, 0) + 16
        ev = {k: self.dcount[k]}
        self.streams[q].append((waits, fn, (k, 16)))
        self._commit(r, w, ev)

    def barrier(self):
        for eng in ENGS:
            waits = [(k, v) for k, v in self.dcount.items()]
            for e in ENGS:
                if e != eng and self.ecount[e] > 0:
                    ep = (self.ecount[e] - 1) // self.EPOCH
                    waits.append((("e", e, ep), self.ecount[e] - ep * self.EPOCH))
            self.streams[eng].append((waits, None, None))

    POOL = None

    def emit(self):
        nc = self.nc
        pool = Prog.POOL
        totals = {}
        keys = []
        for e in ENGS:
            for waits, fn, inc in self.streams[e]:
                for k, v in waits:
                    if k not in totals:
                        totals[k] = 0
                        keys.append(k)
                if inc:
                    if inc[0] not in totals:
                        totals[inc[0]] = 0
                        keys.append(inc[0])
                    totals[inc[0]] += inc[1]
        n = len(pool["h"])
        assert len(keys) <= n, len(keys)
        base = {}
        for i, k in enumerate(sorted(keys, key=str)):
            idx = (pool["next"] + i) % n
            self.sems[k] = pool["h"][idx]
            base[k] = pool["v"][idx]
            pool["v"][idx] += totals[k]
        pool["next"] = (pool["next"] + len(keys)) % n
        with ExitStack() as es:
            block = es.enter_context(nc.Block())
            handles = {"sync": block.sync, "act": block.scalar, "dve": block.vector,
                       "pool": block.gpsimd, "pe": block.tensor}
            for e in ENGS:
                stream = self.streams[e]

                def body(h, stream=stream):
                    for waits, fn, inc in stream:
                        for k, v in waits:
                            h.wait_ge(self.sems[k], base[k] + v)
                        if fn is not None:
                            ins = fn(h)
                            ins.then_inc(self.sems[inc[0]], inc[1])

                handles[e](body)


class Ctx:
    pass


def build(n_layers=2, dbg=None, upto=None, skip=(), small=False, rwsteps=None, zero_mix=False, layers=None):
    dbg = dbg or set()
    nc = bass.Bass("TRN2", target_bir_lowering=False)
    g = Ctx()
    g.nc = nc
    if layers is None:
        layers = list(range(n_layers))
    NL = len(layers)
    g.first = layers[0]
    g.last = layers[-1]
    g.wi = lambda l: l - layers[0]
    if layers[-1] == 0:
        dbg = set(dbg) | {"xcres"}

    def din(name, shape, dt=F32):
        return nc.dram_tensor(name, list(shape), dt, kind="ExternalInput").ap()

    def dscr(name, shape, dt=F32):
        kind = "ExternalOutput" if name in dbg else "Internal"
        return nc.dram_tensor(name, list(shape), dt, kind=kind).ap()

    I = Ctx()
    I.x = din("x", [SEQ, D])
    I.ctx = din("ctx", [CTX, D])
    I.c2T = din("c2T", [128, 8, 2])
    I.ada_w = din("ada_w", [NL, D, 6 * D])
    I.ada_b = din("ada_b", [NL, 1, 6 * D])
    I.n1g = din("norm1_g", [NL, 1, D])
    I.n2g = din("norm2_g", [NL, 1, D])
    I.w_in = din("w_in", [NL, D, PROJ])
    I.w_out = din("w_out", [NL, D, D])
    I.conv_wT = din("conv_wT", [NL, 256, 3])
    I.qg = din("q_norm_g", [NL, 1, 64])
    I.kg = din("k_norm_g", [NL, 1, 64])
    I.mu = din("rw_mu", [NL, 1184, 1])
    I.w0 = din("rw_w0", [NL, 512, 1])
    I.w_b = din("rw_w_b", [NL, 128, 256])
    I.a0 = din("rw_a0", [NL, 512, 1])
    I.a_b = din("rw_a_b", [NL, 128, 256])
    I.g_b = din("rw_g_b", [NL, 160, 256])
    I.k_k = din("rw_k_k", [NL, 256, 1])
    I.k_a = din("rw_k_a", [NL, 256, 1])
    I.r_k = din("rw_r_k", [NL, 256, 1])
    I.ln_w = din("rw_ln_w", [NL, 256, 1])
    I.ln_b = din("rw_ln_b", [NL, 256, 1])
    I.router = din("router_w", [NL, D, NE])
    esh = [NL, 1, 8, 8] if small else [NL, NE, D, D]
    I.wg = din("exp_w_gate", esh)
    I.wu = din("exp_w_up", esh)
    I.wd = din("exp_w_down", esh)
    g.rwsteps = rwsteps
    I.cs = din("cs_tab", [SEQ, 64])
    I.consts = din("consts", [128, 8 * 128])
    out = nc.dram_tensor("out", [SEQ, D], F32, kind="ExternalOutput").ap()

    S = Ctx()
    S.modv = dscr("modv", [NL, 2, 6, D])
    S.pfm = dscr("pfm", [1952, NT])
    S.qT = dscr("qT", [8, 64, NT], BF16)
    S.mixT = dscr("mixT", [D, NT], BF16)
    S.xres = dscr("xres", [SEQ, D])
    S.xcres = dscr("xcres", [CTX, D])
    S.h2l = dscr("h2l", [SEQ, D], BF16)
    S.h2c = dscr("h2c", [CTX, D], BF16)
    S.rwf = dscr("rwf", [10, 256, NT])
    g.I, g.S, g.out = I, S, out

    with ExitStack() as gs:
        def gsb(name, shape, dt=F32):
            return gs.enter_context(nc.sbuf_tensor("g_" + name, list(shape), dt))
        Prog.POOL = {"h": [gs.enter_context(nc.semaphore("gp%d" % i)) for i in range(72)], "v": [0] * 72, "next": 0}
        g.consts = gsb("consts", [128, 8, 128])
        g.identb = gsb("identb", [128, 128], BF16)
        g.kT = gsb("kT", [64, 2, NT], BF16)
        g.Vaug = gsb("Vaug", [128, NTILE, 2, 65], BF16)
        g.affT = gsb("affT", [NE, NT])
        phase_consts(g)
        phases = [phase_adaln, phase_proj, phase_conv, phase_attn, phase_rwfeat, phase_rwscan] + ([phase_zero_mix] if zero_mix else []) + [phase_wout, phase_moe]
        for l in layers:
            for ph in phases:
                if ph.__name__ in skip:
                    continue
                ph(g, l)
                if upto == (ph.__name__, l):
                    return nc
    return nc


C_ID, C_BONES, C_MS_IT, C_MI_IT, C_MS_TI, C_RESET, C_MS_IT_B, C_MI_IT_B = range(8)


def make_consts():
    c = np.zeros((8, 128, 128), np.float32)
    i = np.arange(128)[:, None]
    t = np.arange(128)[None, :]
    same = (i // 64) == (t // 64)
    c[C_ID] = np.eye(128)
    c[C_BONES] = same
    c[C_MS_IT] = same & (i < t)
    c[C_MI_IT] = same & (i <= t)
    c[C_MS_TI] = same & (t < i)
    c[C_RESET] = (t % 64 != 0) * np.ones((128, 1))
    c[C_MS_IT_B] = same & (i > t)
    c[C_MI_IT_B] = same & (i >= t)
    return np.ascontiguousarray(c.transpose(1, 0, 2).reshape(128, 8 * 128))


def phase_consts(g):
    nc = g.nc
    P = Prog(nc)
    P.dma("sync", g.consts[:], g.I.consts.rearrange("p (a b) -> p a b", a=8), w=[g.consts])
    P.op("dve", lambda e: e.tensor_copy(out=g.identb[:], in_=g.consts[:, C_ID, :]), r=[g.consts], w=[g.identb])
    P.op("pool", lambda e: e.memset(g.Vaug[:, :, :, 64:65], 1.0), w=[g.Vaug])
    P.barrier()
    P.emit()


def phase_adaln(g, l):
    nc, I, S = g.nc, g.I, g.S
    with ExitStack() as es:
        def sb(name, shape, dt=F32):
            return es.enter_context(nc.sbuf_tensor("a%d_" % l + name, list(shape), dt))
        c2 = sb("c2", [128, 8, 2])
        sc = sb("sc", [128, 8, 2])
        wt = [sb("wt%d" % i, [128, 8, 512]) for i in range(2)]
        bias = sb("bias", [2, 6 * D])
        mod = sb("mod", [2, 6 * D])
        gg = sb("gg", [2, 2, D])
        mv = sb("mv", [2, 6, D])
        ps = [es.enter_context(nc.psum_tensor("a%d_ps%d" % (l, i), [2, 512], F32)) for i in range(2)]
        P = Prog(nc)
        P.dma("sync", c2[:], I.c2T[:, :, :], w=[c2])
        P.dma("sync", bias[:], I.ada_b[g.wi(l), 0:1, :].to_broadcast([2, 6 * D]), w=[bias])
        P.dma("sync", gg[:, 0, :], I.n1g[g.wi(l), 0:1, :].to_broadcast([2, D]), w=[gg], sem="gg")
        P.dma("sync", gg[:, 1, :], I.n2g[g.wi(l), 0:1, :].to_broadcast([2, D]), w=[gg], sem="gg")
        P.op("act", lambda e: e.activation(out=sc[:], in_=c2[:], func=AF.Silu), r=[c2], w=[sc])
        wv = I.ada_w[g.wi(l)].rearrange("(kc p) n -> p kc n", p=128)
        for cc in range(12):
            b = cc % 2
            P.dma("sync" if b == 0 else "pool", wt[b][:], wv[:, :, cc * 512:(cc + 1) * 512], w=[wt[b]])
            for kc in range(8):
                P.op("pe", lambda e, kc=kc, b=b: e.matmul(out=ps[b][:], lhsT=sc[:, kc, :], rhs=wt[b][:, kc, :],
                                                           start=(kc == 0), stop=(kc == 7)),
                     r=[sc, wt[b]], w=[ps[b]])
            P.op("dve", lambda e, cc=cc, b=b: e.tensor_tensor(out=mod[:, cc * 512:(cc + 1) * 512], in0=ps[b][:],
                                                               in1=bias[:, cc * 512:(cc + 1) * 512], op=ALU.add),
                 r=[ps[b], bias], w=[mod])
        for j, (sci, shi, gti) in enumerate(((1, 0, 2), (4, 3, 5))):
            P.op("dve", lambda e, j=j, sci=sci: e.scalar_tensor_tensor(
                out=mv[:, 3 * j, :], in0=mod[:, sci * D:(sci + 1) * D], scalar=1.0, in1=gg[:, j, :],
                op0=ALU.add, op1=ALU.mult), r=[mod, gg], w=[mv])
            P.op("dve", lambda e, j=j, shi=shi: e.tensor_copy(out=mv[:, 3 * j + 1, :], in_=mod[:, shi * D:(shi + 1) * D]),
                 r=[mod], w=[mv])
            P.op("dve", lambda e, j=j, gti=gti: e.tensor_copy(out=mv[:, 3 * j + 2, :], in_=mod[:, gti * D:(gti + 1) * D]),
                 r=[mod], w=[mv])
        P.dma("sync", S.modv[g.wi(l)], mv[:], r=[mv], sem="mvst")
        P.barrier()
        P.emit()


def phase_proj(g, l):
    nc, I, S = g.nc, g.I, g.S
    with ExitStack() as es:
        def sb(name, shape, dt=F32):
            return es.enter_context(nc.sbuf_tensor("b%d_" % l + name, list(shape), dt))

        def psb(name, shape, dt=F32):
            return es.enter_context(nc.psum_tensor("b%d_" % l + name, list(shape), dt))
        wb = sb("wb", [128, 8, PROJ], BF16)
        m1 = [sb("m1_%d" % i, [128, D]) for i in range(2)]
        sh1 = [sb("sh1_%d" % i, [128, D]) for i in range(2)]
        qkg = sb("qkg", [128, 2, 64])
        cs = sb("cs", [128, 32, 64])
        xt = [sb("xt%d" % i, [128, D]) for i in range(2)]
        junk = sb("junk", [128, D])
        ss = [sb("ss%d" % i, [128, 1]) for i in range(2)]
        hb = [sb("hb%d" % i, [128, D], BF16) for i in range(2)]
        hT = [sb("hT%d" % i, [128, 8, 512], BF16) for i in range(2)]
        fm = [sb("fm%d" % i, [128, 512]) for i in range(3)]
        qkv = [sb("qkv%d" % i, [128, 768]) for i in range(2)]
        sq = sb("sq", [128, 640])
        ssq = sb("ssq", [128, 10])
        qn = sb("qn", [128, 10, 64])
        qr = sb("qr", [128, 10, 64])
        qrb = [sb("qrb%d" % i, [128, 10, 64], BF16) for i in range(2)]
        tmp = sb("tmp", [128, 10, 32])
        qTs = [sb("qTs%d" % i, [64, 8, 128], BF16) for i in range(2)]
        pT = [psb("pT%d" % i, [128, 8, 128], BF16) for i in range(2)]
        pF = [psb("pF%d" % i, [128, 512]) for i in range(2)]
        pA = psb("pA", [128, 512])
        pB = psb("pB", [128, 512])
        pQ = psb("pQ", [64, 8, 128], BF16)
        pQk = psb("pQk", [64, 8, 128], BF16)
        P = Prog(nc)
        wv = I.w_in[g.wi(l)].rearrange("(kc p) n -> p kc n", p=128)
        for (c0, c1) in ((0, 1024), (1024, 2048), (2048, PROJ)):
            P.dma("pool", wb[:, :, c0:c1], wv[:, :, c0:c1], w=[("wb", c0)], sem=("wb", c0))
        wbk = [("wb", 0), ("wb", 1024), ("wb", 2048)]
        for j in range(2):
            P.dma("sync", m1[j][:], S.modv[g.wi(l), j, 0:1, :].to_broadcast([128, D]), w=[m1[j]])
            P.dma("sync", sh1[j][:], S.modv[g.wi(l), j, 1:2, :].to_broadcast([128, D]), w=[sh1[j]])
        P.dma("sync", qkg[:, 0, :], I.qg[g.wi(l), 0:1, :].to_broadcast([128, 64]), w=[qkg], sem="qkg")
        P.dma("sync", qkg[:, 1, :], I.kg[g.wi(l), 0:1, :].to_broadcast([128, 64]), w=[qkg], sem="qkg")
        P.dma("sync", cs[:], I.cs.rearrange("(i p) c -> p i c", p=128), w=[cs])
        fchunks = [(c, 128, c) for c in range(0, 768, 128)]
        for j in range(10):
            c = 1536 + j * 128
            wdt = min(128, PROJ - c)
            fchunks.append((c, wdt, 768 + j * 128))
        sts = [(0, 2)] + [(2 + 4 * s, 4) for s in range(8)]
        fmi = 0
        for si, (t0, ntile) in enumerate(sts):
            hTs = hT[si % 2]
            ntok = ntile * 128
            for ti in range(ntile):
                i = t0 + ti
                b = i % 2
                j = 1 if i < 2 else 0
                if i < 2:
                    src = (I.ctx if l == g.first else S.xcres)[i * 128:(i + 1) * 128, :]
                else:
                    src = (I.x if l == g.first else S.xres)[(i - 2) * 128:(i - 1) * 128, :]
                P.dma("sync", xt[b][:], src, w=[xt[b]])
                P.op("act", lambda e, b=b: e.activation(out=junk[:], in_=xt[b][:], func=AF.Square, accum_out=ss[b][:]),
                     r=[xt[b]], w=[junk, ss[b]])
                P.op("dve", lambda e, b=b: e.tensor_scalar(out=ss[b][:], in0=ss[b][:], scalar1=1.0 / D, scalar2=1e-6,
                                                            op0=ALU.mult, op1=ALU.add), r=[ss[b]], w=[ss[b]])
                P.op("act", lambda e, b=b: e.activation(out=ss[b][:], in_=ss[b][:], func=AF.Sqrt), r=[ss[b]], w=[ss[b]])
                P.op("dve", lambda e, b=b: e.reciprocal(out=ss[b][:], in_=ss[b][:]), r=[ss[b]], w=[ss[b]])
                P.op("dve", lambda e, b=b, j=j: e.scalar_tensor_tensor(out=xt[b][:], in0=xt[b][:], scalar=ss[b][:, 0:1],
                                                                       in1=m1[j][:], op0=ALU.mult, op1=ALU.mult),
                     r=[xt[b], ss[b], m1[j]], w=[xt[b]])
                P.op("pool", lambda e, b=b, j=j: e.tensor_tensor(out=hb[b][:], in0=xt[b][:], in1=sh1[j][:], op=ALU.add),
                     r=[xt[b], sh1[j]], w=[hb[b]])
                for half in range(2):
                    for k4 in range(4):
                        kc = half * 4 + k4
                        P.op("pe", lambda e, b=b, kc=kc, k4=k4, half=half: e.transpose(
                            out=pT[half][:, k4, :], in_=hb[b][:, kc * 128:(kc + 1) * 128], identity=g.identb[:]),
                            r=[hb[b], g.identb], w=[pT[half]])
                    eng = "act" if half == 0 else "dve"
                    if eng == "act":
                        P.op("act", lambda e, half=half, ti=ti, hTs=hTs: e.activation(
                            out=hTs[:, half * 4:(half + 1) * 4, ti * 128:(ti + 1) * 128], in_=pT[half][:, 0:4, :], func=AF.Copy),
                            r=[pT[half]], w=[hTs])
                    else:
                        P.op("dve", lambda e, half=half, ti=ti, hTs=hTs: e.tensor_copy(
                            out=hTs[:, half * 4:(half + 1) * 4, ti * 128:(ti + 1) * 128], in_=pT[half][:, 0:4, :]),
                            r=[pT[half]], w=[hTs])
                for kc in range(8):
                    P.op("pe", lambda e, kc=kc, ti=ti, hTs=hTs: e.matmul(
                        out=pA[:], lhsT=hTs[:, kc, ti * 128:(ti + 1) * 128], rhs=wb[:, kc, 768:1280],
                        start=(kc == 0), stop=(kc == 7)), r=[hTs] + wbk, w=[pA])
                for kc in range(8):
                    P.op("pe", lambda e, kc=kc, ti=ti, hTs=hTs: e.matmul(
                        out=pB[:, 0:256], lhsT=hTs[:, kc, ti * 128:(ti + 1) * 128], rhs=wb[:, kc, 1280:1536],
                        start=(kc == 0), stop=(kc == 7)), r=[hTs] + wbk, w=[pB])
                qv = qkv[b]
                P.op("act", lambda e, qv=qv: e.activation(out=qv[:, 0:512], in_=pA[:], func=AF.Copy), r=[pA], w=[qv])
                P.op("act", lambda e, qv=qv: e.activation(out=qv[:, 512:768], in_=pB[:, 0:256], func=AF.Copy), r=[pB], w=[qv])
                P.op("pool", lambda e, qv=qv, i=i: e.tensor_copy(
                    out=g.Vaug[:, i, :, 0:64], in_=qv[:, 640:768].rearrange("p (g d) -> p g d", g=2)),
                    r=[qv], w=[("Vaug", i)])
                P.op("dve", lambda e, qv=qv: e.tensor_tensor(out=sq[:], in0=qv[:, 0:640], in1=qv[:, 0:640], op=ALU.mult),
                     r=[qv], w=[sq])
                P.op("dve", lambda e: e.tensor_reduce(out=ssq[:], in_=sq[:].rearrange("p (h d) -> p h d", h=10),
                                                       axis=AX.X, op=ALU.add), r=[sq], w=[ssq])
                P.op("dve", lambda e: e.tensor_scalar(out=ssq[:], in0=ssq[:], scalar1=1.0 / 64, scalar2=1e-6,
                                                       op0=ALU.mult, op1=ALU.add), r=[ssq], w=[ssq])
                P.op("act", lambda e: e.activation(out=ssq[:], in_=ssq[:], func=AF.Sqrt), r=[ssq], w=[ssq])
                P.op("dve", lambda e: e.reciprocal(out=ssq[:], in_=ssq[:]), r=[ssq], w=[ssq])
                P.op("dve", lambda e, qv=qv: e.tensor_tensor(
                    out=qn[:], in0=qv[:, 0:640].rearrange("p (h d) -> p h d", h=10),
                    in1=ssq[:].unsqueeze(2).to_broadcast([128, 10, 64]), op=ALU.mult), r=[qv, ssq], w=[qn])
                P.op("dve", lambda e: e.tensor_tensor(out=qn[:, 0:8, :], in0=qn[:, 0:8, :],
                                                       in1=qkg[:, 0:1, :].to_broadcast([128, 8, 64]), op=ALU.mult),
                     r=[qn, qkg], w=[qn])
                P.op("dve", lambda e: e.tensor_tensor(out=qn[:, 8:10, :], in0=qn[:, 8:10, :],
                                                       in1=qkg[:, 1:2, :].to_broadcast([128, 2, 64]), op=ALU.mult),
                     r=[qn, qkg], w=[qn])
                qb = qrb[b]
                if i < 2:
                    P.op("dve", lambda e, qb=qb: e.tensor_copy(out=qb[:], in_=qn[:]), r=[qn], w=[qb])
                else:
                    li = i - 2
                    cosb = cs[:, li:li + 1, 0:32].to_broadcast([128, 10, 32])
                    sinb = cs[:, li:li + 1, 32:64].to_broadcast([128, 10, 32])
                    x1 = qn[:, :, 0:32]
                    x2 = qn[:, :, 32:64]
                    P.op("dve", lambda e, cosb=cosb: e.tensor_tensor(out=qr[:, :, 0:32], in0=qn[:, :, 0:32], in1=cosb, op=ALU.mult),
                         r=[qn, cs], w=[qr])
                    P.op("dve", lambda e, sinb=sinb: e.tensor_tensor(out=tmp[:], in0=qn[:, :, 32:64], in1=sinb, op=ALU.mult),
                         r=[qn, cs], w=[tmp])
                    P.op("dve", lambda e, qb=qb: e.tensor_tensor(out=qb[:, :, 0:32], in0=qr[:, :, 0:32], in1=tmp[:], op=ALU.subtract),
                         r=[qr, tmp], w=[qb])
                    P.op("dve", lambda e, sinb=sinb: e.tensor_tensor(out=qr[:, :, 32:64], in0=qn[:, :, 0:32], in1=sinb, op=ALU.mult),
                         r=[qn, cs], w=[qr])
                    P.op("dve", lambda e, cosb=cosb: e.tensor_tensor(out=tmp[:], in0=qn[:, :, 32:64], in1=cosb, op=ALU.mult),
                         r=[qn, cs, qb], w=[tmp])
                    P.op("dve", lambda e, qb=qb: e.tensor_tensor(out=qb[:, :, 32:64], in0=qr[:, :, 32:64], in1=tmp[:], op=ALU.add),
                         r=[qr, tmp], w=[qb])
                for h in range(8):
                    P.op("pe", lambda e, h=h, qb=qb: e.transpose(out=pQ[:, h, :], in_=qb[:, h, :], identity=g.identb[:]),
                         r=[qb, g.identb], w=[pQ])
                for h in range(2):
                    P.op("pe", lambda e, h=h, qb=qb: e.transpose(out=pQk[:, h, :], in_=qb[:, 8 + h, :], identity=g.identb[:]),
                         r=[qb, g.identb], w=[pQk])
                qs = qTs[b]
                P.op("act", lambda e, qs=qs: e.activation(out=qs[:], in_=pQ[:], func=AF.Copy), r=[pQ], w=[qs])
                P.op("dve", lambda e, i=i: e.tensor_copy(out=g.kT[:, :, i * 128:(i + 1) * 128], in_=pQk[:, 0:2, :]),
                     r=[pQk], w=[("kT", i)])
                P.dma("pool", S.qT[:, :, i * 128:(i + 1) * 128].rearrange("h d t -> d h t"), qs[:], r=[qs], sem=("qs", b))
            for (c0, wdt, r0) in fchunks:
                pb = pF[fmi % 2]
                fb = fm[fmi % 3]
                for kc in range(8):
                    P.op("pe", lambda e, kc=kc, c0=c0, wdt=wdt, pb=pb, hTs=hTs, ntok=ntok: e.matmul(
                        out=pb[0:wdt, 0:ntok], lhsT=wb[:, kc, c0:c0 + wdt], rhs=hTs[:, kc, 0:ntok],
                        start=(kc == 0), stop=(kc == 7)), r=[hTs] + wbk, w=[pb])
                if fmi % 2 == 0:
                    P.op("act", lambda e, wdt=wdt, pb=pb, fb=fb, ntok=ntok: e.activation(
                        out=fb[0:wdt, 0:ntok], in_=pb[0:wdt, 0:ntok], func=AF.Copy), r=[pb], w=[fb])
                else:
                    P.op("dve", lambda e, wdt=wdt, pb=pb, fb=fb, ntok=ntok: e.tensor_copy(
                        out=fb[0:wdt, 0:ntok], in_=pb[0:wdt, 0:ntok]), r=[pb], w=[fb])
                P.dma("sync", S.pfm[r0:r0 + wdt, t0 * 128:t0 * 128 + ntok], fb[0:wdt, 0:ntok], r=[fb], sem=("fm", fmi % 3))
                fmi += 1
        P.barrier()
        P.emit()


def phase_conv(g, l):
    nc, I, S = g.nc, g.I, g.S
    with ExitStack() as es:
        def sb(name, shape, dt=F32):
            return es.enter_context(nc.sbuf_tensor("c%d_" % l + name, list(shape), dt))
        Bt = sb("Bt", [128, SEQ])
        Ct = sb("Ct", [128, SEQ])
        Ut = sb("Ut", [128, SEQ])
        zp = sb("zp", [128, SEQ + 2])
        acc = sb("acc", [128, SEQ])
        ob = sb("ob", [128, SEQ], BF16)
        cw = sb("cw", [128, 2, 3])
        P = Prog(nc)
        P.dma("sync", cw[:], I.conv_wT[g.wi(l)].rearrange("(c p) k -> p c k", p=128), w=[cw])
        seqs = [(CTX, SEQ)] + ([(0, CTX)] if l == 0 else [])
        for (t0, T) in seqs:
            for cc in range(2):
                P.dma("sync", Bt[:, 0:T], S.pfm[cc * 128:(cc + 1) * 128, t0:t0 + T], w=[Bt])
                P.dma("sync", Ct[:, 0:T], S.pfm[256 + cc * 128:256 + (cc + 1) * 128, t0:t0 + T], w=[Ct])
                P.dma("pool", Ut[:, 0:T], S.pfm[512 + cc * 128:512 + (cc + 1) * 128, t0:t0 + T], w=[Ut])
                P.op("pool", lambda e, T=T: e.memset(zp[:, 0:1], 0.0), w=[zp])
                P.op("pool", lambda e, T=T: e.memset(zp[:, T + 1:T + 2], 0.0), w=[zp])
                P.op("dve", lambda e, T=T: e.tensor_tensor(out=zp[:, 1:T + 1], in0=Ct[:, 0:T], in1=Ut[:, 0:T], op=ALU.mult),
                     r=[Ct, Ut], w=[zp])
                P.op("dve", lambda e, T=T, cc=cc: e.tensor_scalar(out=acc[:, 0:T], in0=zp[:, 0:T], scalar1=cw[:, cc, 0:1],
                                                                   scalar2=None, op0=ALU.mult), r=[zp, cw], w=[acc])
                P.op("dve", lambda e, T=T, cc=cc: e.scalar_tensor_tensor(out=acc[:, 0:T], in0=zp[:, 1:T + 1], scalar=cw[:, cc, 1:2],
                                                                         in1=acc[:, 0:T], op0=ALU.mult, op1=ALU.add),
                     r=[zp, cw, acc], w=[acc])
                P.op("dve", lambda e, T=T, cc=cc: e.scalar_tensor_tensor(out=acc[:, 0:T], in0=zp[:, 2:T + 2], scalar=cw[:, cc, 2:3],
                                                                         in1=acc[:, 0:T], op0=ALU.mult, op1=ALU.add),
                     r=[zp, cw, acc], w=[acc])
                P.op("pool", lambda e, T=T: e.tensor_tensor(out=ob[:, 0:T], in0=acc[:, 0:T], in1=Bt[:, 0:T], op=ALU.mult),
                     r=[acc, Bt], w=[ob])
                P.dma("sync", S.mixT[cc * 128:(cc + 1) * 128, t0:t0 + T], ob[:, 0:T], r=[ob], sem="obst")
        P.barrier()
        P.emit()


def phase_attn(g, l):
    nc, I, S = g.nc, g.I, g.S
    with ExitStack() as es:
        def sb(name, shape, dt=F32):
            return es.enter_context(nc.sbuf_tensor("d%d_" % l + name, list(shape), dt))

        def psb(name, shape, dt=F32):
            return es.enter_context(nc.psum_tensor("d%d_" % l + name, list(shape), dt))
        qc = [sb("qc%d" % i, [64, 512], BF16) for i in range(2)]
        eS = [sb("eS%d" % i, [128, 512], BF16) for i in range(4)]
        rs = sb("rs", [128, 512])
        rsb = sb("rsb", [64, 512])
        ob = [sb("ob%d" % i, [64, 512], BF16) for i in range(2)]
        nb = sb("nb", [128, 1])
        pS = [psb("pS%d" % i, [128, 512]) for i in range(4)]
        pO = [psb("pO%d" % i, [128, 512]) for i in range(2)]
        pR = psb("pR", [64, 512])
        P = Prog(nc)
        P.op("pool", lambda e: e.memset(nb[:], -8.0), w=[nb])
        jobs = []
        if l == 0:
            for h in range(8):
                jobs.append((h, 0, CTX, [0, 1]))
        for h in range(8):
            for qi in range(8):
                jobs.append((h, CTX + qi * 512, 512, list(range(NTILE))))
        cnt = 0
        for ji, (h, q0, nq, kts) in enumerate(jobs):
            gkv = h // 4
            qb = qc[ji % 2]
            po = pO[ji % 2]
            P.dma("sync", qb[:, 0:nq], S.qT[h, :, q0:q0 + nq], w=[qb])
            nk = len(kts)
            LOOK = 3
            slots = {}

            def emit_s(ki, cnt0=cnt, kts=kts, qb=qb, nq=nq, gkv=gkv):
                kt = kts[ki]
                ps = pS[(cnt0 + ki) % 4]
                ee = eS[(cnt0 + ki) % 4]
                P.op("pe", lambda e, ps=ps, kt=kt: e.matmul(
                    out=ps[:, 0:nq], lhsT=g.kT[:, gkv, kt * 128:(kt + 1) * 128], rhs=qb[:, 0:nq], start=True, stop=True),
                    r=[qb, ("kT", kt)], w=[ps])
                P.op("act", lambda e, ps=ps, ee=ee: e.activation(out=ee[:, 0:nq], in_=ps[:, 0:nq], func=AF.Exp,
                                                                 bias=nb[:, 0:1], scale=0.125),
                     r=[ps, nb], w=[ee])

            def emit_pv(ki, cnt0=cnt, kts=kts, po=po, nq=nq, gkv=gkv, nk=nk):
                kt = kts[ki]
                ee = eS[(cnt0 + ki) % 4]
                P.op("pe", lambda e, kt=kt, ee=ee: e.matmul(
                    out=po[0:65, 0:nq], lhsT=g.Vaug[:, kt, gkv, :], rhs=ee[:, 0:nq], start=(ki == 0), stop=(ki == nk - 1)),
                    r=[ee, ("Vaug", kt), g.Vaug], w=[po])

            for ki in range(nk + LOOK):
                if ki < nk:
                    emit_s(ki)
                if ki - LOOK >= 0:
                    emit_pv(ki - LOOK)
            cnt += nk
            P.op("dve", lambda e, po=po, nq=nq: e.reciprocal(out=rs[64:65, 0:nq], in_=po[64:65, 0:nq]), r=[po], w=[rs])
            P.op("pe", lambda e, nq=nq: e.matmul(out=pR[:, 0:nq], lhsT=g.consts[64:65, C_BONES, 64:128], rhs=rs[64:65, 0:nq],
                                                  start=True, stop=True), r=[rs, g.consts], w=[pR])
            P.op("act", lambda e, nq=nq: e.activation(out=rsb[:, 0:nq], in_=pR[:, 0:nq], func=AF.Copy), r=[pR], w=[rsb])
            o = ob[ji % 2]
            P.op("dve", lambda e, po=po, o=o, nq=nq: e.tensor_tensor(out=o[:, 0:nq], in0=po[0:64, 0:nq], in1=rsb[:, 0:nq], op=ALU.mult),
                 r=[po, rsb], w=[o])
            P.dma("pool", S.mixT[256 + h * 64:256 + (h + 1) * 64, q0:q0 + nq], o[:, 0:nq], r=[o], sem=("ob", ji % 2))
        P.barrier()
        P.emit()


NEG_EM05 = -math.exp(-0.5)


def phase_rwfeat(g, l):
    nc, I, S = g.nc, g.I, g.S
    with ExitStack() as es:
        def sb(name, shape, dt=F32):
            return es.enter_context(nc.sbuf_tensor("e%d_" % l + name, list(shape), dt))

        def psb(name, shape, dt=F32):
            return es.enter_context(nc.psum_tensor("e%d_" % l + name, list(shape), dt))
        SEG = 512
        rch = [("r0", 768, 128), ("r1", 896, 128), ("k0", 1024, 128), ("k1", 1152, 128), ("v0", 1280, 128),
               ("v1", 1408, 128), ("wl", 1536, 128), ("al", 1664, 128), ("g0", 1792, 128), ("g1", 1920, 32)]
        mu = sb("mu", [128, 10])
        omu = sb("omu", [128, 10])
        hmu = sb("hmu", [128, 10])
        pt = [sb("pt%d" % i, [128, SEG + 2]) for i in range(3)]
        s1 = [sb("s1%d" % i, [128, SEG]) for i in range(2)]
        sh = {nm: sb("sh_" + nm, [128, SEG]) for nm, _, _ in rch}
        w0c = sb("w0c", [128, 4])
        a0c = sb("a0c", [128, 4])
        kkc = sb("kkc", [128, 2])
        kac = sb("kac", [128, 2])
        omka = sb("omka", [128, 2])
        wbt = sb("wbt", [128, 256])
        abt = sb("abt", [128, 256])
        gb0 = sb("gb0", [128, 256])
        gb1 = sb("gb1", [32, 256])
        twl = sb("twl", [128, SEG])
        sg0 = sb("sg0", [128, SEG])
        sg1 = sb("sg1", [32, SEG])
        lw = [sb("lw%d" % i, [128, SEG]) for i in range(4)]
        asg = [sb("asg%d" % i, [128, SEG]) for i in range(4)]
        kk = [sb("kk%d" % i, [128, SEG]) for i in range(2)]
        sq = sb("sq", [128, SEG])
        rn = sb("rn", [128, SEG])
        kd = [sb("kd%d" % i, [128, SEG]) for i in range(4)]
        bd = [sb("bd%d" % i, [128, SEG]) for i in range(4)]
        gt = [sb("gt%d" % i, [128, SEG]) for i in range(2)]
        tq = sb("tq", [128, SEG])
        pp = [psb("pp%d" % i, [128, SEG]) for i in range(6)]
        P = Prog(nc)
        P.op("pool", lambda e: e.memset(mu[:, 9:10], 0.0), w=[mu])
        for ci, (nm, r0, nr) in enumerate(rch):
            P.dma("sync", mu[0:nr, ci:ci + 1], I.mu[g.wi(l), r0 - 768:r0 - 768 + nr, :], w=[mu], sem="mu")
        P.op("dve", lambda e: e.tensor_scalar(out=omu[:], in0=mu[:], scalar1=-1.0, scalar2=1.0, op0=ALU.mult, op1=ALU.add),
             r=[mu], w=[omu])
        P.op("dve", lambda e: e.tensor_scalar(out=hmu[:], in0=mu[:], scalar1=0.5, scalar2=None, op0=ALU.mult), r=[mu], w=[hmu])
        for d in range(2):
            for cc in range(2):
                P.dma("sync", w0c[:, d * 2 + cc:d * 2 + cc + 1], I.w0[g.wi(l), d * 256 + cc * 128:d * 256 + (cc + 1) * 128, :], w=[w0c], sem="w0c")
                P.dma("sync", a0c[:, d * 2 + cc:d * 2 + cc + 1], I.a0[g.wi(l), d * 256 + cc * 128:d * 256 + (cc + 1) * 128, :], w=[a0c], sem="a0c")
        for cc in range(2):
            P.dma("sync", kkc[:, cc:cc + 1], I.k_k[g.wi(l), cc * 128:(cc + 1) * 128, :], w=[kkc], sem="kkc")
            P.dma("sync", kac[:, cc:cc + 1], I.k_a[g.wi(l), cc * 128:(cc + 1) * 128, :], w=[kac], sem="kac")
        P.op("dve", lambda e: e.tensor_scalar(out=omka[:], in0=kac[:], scalar1=-1.0, scalar2=1.0, op0=ALU.mult, op1=ALU.add),
             r=[kac], w=[omka])
        P.dma("sync", wbt[:], I.w_b[g.wi(l)], w=[wbt])
        P.dma("sync", abt[:], I.a_b[g.wi(l)], w=[abt])
        P.dma("sync", gb0[:], I.g_b[g.wi(l), 0:128, :], w=[gb0])
        P.dma("sync", gb1[:], I.g_b[g.wi(l), 128:160, :], w=[gb1])
        segs = [(0, 0, CTX, CTX)] + [(CTX, CTX + i * SEG, SEG, SEQ) for i in range(8)]
        pti = 0
        sti = 0
        ppi = 0

        def store(idx, cc, src, n, t0):
            nonlocal sti
            q = "sync" if sti % 2 == 0 else "pool"
            sti += 1
            P.dma(q, S.rwf[idx, cc * 128:(cc + 1) * 128, t0:t0 + n], src[:, 0:n], r=[src], sem=("st", src.name))

        for (sq0, t0, n, slen) in segs:
            for ci, (nm, r0, nr) in enumerate(rch):
                p_ = pt[pti % 3]
                s_ = s1[pti % 2]
                pti += 1
                lo = t0 - 1
                hi = t0 + n + 1
                dlo, dhi = 0, n + 2
                if t0 == sq0:
                    lo += 1
                    dlo = 1
                    P.op("pool", lambda e, p_=p_, nr=nr: e.memset(p_[0:nr, 0:1], 0.0), w=[p_])
                if t0 + n == sq0 + slen:
                    hi -= 1
                    dhi = n + 1
                    P.op("pool", lambda e, p_=p_, nr=nr, n=n: e.memset(p_[0:nr, n + 1:n + 2], 0.0), w=[p_])
                P.dma("sync" if ci % 2 == 0 else "pool", p_[0:nr, dlo:dhi], S.pfm[r0:r0 + nr, lo:hi], w=[p_])
                P.op("pool", lambda e, p_=p_, s_=s_, nr=nr, n=n: e.tensor_tensor(out=s_[0:nr, 0:n], in0=p_[0:nr, 0:n], in1=p_[0:nr, 2:n + 2], op=ALU.add),
                     r=[p_], w=[s_])
                P.op("dve", lambda e, p_=p_, nr=nr, n=n, ci=ci, nm=nm: e.tensor_scalar(out=sh[nm][0:nr, 0:n], in0=p_[0:nr, 1:n + 1], scalar1=omu[0:nr, ci:ci + 1],
                                                                                scalar2=None, op0=ALU.mult), r=[p_, omu], w=[sh[nm]])
                P.op("dve", lambda e, s_=s_, nr=nr, n=n, ci=ci, nm=nm: e.scalar_tensor_tensor(out=sh[nm][0:nr, 0:n], in0=s_[0:nr, 0:n], scalar=hmu[0:nr, ci:ci + 1],
                                                                                       in1=sh[nm][0:nr, 0:n], op0=ALU.mult, op1=ALU.add),
                     r=[s_, hmu, sh[nm]], w=[sh[nm]])
            P.op("act", lambda e, n=n: e.activation(out=twl[:, 0:n], in_=sh["wl"][:, 0:n], func=AF.Tanh), r=[sh["wl"]], w=[twl])
            P.op("act", lambda e, n=n: e.activation(out=sg0[:, 0:n], in_=sh["g0"][:, 0:n], func=AF.Sigmoid), r=[sh["g0"]], w=[sg0])
            P.op("act", lambda e, n=n: e.activation(out=sg1[:, 0:n], in_=sh["g1"][0:32, 0:n], func=AF.Sigmoid), r=[sh["g1"]], w=[sg1])
            for d in range(2):
                for cc in range(2):
                    ix = d * 2 + cc
                    pw = pp[ppi % 6]
                    ppi += 1
                    P.op("pe", lambda e, pw=pw, d=d, cc=cc, n=n: e.matmul(out=pw[:, 0:n], lhsT=wbt[d * 64:(d + 1) * 64, cc * 128:(cc + 1) * 128],
                                                                       rhs=twl[d * 64:(d + 1) * 64, 0:n], start=True, stop=True),
                         r=[wbt, twl], w=[pw])
                    P.op("act", lambda e, pw=pw, ix=ix, n=n: e.activation(out=lw[ix][:, 0:n], in_=pw[:, 0:n], func=AF.Sigmoid, bias=w0c[:, ix:ix + 1]),
                         r=[pw, w0c], w=[lw[ix]])
                    P.op("pool", lambda e, ix=ix, n=n: e.tensor_scalar(out=lw[ix][:, 0:n], in0=lw[ix][:, 0:n], scalar1=NEG_EM05, scalar2=None, op0=ALU.mult),
                         r=[lw[ix]], w=[lw[ix]])
                    store(7 + d, cc, lw[ix], n, t0)
                    pa = pp[ppi % 6]
                    ppi += 1
                    P.op("pe", lambda e, pa=pa, d=d, cc=cc, n=n: e.matmul(out=pa[:, 0:n], lhsT=abt[d * 64:(d + 1) * 64, cc * 128:(cc + 1) * 128],
                                                                       rhs=sh["al"][d * 64:(d + 1) * 64, 0:n], start=True, stop=True),
                         r=[abt, sh["al"]], w=[pa])
                    P.op("act", lambda e, pa=pa, ix=ix, n=n: e.activation(out=asg[ix][:, 0:n], in_=pa[:, 0:n], func=AF.Sigmoid, bias=a0c[:, ix:ix + 1]),
                         r=[pa, a0c], w=[asg[ix]])
            for cc in range(2):
                kx = sh["k%d" % cc]
                P.op("dve", lambda e, cc=cc, kx=kx, n=n: e.tensor_scalar(out=kk[cc][:, 0:n], in0=kx[:, 0:n], scalar1=kkc[:, cc:cc + 1], scalar2=None, op0=ALU.mult),
                     r=[kx, kkc], w=[kk[cc]])
                P.op("pool", lambda e, cc=cc, n=n: e.tensor_tensor(out=sq[:, 0:n], in0=kk[cc][:, 0:n], in1=kk[cc][:, 0:n], op=ALU.mult),
                     r=[kk[cc]], w=[sq])
                pn = pp[ppi % 6]
                ppi += 1
                P.op("pe", lambda e, pn=pn, n=n: e.matmul(out=pn[:, 0:n], lhsT=g.consts[:, C_BONES, :], rhs=sq[:, 0:n], start=True, stop=True),
                     r=[sq, g.consts], w=[pn])
                P.op("act", lambda e, pn=pn, n=n: e.activation(out=rn[:, 0:n], in_=pn[:, 0:n], func=AF.Sqrt), r=[pn], w=[rn])
                P.op("dve", lambda e, n=n: e.tensor_scalar(out=rn[:, 0:n], in0=rn[:, 0:n], scalar1=1e-12, scalar2=None, op0=ALU.max), r=[rn], w=[rn])
                P.op("dve", lambda e, n=n: e.reciprocal(out=rn[:, 0:n], in_=rn[:, 0:n]), r=[rn], w=[rn])
                P.op("dve", lambda e, cc=cc, n=n: e.tensor_tensor(out=kk[cc][:, 0:n], in0=kk[cc][:, 0:n], in1=rn[:, 0:n], op=ALU.mult),
                     r=[kk[cc], rn], w=[kk[cc]])
                store(4, cc, kk[cc], n, t0)
                store(0, cc, sh["r%d" % cc], n, t0)
                store(3, cc, sh["v%d" % cc], n, t0)
                for d in range(2):
                    ix = d * 2 + cc
                    P.op("dve", lambda e, ix=ix, cc=cc, n=n: e.tensor_scalar(out=tq[:, 0:n], in0=asg[ix][:, 0:n], scalar1=kac[:, cc:cc + 1],
                                                                         scalar2=omka[:, cc:cc + 1], op0=ALU.mult, op1=ALU.add),
                         r=[asg[ix], kac, omka], w=[tq])
                    P.op("dve", lambda e, ix=ix, kx=kx, n=n: e.tensor_tensor(out=kd[ix][:, 0:n], in0=tq[:, 0:n], in1=kx[:, 0:n], op=ALU.mult),
                         r=[tq, kx], w=[kd[ix]])
                    store(1 + d, cc, kd[ix], n, t0)
                    P.op("pool", lambda e, ix=ix, cc=cc, n=n: e.tensor_tensor(out=bd[ix][:, 0:n], in0=kk[cc][:, 0:n], in1=asg[ix][:, 0:n], op=ALU.mult),
                         r=[kk[cc], asg[ix]], w=[bd[ix]])
                    store(5 + d, cc, bd[ix], n, t0)
                pg = pp[ppi % 6]
                ppi += 1
                P.op("pe", lambda e, pg=pg, cc=cc, n=n: e.matmul(out=pg[:, 0:n], lhsT=gb0[:, cc * 128:(cc + 1) * 128], rhs=sg0[:, 0:n], start=True, stop=False),
                     r=[gb0, sg0], w=[pg])
                P.op("pe", lambda e, pg=pg, cc=cc, n=n: e.matmul(out=pg[:, 0:n], lhsT=gb1[:, cc * 128:(cc + 1) * 128], rhs=sg1[:, 0:n], start=False, stop=True),
                     r=[gb1, sg1], w=[pg])
                P.op("act", lambda e, pg=pg, cc=cc, n=n: e.activation(out=gt[cc][:, 0:n], in_=pg[:, 0:n], func=AF.Copy), r=[pg], w=[gt[cc]])
                store(9, cc, gt[cc], n, t0)
        P.barrier()
        P.emit()


def phase_rwscan(g, l):
    nc, I, S = g.nc, g.I, g.S
    with ExitStack() as es:
        def sb(name, shape, dt=F32):
            return es.enter_context(nc.sbuf_tensor("f%d_" % l + name, list(shape), dt))

        def psb(name, shape, dt=F32):
            return es.enter_context(nc.psum_tensor("f%d_" % l + name, list(shape), dt))
        ident = g.consts[:, C_ID, :]
        ybuf = [[sb("y%d%d" % (p, d), [128, NT]) for d in range(2)] for p in range(2)]
        E64 = sb("E64", [128, 64])
        U = [[None, None], [None, None]]
        for d in range(2):
            for p in range(2):
                if p == 1:
                    U[d][1] = U[d][0]
                    continue
                u = Ctx()
                n = "%d%d" % (d, p)
                u.f = [sb("ld%d_" % i + n, [128, 128]) for i in range(6)]
                u.Lc = sb("Lc" + n, [128, 128])
                u.LC = sb("LC" + n, [128, 128])
                u.t1 = sb("t1" + n, [128, 128])
                u.tA = sb("tA" + n, [128, 128])
                u.tW = sb("tW" + n, [128, 128])
                u.eP = sb("eP" + n, [128, 128])
                u.eN = sb("eN" + n, [128, 128])
                u.eA = sb("eA" + n, [128, 128])
                u.eW = sb("eW" + n, [128, 128])
                u.WL = sb("WL" + n, [128, 2])
                u.ar = sb("ar" + n, [128, 256])
                u.bt = sb("bt" + n, [128, 128])
                u.kt = sb("kt" + n, [128, 128])
                u.bW = sb("bW" + n, [128, 128])
                u.kW = sb("kW" + n, [128, 128])
                u.Dg = sb("Dg" + n, [128, 2, 64])
                u.TM = sb("TM" + n, [128, 4, 128])
                u.q = []
                for hh in range(2):
                    q = Ctx()
                    m = n + "%d" % hh
                    q.XTR = sb("XTR" + m, [128, 256])
                    q.KTR = sb("KTR" + m, [128, 256])
                    q.X = [sb("X%d_" % i + m, [128, 128]) for i in range(2)]
                    q.XT = [sb("XT%d_" % i + m, [128, 128]) for i in range(2)]
                    q.PT = [sb("PT%d_" % i + m, [128, 128]) for i in range(2)]
                    q.Gs = sb("Gs" + m, [128, 64])
                    q.MAG = sb("MAG" + m, [128, 128])
                    q.Phi = sb("Phi" + m, [64, 2, 64])
                    q.Psi = sb("Psi" + m, [64, 2, 64])
                    q.RAT = sb("RAT" + m, [64, 128])
                    q.YCT = sb("YCT" + m, [64, 128])
                    u.q.append(q)
                U[d][p] = u
        ST = [[[sb("ST%d%d%d" % (h, d, i), [64, 64]) for i in range(2)] for d in range(2)] for h in range(4)]
        stpar = [[0, 0] for _ in range(4)]
        pq = [psb("pq%d" % i, [128, 512]) for i in range(4)]
        pTr = [psb("pTr%d" % i, [128, 4, 128]) for i in range(2)]
        pSq = [psb("pSq%d" % i, [64, 512]) for i in range(2)]
        P = Prog(nc)
        P.op("dve", lambda e: e.tensor_tensor(out=E64[:], in0=g.consts[:, C_ID, 0:64], in1=g.consts[:, C_ID, 64:128], op=ALU.add),
             r=[g.consts], w=[E64])
        for h in range(4):
            for d in range(2):
                P.op("pool", lambda e, h=h, d=d: e.memset(ST[h][d][0][:], 0.0), w=[ST[h][d][0]])
        order_f = list(range(NTILE))
        order_b = [1, 0] + list(range(NTILE - 1, 1, -1))
        fidx = [[0, 1, 3, 4, 5, 7], [0, 2, 3, 4, 6, 8]]
        slotc = [0, 0, 0, 0]

        def slot(qi):
            sl = slotc[qi] % 4
            slotc[qi] += 1
            return sl

        for step in range(NTILE if g.rwsteps is None else g.rwsteps):
            for p in range(2):
                units = [(0, order_f[step]), (1, order_b[step])]
                for (d, j) in units:
                    u = U[d][p]
                    for i6 in range(6):
                        P.dma("sync" if i6 % 2 == 0 else "pool", u.f[i6][:], S.rwf[fidx[d][i6], p * 128:(p + 1) * 128, j * 128:(j + 1) * 128],
                              w=[u.f[i6]])
                    fr, fkd, fv, fkk, fbd, flw = u.f
                    P.op("dve", lambda e, u=u, flw=flw: e.tensor_tensor_scan(out=u.Lc[:], data0=g.consts[:, C_RESET, :], data1=flw[:], initial=0.0,
                                                                           op0=ALU.mult, op1=ALU.add), r=[flw, g.consts], w=[u.Lc])
                    totv = u.Lc[:].rearrange("p (c l) -> p c l", c=2)[:, :, 63:64]
                    if d == 0:
                        LC = u.Lc
                    else:
                        LC = u.LC
                        P.op("pool", lambda e, u=u, flw=flw: e.tensor_tensor(out=u.t1[:], in0=flw[:], in1=u.Lc[:], op=ALU.subtract),
                             r=[flw, u.Lc], w=[u.t1])
                        P.op("pool", lambda e, u=u, totv=totv: e.tensor_tensor(out=u.LC[:].rearrange("p (c l) -> p c l", c=2),
                                                                            in0=u.t1[:].rearrange("p (c l) -> p c l", c=2),
                                                                            in1=totv.to_broadcast([128, 2, 64]), op=ALU.add),
                             r=[u.t1, u.Lc], w=[u.LC])
                    P.op("pool", lambda e, u=u, LC=LC, flw=flw: e.tensor_tensor(out=u.tA[:], in0=LC[:], in1=flw[:], op=ALU.subtract),
                         r=[LC, flw], w=[u.tA])
                    P.op("pool", lambda e, u=u, LC=LC, totv=totv: e.tensor_tensor(out=u.tW[:].rearrange("p (c l) -> p c l", c=2),
                                                                               in0=totv.to_broadcast([128, 2, 64]),
                                                                               in1=LC[:].rearrange("p (c l) -> p c l", c=2), op=ALU.subtract),
                         r=[LC, u.Lc], w=[u.tW])
                    P.op("act", lambda e, u=u, LC=LC: e.activation(out=u.eP[:], in_=LC[:], func=AF.Exp), r=[LC], w=[u.eP])
                    P.op("act", lambda e, u=u, LC=LC: e.activation(out=u.eN[:], in_=LC[:], func=AF.Exp, scale=-1.0), r=[LC], w=[u.eN])
                    P.op("act", lambda e, u=u: e.activation(out=u.eA[:], in_=u.tA[:], func=AF.Exp), r=[u.tA], w=[u.eA])
                    P.op("act", lambda e, u=u: e.activation(out=u.eW[:], in_=u.tW[:], func=AF.Exp), r=[u.tW], w=[u.eW])
                    P.op("act", lambda e, u=u, totv=totv: e.activation(out=u.WL[:].unsqueeze(2), in_=totv, func=AF.Exp), r=[u.Lc], w=[u.WL])
                    P.op("dve", lambda e, u=u, fkk=fkk: e.scalar_tensor_tensor(out=u.ar[:, 0:128], in0=fkk[:], scalar=-1.0, in1=u.eA[:],
                                                                             op0=ALU.mult, op1=ALU.mult), r=[fkk, u.eA], w=[(u.ar.name, 0)])
                    P.op("pool", lambda e, u=u, fr=fr: e.tensor_tensor(out=u.ar[:, 128:256], in0=fr[:], in1=u.eP[:], op=ALU.mult),
                         r=[fr, u.eP], w=[(u.ar.name, 1)])
                    P.op("dve", lambda e, u=u, fbd=fbd: e.tensor_tensor(out=u.bt[:], in0=fbd[:], in1=u.eN[:], op=ALU.mult), r=[fbd, u.eN], w=[u.bt])
                    P.op("pool", lambda e, u=u, fkd=fkd: e.tensor_tensor(out=u.kt[:], in0=fkd[:], in1=u.eN[:], op=ALU.mult), r=[fkd, u.eN], w=[u.kt])
                    P.op("dve", lambda e, u=u, fbd=fbd: e.tensor_tensor(out=u.bW[:], in0=fbd[:], in1=u.eW[:], op=ALU.mult), r=[fbd, u.eW], w=[u.bW])
                    P.op("pool", lambda e, u=u, fkd=fkd: e.tensor_tensor(out=u.kW[:], in0=fkd[:], in1=u.eW[:], op=ALU.mult), r=[fkd, u.eW], w=[u.kW])
                    for c in range(2):
                        P.op("pool", lambda e, u=u, c=c: e.tensor_scalar(out=u.Dg[:, c, :], in0=E64[:], scalar1=u.WL[:, c:c + 1], scalar2=None, op0=ALU.mult),
                             r=[E64, u.WL], w=[u.Dg])
                    srcs = [(u.ar, 0, (u.ar.name, 0)), (u.bW, None, u.bW.name), (u.kW, None, u.kW.name), (fv, None, fv.name)]
                    for k4, (src, off, key) in enumerate(srcs):
                        in_ap = src[:, 0:128]
                        P.op("pe", lambda e, d=d, k4=k4, in_ap=in_ap: e.transpose(out=pTr[d][:, k4, :], in_=in_ap, identity=ident),
                             r=[key, g.consts], w=[pTr[d]])
                    P.op("act", lambda e, u=u, d=d: e.activation(out=u.TM[:], in_=pTr[d][:], func=AF.Copy), r=[pTr[d]], w=[u.TM])
                probs = []
                for (d, j) in units:
                    for hh in range(2):
                        probs.append((d, j, hh, U[d][p], U[d][p].q[hh], d * 2 + hh))
                for (d, j, hh, u, q, qi) in probs:
                    pb = hh * 64
                    P.op("pe", lambda e, u=u, pb=pb, qi=qi: e.matmul(out=pq[qi][:, 0:256], lhsT=u.bt[pb:pb + 64, :], rhs=u.ar[pb:pb + 64, :], start=True, stop=True),
                         r=[u.bt, (u.ar.name, 0), (u.ar.name, 1)], w=[("pqb", qi), ("pqb", qi)], rows=pb)
                    P.op("pe", lambda e, u=u, pb=pb, qi=qi: e.matmul(out=pq[qi][:, 256:384], lhsT=u.ar[pb:pb + 64, 0:128], rhs=u.bt[pb:pb + 64, :], start=True, stop=True),
                         r=[u.bt, (u.ar.name, 0)], w=[("pqb", qi)], rows=pb)
                for (d, j, hh, u, q, qi) in probs:
                    m2 = (C_MS_IT if d == 0 else C_MS_IT_B)
                    mti = (C_MS_TI if d == 0 else C_MS_IT)
                    P.op("dve", lambda e, q=q, qi=qi, m2=m2: e.tensor_tensor(out=q.XTR[:], in0=pq[qi][:, 0:256],
                                                                          in1=g.consts[:, m2:m2 + 2, :].rearrange("p a b -> p (a b)"), op=ALU.mult),
                         r=[("pqb", qi), ("pqb", qi), g.consts], w=[q.XTR])
                    P.op("dve", lambda e, q=q, qi=qi, mti=mti: e.tensor_tensor(out=q.X[0][:], in0=pq[qi][:, 256:384], in1=g.consts[:, mti, :], op=ALU.mult),
                         r=[("pqb", qi), g.consts], w=[q.X[0]])
                    P.op("pool", lambda e, q=q: e.tensor_tensor(out=q.PT[0][:], in0=q.XTR[:, 0:128], in1=ident, op=ALU.add),
                         r=[q.XTR, g.consts], w=[q.PT[0]])
                for (d, j, hh, u, q, qi) in probs:
                    pb = hh * 64
                    P.op("pe", lambda e, u=u, pb=pb, qi=qi: e.matmul(out=pq[qi][:, 0:256], lhsT=u.kt[pb:pb + 64, :], rhs=u.ar[pb:pb + 64, :], start=True, stop=True),
                         r=[u.kt, (u.ar.name, 0), (u.ar.name, 1)], w=[("pqb", qi), ("pqb", qi)], rows=pb)
                for (d, j, hh, u, q, qi) in probs:
                    m2 = (C_MS_IT if d == 0 else C_MS_IT_B)
                    P.op("dve", lambda e, q=q, qi=qi, m2=m2: e.tensor_tensor(out=q.KTR[:], in0=pq[qi][:, 0:256],
                                                                          in1=g.consts[:, m2:m2 + 2, :].rearrange("p a b -> p (a b)"), op=ALU.mult),
                         r=[("pqb", qi), ("pqb", qi), g.consts], w=[q.KTR])
                curX = {qi: None for qi in range(4)}
                for lev in range(5):
                    last = (lev == 4)
                    for (d, j, hh, u, q, qi) in probs:
                        Xc = q.X[lev % 2]
                        XTc = q.XTR if lev == 0 else q.XT[lev % 2]
                        XTc_ap = XTc[:, 0:128]
                        P.op("pe", lambda e, qi=qi, Xc=Xc, XTc_ap=XTc_ap: e.matmul(out=pq[qi][:, 384:512], lhsT=XTc_ap, rhs=Xc[:], start=True, stop=True),
                             r=[Xc, XTc], w=[("pqb", qi)])
                        if not last:
                            P.op("pe", lambda e, qi=qi, Xc=Xc, XTc_ap=XTc_ap: e.matmul(out=pq[qi][:, 256:384], lhsT=Xc[:], rhs=XTc_ap, start=True, stop=True),
                                 r=[Xc, XTc], w=[("pqb", qi)])
                    for (d, j, hh, u, q, qi) in probs:
                        Xn = q.X[(lev + 1) % 2]
                        XTn = q.XT[(lev + 1) % 2]
                        P.op("act", lambda e, qi=qi, Xn=Xn: e.activation(out=Xn[:], in_=pq[qi][:, 384:512], func=AF.Copy), r=[("pqb", qi)], w=[Xn])
                        if not last:
                            P.op("act", lambda e, qi=qi, XTn=XTn: e.activation(out=XTn[:], in_=pq[qi][:, 256:384], func=AF.Copy), r=[("pqb", qi)], w=[XTn])
                    for (d, j, hh, u, q, qi) in probs:
                        Xn = q.X[(lev + 1) % 2]
                        PTc = q.PT[lev % 2]
                        P.op("pe", lambda e, qi=qi, Xn=Xn, PTc=PTc: e.matmul(out=pq[qi][:, 0:128], lhsT=Xn[:], rhs=PTc[:], start=True, stop=True),
                             r=[Xn, PTc], w=[("pqb", qi)])
                    for (d, j, hh, u, q, qi) in probs:
                        PTc = q.PT[lev % 2]
                        PTn = q.PT[(lev + 1) % 2]
                        P.op("dve", lambda e, qi=qi, PTc=PTc, PTn=PTn: e.tensor_tensor(out=PTn[:], in0=pq[qi][:, 0:128], in1=PTc[:], op=ALU.add),
                             r=[("pqb", qi), PTc], w=[PTn])
                for (d, j, hh, u, q, qi) in probs:
                    cb = hh * 64
                    P.op("pe", lambda e, qi=qi, q=q, u=u, cb=cb: e.matmul(out=pq[qi][:, 128:192], lhsT=q.KTR[:, 0:128], rhs=u.TM[:, 3, cb:cb + 64], start=True, stop=True),
                         r=[q.KTR, u.TM], w=[("pqb", qi)])
                for (d, j, hh, u, q, qi) in probs:
                    P.op("act", lambda e, qi=qi, q=q: e.activation(out=q.Gs[:], in_=pq[qi][:, 128:192], func=AF.Copy), r=[("pqb", qi)], w=[q.Gs])
                for (d, j, hh, u, q, qi) in probs:
                    cb = hh * 64
                    PTf = q.PT[1]
                    P.op("pe", lambda e, qi=qi, PTf=PTf, u=u, cb=cb: e.matmul(out=pq[qi][:, 256:320], lhsT=PTf[:], rhs=u.TM[:, 0, cb:cb + 64], start=True, stop=True),
                         r=[PTf, u.TM], w=[("pqb", qi)])
                    P.op("pe", lambda e, qi=qi, PTf=PTf, q=q: e.matmul(out=pq[qi][:, 320:384], lhsT=PTf[:], rhs=q.Gs[:], start=True, stop=True),
                         r=[PTf, q.Gs], w=[("pqb", qi)])
                for (d, j, hh, u, q, qi) in probs:
                    P.op("act", lambda e, qi=qi, q=q: e.activation(out=q.MAG[:], in_=pq[qi][:, 256:384], func=AF.Copy), r=[("pqb", qi)], w=[q.MAG])
                for (d, j, hh, u, q, qi) in probs:
                    cb = hh * 64
                    pb = hh * 64
                    for c in range(2):
                        rb = c * 64
                        P.op("pe", lambda e, qi=qi, q=q, u=u, rb=rb, cb=cb, c=c: e.matmul(out=pq[qi][0:64, 384 + c * 64:448 + c * 64], lhsT=q.MAG[rb:rb + 64, 0:64],
                                                                                       rhs=u.TM[rb:rb + 64, 1, cb:cb + 64], start=True, stop=False),
                             r=[q.MAG, u.TM], w=[("pqb", qi)], rows=rb)
                        P.op("pe", lambda e, qi=qi, u=u, pb=pb, c=c: e.matmul(out=pq[qi][0:64, 384 + c * 64:448 + c * 64], lhsT=E64[pb:pb + 64, :],
                                                                            rhs=u.Dg[pb:pb + 64, c, :], start=False, stop=True),
                             r=[E64, u.Dg], w=[("pqb", qi)], rows=pb)
                        P.op("pe", lambda e, qi=qi, q=q, u=u, rb=rb, cb=cb, c=c: e.matmul(out=pq[qi][0:64, c * 64:c * 64 + 64], lhsT=u.TM[rb:rb + 64, 1, cb:cb + 64],
                                                                                       rhs=q.MAG[rb:rb + 64, 64:128], start=True, stop=False),
                             r=[q.MAG, u.TM], w=[("pqb", qi)], rows=rb)
                        P.op("pe", lambda e, qi=qi, u=u, rb=rb, cb=cb, c=c: e.matmul(out=pq[qi][0:64, c * 64:c * 64 + 64], lhsT=u.TM[rb:rb + 64, 2, cb:cb + 64],
                                                                                  rhs=u.TM[rb:rb + 64, 3, cb:cb + 64], start=False, stop=True),
                             r=[u.TM], w=[("pqb", qi)], rows=rb)
                    P.op("pe", lambda e, qi=qi, q=q: e.matmul(out=pq[qi][0:64, 128:256], lhsT=q.MAG[:, 0:64], rhs=q.XTR[:, 128:256], start=True, stop=False),
                         r=[q.MAG, q.XTR], w=[("pqb", qi)])
                    P.op("pe", lambda e, qi=qi, u=u, pb=pb: e.matmul(out=pq[qi][0:64, 128:256], lhsT=E64[pb:pb + 64, :], rhs=u.ar[pb:pb + 64, 128:256], start=False, stop=True),
                         r=[E64, (u.ar.name, 1)], w=[("pqb", qi)], rows=pb)
                    P.op("pe", lambda e, qi=qi, q=q: e.matmul(out=pq[qi][0:64, 256:384], lhsT=q.MAG[:, 64:128], rhs=q.XTR[:, 128:256], start=True, stop=False),
                         r=[q.MAG, q.XTR], w=[("pqb", qi)])
                    P.op("pe", lambda e, qi=qi, q=q, u=u, cb=cb: e.matmul(out=pq[qi][0:64, 256:384], lhsT=u.TM[:, 3, cb:cb + 64], rhs=q.KTR[:, 128:256], start=False, stop=True),
                         r=[u.TM, q.KTR], w=[("pqb", qi)])
                for (d, j, hh, u, q, qi) in probs:
                    P.op("act", lambda e, qi=qi, q=q: e.activation(out=q.Phi[:].rearrange("p c k -> p (c k)"), in_=pq[qi][0:64, 384:512], func=AF.Copy),
                         r=[("pqb", qi)], w=[q.Phi])
                    P.op("dve", lambda e, qi=qi, q=q: e.tensor_copy(out=q.Psi[:].rearrange("p c k -> p (c k)"), in_=pq[qi][0:64, 0:128]),
                         r=[("pqb", qi)], w=[q.Psi])
                    P.op("act", lambda e, qi=qi, q=q: e.activation(out=q.RAT[:], in_=pq[qi][0:64, 128:256], func=AF.Copy), r=[("pqb", qi)], w=[q.RAT])
                    P.op("dve", lambda e, qi=qi, q=q: e.tensor_copy(out=q.YCT[:], in_=pq[qi][0:64, 256:384]), r=[("pqb", qi)], w=[q.YCT])
                for ci in range(2):
                    for (d, j, hh, u, q, qi) in probs:
                        c = ci if d == 0 else 1 - ci
                        h = p * 2 + hh
                        sp = stpar[h][d]
                        Sc = ST[h][d][sp]
                        Sn = ST[h][d][1 - sp]
                        stpar[h][d] = 1 - sp
                        psq = pSq[ci]
                        yc0 = qi * 128
                        P.op("pe", lambda e, psq=psq, yc0=yc0, Sc=Sc, q=q, c=c: e.matmul(out=psq[:, yc0:yc0 + 64], lhsT=Sc[:], rhs=q.RAT[:, c * 64:(c + 1) * 64], start=True, stop=False),
                             r=[Sc, q.RAT], w=[("sqb", ci)])
                        P.op("pe", lambda e, psq=psq, yc0=yc0, q=q, c=c: e.matmul(out=psq[:, yc0:yc0 + 64], lhsT=E64[0:64, :], rhs=q.YCT[:, c * 64:(c + 1) * 64], start=False, stop=True),
                             r=[E64, q.YCT], w=[("sqb", ci)])
                        P.op("pe", lambda e, psq=psq, yc0=yc0, Sc=Sc, q=q, c=c: e.matmul(out=psq[:, yc0 + 64:yc0 + 128], lhsT=q.Phi[:, c, :], rhs=Sc[:], start=True, stop=False),
                             r=[Sc, q.Phi], w=[("sqb", ci)])
                        P.op("pe", lambda e, psq=psq, yc0=yc0, q=q, c=c: e.matmul(out=psq[:, yc0 + 64:yc0 + 128], lhsT=E64[0:64, :], rhs=q.Psi[:, c, :], start=False, stop=True),
                             r=[E64, q.Psi], w=[("sqb", ci)])
                        tcol = j * 128 + c * 64
                        yb = ybuf[p][d]
                        P.op("act", lambda e, psq=psq, yc0=yc0, yb=yb, hh=hh, tcol=tcol: e.activation(out=yb[hh * 64:(hh + 1) * 64, tcol:tcol + 64], in_=psq[:, yc0:yc0 + 64], func=AF.Copy),
                             r=[("sqb", ci)], w=[(yb.name, j)])
                        P.op("dve", lambda e, psq=psq, yc0=yc0, Sn=Sn: e.tensor_copy(out=Sn[:], in_=psq[:, yc0 + 64:yc0 + 128]), r=[("sqb", ci)], w=[Sn])
        SEG = 512
        prm = sb("prm", [128, 2, 3])
        ld = [sb("o_ld%d" % i, [128, SEG]) for i in range(5)]
        ysum = sb("ysum", [128, SEG])
        yc_ = sb("yc_", [128, SEG])
        sq = sb("osq", [128, SEG])
        rstd = sb("rstd", [128, SEG])
        prod = sb("prod", [128, SEG])
        ob = [sb("oob%d" % i, [128, SEG], BF16) for i in range(2)]
        for p in range(2):
            for k3, src in enumerate((I.r_k, I.ln_w, I.ln_b)):
                P.dma("sync", prm[:, p, k3:k3 + 1], src[g.wi(l), p * 128:(p + 1) * 128, :], w=[prm], sem="prm")
        segs = ([(0, CTX)] if l == 0 else []) + [(CTX + i * SEG, SEG) for i in range(8)]
        oi = 0
        for (t0, n) in segs:
            for p in range(2):
                for k5, idx in enumerate((0, 1, 2, 3, 9)):
                    P.dma("sync" if k5 % 2 == 0 else "pool", ld[k5][:, 0:n], S.rwf[idx, p * 128:(p + 1) * 128, t0:t0 + n], w=[ld[k5]])
                ykeys = [(ybuf[p][dd].name, jj) for dd in range(2) for jj in range(t0 // 128, (t0 + n) // 128)]
                P.op("pool", lambda e, p=p, t0=t0, n=n: e.tensor_tensor(out=ysum[:, 0:n], in0=ybuf[p][0][:, t0:t0 + n], in1=ybuf[p][1][:, t0:t0 + n], op=ALU.add),
                     r=ykeys, w=[ysum])
                pm = pq[0]
                P.op("pe", lambda e, pm=pm, n=n: e.matmul(out=pm[:, 0:n], lhsT=g.consts[:, C_BONES, :], rhs=ysum[:, 0:n], start=True, stop=True),
                     r=[ysum, g.consts], w=[("pqb", 0), ("pqb", 0), ("pqb", 0), ("pqb", 0)])
                P.op("dve", lambda e, pm=pm, n=n: e.scalar_tensor_tensor(out=yc_[:, 0:n], in0=pm[:, 0:n], scalar=-1.0 / 64, in1=ysum[:, 0:n], op0=ALU.mult, op1=ALU.add),
                     r=[("pqb", 0), ("pqb", 0), ("pqb", 0), ("pqb", 0), ysum], w=[yc_])
                P.op("pool", lambda e, n=n: e.tensor_tensor(out=sq[:, 0:n], in0=yc_[:, 0:n], in1=yc_[:, 0:n], op=ALU.mult), r=[yc_], w=[sq])
                pv = pq[1]
                P.op("pe", lambda e, pv=pv, n=n: e.matmul(out=pv[:, 0:n], lhsT=g.consts[:, C_BONES, :], rhs=sq[:, 0:n], start=True, stop=True),
                     r=[sq, g.consts], w=[("pqb", 1), ("pqb", 1), ("pqb", 1), ("pqb", 1)])
                P.op("dve", lambda e, pv=pv, n=n: e.tensor_scalar(out=rstd[:, 0:n], in0=pv[:, 0:n], scalar1=1.0 / 64, scalar2=64e-5, op0=ALU.mult, op1=ALU.add),
                     r=[("pqb", 1), ("pqb", 1), ("pqb", 1), ("pqb", 1)], w=[rstd])
                P.op("act", lambda e, n=n: e.activation(out=rstd[:, 0:n], in_=rstd[:, 0:n], func=AF.Sqrt), r=[rstd], w=[rstd])
                P.op("dve", lambda e, n=n: e.reciprocal(out=rstd[:, 0:n], in_=rstd[:, 0:n]), r=[rstd], w=[rstd])
                P.op("dve", lambda e, n=n: e.tensor_tensor(out=yc_[:, 0:n], in0=yc_[:, 0:n], in1=rstd[:, 0:n], op=ALU.mult), r=[yc_, rstd], w=[yc_])
                P.op("dve", lambda e, n=n, p=p: e.tensor_scalar(out=yc_[:, 0:n], in0=yc_[:, 0:n], scalar1=prm[:, p, 1:2], scalar2=prm[:, p, 2:3], op0=ALU.mult, op1=ALU.add),
                     r=[yc_, prm], w=[yc_])
                P.op("pool", lambda e, n=n: e.tensor_tensor(out=prod[:, 0:n], in0=ld[1][:, 0:n], in1=ld[2][:, 0:n], op=ALU.add), r=[ld[1], ld[2]], w=[prod])
                P.op("pool", lambda e, n=n: e.tensor_tensor(out=prod[:, 0:n], in0=prod[:, 0:n], in1=ld[0][:, 0:n], op=ALU.mult), r=[prod, ld[0]], w=[prod])
                P.op("pool", lambda e, n=n, p=p: e.tensor_scalar(out=prod[:, 0:n], in0=prod[:, 0:n], scalar1=prm[:, p, 0:1], scalar2=0.5, op0=ALU.mult, op1=ALU.mult),
                     r=[prod, prm], w=[prod])
                pbn = pq[2]
                P.op("pe", lambda e, pbn=pbn, n=n: e.matmul(out=pbn[:, 0:n], lhsT=g.consts[:, C_BONES, :], rhs=prod[:, 0:n], start=True, stop=True),
                     r=[prod, g.consts], w=[("pqb", 2), ("pqb", 2), ("pqb", 2), ("pqb", 2)])
                P.op("dve", lambda e, pbn=pbn, n=n: e.tensor_tensor(out=sq[:, 0:n], in0=pbn[:, 0:n], in1=ld[3][:, 0:n], op=ALU.mult),
                     r=[("pqb", 2), ("pqb", 2), ("pqb", 2), ("pqb", 2), ld[3]], w=[sq])
                P.op("dve", lambda e, n=n: e.tensor_tensor(out=yc_[:, 0:n], in0=yc_[:, 0:n], in1=sq[:, 0:n], op=ALU.add), r=[yc_, sq], w=[yc_])
                o = ob[oi % 2]
                oi += 1
                P.op("dve", lambda e, n=n, o=o: e.tensor_tensor(out=o[:, 0:n], in0=yc_[:, 0:n], in1=ld[4][:, 0:n], op=ALU.mult), r=[yc_, ld[4]], w=[o])
                P.dma("sync", S.mixT[768 + p * 128:768 + (p + 1) * 128, t0:t0 + n], o[:, 0:n], r=[o], sem=("oob", oi % 2))
        P.barrier()
        P.emit()


def phase_wout(g, l):
    nc, I, S = g.nc, g.I, g.S
    with ExitStack() as es:
        def sb(name, shape, dt=F32):
            return es.enter_context(nc.sbuf_tensor("w%d_" % l + name, list(shape), dt))

        def psb(name, shape, dt=F32):
            return es.enter_context(nc.psum_tensor("w%d_" % l + name, list(shape), dt))
        wo = sb("wo", [128, 8, D], BF16)
        rw = sb("rw", [128, 8, NE])
        bc = [[sb("bc%d%d" % (j, k), [128, D]) for k in range(3)] for j in range(2)]
        mt = [sb("mt%d" % i, [128, 8, 128], BF16) for i in range(2)]
        xt = [sb("xt%d" % i, [128, D]) for i in range(2)]
        x1 = [sb("x1%d" % i, [128, D]) for i in range(2)]
        junk = sb("junk", [128, D])
        ss = [sb("ss%d" % i, [128, 1]) for i in range(2)]
        h2f = [sb("h2f%d" % i, [128, D]) for i in range(2)]
        h2b = [sb("h2b%d" % i, [128, D], BF16) for i in range(2)]
        h2T = sb("h2T", [128, 8, 128])
        lg = sb("lg", [128, NE])
        mx = sb("mx", [128, 1])
        sm = sb("sm", [128, 1])
        aff = sb("aff", [128, NE])
        pO = [psb("pO%d" % i, [128, 512]) for i in range(2)]
        pT = [psb("pT%d" % i, [128, 4, 128]) for i in range(2)]
        pL_ = psb("pL", [128, 512])
        pL = pL_[:, 0:NE]
        pA_ = psb("pA", [NE, 512])
        pA = pA_[:, 0:128]
        P = Prog(nc)
        P.dma("pool", wo[:], I.w_out[g.wi(l)].rearrange("(kc p) n -> p kc n", p=128), w=[wo])
        P.dma("sync", rw[:], I.router[g.wi(l)].rearrange("(kc p) n -> p kc n", p=128), w=[rw])
        for j in range(2):
            for k, mi in enumerate((2, 3, 4)):
                P.dma("sync", bc[j][k][:], S.modv[g.wi(l), j, mi:mi + 1, :].to_broadcast([128, D]), w=[bc[j][k]])
        tiles = list(range(NTILE)) if l == 0 else list(range(2, NTILE))
        for i in tiles:
            b = i % 2
            j = 1 if i < 2 else 0
            if i < 2:
                src = (I.ctx if l == g.first else S.xcres)[i * 128:(i + 1) * 128, :]
                dst = S.xcres[i * 128:(i + 1) * 128, :]
                h2dst = S.h2c[i * 128:(i + 1) * 128, :]
            else:
                src = (I.x if l == g.first else S.xres)[(i - 2) * 128:(i - 1) * 128, :]
                dst = (g.out if l == g.last else S.xres)[(i - 2) * 128:(i - 1) * 128, :]
                h2dst = S.h2l[(i - 2) * 128:(i - 1) * 128, :]
            P.dma("sync", mt[b][:], S.mixT[:, i * 128:(i + 1) * 128].rearrange("(kc p) t -> p kc t", p=128), w=[mt[b]])
            P.dma("sync", xt[b][:], src, w=[xt[b]])
            for half in range(2):
                for kc in range(8):
                    P.op("pe", lambda e, half=half, kc=kc, b=b: e.matmul(out=pO[half][:], lhsT=mt[b][:, kc, :], rhs=wo[:, kc, half * 512:(half + 1) * 512],
                                                                       start=(kc == 0), stop=(kc == 7)), r=[mt[b], wo], w=[pO[half]])
                P.op("dve", lambda e, half=half, b=b, j=j: e.tensor_tensor(out=x1[b][:, half * 512:(half + 1) * 512], in0=pO[half][:],
                                                                          in1=bc[j][0][:, half * 512:(half + 1) * 512], op=ALU.mult),
                     r=[pO[half], bc[j][0]], w=[(x1[b].name, half)])
            P.op("pool", lambda e, b=b: e.tensor_tensor(out=x1[b][:], in0=x1[b][:], in1=xt[b][:], op=ALU.add),
                 r=[xt[b]], w=[(x1[b].name, 0), (x1[b].name, 1)])
            P.dma("pool", dst, x1[b][:], r=[(x1[b].name, 0), (x1[b].name, 1)], sem=("x1st", b))
            P.op("act", lambda e, b=b: e.activation(out=junk[:], in_=x1[b][:], func=AF.Square, accum_out=ss[b][:]), r=[(x1[b].name, 0), (x1[b].name, 1)], w=[junk, ss[b]])
            P.op("dve", lambda e, b=b: e.tensor_scalar(out=ss[b][:], in0=ss[b][:], scalar1=1.0 / D, scalar2=1e-6, op0=ALU.mult, op1=ALU.add),
                 r=[ss[b]], w=[ss[b]])
            P.op("act", lambda e, b=b: e.activation(out=ss[b][:], in_=ss[b][:], func=AF.Sqrt), r=[ss[b]], w=[ss[b]])
            P.op("dve", lambda e, b=b: e.reciprocal(out=ss[b][:], in_=ss[b][:]), r=[ss[b]], w=[ss[b]])
            P.op("dve", lambda e, b=b, j=j: e.scalar_tensor_tensor(out=h2f[b][:], in0=x1[b][:], scalar=ss[b][:, 0:1], in1=bc[j][1][:], op0=ALU.mult, op1=ALU.mult),
                 r=[(x1[b].name, 0), (x1[b].name, 1), ss[b], bc[j][1]], w=[h2f[b]])
            P.op("pool", lambda e, b=b, j=j: e.tensor_tensor(out=h2f[b][:], in0=h2f[b][:], in1=bc[j][2][:], op=ALU.add), r=[h2f[b], bc[j][2]], w=[h2f[b]])
            P.op("act", lambda e, b=b: e.activation(out=h2b[b][:], in_=h2f[b][:], func=AF.Copy), r=[h2f[b]], w=[h2b[b]])
            P.dma("sync", h2dst, h2b[b][:], r=[h2b[b]], sem=("h2st", b))
            for half in range(2):
                for k4 in range(4):
                    kc = half * 4 + k4
                    P.op("pe", lambda e, half=half, k4=k4, kc=kc, b=b: e.transpose(out=pT[half][:, k4, :], in_=h2f[b][:, kc * 128:(kc + 1) * 128],
                                                                                identity=g.consts[:, C_ID, :]), r=[h2f[b], g.consts], w=[pT[half]])
                if half == 0:
                    P.op("act", lambda e, half=half: e.activation(out=h2T[:, 0:4, :], in_=pT[0][:], func=AF.Copy), r=[pT[0]], w=[("h2T", 0)])
                else:
                    P.op("dve", lambda e, half=half: e.tensor_copy(out=h2T[:, 4:8, :], in_=pT[1][:]), r=[pT[1]], w=[("h2T", 1)])
            for kc in range(8):
                P.op("pe", lambda e, kc=kc: e.matmul(out=pL, lhsT=h2T[:, kc, :], rhs=rw[:, kc, :], start=(kc == 0), stop=(kc == 7)),
                     r=[("h2T", 0), ("h2T", 1), rw], w=["pL"])
            P.op("dve", lambda e: e.tensor_copy(out=lg[:], in_=pL), r=["pL"], w=[lg])
            P.op("dve", lambda e: e.tensor_reduce(out=mx[:], in_=lg[:], axis=AX.X, op=ALU.max), r=[lg], w=[mx])
            P.op("dve", lambda e: e.tensor_scalar(out=mx[:], in0=mx[:], scalar1=-1.0, scalar2=None, op0=ALU.mult), r=[mx], w=[mx])
            P.op("act", lambda e: e.activation(out=aff[:], in_=lg[:], func=AF.Exp, bias=mx[:, 0:1], accum_out=sm[:]), r=[lg, mx], w=[aff, sm])
            P.op("dve", lambda e: e.reciprocal(out=sm[:], in_=sm[:]), r=[sm], w=[sm])
            P.op("dve", lambda e: e.tensor_scalar(out=aff[:], in0=aff[:], scalar1=sm[:, 0:1], scalar2=None, op0=ALU.mult), r=[aff, sm], w=[aff])
            P.op("pe", lambda e: e.transpose(out=pA, in_=aff[:], identity=g.consts[:, C_ID, :]), r=[aff, g.consts], w=["pA"])
            P.op("act", lambda e, i=i: e.activation(out=g.affT[:, i * 128:(i + 1) * 128], in_=pA, func=AF.Copy), r=["pA"], w=[("affT", i)])
        P.barrier()
        P.emit()


def phase_moe(g, l):
    nc, I, S = g.nc, g.I, g.S
    with ExitStack() as es:
        def sb(name, shape, dt=F32):
            return es.enter_context(nc.sbuf_tensor("m%d_" % l + name, list(shape), dt))

        def psb(name, shape, dt=F32):
            return es.enter_context(nc.psum_tensor("m%d_" % l + name, list(shape), dt))
        work = sb("work", [NE, SEQ])
        vals = sb("vals", [NE, CAP_L])
        idxu = sb("idxu", [NE, CAP_L], U32)
        idxf = sb("idxf", [NE, CAP_L])
        idxT = sb("idxT", [128, 4, NE], I32)
        gT = sb("gT", [128, 4, NE])
        gt2 = [sb("gt2_%d" % j, [128, D]) for j in range(2)]
        wgt = [sb("wg%d" % i, [128, 8, D], BF16) for i in range(2)]
        wut = [sb("wu%d" % i, [128, 8, D], BF16) for i in range(2)]
        wdt = [sb("wd%d" % i, [128, 8, D], BF16) for i in range(2)]
        xs = [sb("xs%d" % i, [128, D], BF16) for i in range(2)]
        xsT = sb("xsT", [128, 8, 512], BF16)
        hidT = sb("hidT", [128, 8, 512], BF16)
        sg = [sb("sg%d" % i, [128, 512]) for i in range(2)]
        y = [sb("y%d" % i, [128, D]) for i in range(2)]
        pX = [psb("pX%d" % i, [128, 8, 128], BF16) for i in range(2)]
        pGs = [psb("pG%d" % i, [128, 512]) for i in range(2)]
        pUs = [psb("pU%d" % i, [128, 512]) for i in range(2)]
        pY = [psb("pY%d" % i, [128, 512]) for i in range(2)]
        pTi_t = pY[1]
        pTi = pY[1][:, :].rearrange("p (a b) -> p a b", a=32)
        P = Prog(nc)
        for j in range(2):
            P.dma("sync", gt2[j][:], S.modv[g.wi(l), j, 5:6, :].to_broadcast([128, D]), w=[gt2[j]])
        sets = [(0, CTX, SEQ, CAP_L, S.h2l, (g.out if l == g.last else S.xres))]
        if l == 0:
            sets.append((1, 0, CTX, CAP_C, S.h2c, S.xcres))
        wi = 0
        xi = 0
        yi = 0
        for (j, a0, N, cap, h2src, dest) in sets:
            nch = (cap + 127) // 128
            npc = min(cap, 128)
            akeys = [("affT", i) for i in range(a0 // 128, (a0 + N) // 128)]
            P.op("pool", lambda e, a0=a0, N=N: e.tensor_copy(out=work[:, 0:N], in_=g.affT[:, a0:a0 + N]), r=akeys, w=[work])
            for r8 in range(cap // 8):
                P.op("dve", lambda e, r8=r8, N=N: e.max(out=vals[:, r8 * 8:(r8 + 1) * 8], in_=work[:, 0:N]), r=[work], w=[vals])
                P.op("dve", lambda e, r8=r8, N=N: e.max_index(out=idxu[:, r8 * 8:(r8 + 1) * 8], in_max=vals[:, r8 * 8:(r8 + 1) * 8], in_values=work[:, 0:N]),
                     r=[work, vals], w=[idxu])
                P.op("dve", lambda e, r8=r8, N=N: e.match_replace(out=work[:, 0:N], in_to_replace=vals[:, r8 * 8:(r8 + 1) * 8], in_values=work[:, 0:N], imm_value=-1.0),
                     r=[work, vals], w=[work])
            P.op("dve", lambda e, cap=cap: e.tensor_copy(out=idxf[:, 0:cap], in_=idxu[:, 0:cap]), r=[idxu], w=[idxf])
            for ch in range(nch):
                P.op("pe", lambda e, ch=ch, npc=npc: e.transpose(out=pTi[0:npc, 0, :], in_=idxf[:, ch * 128:ch * 128 + npc], identity=g.consts[0:NE, C_ID, 0:NE]),
                     r=[idxf, g.consts], w=[pTi_t])
                P.op("pe", lambda e, ch=ch, npc=npc: e.transpose(out=pTi[0:npc, 1, :], in_=vals[:, ch * 128:ch * 128 + npc], identity=g.consts[0:NE, C_ID, 0:NE]),
                     r=[vals, g.consts], w=[pTi_t])
                P.op("dve", lambda e, ch=ch, npc=npc: e.tensor_copy(out=idxT[0:npc, ch, :], in_=pTi[0:npc, 0, :]), r=[pTi_t], w=[idxT])
                P.op("dve", lambda e, ch=ch, npc=npc: e.tensor_copy(out=gT[0:npc, ch, :], in_=pTi[0:npc, 1, :]), r=[], w=[gT, pTi_t])
            ncol = nch * npc
            for ex in range(NE):
                wb_ = wi % 2
                wi += 1
                P.dma("pool", wgt[wb_][:], I.wg[g.wi(l), ex].rearrange("(kc p) n -> p kc n", p=128), w=[wgt[wb_]])
                P.dma("pool", wut[wb_][:], I.wu[g.wi(l), ex].rearrange("(kc p) n -> p kc n", p=128), w=[wut[wb_]])
                P.dma("pool", wdt[wb_][:], I.wd[g.wi(l), ex].rearrange("(kc p) n -> p kc n", p=128), w=[wdt[wb_]])
                for ch in range(nch):
                    xb = xs[xi % 2]
                    xi += 1
                    P.dma_fn("pool", lambda e, xb=xb, ch=ch, ex=ex, npc=npc, h2src=h2src: e.indirect_dma_start(
                        out=xb[0:npc, :], out_offset=None, in_=h2src[:, :],
                        in_offset=bass.IndirectOffsetOnAxis(ap=idxT[0:npc, ch, ex:ex + 1], axis=0)),
                        r=[idxT], w=[xb], sem=("xg", xb.name))
                    for half in range(2):
                        for k4 in range(4):
                            kc = half * 4 + k4
                            P.op("pe", lambda e, half=half, k4=k4, kc=kc, xb=xb, npc=npc: e.transpose(out=pX[half][:, k4, 0:npc], in_=xb[0:npc, kc * 128:(kc + 1) * 128],
                                                                                                 identity=g.identb[0:npc, 0:npc]), r=[xb, g.identb], w=[pX[half]])
                        if half == 0:
                            P.op("act", lambda e, ch=ch, npc=npc: e.activation(out=xsT[:, 0:4, ch * 128:ch * 128 + npc], in_=pX[0][:, 0:4, 0:npc], func=AF.Copy),
                                 r=[pX[0]], w=[("xsT", 0)])
                        else:
                            P.op("dve", lambda e, ch=ch, npc=npc: e.tensor_copy(out=xsT[:, 4:8, ch * 128:ch * 128 + npc], in_=pX[1][:, 0:4, 0:npc]),
                                 r=[pX[1]], w=[("xsT", 1)])
                for fc in range(8):
                    pG = pGs[fc % 2]
                    pU = pUs[fc % 2]
                    for kc in range(8):
                        P.op("pe", lambda e, fc=fc, kc=kc, wb_=wb_, ncol=ncol, pG=pG: e.matmul(out=pG[:, 0:ncol], lhsT=wgt[wb_][:, kc, fc * 128:(fc + 1) * 128], rhs=xsT[:, kc, 0:ncol],
                                                                                     start=(kc == 0), stop=(kc == 7)), r=[wgt[wb_], ("xsT", 0), ("xsT", 1)], w=[pG])
                    for kc in range(8):
                        P.op("pe", lambda e, fc=fc, kc=kc, wb_=wb_, ncol=ncol, pU=pU: e.matmul(out=pU[:, 0:ncol], lhsT=wut[wb_][:, kc, fc * 128:(fc + 1) * 128], rhs=xsT[:, kc, 0:ncol],
                                                                                     start=(kc == 0), stop=(kc == 7)), r=[wut[wb_], ("xsT", 0), ("xsT", 1)], w=[pU])
                    s_ = sg[fc % 2]
                    P.op("act", lambda e, s_=s_, ncol=ncol, pG=pG: e.activation(out=s_[:, 0:ncol], in_=pG[:, 0:ncol], func=AF.Silu), r=[pG], w=[s_])
                    P.op("dve", lambda e, s_=s_, fc=fc, ncol=ncol, pU=pU: e.tensor_tensor(out=hidT[:, fc, 0:ncol], in0=pU[:, 0:ncol], in1=s_[:, 0:ncol], op=ALU.mult),
                         r=[pU, s_], w=[("hidT", fc)])
                hk = [("hidT", fc) for fc in range(8)]
                for ch in range(nch):
                    yb = y[yi % 2]
                    yi += 1
                    for half in range(2):
                        for fc in range(8):
                            P.op("pe", lambda e, half=half, fc=fc, ch=ch, wb_=wb_, npc=npc: e.matmul(out=pY[half][0:npc, :], lhsT=hidT[:, fc, ch * 128:ch * 128 + npc],
                                                                                                 rhs=wdt[wb_][:, fc, half * 512:(half + 1) * 512], start=(fc == 0), stop=(fc == 7)),
                                 r=hk + [wdt[wb_]], w=[pY[half]])
                        P.op("dve", lambda e, half=half, yb=yb, ch=ch, ex=ex, npc=npc, j=j: e.scalar_tensor_tensor(
                            out=yb[0:npc, half * 512:(half + 1) * 512], in0=pY[half][0:npc, :], scalar=gT[0:npc, ch, ex:ex + 1],
                            in1=gt2[j][0:npc, half * 512:(half + 1) * 512], op0=ALU.mult, op1=ALU.mult), r=[pY[half], gT, gt2[j]], w=[(yb.name, half)])
                    P.dma_fn("pool", lambda e, yb=yb, ch=ch, ex=ex, npc=npc, dest=dest: e.indirect_dma_start(
                        out=dest[:, :], out_offset=bass.IndirectOffsetOnAxis(ap=idxT[0:npc, ch, ex:ex + 1], axis=0),
                        in_=yb[0:npc, :], in_offset=None, compute_op=ALU.add),
                        r=[(yb.name, 0), (yb.name, 1), idxT], w=[("dest", j)], sem=("ysc", j))
        P.barrier()
        P.emit()


def phase_zero_mix(g, l):
    nc, S = g.nc, g.S
    with ExitStack() as es:
        z = es.enter_context(nc.sbuf_tensor("z%d_z" % l, [128, NT], BF16))
        P = Prog(nc)
        P.op("pool", lambda e: e.memset(z[:], 0.0), w=[z])
        for r in range(2, 8):
            P.dma("sync", S.mixT[r * 128:(r + 1) * 128, :], z[:], r=[z], sem="zst")
        P.barrier()
        P.emit()


def prep_inputs(inputs):
    f = lambda a: np.ascontiguousarray(np.asarray(a, dtype=np.float32))
    x = f(inputs["x"])
    c = f(inputs["c"])
    ctx = f(inputs["ctx"])
    c_ctx = f(inputs["c_ctx"])
    shared = {
        "ada_w": f(inputs["ada_w"]),
        "ada_b": f(inputs["ada_b"]).reshape(2, 1, 6 * D),
        "norm1_g": f(inputs["norm1_g"]).reshape(2, 1, D),
        "norm2_g": f(inputs["norm2_g"]).reshape(2, 1, D),
        "w_in": f(inputs["w_in"]),
        "w_out": f(inputs["w_out"]),
        "conv_wT": f(np.transpose(f(inputs["conv_w"]), (0, 2, 1))),
        "q_norm_g": f(inputs["q_norm_g"]).reshape(2, 1, 64),
        "k_norm_g": f(inputs["k_norm_g"]).reshape(2, 1, 64),
        "rw_mu": f(inputs["rw_mu"]).reshape(2, 1184, 1),
        "rw_w0": f(inputs["rw_w0"]).reshape(2, 512, 1),
        "rw_w_b": f(inputs["rw_w_b"]).reshape(2, 128, 256),
        "rw_a0": f(inputs["rw_a0"]).reshape(2, 512, 1),
        "rw_a_b": f(inputs["rw_a_b"]).reshape(2, 128, 256),
        "rw_g_b": f(inputs["rw_g_b"]),
        "rw_k_k": f(inputs["rw_k_k"]).reshape(2, 256, 1),
        "rw_k_a": f(inputs["rw_k_a"]).reshape(2, 256, 1),
        "rw_r_k": f(inputs["rw_r_k"]).reshape(2, 256, 1),
        "rw_ln_w": f(inputs["rw_ln_w"]).reshape(2, 256, 1),
        "rw_ln_b": f(inputs["rw_ln_b"]).reshape(2, 256, 1),
        "router_w": f(inputs["router_w"]),
        "exp_w_gate": f(inputs["exp_w_gate"]),
        "exp_w_up": f(inputs["exp_w_up"]),
        "exp_w_down": f(inputs["exp_w_down"]),
        "consts": make_consts(),
    }
    t = np.arange(SEQ)
    row = (t // 64).astype(np.float32)
    col = (t % 64).astype(np.float32)
    inv = (10000.0 ** (-np.arange(0, 32, 2, dtype=np.float32) / 32)).astype(np.float32)
    ang = np.concatenate([row[:, None] * inv, col[:, None] * inv], axis=-1).astype(np.float32)
    shared["cs_tab"] = np.ascontiguousarray(np.concatenate([np.cos(ang), np.sin(ang)], axis=-1).astype(np.float32))
    maps = []
    for b in range(x.shape[0]):
        m = dict(shared)
        m["x"] = x[b]
        m["ctx"] = ctx[b]
        c2 = np.stack([c[b], c_ctx], axis=-1)
        m["c2T"] = np.ascontiguousarray(c2.reshape(8, 128, 2).transpose(1, 0, 2))
        maps.append(m)
    return maps


_NC_CACHE = {}

W_KEYS = ["ada_w", "ada_b", "norm1_g", "norm2_g", "w_in", "w_out", "conv_wT", "q_norm_g", "k_norm_g", "rw_mu", "rw_w0", "rw_w_b",
          "rw_a0", "rw_a_b", "rw_g_b", "rw_k_k", "rw_k_a", "rw_r_k", "rw_ln_w", "rw_ln_b", "router_w",
          "exp_w_gate", "exp_w_up", "exp_w_down"]


def kernel(**inputs):
    maps = prep_inputs(inputs)
    if "nc" not in _NC_CACHE:
        _NC_CACHE["nc"] = build(layers=[0, 1])
    nc = _NC_CACHE["nc"]
    res = run_bass_kernel_spmd(nc, maps, core_ids=list(range(8)))
    return np.stack([np.asarray(r["out"], dtype=np.float32) for r in res.results], axis=0)
```

```python
import math
from contextlib import ExitStack

import numpy as np
import concourse.bass as bass
import concourse.mybir as mybir
from concourse.bass_utils import run_bass_kernel_spmd

F32 = mybir.dt.float32
BF16 = mybir.dt.bfloat16
I32 = mybir.dt.int32
U32 = mybir.dt.uint32
AF = mybir.ActivationFunctionType
ALU = mybir.AluOpType
AX = mybir.AxisListType

ENGS = ("sync", "act", "dve", "pool", "pe")

D = 1024
SEQ = 4096
CTX = 256
NT = SEQ + CTX
NTILE = NT // 128
PROJ = 2720
NE = 16
CAP_L = 512
CAP_C = 32
LCH = 64


class Prog:
    SEMID = 0

    def __init__(self, nc):
        self.nc = nc
        self.streams = {e: [] for e in ENGS}
        self.ecount = {e: 0 for e in ENGS}
        self.seen = {e: {} for e in ENGS}
        self.bufs = {}
        self.dcount = {}
        self.sems = {}

    @staticmethod
    def _k(b):
        if isinstance(b, (str, tuple, int)):
            return b
        return b.name

    def _deps(self, eng, reads, writes):
        need = {}

        def add(ev):
            for k, v in ev.items():
                if need.get(k, 0) < v:
                    need[k] = v

        for b in reads:
            st = self.bufs.get(b)
            if st:
                add(st["w"])
        for b in writes:
            st = self.bufs.get(b)
            if st:
                add(st["w"])
                add(st["r"])
        waits = []
        seen = self.seen[eng]
        for k, v in need.items():
            if k[0] == "e" and k[1] == eng and eng == "pe":
                continue
            if seen.get(k, 0) >= v:
                continue
            seen[k] = v
            waits.append((k, v))
        return waits

    def _commit(self, reads, writes, ev):
        for b in reads:
            st = self.bufs.setdefault(b, {"w": {}, "r": {}})
            for k, v in ev.items():
                if st["r"].get(k, 0) < v:
                    st["r"][k] = v
        for b in writes:
            self.bufs[b] = {"w": dict(ev), "r": {}}

    EPOCH = 3000

    def op(self, eng, fn, r=(), w=(), rows=None):
        r = [self._k(b) for b in r]
        w = [self._k(b) for b in w]
        bk = [k for k in r if isinstance(k, tuple) and k[0] in ("pqb", "sqb")]
        if bk:
            r = [k for k in r if k not in bk]
            w = w + bk
        waits = self._deps(eng, r, w)
        self.ecount[eng] += 1
        ep = (self.ecount[eng] - 1) // self.EPOCH
        ek = ("e", eng, ep)
        ev = {ek: self.ecount[eng] - ep * self.EPOCH}
        if eng == "pe":
            if not hasattr(self, "pe_rows"):
                self.pe_rows = {}
            for bk_ in w:
                last = self.pe_rows.get(bk_)
                if last is not None and rows in (0, 64) and last[0] in (0, 64) and last[0] != rows:
                    for k_, v_ in last[1].items():
                        if self.seen[eng].get(k_, 0) < v_:
                            self.seen[eng][k_] = v_
                            waits.append((k_, v_))
                self.pe_rows[bk_] = (rows, ev)
        self.streams[eng].append((waits, fn, (ek, 1)))
        self._commit(r, w, ev)

    def dma(self, q, out, in_, r=(), w=(), sem=None, **kw):
        r = [self._k(b) for b in r]
        w = [self._k(b) for b in w]
        if sem is None:
            sem = (w[0] if w else r[0])
        waits = self._deps(q, r, w)
        k = ("d", sem)
        self.dcount[k] = self.dcount.get(k, 0) + 16
        ev = {k: self.dcount[k]}
        self.streams[q].append((waits, (lambda e: e.dma_start(out=out, in_=in_, **kw)), (k, 16)))
        self._commit(r, w, ev)

    def dma_fn(self, q, fn, r=(), w=(), sem=None):
        r = [self._k(b) for b in r]
        w = [self._k(b) for b in w]
        waits = self._deps(q, r, w)
        k = ("d", sem)
        self.dcount[k] = self.dcount.get(k, 0) + 16
        ev = {k: self.dcount[k]}
        self.streams[q].append((waits, fn, (k, 16)))
        self._commit(r, w, ev)

    def barrier(self):
        for eng in ENGS:
            waits = [(k, v) for k, v in self.dcount.items()]
            for e in ENGS:
                if e != eng and self.ecount[e] > 0:
                    ep = (self.ecount[e] - 1) // self.EPOCH
                    waits.append((("e", e, ep), self.ecount[e] - ep * self.EPOCH))
            self.streams[eng].append((waits, None, None))

    POOL = None

    def emit(self):
        nc = self.nc
        pool = Prog.POOL
        totals = {}
        keys = []
        for e in ENGS:
            for waits, fn, inc in self.streams[e]:
                for k, v in waits:
                    if k not in totals:
                        totals[k] = 0
                        keys.append(k)
                if inc:
                    if inc[0] not in totals:
                        totals[inc[0]] = 0
                        keys.append(inc[0])
                    totals[inc[0]] += inc[1]
        n = len(pool["h"])
        assert len(keys) <= n, len(keys)
        base = {}
        for i, k in enumerate(sorted(keys, key=str)):
            idx = (pool["next"] + i) % n
            self.sems[k] = pool["h"][idx]
            base[k] = pool["v"][idx]
            pool["v"][idx] += totals[k]
        pool["next"] = (pool["next"] + len(keys)) % n
        with ExitStack() as es:
            block = es.enter_context(nc.Block())
            handles = {"sync": block.sync, "act": block.scalar, "dve": block.vector,
                       "pool": block.gpsimd, "pe": block.tensor}
            for e in ENGS:
                stream = self.streams[e]

                def body(h, stream=stream):
                    for waits, fn, inc in stream:
                        for k, v in waits:
                            h.wait_ge(self.sems[k], base[k] + v)
                        if fn is not None:
                            ins = fn(h)
                            ins.then_inc(self.sems[inc[0]], inc[1])

                handles[e](body)


class Ctx:
    pass


def build(n_layers=2, dbg=None, upto=None, skip=(), small=False, rwsteps=None, zero_mix=False, layers=None):
    dbg = dbg or set()
    nc = bass.Bass("TRN2", target_bir_lowering=False)
    g = Ctx()
    g.nc = nc
    if layers is None:
        layers = list(range(n_layers))
    NL = len(layers)
    g.first = layers[0]
    g.last = layers[-1]
    g.wi = lambda l: l - layers[0]
    if layers[-1] == 0:
        dbg = set(dbg) | {"xcres"}

    def din(name, shape, dt=F32):
        return nc.dram_tensor(name, list(shape), dt, kind="ExternalInput").ap()

    def dscr(name, shape, dt=F32):
        kind = "ExternalOutput" if name in dbg else "Internal"
        return nc.dram_tensor(name, list(shape), dt, kind=kind).ap()

    I = Ctx()
    I.x = din("x", [SEQ, D])
    I.ctx = din("ctx", [CTX, D])
    I.c2T = din("c2T", [128, 8, 2])
    I.ada_w = din("ada_w", [NL, D, 6 * D])
    I.ada_b = din("ada_b", [NL, 1, 6 * D])
    I.n1g = din("norm1_g", [NL, 1, D])
    I.n2g = din("norm2_g", [NL, 1, D])
    I.w_in = din("w_in", [NL, D, PROJ])
    I.w_out = din("w_out", [NL, D, D])
    I.conv_wT = din("conv_wT", [NL, 256, 3])
    I.qg = din("q_norm_g", [NL, 1, 64])
    I.kg = din("k_norm_g", [NL, 1, 64])
    I.mu = din("rw_mu", [NL, 1184, 1])
    I.w0 = din("rw_w0", [NL, 512, 1])
    I.w_b = din("rw_w_b", [NL, 128, 256])
    I.a0 = din("rw_a0", [NL, 512, 1])
    I.a_b = din("rw_a_b", [NL, 128, 256])
    I.g_b = din("rw_g_b", [NL, 160, 256])
    I.k_k = din("rw_k_k", [NL, 256, 1])
    I.k_a = din("rw_k_a", [NL, 256, 1])
    I.r_k = din("rw_r_k", [NL, 256, 1])
    I.ln_w = din("rw_ln_w", [NL, 256, 1])
    I.ln_b = din("rw_ln_b", [NL, 256, 1])
    I.router = din("router_w", [NL, D, NE])
    esh = [NL, 1, 8, 8] if small else [NL, NE, D, D]
    I.wg = din("exp_w_gate", esh)
    I.wu = din("exp_w_up", esh)
    I.wd = din("exp_w_down", esh)
    g.rwsteps = rwsteps
    I.cs = din("cs_tab", [SEQ, 64])
    I.consts = din("consts", [128, 8 * 128])
    out = nc.dram_tensor("out", [SEQ, D], F32, kind="ExternalOutput").ap()

    S = Ctx()
    S.modv = dscr("modv", [NL, 2, 6, D])
    S.pfm = dscr("pfm", [1952, NT])
    S.qT = dscr("qT", [8, 64, NT], BF16)
    S.mixT = dscr("mixT", [D, NT], BF16)
    S.xres = dscr("xres", [SEQ, D])
    S.xcres = dscr("xcres", [CTX, D])
    S.h2l = dscr("h2l", [SEQ, D], BF16)
    S.h2c = dscr("h2c", [CTX, D], BF16)
    S.rwf = dscr("rwf", [10, 256, NT])
    g.I, g.S, g.out = I, S, out

    with ExitStack() as gs:
        def gsb(name, shape, dt=F32):
            return gs.enter_context(nc.sbuf_tensor("g_" + name, list(shape), dt))
        Prog.POOL = {"h": [gs.enter_context(nc.semaphore("gp%d" % i)) for i in range(72)], "v": [0] * 72, "next": 0}
        g.consts = gsb("consts", [128, 8, 128])
        g.identb = gsb("identb", [128, 128], BF16)
        g.kT = gsb("kT", [128, 2, NT], BF16)
        g.Vaug = gsb("Vaug", [128, NTILE, 2, 65], BF16)
        g.affT = gsb("affT", [NE, NT])
        phase_consts(g)
        phases = [phase_adaln, phase_proj, phase_attn, phase_rwfeat, phase_rwscan] + ([phase_zero_mix] if zero_mix else []) + [phase_wout, phase_moe]
        for l in layers:
            for ph in phases:
                if ph.__name__ in skip:
                    continue
                ph(g, l)
                if upto == (ph.__name__, l):
                    return nc
    return nc


C_ID, C_BONES, C_MS_IT, C_MI_IT, C_MS_TI, C_RESET, C_MS_IT_B, C_MI_IT_B = range(8)


def make_consts():
    c = np.zeros((8, 128, 128), np.float32)
    i = np.arange(128)[:, None]
    t = np.arange(128)[None, :]
    same = (i // 64) == (t // 64)
    c[C_ID] = np.eye(128)
    c[C_BONES] = same
    c[C_MS_IT] = same & (i < t)
    c[C_MI_IT] = same & (i <= t)
    c[C_MS_TI] = same & (t < i)
    c[C_RESET] = (t % 64 != 0) * np.ones((128, 1))
    c[C_MS_IT_B] = same & (i > t)
    c[C_MI_IT_B] = same & (i >= t)
    return np.ascontiguousarray(c.transpose(1, 0, 2).reshape(128, 8 * 128))


def phase_consts(g):
    nc = g.nc
    P = Prog(nc)
    P.dma("sync", g.consts[:], g.I.consts.rearrange("p (a b) -> p a b", a=8), w=[g.consts])
    P.op("dve", lambda e: e.tensor_copy(out=g.identb[:], in_=g.consts[:, C_ID, :]), r=[g.consts], w=[g.identb])
    P.op("pool", lambda e: e.memset(g.Vaug[:, :, :, 64:65], 1.0), w=[g.Vaug])
    P.barrier()
    P.emit()


def phase_adaln(g, l):
    nc, I, S = g.nc, g.I, g.S
    with ExitStack() as es:
        def sb(name, shape, dt=F32):
            return es.enter_context(nc.sbuf_tensor("a%d_" % l + name, list(shape), dt))
        c2 = sb("c2", [128, 8, 2])
        sc = sb("sc", [128, 8, 2])
        wt = [sb("wt%d" % i, [128, 8, 512]) for i in range(2)]
        bias = sb("bias", [2, 6 * D])
        mod = sb("mod", [2, 6 * D])
        gg = sb("gg", [2, 2, D])
        mv = sb("mv", [2, 6, D])
        ps = [es.enter_context(nc.psum_tensor("a%d_ps%d" % (l, i), [2, 512], F32)) for i in range(2)]
        P = Prog(nc)
        P.dma("sync", c2[:], I.c2T[:, :, :], w=[c2])
        P.dma("sync", bias[:], I.ada_b[g.wi(l), 0:1, :].to_broadcast([2, 6 * D]), w=[bias])
        P.dma("sync", gg[:, 0, :], I.n1g[g.wi(l), 0:1, :].to_broadcast([2, D]), w=[gg], sem="gg")
        P.dma("sync", gg[:, 1, :], I.n2g[g.wi(l), 0:1, :].to_broadcast([2, D]), w=[gg], sem="gg")
        P.op("act", lambda e: e.activation(out=sc[:], in_=c2[:], func=AF.Silu), r=[c2], w=[sc])
        wv = I.ada_w[g.wi(l)].rearrange("(kc p) n -> p kc n", p=128)
        for cc in range(12):
            b = cc % 2
            P.dma("sync" if b == 0 else "pool", wt[b][:], wv[:, :, cc * 512:(cc + 1) * 512], w=[wt[b]])
            for kc in range(8):
                P.op("pe", lambda e, kc=kc, b=b: e.matmul(out=ps[b][:], lhsT=sc[:, kc, :], rhs=wt[b][:, kc, :],
                                                           start=(kc == 0), stop=(kc == 7)),
                     r=[sc, wt[b]], w=[ps[b]])
            P.op("dve", lambda e, cc=cc, b=b: e.tensor_tensor(out=mod[:, cc * 512:(cc + 1) * 512], in0=ps[b][:],
                                                               in1=bias[:, cc * 512:(cc + 1) * 512], op=ALU.add),
                 r=[ps[b], bias], w=[mod])
        for j, (sci, shi, gti) in enumerate(((1, 0, 2), (4, 3, 5))):
            P.op("dve", lambda e, j=j, sci=sci: e.scalar_tensor_tensor(
                out=mv[:, 3 * j, :], in0=mod[:, sci * D:(sci + 1) * D], scalar=1.0, in1=gg[:, j, :],
                op0=ALU.add, op1=ALU.mult), r=[mod, gg], w=[mv])
            P.op("dve", lambda e, j=j, shi=shi: e.tensor_copy(out=mv[:, 3 * j + 1, :], in_=mod[:, shi * D:(shi + 1) * D]),
                 r=[mod], w=[mv])
            P.op("dve", lambda e, j=j, gti=gti: e.tensor_copy(out=mv[:, 3 * j + 2, :], in_=mod[:, gti * D:(gti + 1) * D]),
                 r=[mod], w=[mv])
        P.dma("sync", S.modv[g.wi(l)], mv[:], r=[mv], sem="mvst")
        P.barrier()
        P.emit()


def phase_proj(g, l):
    nc, I, S = g.nc, g.I, g.S
    with ExitStack() as es:
        def sb(name, shape, dt=F32):
            return es.enter_context(nc.sbuf_tensor("b%d_" % l + name, list(shape), dt))

        def psb(name, shape, dt=F32):
            return es.enter_context(nc.psum_tensor("b%d_" % l + name, list(shape), dt))
        wb = sb("wb", [128, 8, PROJ], BF16)
        m1 = [sb("m1_%d" % i, [128, D]) for i in range(2)]
        sh1 = [sb("sh1_%d" % i, [128, D]) for i in range(2)]
        qkg = sb("qkg", [128, 2, 64])
        cs = sb("cs", [128, 32, 64])
        xt = [sb("xt%d" % i, [128, D]) for i in range(2)]
        junk = sb("junk", [128, D])
        ss = [sb("ss%d" % i, [128, 1]) for i in range(2)]
        hb = [sb("hb%d" % i, [128, D], BF16) for i in range(2)]
        hT = [sb("hT%d" % i, [128, 8, 512], BF16) for i in range(2)]
        fm = [sb("fm%d" % i, [128, 512]) for i in range(3)]
        qkv = [sb("qkv%d" % i, [128, 768]) for i in range(2)]
        sq = sb("sq", [128, 640])
        ssq = sb("ssq", [128, 10])
        qn = sb("qn", [128, 10, 64])
        qr = sb("qr", [128, 10, 64])
        qrb = [sb("qrb%d" % i, [128, 10, 64], BF16) for i in range(2)]
        tmp = sb("tmp", [128, 10, 32])
        qTs = [sb("qTs%d" % i, [64, 8, 128], BF16) for i in range(2)]
        pT = [psb("pT%d" % i, [128, 8, 128], BF16) for i in range(2)]
        pF = [psb("pF%d" % i, [128, 512]) for i in range(2)]
        pA = psb("pA", [128, 512])
        pB = psb("pB", [128, 512])
        pQ = psb("pQ", [64, 8, 128], BF16)
        pQk = psb("pQk", [64, 8, 128], BF16)
        P = Prog(nc)
        wv = I.w_in[g.wi(l)].rearrange("(kc p) n -> p kc n", p=128)
        for (c0, c1) in ((0, 1024), (1024, 2048), (2048, PROJ)):
            P.dma("pool", wb[:, :, c0:c1], wv[:, :, c0:c1], w=[("wb", c0)], sem=("wb", c0))
        wbk = [("wb", 0), ("wb", 1024), ("wb", 2048)]
        for j in range(2):
            P.dma("sync", m1[j][:], S.modv[g.wi(l), j, 0:1, :].to_broadcast([128, D]), w=[m1[j]])
            P.dma("sync", sh1[j][:], S.modv[g.wi(l), j, 1:2, :].to_broadcast([128, D]), w=[sh1[j]])
        P.dma("sync", qkg[:, 0, :], I.qg[g.wi(l), 0:1, :].to_broadcast([128, 64]), w=[qkg], sem="qkg")
        P.dma("sync", qkg[:, 1, :], I.kg[g.wi(l), 0:1, :].to_broadcast([128, 64]), w=[qkg], sem="qkg")
        P.dma("sync", cs[:], I.cs.rearrange("(i p) c -> p i c", p=128), w=[cs])
        fchunks = [(c, 128, c) for c in range(0, 768, 128)]
        for j in range(10):
            c = 1536 + j * 128
            wdt = min(128, PROJ - c)
            fchunks.append((c, wdt, 768 + j * 128))
        sts = [(0, 2)] + [(2 + 4 * s, 4) for s in range(8)]
        fmi = 0
        for si, (t0, ntile) in enumerate(sts):
            hTs = hT[si % 2]
            ntok = ntile * 128
            for ti in range(ntile):
                i = t0 + ti
                b = i % 2
                j = 1 if i < 2 else 0
                if i < 2:
                    src = (I.ctx if l == g.first else S.xcres)[i * 128:(i + 1) * 128, :]
                else:
                    src = (I.x if l == g.first else S.xres)[(i - 2) * 128:(i - 1) * 128, :]
                P.dma("sync", xt[b][:], src, w=[xt[b]])
                P.op("act", lambda e, b=b: e.activation(out=junk[:], in_=xt[b][:], func=AF.Square, accum_out=ss[b][:]),
                     r=[xt[b]], w=[junk, ss[b]])
                P.op("dve", lambda e, b=b: e.tensor_scalar(out=ss[b][:], in0=ss[b][:], scalar1=1.0 / D, scalar2=1e-6,
                                                            op0=ALU.mult, op1=ALU.add), r=[ss[b]], w=[ss[b]])
                P.op("act", lambda e, b=b: e.activation(out=ss[b][:], in_=ss[b][:], func=AF.Sqrt), r=[ss[b]], w=[ss[b]])
                P.op("dve", lambda e, b=b: e.reciprocal(out=ss[b][:], in_=ss[b][:]), r=[ss[b]], w=[ss[b]])
                P.op("dve", lambda e, b=b, j=j: e.scalar_tensor_tensor(out=xt[b][:], in0=xt[b][:], scalar=ss[b][:, 0:1],
                                                                       in1=m1[j][:], op0=ALU.mult, op1=ALU.mult),
                     r=[xt[b], ss[b], m1[j]], w=[xt[b]])
                P.op("pool", lambda e, b=b, j=j: e.tensor_tensor(out=hb[b][:], in0=xt[b][:], in1=sh1[j][:], op=ALU.add),
                     r=[xt[b], sh1[j]], w=[hb[b]])
                for half in range(2):
                    for k4 in range(4):
                        kc = half * 4 + k4
                        P.op("pe", lambda e, b=b, kc=kc, k4=k4, half=half: e.transpose(
                            out=pT[half][:, k4, :], in_=hb[b][:, kc * 128:(kc + 1) * 128], identity=g.identb[:]),
                            r=[hb[b], g.identb], w=[pT[half]])
                    eng = "act" if half == 0 else "dve"
                    if eng == "act":
                        P.op("act", lambda e, half=half, ti=ti, hTs=hTs: e.activation(
                            out=hTs[:, half * 4:(half + 1) * 4, ti * 128:(ti + 1) * 128], in_=pT[half][:, 0:4, :], func=AF.Copy),
                            r=[pT[half]], w=[hTs])
                    else:
                        P.op("dve", lambda e, half=half, ti=ti, hTs=hTs: e.tensor_copy(
                            out=hTs[:, half * 4:(half + 1) * 4, ti * 128:(ti + 1) * 128], in_=pT[half][:, 0:4, :]),
                            r=[pT[half]], w=[hTs])
                for kc in range(8):
                    P.op("pe", lambda e, kc=kc, ti=ti, hTs=hTs: e.matmul(
                        out=pA[:], lhsT=hTs[:, kc, ti * 128:(ti + 1) * 128], rhs=wb[:, kc, 768:1280],
                        start=(kc == 0), stop=(kc == 7)), r=[hTs] + wbk, w=[pA])
                for kc in range(8):
                    P.op("pe", lambda e, kc=kc, ti=ti, hTs=hTs: e.matmul(
                        out=pB[:, 0:256], lhsT=hTs[:, kc, ti * 128:(ti + 1) * 128], rhs=wb[:, kc, 1280:1536],
                        start=(kc == 0), stop=(kc == 7)), r=[hTs] + wbk, w=[pB])
                qv = qkv[b]
                P.op("act", lambda e, qv=qv: e.activation(out=qv[:, 0:512], in_=pA[:], func=AF.Copy), r=[pA], w=[qv])
                P.op("act", lambda e, qv=qv: e.activation(out=qv[:, 512:768], in_=pB[:, 0:256], func=AF.Copy), r=[pB], w=[qv])
                P.op("pool", lambda e, qv=qv, i=i: e.tensor_copy(
                    out=g.Vaug[:, i, :, 0:64], in_=qv[:, 640:768].rearrange("p (g d) -> p g d", g=2)),
                    r=[qv], w=[("Vaug", i)])
                P.op("dve", lambda e, qv=qv: e.tensor_tensor(out=sq[:], in0=qv[:, 0:640], in1=qv[:, 0:640], op=ALU.mult),
                     r=[qv], w=[sq])
                P.op("dve", lambda e: e.tensor_reduce(out=ssq[:], in_=sq[:].rearrange("p (h d) -> p h d", h=10),
                                                       axis=AX.X, op=ALU.add), r=[sq], w=[ssq])
                P.op("dve", lambda e: e.tensor_scalar(out=ssq[:], in0=ssq[:], scalar1=1.0 / 64, scalar2=1e-6,
                                                       op0=ALU.mult, op1=ALU.add), r=[ssq], w=[ssq])
                P.op("act", lambda e: e.activation(out=ssq[:], in_=ssq[:], func=AF.Sqrt), r=[ssq], w=[ssq])
                P.op("dve", lambda e: e.reciprocal(out=ssq[:], in_=ssq[:]), r=[ssq], w=[ssq])
                P.op("dve", lambda e, qv=qv: e.tensor_tensor(
                    out=qn[:], in0=qv[:, 0:640].rearrange("p (h d) -> p h d", h=10),
                    in1=ssq[:].unsqueeze(2).to_broadcast([128, 10, 64]), op=ALU.mult), r=[qv, ssq], w=[qn])
                P.op("dve", lambda e: e.tensor_tensor(out=qn[:, 0:8, :], in0=qn[:, 0:8, :],
                                                       in1=qkg[:, 0:1, :].to_broadcast([128, 8, 64]), op=ALU.mult),
                     r=[qn, qkg], w=[qn])
                P.op("dve", lambda e: e.tensor_tensor(out=qn[:, 8:10, :], in0=qn[:, 8:10, :],
                                                       in1=qkg[:, 1:2, :].to_broadcast([128, 2, 64]), op=ALU.mult),
                     r=[qn, qkg], w=[qn])
                qb = qrb[b]
                if i < 2:
                    P.op("dve", lambda e, qb=qb: e.tensor_copy(out=qb[:], in_=qn[:]), r=[qn], w=[qb])
                else:
                    li = i - 2
                    cosb = cs[:, li:li + 1, 0:32].to_broadcast([128, 10, 32])
                    sinb = cs[:, li:li + 1, 32:64].to_broadcast([128, 10, 32])
                    x1 = qn[:, :, 0:32]
                    x2 = qn[:, :, 32:64]
                    P.op("dve", lambda e, cosb=cosb: e.tensor_tensor(out=qr[:, :, 0:32], in0=qn[:, :, 0:32], in1=cosb, op=ALU.mult),
                         r=[qn, cs], w=[qr])
                    P.op("dve", lambda e, sinb=sinb: e.tensor_tensor(out=tmp[:], in0=qn[:, :, 32:64], in1=sinb, op=ALU.mult),
                         r=[qn, cs], w=[tmp])
                    P.op("dve", lambda e, qb=qb: e.tensor_tensor(out=qb[:, :, 0:32], in0=qr[:, :, 0:32], in1=tmp[:], op=ALU.subtract),
                         r=[qr, tmp], w=[qb])
                    P.op("dve", lambda e, sinb=sinb: e.tensor_tensor(out=qr[:, :, 32:64], in0=qn[:, :, 0:32], in1=sinb, op=ALU.mult),
                         r=[qn, cs], w=[qr])
                    P.op("dve", lambda e, cosb=cosb: e.tensor_tensor(out=tmp[:], in0=qn[:, :, 32:64], in1=cosb, op=ALU.mult),
                         r=[qn, cs, qb], w=[tmp])
                    P.op("dve", lambda e, qb=qb: e.tensor_tensor(out=qb[:, :, 32:64], in0=qr[:, :, 32:64], in1=tmp[:], op=ALU.add),
                         r=[qr, tmp], w=[qb])
                for h in range(8):
                    P.op("pe", lambda e, h=h, qb=qb: e.transpose(out=pQ[:, h, :], in_=qb[:, h, :], identity=g.identb[:]),
                         r=[qb, g.identb], w=[pQ])
                for h in range(2):
                    P.op("pe", lambda e, h=h, qb=qb: e.transpose(out=pQk[:, h, :], in_=qb[:, 8 + h, :], identity=g.identb[:]),
                         r=[qb, g.identb], w=[pQk])
                qs = qTs[b]
                P.op("act", lambda e, qs=qs: e.activation(out=qs[:], in_=pQ[:], func=AF.Copy), r=[pQ], w=[qs])
                P.op("dve", lambda e, i=i: e.tensor_copy(out=g.kT[0:64, :, i * 128:(i + 1) * 128], in_=pQk[:, 0:2, :]),
                     r=[pQk], w=[("kT", i)])
                P.op("dve", lambda e, i=i: e.tensor_copy(out=g.kT[64:128, :, i * 128:(i + 1) * 128], in_=pQk[:, 0:2, :]),
                     r=[pQk], w=[("kT", i)])
                P.dma("pool", S.qT[:, :, i * 128:(i + 1) * 128].rearrange("h d t -> d h t"), qs[:], r=[qs], sem=("qs", b))
            for (c0, wdt, r0) in fchunks:
                pb = pF[fmi % 2]
                fb = fm[fmi % 3]
                for kc in range(8):
                    P.op("pe", lambda e, kc=kc, c0=c0, wdt=wdt, pb=pb, hTs=hTs, ntok=ntok: e.matmul(
                        out=pb[0:wdt, 0:ntok], lhsT=wb[:, kc, c0:c0 + wdt], rhs=hTs[:, kc, 0:ntok],
                        start=(kc == 0), stop=(kc == 7)), r=[hTs] + wbk, w=[pb])
                if fmi % 2 == 0:
                    P.op("act", lambda e, wdt=wdt, pb=pb, fb=fb, ntok=ntok: e.activation(
                        out=fb[0:wdt, 0:ntok], in_=pb[0:wdt, 0:ntok], func=AF.Copy), r=[pb], w=[fb])
                else:
                    P.op("dve", lambda e, wdt=wdt, pb=pb, fb=fb, ntok=ntok: e.tensor_copy(
                        out=fb[0:wdt, 0:ntok], in_=pb[0:wdt, 0:ntok]), r=[pb], w=[fb])
                P.dma("sync", S.pfm[r0:r0 + wdt, t0 * 128:t0 * 128 + ntok], fb[0:wdt, 0:ntok], r=[fb], sem=("fm", fmi % 3))
                fmi += 1
        P.barrier()
        P.emit()


def conv_ops(g, l, P, es):
    nc, I, S = g.nc, g.I, g.S
    todo = []
    if True:
        def sb(name, shape, dt=F32):
            return es.enter_context(nc.sbuf_tensor("c%d_" % l + name, list(shape), dt))
        Bt = sb("Bt", [128, SEQ])
        Ct = sb("Ct", [128, SEQ])
        Ut = sb("Ut", [128, SEQ])
        zp = sb("zp", [128, SEQ + 2])
        acc = sb("acc", [128, SEQ])
        ob = sb("ob", [128, SEQ], BF16)
        cw = sb("cw", [128, 2, 3])
        todo.append(lambda: P.dma("sync", cw[:], I.conv_wT[g.wi(l)].rearrange("(c p) k -> p c k", p=128), w=[cw]))
        seqs = [(CTX, SEQ)] + ([(0, CTX)] if l == 0 else [])
        for (t0, T) in seqs:
            for cc in range(2):
                todo.append(lambda T=T, cc=cc, t0=t0: P.dma("sync", Bt[:, 0:T], S.pfm[cc * 128:(cc + 1) * 128, t0:t0 + T], w=[Bt]))
                todo.append(lambda T=T, cc=cc, t0=t0: P.dma("sync", Ct[:, 0:T], S.pfm[256 + cc * 128:256 + (cc + 1) * 128, t0:t0 + T], w=[Ct]))
                todo.append(lambda T=T, cc=cc, t0=t0: P.dma("pool", Ut[:, 0:T], S.pfm[512 + cc * 128:512 + (cc + 1) * 128, t0:t0 + T], w=[Ut]))
                todo.append(lambda T=T, cc=cc, t0=t0: P.op("pool", lambda e, T=T: e.memset(zp[:, 0:1], 0.0), w=[zp]))
                todo.append(lambda T=T, cc=cc, t0=t0: P.op("pool", lambda e, T=T: e.memset(zp[:, T + 1:T + 2], 0.0), w=[zp]))
                todo.append(lambda T=T, cc=cc, t0=t0: P.op("dve", lambda e, T=T: e.tensor_tensor(out=zp[:, 1:T + 1], in0=Ct[:, 0:T], in1=Ut[:, 0:T], op=ALU.mult),
                     r=[Ct, Ut], w=[zp]))
                todo.append(lambda T=T, cc=cc, t0=t0: P.op("dve", lambda e, T=T, cc=cc: e.tensor_scalar(out=acc[:, 0:T], in0=zp[:, 0:T], scalar1=cw[:, cc, 0:1],
                                                                   scalar2=None, op0=ALU.mult), r=[zp, cw], w=[acc]))
                todo.append(lambda T=T, cc=cc, t0=t0: P.op("dve", lambda e, T=T, cc=cc: e.scalar_tensor_tensor(out=acc[:, 0:T], in0=zp[:, 1:T + 1], scalar=cw[:, cc, 1:2],
                                                                         in1=acc[:, 0:T], op0=ALU.mult, op1=ALU.add),
                     r=[zp, cw, acc], w=[acc]))
                todo.append(lambda T=T, cc=cc, t0=t0: P.op("dve", lambda e, T=T, cc=cc: e.scalar_tensor_tensor(out=acc[:, 0:T], in0=zp[:, 2:T + 2], scalar=cw[:, cc, 2:3],
                                                                         in1=acc[:, 0:T], op0=ALU.mult, op1=ALU.add),
                     r=[zp, cw, acc], w=[acc]))
                todo.append(lambda T=T, cc=cc, t0=t0: P.op("pool", lambda e, T=T: e.tensor_tensor(out=ob[:, 0:T], in0=acc[:, 0:T], in1=Bt[:, 0:T], op=ALU.mult),
                     r=[acc, Bt], w=[ob]))
                todo.append(lambda T=T, cc=cc, t0=t0: P.dma("sync", S.mixT[cc * 128:(cc + 1) * 128, t0:t0 + T], ob[:, 0:T], r=[ob], sem="obst"))
    return todo


def phase_attn(g, l):
    nc, I, S = g.nc, g.I, g.S
    with ExitStack() as es:
        def sb(name, shape, dt=F32):
            return es.enter_context(nc.sbuf_tensor("d%d_" % l + name, list(shape), dt))

        def psb(name, shape, dt=F32):
            return es.enter_context(nc.psum_tensor("d%d_" % l + name, list(shape), dt))
        qc = [sb("qc%d" % i, [128, 512], BF16) for i in range(2)]
        eS = [sb("eS%d" % i, [128, 512], BF16) for i in range(4)]
        rs = sb("rs", [128, 512])
        rsb = sb("rsb", [64, 512])
        ob = [sb("ob%d" % i, [64, 512], BF16) for i in range(2)]
        nb = sb("nb", [128, 1])
        pS = [psb("pS%d" % i, [128, 512]) for i in range(4)]
        pO = [psb("pO%d" % i, [128, 512]) for i in range(2)]
        pR = psb("pR", [64, 512])
        P = Prog(nc)
        P.op("pool", lambda e: e.memset(nb[:], -8.0), w=[nb])
        todo = conv_ops(g, l, P, es)
        jobs = []
        if l == 0:
            for h in range(8):
                jobs.append((h, 0, CTX, [0, 1]))
        for h in range(8):
            for qi in range(8):
                jobs.append((h, CTX + qi * 512, 512, list(range(NTILE))))
        cnt = 0
        for ji, (h, q0, nq, kts) in enumerate(jobs):
            gkv = h // 4
            qb = qc[ji % 2]
            po = pO[ji % 2]
            P.dma("sync", qb[0:64, 0:nq], S.qT[h, :, q0:q0 + nq], w=[qb], sem=("qb", ji % 2))
            P.dma("sync", qb[64:128, 0:nq], S.qT[h, :, q0:q0 + nq], w=[qb], sem=("qb", ji % 2))
            nk = len(kts)
            LOOK = 3
            slots = {}

            def emit_s(ki, cnt0=cnt, kts=kts, qb=qb, nq=nq, gkv=gkv):
                kt = kts[ki]
                ps = pS[(cnt0 + ki) % 4]
                ee = eS[(cnt0 + ki) % 4]
                rb = 64 * (ki % 2)
                P.op("pe", lambda e, ps=ps, kt=kt, rb=rb: e.matmul(
                    out=ps[:, 0:nq], lhsT=g.kT[rb:rb + 64, gkv, kt * 128:(kt + 1) * 128], rhs=qb[rb:rb + 64, 0:nq], start=True, stop=True),
                    r=[qb, ("kT", kt)], w=[ps], rows=rb)
                P.op("act", lambda e, ps=ps, ee=ee: e.activation(out=ee[:, 0:nq], in_=ps[:, 0:nq], func=AF.Exp,
                                                                 bias=nb[:, 0:1], scale=0.125),
                     r=[ps, nb], w=[ee])

            def emit_pv(ki, cnt0=cnt, kts=kts, po=po, nq=nq, gkv=gkv, nk=nk):
                kt = kts[ki]
                ee = eS[(cnt0 + ki) % 4]
                P.op("pe", lambda e, kt=kt, ee=ee: e.matmul(
                    out=po[0:65, 0:nq], lhsT=g.Vaug[:, kt, gkv, :], rhs=ee[:, 0:nq], start=(ki == 0), stop=(ki == nk - 1)),
                    r=[ee, ("Vaug", kt), g.Vaug], w=[po])

            for ki in range(nk + LOOK):
                if ki < nk:
                    emit_s(ki)
                if ki - LOOK >= 0:
                    emit_pv(ki - LOOK)
            cnt += nk
            P.op("dve", lambda e, po=po, nq=nq: e.reciprocal(out=rs[64:65, 0:nq], in_=po[64:65, 0:nq]), r=[po], w=[rs])
            P.op("pe", lambda e, nq=nq: e.matmul(out=pR[:, 0:nq], lhsT=g.consts[64:65, C_BONES, 64:128], rhs=rs[64:65, 0:nq],
                                                  start=True, stop=True), r=[rs, g.consts], w=[pR])
            P.op("act", lambda e, nq=nq: e.activation(out=rsb[:, 0:nq], in_=pR[:, 0:nq], func=AF.Copy), r=[pR], w=[rsb])
            o = ob[ji % 2]
            P.op("dve", lambda e, po=po, o=o, nq=nq: e.tensor_tensor(out=o[:, 0:nq], in0=po[0:64, 0:nq], in1=rsb[:, 0:nq], op=ALU.mult),
                 r=[po, rsb], w=[o])
            P.dma("pool", S.mixT[256 + h * 64:256 + (h + 1) * 64, q0:q0 + nq], o[:, 0:nq], r=[o], sem=("ob", ji % 2))
            for _ in range(2):
                if todo:
                    todo.pop(0)()
        while todo:
            todo.pop(0)()
        P.barrier()
        P.emit()


NEG_EM05 = -math.exp(-0.5)


def phase_rwfeat(g, l):
    nc, I, S = g.nc, g.I, g.S
    with ExitStack() as es:
        def sb(name, shape, dt=F32):
            return es.enter_context(nc.sbuf_tensor("e%d_" % l + name, list(shape), dt))

        def psb(name, shape, dt=F32):
            return es.enter_context(nc.psum_tensor("e%d_" % l + name, list(shape), dt))
        SEG = 512
        rch = [("r0", 768, 128), ("r1", 896, 128), ("k0", 1024, 128), ("k1", 1152, 128), ("v0", 1280, 128),
               ("v1", 1408, 128), ("wl", 1536, 128), ("al", 1664, 128), ("g0", 1792, 128), ("g1", 1920, 32)]
        mu = sb("mu", [128, 10])
        omu = sb("omu", [128, 10])
        hmu = sb("hmu", [128, 10])
        pt = [sb("pt%d" % i, [128, SEG + 2]) for i in range(3)]
        s1 = [sb("s1%d" % i, [128, SEG]) for i in range(2)]
        sh = {nm: sb("sh_" + nm, [128, SEG]) for nm, _, _ in rch}
        w0c = sb("w0c", [128, 4])
        a0c = sb("a0c", [128, 4])
        kkc = sb("kkc", [128, 2])
        kac = sb("kac", [128, 2])
        omka = sb("omka", [128, 2])
        wbt = sb("wbt", [128, 256])
        abt = sb("abt", [128, 256])
        gb0 = sb("gb0", [128, 256])
        gb1 = sb("gb1", [32, 256])
        twl = sb("twl", [128, SEG])
        sg0 = sb("sg0", [128, SEG])
        sg1 = sb("sg1", [32, SEG])
        lw = [sb("lw%d" % i, [128, SEG]) for i in range(4)]
        asg = [sb("asg%d" % i, [128, SEG]) for i in range(4)]
        kk = [sb("kk%d" % i, [128, SEG]) for i in range(2)]
        sq = sb("sq", [128, SEG])
        rn = sb("rn", [128, SEG])
        kd = [sb("kd%d" % i, [128, SEG]) for i in range(4)]
        bd = [sb("bd%d" % i, [128, SEG]) for i in range(4)]
        gt = [sb("gt%d" % i, [128, SEG]) for i in range(2)]
        tq = sb("tq", [128, SEG])
        pp = [psb("pp%d" % i, [128, SEG]) for i in range(6)]
        P = Prog(nc)
        P.op("pool", lambda e: e.memset(mu[:, 9:10], 0.0), w=[mu])
        for ci, (nm, r0, nr) in enumerate(rch):
            P.dma("sync", mu[0:nr, ci:ci + 1], I.mu[g.wi(l), r0 - 768:r0 - 768 + nr, :], w=[mu], sem="mu")
        P.op("dve", lambda e: e.tensor_scalar(out=omu[:], in0=mu[:], scalar1=-1.0, scalar2=1.0, op0=ALU.mult, op1=ALU.add),
             r=[mu], w=[omu])
        P.op("dve", lambda e: e.tensor_scalar(out=hmu[:], in0=mu[:], scalar1=0.5, scalar2=None, op0=ALU.mult), r=[mu], w=[hmu])
        for d in range(2):
            for cc in range(2):
                P.dma("sync", w0c[:, d * 2 + cc:d * 2 + cc + 1], I.w0[g.wi(l), d * 256 + cc * 128:d * 256 + (cc + 1) * 128, :], w=[w0c], sem="w0c")
                P.dma("sync", a0c[:, d * 2 + cc:d * 2 + cc + 1], I.a0[g.wi(l), d * 256 + cc * 128:d * 256 + (cc + 1) * 128, :], w=[a0c], sem="a0c")
        for cc in range(2):
            P.dma("sync", kkc[:, cc:cc + 1], I.k_k[g.wi(l), cc * 128:(cc + 1) * 128, :], w=[kkc], sem="kkc")
            P.dma("sync", kac[:, cc:cc + 1], I.k_a[g.wi(l), cc * 128:(cc + 1) * 128, :], w=[kac], sem="kac")
        P.op("dve", lambda e: e.tensor_scalar(out=omka[:], in0=kac[:], scalar1=-1.0, scalar2=1.0, op0=ALU.mult, op1=ALU.add),
             r=[kac], w=[omka])
        P.dma("sync", wbt[:], I.w_b[g.wi(l)], w=[wbt])
        P.dma("sync", abt[:], I.a_b[g.wi(l)], w=[abt])
        P.dma("sync", gb0[:], I.g_b[g.wi(l), 0:128, :], w=[gb0])
        P.dma("sync", gb1[:], I.g_b[g.wi(l), 128:160, :], w=[gb1])
        segs = [(0, 0, CTX, CTX)] + [(CTX, CTX + i * SEG, SEG, SEQ) for i in range(8)]
        pti = 0
        sti = 0
        ppi = 0

        def store(idx, cc, src, n, t0):
            nonlocal sti
            q = "sync" if sti % 2 == 0 else "pool"
            sti += 1
            P.dma(q, S.rwf[idx, cc * 128:(cc + 1) * 128, t0:t0 + n], src[:, 0:n], r=[src], sem=("st", src.name))

        for (sq0, t0, n, slen) in segs:
            for ci, (nm, r0, nr) in enumerate(rch):
                p_ = pt[pti % 3]
                s_ = s1[pti % 2]
                pti += 1
                lo = t0 - 1
                hi = t0 + n + 1
                dlo, dhi = 0, n + 2
                if t0 == sq0:
                    lo += 1
                    dlo = 1
                    P.op("pool", lambda e, p_=p_, nr=nr: e.memset(p_[0:nr, 0:1], 0.0), w=[p_])
                if t0 + n == sq0 + slen:
                    hi -= 1
                    dhi = n + 1
                    P.op("pool", lambda e, p_=p_, nr=nr, n=n: e.memset(p_[0:nr, n + 1:n + 2], 0.0), w=[p_])
                P.dma("sync" if ci % 2 == 0 else "pool", p_[0:nr, dlo:dhi], S.pfm[r0:r0 + nr, lo:hi], w=[p_])
                P.op("pool", lambda e, p_=p_, s_=s_, nr=nr, n=n: e.tensor_tensor(out=s_[0:nr, 0:n], in0=p_[0:nr, 0:n], in1=p_[0:nr, 2:n + 2], op=ALU.add),
                     r=[p_], w=[s_])
                P.op("dve", lambda e, p_=p_, nr=nr, n=n, ci=ci, nm=nm: e.tensor_scalar(out=sh[nm][0:nr, 0:n], in0=p_[0:nr, 1:n + 1], scalar1=omu[0:nr, ci:ci + 1],
                                                                                scalar2=None, op0=ALU.mult), r=[p_, omu], w=[sh[nm]])
                P.op("dve", lambda e, s_=s_, nr=nr, n=n, ci=ci, nm=nm: e.scalar_tensor_tensor(out=sh[nm][0:nr, 0:n], in0=s_[0:nr, 0:n], scalar=hmu[0:nr, ci:ci + 1],
                                                                                       in1=sh[nm][0:nr, 0:n], op0=ALU.mult, op1=ALU.add),
                     r=[s_, hmu, sh[nm]], w=[sh[nm]])
            P.op("act", lambda e, n=n: e.activation(out=twl[:, 0:n], in_=sh["wl"][:, 0:n], func=AF.Tanh), r=[sh["wl"]], w=[twl])
            P.op("act", lambda e, n=n: e.activation(out=sg0[:, 0:n], in_=sh["g0"][:, 0:n], func=AF.Sigmoid), r=[sh["g0"]], w=[sg0])
            P.op("act", lambda e, n=n: e.activation(out=sg1[:, 0:n], in_=sh["g1"][0:32, 0:n], func=AF.Sigmoid), r=[sh["g1"]], w=[sg1])
            for d in range(2):
                for cc in range(2):
                    ix = d * 2 + cc
                    pw = pp[ppi % 6]
                    ppi += 1
                    P.op("pe", lambda e, pw=pw, d=d, cc=cc, n=n: e.matmul(out=pw[:, 0:n], lhsT=wbt[d * 64:(d + 1) * 64, cc * 128:(cc + 1) * 128],
                                                                       rhs=twl[d * 64:(d + 1) * 64, 0:n], start=True, stop=True),
                         r=[wbt, twl], w=[pw])
                    P.op("act", lambda e, pw=pw, ix=ix, n=n: e.activation(out=lw[ix][:, 0:n], in_=pw[:, 0:n], func=AF.Sigmoid, bias=w0c[:, ix:ix + 1]),
                         r=[pw, w0c], w=[lw[ix]])
                    P.op("pool", lambda e, ix=ix, n=n: e.tensor_scalar(out=lw[ix][:, 0:n], in0=lw[ix][:, 0:n], scalar1=NEG_EM05, scalar2=None, op0=ALU.mult),
                         r=[lw[ix]], w=[lw[ix]])
                    store(7 + d, cc, lw[ix], n, t0)
                    pa = pp[ppi % 6]
                    ppi += 1
                    P.op("pe", lambda e, pa=pa, d=d, cc=cc, n=n: e.matmul(out=pa[:, 0:n], lhsT=abt[d * 64:(d + 1) * 64, cc * 128:(cc + 1) * 128],
                                                                       rhs=sh["al"][d * 64:(d + 1) * 64, 0:n], start=True, stop=True),
                         r=[abt, sh["al"]], w=[pa])
                    P.op("act", lambda e, pa=pa, ix=ix, n=n: e.activation(out=asg[ix][:, 0:n], in_=pa[:, 0:n], func=AF.Sigmoid, bias=a0c[:, ix:ix + 1]),
                         r=[pa, a0c], w=[asg[ix]])
            for cc in range(2):
                kx = sh["k%d" % cc]
                P.op("dve", lambda e, cc=cc, kx=kx, n=n: e.tensor_scalar(out=kk[cc][:, 0:n], in0=kx[:, 0:n], scalar1=kkc[:, cc:cc + 1], scalar2=None, op0=ALU.mult),
                     r=[kx, kkc], w=[kk[cc]])
                P.op("pool", lambda e, cc=cc, n=n: e.tensor_tensor(out=sq[:, 0:n], in0=kk[cc][:, 0:n], in1=kk[cc][:, 0:n], op=ALU.mult),
                     r=[kk[cc]], w=[sq])
                pn = pp[ppi % 6]
                ppi += 1
                P.op("pe", lambda e, pn=pn, n=n: e.matmul(out=pn[:, 0:n], lhsT=g.consts[:, C_BONES, :], rhs=sq[:, 0:n], start=True, stop=True),
                     r=[sq, g.consts], w=[pn])
                P.op("act", lambda e, pn=pn, n=n: e.activation(out=rn[:, 0:n], in_=pn[:, 0:n], func=AF.Sqrt), r=[pn], w=[rn])
                P.op("dve", lambda e, n=n: e.tensor_scalar(out=rn[:, 0:n], in0=rn[:, 0:n], scalar1=1e-12, scalar2=None, op0=ALU.max), r=[rn], w=[rn])
                P.op("dve", lambda e, n=n: e.reciprocal(out=rn[:, 0:n], in_=rn[:, 0:n]), r=[rn], w=[rn])
                P.op("dve", lambda e, cc=cc, n=n: e.tensor_tensor(out=kk[cc][:, 0:n], in0=kk[cc][:, 0:n], in1=rn[:, 0:n], op=ALU.mult),
                     r=[kk[cc], rn], w=[kk[cc]])
                store(4, cc, kk[cc], n, t0)
                store(0, cc, sh["r%d" % cc], n, t0)
                store(3, cc, sh["v%d" % cc], n, t0)
                for d in range(2):
                    ix = d * 2 + cc
                    P.op("dve", lambda e, ix=ix, cc=cc, n=n: e.tensor_scalar(out=tq[:, 0:n], in0=asg[ix][:, 0:n], scalar1=kac[:, cc:cc + 1],
                                                                         scalar2=omka[:, cc:cc + 1], op0=ALU.mult, op1=ALU.add),
                         r=[asg[ix], kac, omka], w=[tq])
                    P.op("dve", lambda e, ix=ix, kx=kx, n=n: e.tensor_tensor(out=kd[ix][:, 0:n], in0=tq[:, 0:n], in1=kx[:, 0:n], op=ALU.mult),
                         r=[tq, kx], w=[kd[ix]])
                    store(1 + d, cc, kd[ix], n, t0)
                    P.op("pool", lambda e, ix=ix, cc=cc, n=n: e.tensor_tensor(out=bd[ix][:, 0:n], in0=kk[cc][:, 0:n], in1=asg[ix][:, 0:n], op=ALU.mult),
                         r=[kk[cc], asg[ix]], w=[bd[ix]])
                    store(5 + d, cc, bd[ix], n, t0)
                pg = pp[ppi % 6]
                ppi += 1
                P.op("pe", lambda e, pg=pg, cc=cc, n=n: e.matmul(out=pg[:, 0:n], lhsT=gb0[:, cc * 128:(cc + 1) * 128], rhs=sg0[:, 0:n], start=True, stop=False),
                     r=[gb0, sg0], w=[pg])
                P.op("pe", lambda e, pg=pg, cc=cc, n=n: e.matmul(out=pg[:, 0:n], lhsT=gb1[:, cc * 128:(cc + 1) * 128], rhs=sg1[:, 0:n], start=False, stop=True),
                     r=[gb1, sg1], w=[pg])
                P.op("act", lambda e, pg=pg, cc=cc, n=n: e.activation(out=gt[cc][:, 0:n], in_=pg[:, 0:n], func=AF.Copy), r=[pg], w=[gt[cc]])
                store(9, cc, gt[cc], n, t0)
        P.barrier()
        P.emit()


def phase_rwscan(g, l):
    nc, I, S = g.nc, g.I, g.S
    with ExitStack() as es:
        def sb(name, shape, dt=F32):
            return es.enter_context(nc.sbuf_tensor("f%d_" % l + name, list(shape), dt))

        def psb(name, shape, dt=F32):
            return es.enter_context(nc.psum_tensor("f%d_" % l + name, list(shape), dt))
        ident = g.consts[:, C_ID, :]
        ybuf = [[sb("y%d%d" % (p, d), [128, NT], BF16) for d in range(2)] for p in range(2)]
        E64 = sb("E64", [128, 64])
        U = [[None, None], [None, None]]
        for d in range(2):
            for p in range(2):
                u = Ctx()
                n = "%d%d" % (d, p)
                u.f = [sb("ld%d_" % i + n, [128, 128]) for i in range(6)]
                u.Lc = sb("Lc" + n, [128, 128])
                u.LC = sb("LC" + n, [128, 128])
                u.t1 = sb("t1" + n, [128, 128])
                u.tA = sb("tA" + n, [128, 128])
                u.tW = sb("tW" + n, [128, 128])
                u.eP = sb("eP" + n, [128, 128])
                u.eN = sb("eN" + n, [128, 128])
                u.eA = sb("eA" + n, [128, 128])
                u.eW = sb("eW" + n, [128, 128])
                u.WL = sb("WL" + n, [128, 2])
                u.ar = sb("ar" + n, [128, 256])
                u.bt = sb("bt" + n, [128, 128])
                u.kt = sb("kt" + n, [128, 128])
                u.bW = sb("bW" + n, [128, 128])
                u.kW = sb("kW" + n, [128, 128])
                u.Dg = sb("Dg" + n, [128, 2, 64])
                u.TM = sb("TM" + n, [128, 4, 128])
                u.q = []
                if p == 1:
                    u.q = U[d][0].q
                    U[d][p] = u
                    continue
                for hh in range(2):
                    q = Ctx()
                    m = n + "%d" % hh
                    q.XTR = sb("XTR" + m, [128, 256])
                    q.KTR = sb("KTR" + m, [128, 256])
                    q.X = [sb("X%d_" % i + m, [128, 128]) for i in range(2)]
                    q.XT = [sb("XT%d_" % i + m, [128, 128]) for i in range(2)]
                    q.PT = [sb("PT%d_" % i + m, [128, 128]) for i in range(2)]
                    q.Gs = sb("Gs" + m, [128, 64])
                    q.MAG = sb("MAG" + m, [128, 128])
                    q.Phi = sb("Phi" + m, [64, 2, 64])
                    q.Psi = sb("Psi" + m, [64, 2, 64])
                    q.RAT = sb("RAT" + m, [64, 128])
                    q.YCT = sb("YCT" + m, [64, 128])
                    u.q.append(q)
                U[d][p] = u
        ST = [[[sb("ST%d%d%d" % (h, d, i), [64, 64]) for i in range(2)] for d in range(2)] for h in range(4)]
        stpar = [[0, 0] for _ in range(4)]
        pq = [psb("pq%d" % i, [128, 512]) for i in range(4)]
        pTr = [psb("pTr%d" % i, [128, 4, 128]) for i in range(2)]
        pSq = [psb("pSq%d" % i, [64, 512]) for i in range(2)]
        P = Prog(nc)
        P.op("dve", lambda e: e.tensor_tensor(out=E64[:], in0=g.consts[:, C_ID, 0:64], in1=g.consts[:, C_ID, 64:128], op=ALU.add),
             r=[g.consts], w=[E64])
        for h in range(4):
            for d in range(2):
                P.op("pool", lambda e, h=h, d=d: e.memset(ST[h][d][0][:], 0.0), w=[ST[h][d][0]])
        order_f = list(range(NTILE))
        order_b = [1, 0] + list(range(NTILE - 1, 1, -1))
        fidx = [[0, 1, 3, 4, 5, 7], [0, 2, 3, 4, 6, 8]]
        slotc = [0, 0, 0, 0]

        def slot(qi):
            sl = slotc[qi] % 4
            slotc[qi] += 1
            return sl

        def fm_part(step, p):
            units = [(0, order_f[step]), (1, order_b[step])]
            for (d, j) in units:
                u = U[d][p]
                for i6 in range(6):
                    P.dma("sync" if i6 % 2 == 0 else "pool", u.f[i6][:], S.rwf[fidx[d][i6], p * 128:(p + 1) * 128, j * 128:(j + 1) * 128],
                          w=[u.f[i6]])
                fr, fkd, fv, fkk, fbd, flw = u.f
                P.op("dve", lambda e, u=u, flw=flw: e.tensor_tensor_scan(out=u.Lc[:], data0=g.consts[:, C_RESET, :], data1=flw[:], initial=0.0,
                                                                       op0=ALU.mult, op1=ALU.add), r=[flw, g.consts], w=[u.Lc])
                totv = u.Lc[:].rearrange("p (c l) -> p c l", c=2)[:, :, 63:64]
                if d == 0:
                    LC = u.Lc
                else:
                    LC = u.LC
                    P.op("pool", lambda e, u=u, flw=flw: e.tensor_tensor(out=u.t1[:], in0=flw[:], in1=u.Lc[:], op=ALU.subtract),
                         r=[flw, u.Lc], w=[u.t1])
                    P.op("pool", lambda e, u=u, totv=totv: e.tensor_tensor(out=u.LC[:].rearrange("p (c l) -> p c l", c=2),
                                                                        in0=u.t1[:].rearrange("p (c l) -> p c l", c=2),
                                                                        in1=totv.to_broadcast([128, 2, 64]), op=ALU.add),
                         r=[u.t1, u.Lc], w=[u.LC])
                P.op("pool", lambda e, u=u, LC=LC, flw=flw: e.tensor_tensor(out=u.tA[:], in0=LC[:], in1=flw[:], op=ALU.subtract),
                     r=[LC, flw], w=[u.tA])
                P.op("pool", lambda e, u=u, LC=LC, totv=totv: e.tensor_tensor(out=u.tW[:].rearrange("p (c l) -> p c l", c=2),
                                                                           in0=totv.to_broadcast([128, 2, 64]),
                                                                           in1=LC[:].rearrange("p (c l) -> p c l", c=2), op=ALU.subtract),
                     r=[LC, u.Lc], w=[u.tW])
                P.op("act", lambda e, u=u, LC=LC: e.activation(out=u.eP[:], in_=LC[:], func=AF.Exp), r=[LC], w=[u.eP])
                P.op("act", lambda e, u=u, LC=LC: e.activation(out=u.eN[:], in_=LC[:], func=AF.Exp, scale=-1.0), r=[LC], w=[u.eN])
                P.op("act", lambda e, u=u: e.activation(out=u.eA[:], in_=u.tA[:], func=AF.Exp), r=[u.tA], w=[u.eA])
                P.op("act", lambda e, u=u: e.activation(out=u.eW[:], in_=u.tW[:], func=AF.Exp), r=[u.tW], w=[u.eW])
                P.op("act", lambda e, u=u, totv=totv: e.activation(out=u.WL[:].unsqueeze(2), in_=totv, func=AF.Exp), r=[u.Lc], w=[u.WL])
                P.op("dve", lambda e, u=u, fkk=fkk: e.scalar_tensor_tensor(out=u.ar[:, 0:128], in0=fkk[:], scalar=-1.0, in1=u.eA[:],
                                                                         op0=ALU.mult, op1=ALU.mult), r=[fkk, u.eA], w=[(u.ar.name, 0)])
                P.op("pool", lambda e, u=u, fr=fr: e.tensor_tensor(out=u.ar[:, 128:256], in0=fr[:], in1=u.eP[:], op=ALU.mult),
                     r=[fr, u.eP], w=[(u.ar.name, 1)])
                P.op("dve", lambda e, u=u, fbd=fbd: e.tensor_tensor(out=u.bt[:], in0=fbd[:], in1=u.eN[:], op=ALU.mult), r=[fbd, u.eN], w=[u.bt])
                P.op("pool", lambda e, u=u, fkd=fkd: e.tensor_tensor(out=u.kt[:], in0=fkd[:], in1=u.eN[:], op=ALU.mult), r=[fkd, u.eN], w=[u.kt])
                P.op("dve", lambda e, u=u, fbd=fbd: e.tensor_tensor(out=u.bW[:], in0=fbd[:], in1=u.eW[:], op=ALU.mult), r=[fbd, u.eW], w=[u.bW])
                P.op("pool", lambda e, u=u, fkd=fkd: e.tensor_tensor(out=u.kW[:], in0=fkd[:], in1=u.eW[:], op=ALU.mult), r=[fkd, u.eW], w=[u.kW])
                for c in range(2):
                    P.op("pool", lambda e, u=u, c=c: e.tensor_scalar(out=u.Dg[:, c, :], in0=E64[:], scalar1=u.WL[:, c:c + 1], scalar2=None, op0=ALU.mult),
                         r=[E64, u.WL], w=[u.Dg])
                srcs = [(u.ar, 0, (u.ar.name, 0)), (u.bW, None, u.bW.name), (u.kW, None, u.kW.name), (fv, None, fv.name)]
                for k4, (src, off, key) in enumerate(srcs):
                    in_ap = src[:, 0:128]
                    P.op("pe", lambda e, d=d, k4=k4, in_ap=in_ap: e.transpose(out=pTr[d][:, k4, :], in_=in_ap, identity=ident),
                         r=[key, g.consts], w=[pTr[d]])
                P.op("act", lambda e, u=u, d=d: e.activation(out=u.TM[:], in_=pTr[d][:], func=AF.Copy), r=[pTr[d]], w=[u.TM])

        def rest_part(step, p):
            units = [(0, order_f[step]), (1, order_b[step])]
            probs = []
            for (d, j) in units:
                for hh in range(2):
                    probs.append((d, j, hh, U[d][p], U[d][p].q[hh], d * 2 + hh))
            for (d, j, hh, u, q, qi) in probs:
                pb = hh * 64
                P.op("pe", lambda e, u=u, pb=pb, qi=qi: e.matmul(out=pq[qi][:, 0:256], lhsT=u.bt[pb:pb + 64, :], rhs=u.ar[pb:pb + 64, :], start=True, stop=True),
                     r=[u.bt, (u.ar.name, 0), (u.ar.name, 1)], w=[("pqb", qi), ("pqb", qi)], rows=pb)
                P.op("pe", lambda e, u=u, pb=pb, qi=qi: e.matmul(out=pq[qi][:, 256:384], lhsT=u.ar[pb:pb + 64, 0:128], rhs=u.bt[pb:pb + 64, :], start=True, stop=True),
                     r=[u.bt, (u.ar.name, 0)], w=[("pqb", qi)], rows=pb)
            for (d, j, hh, u, q, qi) in probs:
                m2 = (C_MS_IT if d == 0 else C_MS_IT_B)
                mti = (C_MS_TI if d == 0 else C_MS_IT)
                P.op("dve", lambda e, q=q, qi=qi, m2=m2: e.tensor_tensor(out=q.XTR[:], in0=pq[qi][:, 0:256],
                                                                      in1=g.consts[:, m2:m2 + 2, :].rearrange("p a b -> p (a b)"), op=ALU.mult),
                     r=[("pqb", qi), ("pqb", qi), g.consts], w=[q.XTR])
                P.op("dve", lambda e, q=q, qi=qi, mti=mti: e.tensor_tensor(out=q.X[0][:], in0=pq[qi][:, 256:384], in1=g.consts[:, mti, :], op=ALU.mult),
                     r=[("pqb", qi), g.consts], w=[q.X[0]])
                P.op("pool", lambda e, q=q: e.tensor_tensor(out=q.PT[0][:], in0=q.XTR[:, 0:128], in1=ident, op=ALU.add),
                     r=[q.XTR, g.consts], w=[q.PT[0]])
            for (d, j, hh, u, q, qi) in probs:
                pb = hh * 64
                P.op("pe", lambda e, u=u, pb=pb, qi=qi: e.matmul(out=pq[qi][:, 0:256], lhsT=u.kt[pb:pb + 64, :], rhs=u.ar[pb:pb + 64, :], start=True, stop=True),
                     r=[u.kt, (u.ar.name, 0), (u.ar.name, 1)], w=[("pqb", qi), ("pqb", qi)], rows=pb)
            for (d, j, hh, u, q, qi) in probs:
                m2 = (C_MS_IT if d == 0 else C_MS_IT_B)
                P.op("dve", lambda e, q=q, qi=qi, m2=m2: e.tensor_tensor(out=q.KTR[:], in0=pq[qi][:, 0:256],
                                                                      in1=g.consts[:, m2:m2 + 2, :].rearrange("p a b -> p (a b)"), op=ALU.mult),
                     r=[("pqb", qi), ("pqb", qi), g.consts], w=[q.KTR])
            curX = {qi: None for qi in range(4)}
            for lev in range(5):
                last = (lev == 4)
                for (d, j, hh, u, q, qi) in probs:
                    Xc = q.X[lev % 2]
                    XTc = q.XTR if lev == 0 else q.XT[lev % 2]
                    XTc_ap = XTc[:, 0:128]
                    P.op("pe", lambda e, qi=qi, Xc=Xc, XTc_ap=XTc_ap: e.matmul(out=pq[qi][:, 384:512], lhsT=XTc_ap, rhs=Xc[:], start=True, stop=True),
                         r=[Xc, XTc], w=[("pqb", qi)])
                    if not last:
                        P.op("pe", lambda e, qi=qi, Xc=Xc, XTc_ap=XTc_ap: e.matmul(out=pq[qi][:, 256:384], lhsT=Xc[:], rhs=XTc_ap, start=True, stop=True),
                             r=[Xc, XTc], w=[("pqb", qi)])
                for (d, j, hh, u, q, qi) in probs:
                    Xn = q.X[(lev + 1) % 2]
                    XTn = q.XT[(lev + 1) % 2]
                    P.op("act", lambda e, qi=qi, Xn=Xn: e.activation(out=Xn[:], in_=pq[qi][:, 384:512], func=AF.Copy), r=[("pqb", qi)], w=[Xn])
                    if not last:
                        P.op("act", lambda e, qi=qi, XTn=XTn: e.activation(out=XTn[:], in_=pq[qi][:, 256:384], func=AF.Copy), r=[("pqb", qi)], w=[XTn])
                for (d, j, hh, u, q, qi) in probs:
                    Xn = q.X[(lev + 1) % 2]
                    PTc = q.PT[lev % 2]
                    P.op("pe", lambda e, qi=qi, Xn=Xn, PTc=PTc: e.matmul(out=pq[qi][:, 0:128], lhsT=Xn[:], rhs=PTc[:], start=True, stop=True),
                         r=[Xn, PTc], w=[("pqb", qi)])
                for (d, j, hh, u, q, qi) in probs:
                    PTc = q.PT[lev % 2]
                    PTn = q.PT[(lev + 1) % 2]
                    P.op("dve", lambda e, qi=qi, PTc=PTc, PTn=PTn: e.tensor_tensor(out=PTn[:], in0=pq[qi][:, 0:128], in1=PTc[:], op=ALU.add),
                         r=[("pqb", qi), PTc], w=[PTn])
            for (d, j, hh, u, q, qi) in probs:
                cb = hh * 64
                P.op("pe", lambda e, qi=qi, q=q, u=u, cb=cb: e.matmul(out=pq[qi][:, 128:192], lhsT=q.KTR[:, 0:128], rhs=u.TM[:, 3, cb:cb + 64], start=True, stop=True),
                     r=[q.KTR, u.TM], w=[("pqb", qi)])
            for (d, j, hh, u, q, qi) in probs:
                P.op("act", lambda e, qi=qi, q=q: e.activation(out=q.Gs[:], in_=pq[qi][:, 128:192], func=AF.Copy), r=[("pqb", qi)], w=[q.Gs])
            for (d, j, hh, u, q, qi) in probs:
                cb = hh * 64
                PTf = q.PT[1]
                P.op("pe", lambda e, qi=qi, PTf=PTf, u=u, cb=cb: e.matmul(out=pq[qi][:, 256:320], lhsT=PTf[:], rhs=u.TM[:, 0, cb:cb + 64], start=True, stop=True),
                     r=[PTf, u.TM], w=[("pqb", qi)])
                P.op("pe", lambda e, qi=qi, PTf=PTf, q=q: e.matmul(out=pq[qi][:, 320:384], lhsT=PTf[:], rhs=q.Gs[:], start=True, stop=True),
                     r=[PTf, q.Gs], w=[("pqb", qi)])
            for (d, j, hh, u, q, qi) in probs:
                P.op("act", lambda e, qi=qi, q=q: e.activation(out=q.MAG[:], in_=pq[qi][:, 256:384], func=AF.Copy), r=[("pqb", qi)], w=[q.MAG])
            for (d, j, hh, u, q, qi) in probs:
                cb = hh * 64
                pb = hh * 64
                for c in range(2):
                    rb = c * 64
                    P.op("pe", lambda e, qi=qi, q=q, u=u, rb=rb, cb=cb, c=c: e.matmul(out=pq[qi][0:64, 384 + c * 64:448 + c * 64], lhsT=q.MAG[rb:rb + 64, 0:64],
                                                                                   rhs=u.TM[rb:rb + 64, 1, cb:cb + 64], start=True, stop=False),
                         r=[q.MAG, u.TM], w=[("pqb", qi)], rows=rb)
                    P.op("pe", lambda e, qi=qi, u=u, pb=pb, c=c: e.matmul(out=pq[qi][0:64, 384 + c * 64:448 + c * 64], lhsT=E64[pb:pb + 64, :],
                                                                        rhs=u.Dg[pb:pb + 64, c, :], start=False, stop=True),
                         r=[E64, u.Dg], w=[("pqb", qi)], rows=pb)
                    P.op("pe", lambda e, qi=qi, q=q, u=u, rb=rb, cb=cb, c=c: e.matmul(out=pq[qi][0:64, c * 64:c * 64 + 64], lhsT=u.TM[rb:rb + 64, 1, cb:cb + 64],
                                                                                   rhs=q.MAG[rb:rb + 64, 64:128], start=True, stop=False),
                         r=[q.MAG, u.TM], w=[("pqb", qi)], rows=rb)
                    P.op("pe", lambda e, qi=qi, u=u, rb=rb, cb=cb, c=c: e.matmul(out=pq[qi][0:64, c * 64:c * 64 + 64], lhsT=u.TM[rb:rb + 64, 2, cb:cb + 64],
                                                                              rhs=u.TM[rb:rb + 64, 3, cb:cb + 64], start=False, stop=True),
                         r=[u.TM], w=[("pqb", qi)], rows=rb)
                P.op("pe", lambda e, qi=qi, q=q: e.matmul(out=pq[qi][0:64, 128:256], lhsT=q.MAG[:, 0:64], rhs=q.XTR[:, 128:256], start=True, stop=False),
                     r=[q.MAG, q.XTR], w=[("pqb", qi)])
                P.op("pe", lambda e, qi=qi, u=u, pb=pb: e.matmul(out=pq[qi][0:64, 128:256], lhsT=E64[pb:pb + 64, :], rhs=u.ar[pb:pb + 64, 128:256], start=False, stop=True),
                     r=[E64, (u.ar.name, 1)], w=[("pqb", qi)], rows=pb)
                P.op("pe", lambda e, qi=qi, q=q: e.matmul(out=pq[qi][0:64, 256:384], lhsT=q.MAG[:, 64:128], rhs=q.XTR[:, 128:256], start=True, stop=False),
                     r=[q.MAG, q.XTR], w=[("pqb", qi)])
                P.op("pe", lambda e, qi=qi, q=q, u=u, cb=cb: e.matmul(out=pq[qi][0:64, 256:384], lhsT=u.TM[:, 3, cb:cb + 64], rhs=q.KTR[:, 128:256], start=False, stop=True),
                     r=[u.TM, q.KTR], w=[("pqb", qi)])
            for (d, j, hh, u, q, qi) in probs:
                P.op("act", lambda e, qi=qi, q=q: e.activation(out=q.Phi[:].rearrange("p c k -> p (c k)"), in_=pq[qi][0:64, 384:512], func=AF.Copy),
                     r=[("pqb", qi)], w=[q.Phi])
                P.op("dve", lambda e, qi=qi, q=q: e.tensor_copy(out=q.Psi[:].rearrange("p c k -> p (c k)"), in_=pq[qi][0:64, 0:128]),
                     r=[("pqb", qi)], w=[q.Psi])
                P.op("act", lambda e, qi=qi, q=q: e.activation(out=q.RAT[:], in_=pq[qi][0:64, 128:256], func=AF.Copy), r=[("pqb", qi)], w=[q.RAT])
                P.op("dve", lambda e, qi=qi, q=q: e.tensor_copy(out=q.YCT[:], in_=pq[qi][0:64, 256:384]), r=[("pqb", qi)], w=[q.YCT])
            for ci in range(2):
                for (d, j, hh, u, q, qi) in probs:
                    c = ci if d == 0 else 1 - ci
                    h = p * 2 + hh
                    sp = stpar[h][d]
                    Sc = ST[h][d][sp]
                    Sn = ST[h][d][1 - sp]
                    stpar[h][d] = 1 - sp
                    psq = pSq[ci]
                    yc0 = qi * 128
                    P.op("pe", lambda e, psq=psq, yc0=yc0, Sc=Sc, q=q, c=c: e.matmul(out=psq[:, yc0:yc0 + 64], lhsT=Sc[:], rhs=q.RAT[:, c * 64:(c + 1) * 64], start=True, stop=False),
                         r=[Sc, q.RAT], w=[("sqb", ci)])
                    P.op("pe", lambda e, psq=psq, yc0=yc0, q=q, c=c: e.matmul(out=psq[:, yc0:yc0 + 64], lhsT=E64[0:64, :], rhs=q.YCT[:, c * 64:(c + 1) * 64], start=False, stop=True),
                         r=[E64, q.YCT], w=[("sqb", ci)])
                    P.op("pe", lambda e, psq=psq, yc0=yc0, Sc=Sc, q=q, c=c: e.matmul(out=psq[:, yc0 + 64:yc0 + 128], lhsT=q.Phi[:, c, :], rhs=Sc[:], start=True, stop=False),
                         r=[Sc, q.Phi], w=[("sqb", ci)])
                    P.op("pe", lambda e, psq=psq, yc0=yc0, q=q, c=c: e.matmul(out=psq[:, yc0 + 64:yc0 + 128], lhsT=E64[0:64, :], rhs=q.Psi[:, c, :], start=False, stop=True),
                         r=[E64, q.Psi], w=[("sqb", ci)])
                    tcol = j * 128 + c * 64
                    yb = ybuf[p][d]
                    P.op("act", lambda e, psq=psq, yc0=yc0, yb=yb, hh=hh, tcol=tcol: e.activation(out=yb[hh * 64:(hh + 1) * 64, tcol:tcol + 64], in_=psq[:, yc0:yc0 + 64], func=AF.Copy),
                         r=[("sqb", ci)], w=[(yb.name, j)])
                    P.op("dve", lambda e, psq=psq, yc0=yc0, Sn=Sn: e.tensor_copy(out=Sn[:], in_=psq[:, yc0 + 64:yc0 + 128]), r=[("sqb", ci)], w=[Sn])

        nsteps = NTILE if g.rwsteps is None else g.rwsteps
        groups = [(st_, p_) for st_ in range(nsteps) for p_ in range(2)]
        fm_part(*groups[0])
        for gi, (st_, p_) in enumerate(groups):
            if gi + 1 < len(groups):
                fm_part(*groups[gi + 1])
            rest_part(st_, p_)
        SEG = 512
        prm = sb("prm", [128, 2, 3])
        ld = [sb("o_ld%d" % i, [128, SEG]) for i in range(5)]
        ysum = sb("ysum", [128, SEG])
        yc_ = sb("yc_", [128, SEG])
        sq = sb("osq", [128, SEG])
        rstd = sb("rstd", [128, SEG])
        prod = sb("prod", [128, SEG])
        ob = [sb("oob%d" % i, [128, SEG], BF16) for i in range(2)]
        for p in range(2):
            for k3, src in enumerate((I.r_k, I.ln_w, I.ln_b)):
                P.dma("sync", prm[:, p, k3:k3 + 1], src[g.wi(l), p * 128:(p + 1) * 128, :], w=[prm], sem="prm")
        segs = ([(0, CTX)] if l == 0 else []) + [(CTX + i * SEG, SEG) for i in range(8)]
        oi = 0
        for (t0, n) in segs:
            for p in range(2):
                for k5, idx in enumerate((0, 1, 2, 3, 9)):
                    P.dma("sync" if k5 % 2 == 0 else "pool", ld[k5][:, 0:n], S.rwf[idx, p * 128:(p + 1) * 128, t0:t0 + n], w=[ld[k5]])
                ykeys = [(ybuf[p][dd].name, jj) for dd in range(2) for jj in range(t0 // 128, (t0 + n) // 128)]
                P.op("pool", lambda e, p=p, t0=t0, n=n: e.tensor_tensor(out=ysum[:, 0:n], in0=ybuf[p][0][:, t0:t0 + n], in1=ybuf[p][1][:, t0:t0 + n], op=ALU.add),
                     r=ykeys, w=[ysum])
                pm = pq[0]
                P.op("pe", lambda e, pm=pm, n=n: e.matmul(out=pm[:, 0:n], lhsT=g.consts[:, C_BONES, :], rhs=ysum[:, 0:n], start=True, stop=True),
                     r=[ysum, g.consts], w=[("pqb", 0), ("pqb", 0), ("pqb", 0), ("pqb", 0)])
                P.op("dve", lambda e, pm=pm, n=n: e.scalar_tensor_tensor(out=yc_[:, 0:n], in0=pm[:, 0:n], scalar=-1.0 / 64, in1=ysum[:, 0:n], op0=ALU.mult, op1=ALU.add),
                     r=[("pqb", 0), ("pqb", 0), ("pqb", 0), ("pqb", 0), ysum], w=[yc_])
                P.op("pool", lambda e, n=n: e.tensor_tensor(out=sq[:, 0:n], in0=yc_[:, 0:n], in1=yc_[:, 0:n], op=ALU.mult), r=[yc_], w=[sq])
                pv = pq[1]
                P.op("pe", lambda e, pv=pv, n=n: e.matmul(out=pv[:, 0:n], lhsT=g.consts[:, C_BONES, :], rhs=sq[:, 0:n], start=True, stop=True),
                     r=[sq, g.consts], w=[("pqb", 1), ("pqb", 1), ("pqb", 1), ("pqb", 1)])
                P.op("dve", lambda e, pv=pv, n=n: e.tensor_scalar(out=rstd[:, 0:n], in0=pv[:, 0:n], scalar1=1.0 / 64, scalar2=64e-5, op0=ALU.mult, op1=ALU.add),
                     r=[("pqb", 1), ("pqb", 1), ("pqb", 1), ("pqb", 1)], w=[rstd])
                P.op("act", lambda e, n=n: e.activation(out=rstd[:, 0:n], in_=rstd[:, 0:n], func=AF.Sqrt), r=[rstd], w=[rstd])
                P.op("dve", lambda e, n=n: e.reciprocal(out=rstd[:, 0:n], in_=rstd[:, 0:n]), r=[rstd], w=[rstd])
                P.op("dve", lambda e, n=n: e.tensor_tensor(out=yc_[:, 0:n], in0=yc_[:, 0:n], in1=rstd[:, 0:n], op=ALU.mult), r=[yc_, rstd], w=[yc_])
                P.op("dve", lambda e, n=n, p=p: e.tensor_scalar(out=yc_[:, 0:n], in0=yc_[:, 0:n], scalar1=prm[:, p, 1:2], scalar2=prm[:, p, 2:3], op0=ALU.mult, op1=ALU.add),
                     r=[yc_, prm], w=[yc_])
                P.op("pool", lambda e, n=n: e.tensor_tensor(out=prod[:, 0:n], in0=ld[1][:, 0:n], in1=ld[2][:, 0:n], op=ALU.add), r=[ld[1], ld[2]], w=[prod])
                P.op("pool", lambda e, n=n: e.tensor_tensor(out=prod[:, 0:n], in0=prod[:, 0:n], in1=ld[0][:, 0:n], op=ALU.mult), r=[prod, ld[0]], w=[prod])
                P.op("pool", lambda e, n=n, p=p: e.tensor_scalar(out=prod[:, 0:n], in0=prod[:, 0:n], scalar1=prm[:, p, 0:1], scalar2=0.5, op0=ALU.mult, op1=ALU.mult),
                     r=[prod, prm], w=[prod])
                pbn = pq[2]
                P.op("pe", lambda e, pbn=pbn, n=n: e.matmul(out=pbn[:, 0:n], lhsT=g.consts[:, C_BONES, :], rhs=prod[:, 0:n], start=True, stop=True),
                     r=[prod, g.consts], w=[("pqb", 2), ("pqb", 2), ("pqb", 2), ("pqb", 2)])
                P.op("dve", lambda e, pbn=pbn, n=n: e.tensor_tensor(out=sq[:, 0:n], in0=pbn[:, 0:n], in1=ld[3][:, 0:n], op=ALU.mult),
                     r=[("pqb", 2), ("pqb", 2), ("pqb", 2), ("pqb", 2), ld[3]], w=[sq])
                P.op("dve", lambda e, n=n: e.tensor_tensor(out=yc_[:, 0:n], in0=yc_[:, 0:n], in1=sq[:, 0:n], op=ALU.add), r=[yc_, sq], w=[yc_])
                o = ob[oi % 2]
                oi += 1
                P.op("dve", lambda e, n=n, o=o: e.tensor_tensor(out=o[:, 0:n], in0=yc_[:, 0:n], in1=ld[4][:, 0:n], op=ALU.mult), r=[yc_, ld[4]], w=[o])
                P.dma("sync", S.mixT[768 + p * 128:768 + (p + 1) * 128, t0:t0 + n], o[:, 0:n], r=[o], sem=("oob", oi % 2))
        P.barrier()
        P.emit()


def phase_wout(g, l):
    nc, I, S = g.nc, g.I, g.S
    with ExitStack() as es:
        def sb(name, shape, dt=F32):
            return es.enter_context(nc.sbuf_tensor("w%d_" % l + name, list(shape), dt))

        def psb(name, shape, dt=F32):
            return es.enter_context(nc.psum_tensor("w%d_" % l + name, list(shape), dt))
        wo = sb("wo", [128, 8, D], BF16)
        rw = sb("rw", [128, 8, NE])
        bc = [[sb("bc%d%d" % (j, k), [128, D]) for k in range(3)] for j in range(2)]
        mt = [sb("mt%d" % i, [128, 8, 128], BF16) for i in range(2)]
        xt = [sb("xt%d" % i, [128, D]) for i in range(2)]
        x1 = [sb("x1%d" % i, [128, D]) for i in range(2)]
        junk = sb("junk", [128, D])
        ss = [sb("ss%d" % i, [128, 1]) for i in range(2)]
        h2f = [sb("h2f%d" % i, [128, D]) for i in range(2)]
        h2b = [sb("h2b%d" % i, [128, D], BF16) for i in range(2)]
        h2T = sb("h2T", [128, 8, 128])
        lg = sb("lg", [128, NE])
        mx = sb("mx", [128, 1])
        sm = sb("sm", [128, 1])
        aff = sb("aff", [128, NE])
        pO = [psb("pO%d" % i, [128, 512]) for i in range(2)]
        pT = [psb("pT%d" % i, [128, 4, 128]) for i in range(2)]
        pL_ = psb("pL", [128, 512])
        pL = pL_[:, 0:NE]
        pA_ = psb("pA", [NE, 512])
        pA = pA_[:, 0:128]
        P = Prog(nc)
        P.dma("pool", wo[:], I.w_out[g.wi(l)].rearrange("(kc p) n -> p kc n", p=128), w=[wo])
        P.dma("sync", rw[:], I.router[g.wi(l)].rearrange("(kc p) n -> p kc n", p=128), w=[rw])
        for j in range(2):
            for k, mi in enumerate((2, 3, 4)):
                P.dma("sync", bc[j][k][:], S.modv[g.wi(l), j, mi:mi + 1, :].to_broadcast([128, D]), w=[bc[j][k]])
        tiles = list(range(NTILE)) if l == 0 else list(range(2, NTILE))
        for i in tiles:
            b = i % 2
            j = 1 if i < 2 else 0
            if i < 2:
                src = (I.ctx if l == g.first else S.xcres)[i * 128:(i + 1) * 128, :]
                dst = S.xcres[i * 128:(i + 1) * 128, :]
                h2dst = S.h2c[i * 128:(i + 1) * 128, :]
            else:
                src = (I.x if l == g.first else S.xres)[(i - 2) * 128:(i - 1) * 128, :]
                dst = (g.out if l == g.last else S.xres)[(i - 2) * 128:(i - 1) * 128, :]
                h2dst = S.h2l[(i - 2) * 128:(i - 1) * 128, :]
            P.dma("sync", mt[b][:], S.mixT[:, i * 128:(i + 1) * 128].rearrange("(kc p) t -> p kc t", p=128), w=[mt[b]])
            P.dma("sync", xt[b][:], src, w=[xt[b]])
            for half in range(2):
                for kc in range(8):
                    P.op("pe", lambda e, half=half, kc=kc, b=b: e.matmul(out=pO[half][:], lhsT=mt[b][:, kc, :], rhs=wo[:, kc, half * 512:(half + 1) * 512],
                                                                       start=(kc == 0), stop=(kc == 7)), r=[mt[b], wo], w=[pO[half]])
                P.op("dve", lambda e, half=half, b=b, j=j: e.tensor_tensor(out=x1[b][:, half * 512:(half + 1) * 512], in0=pO[half][:],
                                                                          in1=bc[j][0][:, half * 512:(half + 1) * 512], op=ALU.mult),
                     r=[pO[half], bc[j][0]], w=[(x1[b].name, half)])
            P.op("pool", lambda e, b=b: e.tensor_tensor(out=x1[b][:], in0=x1[b][:], in1=xt[b][:], op=ALU.add),
                 r=[xt[b]], w=[(x1[b].name, 0), (x1[b].name, 1)])
            P.dma("pool", dst, x1[b][:], r=[(x1[b].name, 0), (x1[b].name, 1)], sem=("x1st", b))
            P.op("act", lambda e, b=b: e.activation(out=junk[:], in_=x1[b][:], func=AF.Square, accum_out=ss[b][:]), r=[(x1[b].name, 0), (x1[b].name, 1)], w=[junk, ss[b]])
            P.op("dve", lambda e, b=b: e.tensor_scalar(out=ss[b][:], in0=ss[b][:], scalar1=1.0 / D, scalar2=1e-6, op0=ALU.mult, op1=ALU.add),
                 r=[ss[b]], w=[ss[b]])
            P.op("act", lambda e, b=b: e.activation(out=ss[b][:], in_=ss[b][:], func=AF.Sqrt), r=[ss[b]], w=[ss[b]])
            P.op("dve", lambda e, b=b: e.reciprocal(out=ss[b][:], in_=ss[b][:]), r=[ss[b]], w=[ss[b]])
            P.op("dve", lambda e, b=b, j=j: e.scalar_tensor_tensor(out=h2f[b][:], in0=x1[b][:], scalar=ss[b][:, 0:1], in1=bc[j][1][:], op0=ALU.mult, op1=ALU.mult),
                 r=[(x1[b].name, 0), (x1[b].name, 1), ss[b], bc[j][1]], w=[h2f[b]])
            P.op("pool", lambda e, b=b, j=j: e.tensor_tensor(out=h2f[b][:], in0=h2f[b][:], in1=bc[j][2][:], op=ALU.add), r=[h2f[b], bc[j][2]], w=[h2f[b]])
            P.op("act", lambda e, b=b: e.activation(out=h2b[b][:], in_=h2f[b][:], func=AF.Copy), r=[h2f[b]], w=[h2b[b]])
            P.dma("sync", h2dst, h2b[b][:], r=[h2b[b]], sem=("h2st", b))
            for half in range(2):
                for k4 in range(4):
                    kc = half * 4 + k4
                    P.op("pe", lambda e, half=half, k4=k4, kc=kc, b=b: e.transpose(out=pT[half][:, k4, :], in_=h2f[b][:, kc * 128:(kc + 1) * 128],
                                                                                identity=g.consts[:, C_ID, :]), r=[h2f[b], g.consts], w=[pT[half]])
                if half == 0:
                    P.op("act", lambda e, half=half: e.activation(out=h2T[:, 0:4, :], in_=pT[0][:], func=AF.Copy), r=[pT[0]], w=[("h2T", 0)])
                else:
                    P.op("dve", lambda e, half=half: e.tensor_copy(out=h2T[:, 4:8, :], in_=pT[1][:]), r=[pT[1]], w=[("h2T", 1)])
            for kc in range(8):
                P.op("pe", lambda e, kc=kc: e.matmul(out=pL, lhsT=h2T[:, kc, :], rhs=rw[:, kc, :], start=(kc == 0), stop=(kc == 7)),
                     r=[("h2T", 0), ("h2T", 1), rw], w=["pL"])
            P.op("dve", lambda e: e.tensor_copy(out=lg[:], in_=pL), r=["pL"], w=[lg])
            P.op("dve", lambda e: e.tensor_reduce(out=mx[:], in_=lg[:], axis=AX.X, op=ALU.max), r=[lg], w=[mx])
            P.op("dve", lambda e: e.tensor_scalar(out=mx[:], in0=mx[:], scalar1=-1.0, scalar2=None, op0=ALU.mult), r=[mx], w=[mx])
            P.op("act", lambda e: e.activation(out=aff[:], in_=lg[:], func=AF.Exp, bias=mx[:, 0:1], accum_out=sm[:]), r=[lg, mx], w=[aff, sm])
            P.op("dve", lambda e: e.reciprocal(out=sm[:], in_=sm[:]), r=[sm], w=[sm])
            P.op("dve", lambda e: e.tensor_scalar(out=aff[:], in0=aff[:], scalar1=sm[:, 0:1], scalar2=None, op0=ALU.mult), r=[aff, sm], w=[aff])
            P.op("pe", lambda e: e.transpose(out=pA, in_=aff[:], identity=g.consts[:, C_ID, :]), r=[aff, g.consts], w=["pA"])
            P.op("act", lambda e, i=i: e.activation(out=g.affT[:, i * 128:(i + 1) * 128], in_=pA, func=AF.Copy), r=["pA"], w=[("affT", i)])
        P.barrier()
        P.emit()


def phase_moe(g, l):
    nc, I, S = g.nc, g.I, g.S
    with ExitStack() as es:
        def sb(name, shape, dt=F32):
            return es.enter_context(nc.sbuf_tensor("m%d_" % l + name, list(shape), dt))

        def psb(name, shape, dt=F32):
            return es.enter_context(nc.psum_tensor("m%d_" % l + name, list(shape), dt))
        work = sb("work", [NE, SEQ])
        vals = sb("vals", [NE, CAP_L])
        idxu = sb("idxu", [NE, CAP_L], U32)
        idxf = sb("idxf", [NE, CAP_L])
        idxT = sb("idxT", [128, 4, NE], I32)
        gT = sb("gT", [128, 4, NE])
        gt2 = [sb("gt2_%d" % j, [128, D]) for j in range(2)]
        wgt = [sb("wg%d" % i, [128, 8, D], BF16) for i in range(2)]
        wut = [sb("wu%d" % i, [128, 8, D], BF16) for i in range(2)]
        wdt = [sb("wd%d" % i, [128, 8, D], BF16) for i in range(2)]
        xs = [sb("xs%d" % i, [128, D], BF16) for i in range(2)]
        xsT = sb("xsT", [128, 8, 512], BF16)
        hidT = sb("hidT", [128, 8, 512], BF16)
        sg = [sb("sg%d" % i, [128, 512]) for i in range(2)]
        y = [sb("y%d" % i, [128, D]) for i in range(2)]
        pX = [psb("pX%d" % i, [128, 8, 128], BF16) for i in range(2)]
        pGs = [psb("pG%d" % i, [128, 512]) for i in range(2)]
        pUs = [psb("pU%d" % i, [128, 512]) for i in range(2)]
        pY = [psb("pY%d" % i, [128, 512]) for i in range(2)]
        pTi_t = pY[1]
        pTi = pY[1][:, :].rearrange("p (a b) -> p a b", a=32)
        P = Prog(nc)
        for j in range(2):
            P.dma("sync", gt2[j][:], S.modv[g.wi(l), j, 5:6, :].to_broadcast([128, D]), w=[gt2[j]])
        sets = [(0, CTX, SEQ, CAP_L, S.h2l, (g.out if l == g.last else S.xres))]
        if l == 0:
            sets.append((1, 0, CTX, CAP_C, S.h2c, S.xcres))
        wi = 0
        xi = 0
        yi = 0
        for (j, a0, N, cap, h2src, dest) in sets:
            nch = (cap + 127) // 128
            npc = min(cap, 128)
            akeys = [("affT", i) for i in range(a0 // 128, (a0 + N) // 128)]
            P.op("pool", lambda e, a0=a0, N=N: e.tensor_copy(out=work[:, 0:N], in_=g.affT[:, a0:a0 + N]), r=akeys, w=[work])
            for r8 in range(cap // 8):
                P.op("dve", lambda e, r8=r8, N=N: e.max(out=vals[:, r8 * 8:(r8 + 1) * 8], in_=work[:, 0:N]), r=[work], w=[vals])
                P.op("dve", lambda e, r8=r8, N=N: e.max_index(out=idxu[:, r8 * 8:(r8 + 1) * 8], in_max=vals[:, r8 * 8:(r8 + 1) * 8], in_values=work[:, 0:N]),
                     r=[work, vals], w=[idxu])
                P.op("dve", lambda e, r8=r8, N=N: e.match_replace(out=work[:, 0:N], in_to_replace=vals[:, r8 * 8:(r8 + 1) * 8], in_values=work[:, 0:N], imm_value=-1.0),
                     r=[work, vals], w=[work])
            P.op("dve", lambda e, cap=cap: e.tensor_copy(out=idxf[:, 0:cap], in_=idxu[:, 0:cap]), r=[idxu], w=[idxf])
            for ch in range(nch):
                P.op("pe", lambda e, ch=ch, npc=npc: e.transpose(out=pTi[0:npc, 0, :], in_=idxf[:, ch * 128:ch * 128 + npc], identity=g.consts[0:NE, C_ID, 0:NE]),
                     r=[idxf, g.consts], w=[pTi_t])
                P.op("pe", lambda e, ch=ch, npc=npc: e.transpose(out=pTi[0:npc, 1, :], in_=vals[:, ch * 128:ch * 128 + npc], identity=g.consts[0:NE, C_ID, 0:NE]),
                     r=[vals, g.consts], w=[pTi_t])
                P.op("dve", lambda e, ch=ch, npc=npc: e.tensor_copy(out=idxT[0:npc, ch, :], in_=pTi[0:npc, 0, :]), r=[pTi_t], w=[idxT])
                P.op("dve", lambda e, ch=ch, npc=npc: e.tensor_copy(out=gT[0:npc, ch, :], in_=pTi[0:npc, 1, :]), r=[], w=[gT, pTi_t])
            ncol = nch * npc
            for ex in range(NE):
                wb_ = wi % 2
                wi += 1
                P.dma("pool", wgt[wb_][:], I.wg[g.wi(l), ex].rearrange("(kc p) n -> p kc n", p=128), w=[wgt[wb_]])
                P.dma("pool", wut[wb_][:], I.wu[g.wi(l), ex].rearrange("(kc p) n -> p kc n", p=128), w=[wut[wb_]])
                P.dma("pool", wdt[wb_][:], I.wd[g.wi(l), ex].rearrange("(kc p) n -> p kc n", p=128), w=[wdt[wb_]])
                for ch in range(nch):
                    xb = xs[xi % 2]
                    xi += 1
                    P.dma_fn("pool", lambda e, xb=xb, ch=ch, ex=ex, npc=npc, h2src=h2src: e.indirect_dma_start(
                        out=xb[0:npc, :], out_offset=None, in_=h2src[:, :],
                        in_offset=bass.IndirectOffsetOnAxis(ap=idxT[0:npc, ch, ex:ex + 1], axis=0)),
                        r=[idxT], w=[xb], sem=("xg", xb.name))
                    for half in range(2):
                        for k4 in range(4):
                            kc = half * 4 + k4
                            P.op("pe", lambda e, half=half, k4=k4, kc=kc, xb=xb, npc=npc: e.transpose(out=pX[half][:, k4, 0:npc], in_=xb[0:npc, kc * 128:(kc + 1) * 128],
                                                                                                 identity=g.identb[0:npc, 0:npc]), r=[xb, g.identb], w=[pX[half]])
                        if half == 0:
                            P.op("act", lambda e, ch=ch, npc=npc: e.activation(out=xsT[:, 0:4, ch * 128:ch * 128 + npc], in_=pX[0][:, 0:4, 0:npc], func=AF.Copy),
                                 r=[pX[0]], w=[("xsT", 0)])
                        else:
                            P.op("dve", lambda e, ch=ch, npc=npc: e.tensor_copy(out=xsT[:, 4:8, ch * 128:ch * 128 + npc], in_=pX[1][:, 0:4, 0:npc]),
                                 r=[pX[1]], w=[("xsT", 1)])
                for fc in range(8):
                    pG = pGs[fc % 2]
                    pU = pUs[fc % 2]
                    for kc in range(8):
                        P.op("pe", lambda e, fc=fc, kc=kc, wb_=wb_, ncol=ncol, pG=pG: e.matmul(out=pG[:, 0:ncol], lhsT=wgt[wb_][:, kc, fc * 128:(fc + 1) * 128], rhs=xsT[:, kc, 0:ncol],
                                                                                     start=(kc == 0), stop=(kc == 7)), r=[wgt[wb_], ("xsT", 0), ("xsT", 1)], w=[pG])
                    for kc in range(8):
                        P.op("pe", lambda e, fc=fc, kc=kc, wb_=wb_, ncol=ncol, pU=pU: e.matmul(out=pU[:, 0:ncol], lhsT=wut[wb_][:, kc, fc * 128:(fc + 1) * 128], rhs=xsT[:, kc, 0:ncol],
                                                                                     start=(kc == 0), stop=(kc == 7)), r=[wut[wb_], ("xsT", 0), ("xsT", 1)], w=[pU])
                    s_ = sg[fc % 2]
                    P.op("act", lambda e, s_=s_, ncol=ncol, pG=pG: e.activation(out=s_[:, 0:ncol], in_=pG[:, 0:ncol], func=AF.Silu), r=[pG], w=[s_])
                    P.op("dve", lambda e, s_=s_, fc=fc, ncol=ncol, pU=pU: e.tensor_tensor(out=hidT[:, fc, 0:ncol], in0=pU[:, 0:ncol], in1=s_[:, 0:ncol], op=ALU.mult),
                         r=[pU, s_], w=[("hidT", fc)])
                hk = [("hidT", fc) for fc in range(8)]
                for ch in range(nch):
                    yb = y[yi % 2]
                    yi += 1
                    for half in range(2):
                        for fc in range(8):
                            P.op("pe", lambda e, half=half, fc=fc, ch=ch, wb_=wb_, npc=npc: e.matmul(out=pY[half][0:npc, :], lhsT=hidT[:, fc, ch * 128:ch * 128 + npc],
                                                                                                 rhs=wdt[wb_][:, fc, half * 512:(half + 1) * 512], start=(fc == 0), stop=(fc == 7)),
                                 r=hk + [wdt[wb_]], w=[pY[half]])
                        P.op("dve", lambda e, half=half, yb=yb, ch=ch, ex=ex, npc=npc, j=j: e.scalar_tensor_tensor(
                            out=yb[0:npc, half * 512:(half + 1) * 512], in0=pY[half][0:npc, :], scalar=gT[0:npc, ch, ex:ex + 1],
                            in1=gt2[j][0:npc, half * 512:(half + 1) * 512], op0=ALU.mult, op1=ALU.mult), r=[pY[half], gT, gt2[j]], w=[(yb.name, half)])
                    P.dma_fn("pool", lambda e, yb=yb, ch=ch, ex=ex, npc=npc, dest=dest: e.indirect_dma_start(
                        out=dest[:, :], out_offset=bass.IndirectOffsetOnAxis(ap=idxT[0:npc, ch, ex:ex + 1], axis=0),
                        in_=yb[0:npc, :], in_offset=None, compute_op=ALU.add),
                        r=[(yb.name, 0), (yb.name, 1), idxT], w=[("dest", j)], sem=("ysc", j))
        P.barrier()
        P.emit()


def phase_zero_mix(g, l):
    nc, S = g.nc, g.S
    with ExitStack() as es:
        z = es.enter_context(nc.sbuf_tensor("z%d_z" % l, [128, NT], BF16))
        P = Prog(nc)
        P.op("pool", lambda e: e.memset(z[:], 0.0), w=[z])
        for r in range(2, 8):
            P.dma("sync", S.mixT[r * 128:(r + 1) * 128, :], z[:], r=[z], sem="zst")
        P.barrier()
        P.emit()


def prep_inputs(inputs):
    f = lambda a: np.ascontiguousarray(np.asarray(a, dtype=np.float32))
    x = f(inputs["x"])
    c = f(inputs["c"])
    ctx = f(inputs["ctx"])
    c_ctx = f(inputs["c_ctx"])
    shared = {
        "ada_w": f(inputs["ada_w"]),
        "ada_b": f(inputs["ada_b"]).reshape(2, 1, 6 * D),
        "norm1_g": f(inputs["norm1_g"]).reshape(2, 1, D),
        "norm2_g": f(inputs["norm2_g"]).reshape(2, 1, D),
        "w_in": f(inputs["w_in"]),
        "w_out": f(inputs["w_out"]),
        "conv_wT": f(np.transpose(f(inputs["conv_w"]), (0, 2, 1))),
        "q_norm_g": f(inputs["q_norm_g"]).reshape(2, 1, 64),
        "k_norm_g": f(inputs["k_norm_g"]).reshape(2, 1, 64),
        "rw_mu": f(inputs["rw_mu"]).reshape(2, 1184, 1),
        "rw_w0": f(inputs["rw_w0"]).reshape(2, 512, 1),
        "rw_w_b": f(inputs["rw_w_b"]).reshape(2, 128, 256),
        "rw_a0": f(inputs["rw_a0"]).reshape(2, 512, 1),
        "rw_a_b": f(inputs["rw_a_b"]).reshape(2, 128, 256),
        "rw_g_b": f(inputs["rw_g_b"]),
        "rw_k_k": f(inputs["rw_k_k"]).reshape(2, 256, 1),
        "rw_k_a": f(inputs["rw_k_a"]).reshape(2, 256, 1),
        "rw_r_k": f(inputs["rw_r_k"]).reshape(2, 256, 1),
        "rw_ln_w": f(inputs["rw_ln_w"]).reshape(2, 256, 1),
        "rw_ln_b": f(inputs["rw_ln_b"]).reshape(2, 256, 1),
        "router_w": f(inputs["router_w"]),
        "exp_w_gate": f(inputs["exp_w_gate"]),
        "exp_w_up": f(inputs["exp_w_up"]),
        "exp_w_down": f(inputs["exp_w_down"]),
        "consts": make_consts(),
    }
    t = np.arange(SEQ)
    row = (t // 64).astype(np.float32)
    col = (t % 64).astype(np.float32)
    inv = (10000.0 ** (-np.arange(0, 32, 2, dtype=np.float32) / 32)).astype(np.float32)
    ang = np.concatenate([row[:, None] * inv, col[:, None] * inv], axis=-1).astype(np.float32)
    shared["cs_tab"] = np.ascontiguousarray(np.concatenate([np.cos(ang), np.sin(ang)], axis=-1).astype(np.float32))
    maps = []
    for b in range(x.shape[0]):
        m = dict(shared)
        m["x"] = x[b]
        m["ctx"] = ctx[b]
        c2 = np.stack([c[b], c_ctx], axis=-1)
        m["c2T"] = np.ascontiguousarray(c2.reshape(8, 128, 2).transpose(1, 0, 2))
        maps.append(m)
    return maps


_NC_CACHE = {}

W_KEYS = ["ada_w", "ada_b", "norm1_g", "norm2_g", "w_in", "w_out", "conv_wT", "q_norm_g", "k_norm_g", "rw_mu", "rw_w0", "rw_w_b",
          "rw_a0", "rw_a_b", "rw_g_b", "rw_k_k", "rw_k_a", "rw_r_k", "rw_ln_w", "rw_ln_b", "router_w",
          "exp_w_gate", "exp_w_up", "exp_w_down"]


def kernel(**inputs):
    maps = prep_inputs(inputs)
    if "nc" not in _NC_CACHE:
        _NC_CACHE["nc"] = build(layers=[0, 1])
    nc = _NC_CACHE["nc"]
    res = run_bass_kernel_spmd(nc, maps, core_ids=list(range(8)))
    return np.stack([np.asarray(r["out"], dtype=np.float32) for r in res.results], axis=0)
```

```python
import math
from contextlib import ExitStack

import numpy as np
import concourse.bass as bass
import concourse.mybir as mybir
from concourse.bass_utils import run_bass_kernel_spmd

F32 = mybir.dt.float32
BF16 = mybir.dt.bfloat16
I32 = mybir.dt.int32
F32R = mybir.dt.float32r
U32 = mybir.dt.uint32
AF = mybir.ActivationFunctionType
ALU = mybir.AluOpType
AX = mybir.AxisListType

ENGS = ("sync", "act", "dve", "pool", "pe")

D = 1024
SEQ = 4096
CTX = 256
NT = SEQ + CTX
NTILE = NT // 128
PROJ = 2720
NE = 16
CAP_L = 512
CAP_C = 32
LCH = 64


class Prog:
    SEMID = 0

    def __init__(self, nc):
        self.nc = nc
        self.streams = {e: [] for e in ENGS}
        self.ecount = {e: 0 for e in ENGS}
        self.seen = {e: {} for e in ENGS}
        self.bufs = {}
        self.dcount = {}
        self.sems = {}

    @staticmethod
    def _k(b):
        if isinstance(b, (str, tuple, int)):
            return b
        return b.name

    def _deps(self, eng, reads, writes):
        need = {}

        def add(ev):
            for k, v in ev.items():
                if need.get(k, 0) < v:
                    need[k] = v

        for b in reads:
            st = self.bufs.get(b)
            if st:
                add(st["w"])
        for b in writes:
            st = self.bufs.get(b)
            if st:
                add(st["w"])
                add(st["r"])
        waits = []
        seen = self.seen[eng]
        for k, v in need.items():
            if k[0] == "e" and k[1] == eng and eng == "pe":
                continue
            if seen.get(k, 0) >= v:
                continue
            seen[k] = v
            waits.append((k, v))
        return waits

    def _commit(self, reads, writes, ev):
        for b in reads:
            st = self.bufs.setdefault(b, {"w": {}, "r": {}})
            for k, v in ev.items():
                if st["r"].get(k, 0) < v:
                    st["r"][k] = v
        for b in writes:
            self.bufs[b] = {"w": dict(ev), "r": {}}

    EPOCH = 3000

    def op(self, eng, fn, r=(), w=(), rows=None):
        r = [self._k(b) for b in r]
        w = [self._k(b) for b in w]
        bk = [k for k in r if isinstance(k, tuple) and k[0] in ("pqb", "sqb")]
        if bk:
            r = [k for k in r if k not in bk]
            w = w + bk
        waits = self._deps(eng, r, w)
        self.ecount[eng] += 1
        ep = (self.ecount[eng] - 1) // self.EPOCH
        ek = ("e", eng, ep)
        ev = {ek: self.ecount[eng] - ep * self.EPOCH}
        if eng == "pe":
            if not hasattr(self, "pe_rows"):
                self.pe_rows = {}
            for bk_ in w:
                last = self.pe_rows.get(bk_)
                if last is not None and rows in (0, 64) and last[0] in (0, 64) and last[0] != rows:
                    for k_, v_ in last[1].items():
                        if self.seen[eng].get(k_, 0) < v_:
                            self.seen[eng][k_] = v_
                            waits.append((k_, v_))
                self.pe_rows[bk_] = (rows, ev)
        self.streams[eng].append((waits, fn, (ek, 1)))
        self._commit(r, w, ev)

    def dma(self, q, out, in_, r=(), w=(), sem=None, **kw):
        r = [self._k(b) for b in r]
        w = [self._k(b) for b in w]
        if sem is None:
            sem = (w[0] if w else r[0])
        waits = self._deps(q, r, w)
        k = ("d", sem)
        self.dcount[k] = self.dcount.get(k, 0) + 16
        ev = {k: self.dcount[k]}
        self.streams[q].append((waits, (lambda e: e.dma_start(out=out, in_=in_, **kw)), (k, 16)))
        self._commit(r, w, ev)

    def dma_fn(self, q, fn, r=(), w=(), sem=None):
        r = [self._k(b) for b in r]
        w = [self._k(b) for b in w]
        waits = self._deps(q, r, w)
        k = ("d", sem)
        self.dcount[k] = self.dcount.get(k, 0) + 16
        ev = {k: self.dcount[k]}
        self.streams[q].append((waits, fn, (k, 16)))
        self._commit(r, w, ev)

    def barrier(self):
        for eng in ENGS:
            waits = [(k, v) for k, v in self.dcount.items()]
            for e in ENGS:
                if e != eng and self.ecount[e] > 0:
                    ep = (self.ecount[e] - 1) // self.EPOCH
                    waits.append((("e", e, ep), self.ecount[e] - ep * self.EPOCH))
            self.streams[eng].append((waits, None, None))

    POOL = None

    def emit(self):
        nc = self.nc
        pool = Prog.POOL
        totals = {}
        keys = []
        for e in ENGS:
            for waits, fn, inc in self.streams[e]:
                for k, v in waits:
                    if k not in totals:
                        totals[k] = 0
                        keys.append(k)
                if inc:
                    if inc[0] not in totals:
                        totals[inc[0]] = 0
                        keys.append(inc[0])
                    totals[inc[0]] += inc[1]
        n = len(pool["h"])
        assert len(keys) <= n, len(keys)
        base = {}
        for i, k in enumerate(sorted(keys, key=str)):
            idx = (pool["next"] + i) % n
            self.sems[k] = pool["h"][idx]
            base[k] = pool["v"][idx]
            pool["v"][idx] += totals[k]
        pool["next"] = (pool["next"] + len(keys)) % n
        with ExitStack() as es:
            block = es.enter_context(nc.Block())
            handles = {"sync": block.sync, "act": block.scalar, "dve": block.vector,
                       "pool": block.gpsimd, "pe": block.tensor}
            for e in ENGS:
                stream = self.streams[e]

                def body(h, stream=stream):
                    for waits, fn, inc in stream:
                        for k, v in waits:
                            h.wait_ge(self.sems[k], base[k] + v)
                        if fn is not None:
                            ins = fn(h)
                            ins.then_inc(self.sems[inc[0]], inc[1])

                handles[e](body)


class Ctx:
    pass


def build(n_layers=2, dbg=None, upto=None, skip=(), small=False, rwsteps=None, zero_mix=False, layers=None):
    dbg = dbg or set()
    nc = bass.Bass("TRN2", target_bir_lowering=False)
    g = Ctx()
    g.nc = nc
    if layers is None:
        layers = list(range(n_layers))
    NL = len(layers)
    g.first = layers[0]
    g.last = layers[-1]
    g.wi = lambda l: l - layers[0]
    if layers[-1] == 0:
        dbg = set(dbg) | {"xcres"}

    def din(name, shape, dt=F32):
        return nc.dram_tensor(name, list(shape), dt, kind="ExternalInput").ap()

    def dscr(name, shape, dt=F32):
        kind = "ExternalOutput" if name in dbg else "Internal"
        return nc.dram_tensor(name, list(shape), dt, kind=kind).ap()

    I = Ctx()
    I.x = din("x", [SEQ, D])
    I.ctx = din("ctx", [CTX, D])
    I.c2T = din("c2T", [128, 8, 2])
    I.ada_w = din("ada_w", [NL, D, 6 * D])
    I.ada_b = din("ada_b", [NL, 1, 6 * D])
    I.n1g = din("norm1_g", [NL, 1, D])
    I.n2g = din("norm2_g", [NL, 1, D])
    I.w_in = din("w_in", [NL, D, PROJ])
    I.w_out = din("w_out", [NL, D, D])
    I.conv_wT = din("conv_wT", [NL, 256, 3])
    I.qg = din("q_norm_g", [NL, 1, 64])
    I.kg = din("k_norm_g", [NL, 1, 64])
    I.mu = din("rw_mu", [NL, 1184, 1])
    I.w0 = din("rw_w0", [NL, 512, 1])
    I.w_b = din("rw_w_b", [NL, 128, 256])
    I.a0 = din("rw_a0", [NL, 512, 1])
    I.a_b = din("rw_a_b", [NL, 128, 256])
    I.g_b = din("rw_g_b", [NL, 160, 256])
    I.k_k = din("rw_k_k", [NL, 256, 1])
    I.k_a = din("rw_k_a", [NL, 256, 1])
    I.r_k = din("rw_r_k", [NL, 256, 1])
    I.ln_w = din("rw_ln_w", [NL, 256, 1])
    I.ln_b = din("rw_ln_b", [NL, 256, 1])
    I.router = din("router_w", [NL, D, NE])
    esh = [NL, 1, 8, 8] if small else [NL, NE, D, D]
    I.wg = din("exp_w_gate", esh)
    I.wu = din("exp_w_up", esh)
    I.wd = din("exp_w_down", esh)
    g.rwsteps = rwsteps
    I.cs = din("cs_tab", [SEQ, 64])
    I.consts = din("consts", [128, 8 * 128])
    out = nc.dram_tensor("out", [SEQ, D], F32, kind="ExternalOutput").ap()

    S = Ctx()
    S.modv = dscr("modv", [NL, 2, 6, D])
    S.pfm = dscr("pfm", [1952, NT])
    S.qT = dscr("qT", [8, 64, NT], BF16)
    S.mixT = dscr("mixT", [D, NT], BF16)
    S.xres = dscr("xres", [SEQ, D])
    S.xcres = dscr("xcres", [CTX, D])
    S.h2l = dscr("h2l", [SEQ, D], BF16)
    S.h2c = dscr("h2c", [CTX, D], BF16)
    S.rwf = dscr("rwf", [10, 256, NT])
    g.I, g.S, g.out = I, S, out

    with ExitStack() as gs:
        def gsb(name, shape, dt=F32):
            return gs.enter_context(nc.sbuf_tensor("g_" + name, list(shape), dt))
        Prog.POOL = {"h": [gs.enter_context(nc.semaphore("gp%d" % i)) for i in range(72)], "v": [0] * 72, "next": 0}
        g.consts = gsb("consts", [128, 8, 128])
        g.identb = gsb("identb", [128, 128], BF16)
        g.kT = gsb("kT", [128, 2, NT], BF16)
        g.Vaug = gsb("Vaug", [128, NTILE, 2, 65], BF16)
        g.affT = gsb("affT", [NE, NT])
        phase_consts(g)
        phases = [phase_adaln, phase_proj, phase_attn, phase_rwfeat, phase_rwscan] + ([phase_zero_mix] if zero_mix else []) + [phase_wout, phase_moe]
        for l in layers:
            for ph in phases:
                if ph.__name__ in skip:
                    continue
                ph(g, l)
                if upto == (ph.__name__, l):
                    return nc
    return nc


C_ID, C_BONES, C_MS_IT, C_MI_IT, C_MS_TI, C_RESET, C_MS_IT_B, C_MI_IT_B = range(8)


def make_consts():
    c = np.zeros((8, 128, 128), np.float32)
    i = np.arange(128)[:, None]
    t = np.arange(128)[None, :]
    same = (i // 64) == (t // 64)
    c[C_ID] = np.eye(128)
    c[C_BONES] = same
    c[C_MS_IT] = same & (i < t)
    c[C_MI_IT] = same & (i <= t)
    c[C_MS_TI] = same & (t < i)
    c[C_RESET] = (t % 64 != 0) * np.ones((128, 1))
    c[C_MS_IT_B] = same & (i > t)
    c[C_MI_IT_B] = same & (i >= t)
    return np.ascontiguousarray(c.transpose(1, 0, 2).reshape(128, 8 * 128))


def phase_consts(g):
    nc = g.nc
    P = Prog(nc)
    P.dma("sync", g.consts[:], g.I.consts.rearrange("p (a b) -> p a b", a=8), w=[g.consts])
    P.op("dve", lambda e: e.tensor_copy(out=g.identb[:], in_=g.consts[:, C_ID, :]), r=[g.consts], w=[g.identb])
    P.op("pool", lambda e: e.memset(g.Vaug[:, :, :, 64:65], 1.0), w=[g.Vaug])
    P.barrier()
    P.emit()


def phase_adaln(g, l):
    nc, I, S = g.nc, g.I, g.S
    with ExitStack() as es:
        def sb(name, shape, dt=F32):
            return es.enter_context(nc.sbuf_tensor("a%d_" % l + name, list(shape), dt))
        c2 = sb("c2", [128, 8, 2])
        sc = sb("sc", [128, 8, 2])
        wt = [sb("wt%d" % i, [128, 8, 512]) for i in range(2)]
        bias = sb("bias", [2, 6 * D])
        mod = sb("mod", [2, 6 * D])
        gg = sb("gg", [2, 2, D])
        mv = sb("mv", [2, 6, D])
        ps = [es.enter_context(nc.psum_tensor("a%d_ps%d" % (l, i), [2, 512], F32)) for i in range(2)]
        P = Prog(nc)
        P.dma("sync", c2[:], I.c2T[:, :, :], w=[c2])
        P.dma("sync", bias[:], I.ada_b[g.wi(l), 0:1, :].to_broadcast([2, 6 * D]), w=[bias])
        P.dma("sync", gg[:, 0, :], I.n1g[g.wi(l), 0:1, :].to_broadcast([2, D]), w=[gg], sem="gg")
        P.dma("sync", gg[:, 1, :], I.n2g[g.wi(l), 0:1, :].to_broadcast([2, D]), w=[gg], sem="gg")
        P.op("act", lambda e: e.activation(out=sc[:], in_=c2[:], func=AF.Silu), r=[c2], w=[sc])
        wv = I.ada_w[g.wi(l)].rearrange("(kc p) n -> p kc n", p=128)
        for cc in range(12):
            b = cc % 2
            P.dma("sync" if b == 0 else "pool", wt[b][:], wv[:, :, cc * 512:(cc + 1) * 512], w=[wt[b]])
            for kc in range(8):
                P.op("pe", lambda e, kc=kc, b=b: e.matmul(out=ps[b][:], lhsT=sc[:, kc, :], rhs=wt[b][:, kc, :],
                                                           start=(kc == 0), stop=(kc == 7)),
                     r=[sc, wt[b]], w=[ps[b]])
            P.op("dve", lambda e, cc=cc, b=b: e.tensor_tensor(out=mod[:, cc * 512:(cc + 1) * 512], in0=ps[b][:],
                                                               in1=bias[:, cc * 512:(cc + 1) * 512], op=ALU.add),
                 r=[ps[b], bias], w=[mod])
        for j, (sci, shi, gti) in enumerate(((1, 0, 2), (4, 3, 5))):
            P.op("dve", lambda e, j=j, sci=sci: e.scalar_tensor_tensor(
                out=mv[:, 3 * j, :], in0=mod[:, sci * D:(sci + 1) * D], scalar=1.0, in1=gg[:, j, :],
                op0=ALU.add, op1=ALU.mult), r=[mod, gg], w=[mv])
            P.op("dve", lambda e, j=j, shi=shi: e.tensor_copy(out=mv[:, 3 * j + 1, :], in_=mod[:, shi * D:(shi + 1) * D]),
                 r=[mod], w=[mv])
            P.op("dve", lambda e, j=j, gti=gti: e.tensor_copy(out=mv[:, 3 * j + 2, :], in_=mod[:, gti * D:(gti + 1) * D]),
                 r=[mod], w=[mv])
        P.dma("sync", S.modv[g.wi(l)], mv[:], r=[mv], sem="mvst")
        P.barrier()
        P.emit()


def phase_proj(g, l):
    nc, I, S = g.nc, g.I, g.S
    with ExitStack() as es:
        def sb(name, shape, dt=F32):
            return es.enter_context(nc.sbuf_tensor("b%d_" % l + name, list(shape), dt))

        def psb(name, shape, dt=F32):
            return es.enter_context(nc.psum_tensor("b%d_" % l + name, list(shape), dt))
        wb = sb("wb", [128, 8, PROJ], BF16)
        m1 = [sb("m1_%d" % i, [128, D]) for i in range(2)]
        sh1 = [sb("sh1_%d" % i, [128, D]) for i in range(2)]
        qkg = sb("qkg", [128, 2, 64])
        cs = sb("cs", [128, 32, 64])
        xt = [sb("xt%d" % i, [128, D]) for i in range(2)]
        junk = sb("junk", [128, D])
        ss = [sb("ss%d" % i, [128, 1]) for i in range(2)]
        hb = [sb("hb%d" % i, [128, D], BF16) for i in range(2)]
        hT = [sb("hT%d" % i, [128, 8, 512], BF16) for i in range(2)]
        fm = [sb("fm%d" % i, [128, 512]) for i in range(3)]
        qkv = [sb("qkv%d" % i, [128, 768]) for i in range(2)]
        sq = sb("sq", [128, 640])
        ssq = sb("ssq", [128, 10])
        qn = sb("qn", [128, 10, 64])
        qr = sb("qr", [128, 10, 64])
        qrb = [sb("qrb%d" % i, [128, 10, 64], BF16) for i in range(2)]
        tmp = sb("tmp", [128, 10, 32])
        qTs = [sb("qTs%d" % i, [64, 8, 128], BF16) for i in range(2)]
        pT = [psb("pT%d" % i, [128, 8, 128], BF16) for i in range(2)]
        pF = [psb("pF%d" % i, [128, 512]) for i in range(2)]
        pA = psb("pA", [128, 512])
        pB = psb("pB", [128, 512])
        pQ = psb("pQ", [64, 8, 128], BF16)
        pQk = psb("pQk", [64, 8, 128], BF16)
        P = Prog(nc)
        wv = I.w_in[g.wi(l)].rearrange("(kc p) n -> p kc n", p=128)
        for (c0, c1) in ((0, 1024), (1024, 2048), (2048, PROJ)):
            P.dma("pool", wb[:, :, c0:c1], wv[:, :, c0:c1], w=[("wb", c0)], sem=("wb", c0))
        wbk = [("wb", 0), ("wb", 1024), ("wb", 2048)]
        for j in range(2):
            P.dma("sync", m1[j][:], S.modv[g.wi(l), j, 0:1, :].to_broadcast([128, D]), w=[m1[j]])
            P.dma("sync", sh1[j][:], S.modv[g.wi(l), j, 1:2, :].to_broadcast([128, D]), w=[sh1[j]])
        P.dma("sync", qkg[:, 0, :], I.qg[g.wi(l), 0:1, :].to_broadcast([128, 64]), w=[qkg], sem="qkg")
        P.dma("sync", qkg[:, 1, :], I.kg[g.wi(l), 0:1, :].to_broadcast([128, 64]), w=[qkg], sem="qkg")
        P.dma("sync", cs[:], I.cs.rearrange("(i p) c -> p i c", p=128), w=[cs])
        fchunks = [(c, 128, c) for c in range(0, 768, 128)]
        for j in range(10):
            c = 1536 + j * 128
            wdt = min(128, PROJ - c)
            fchunks.append((c, wdt, 768 + j * 128))
        sts = [(0, 2)] + [(2 + 4 * s, 4) for s in range(8)]
        fmi = 0
        for si, (t0, ntile) in enumerate(sts):
            hTs = hT[si % 2]
            ntok = ntile * 128
            for ti in range(ntile):
                i = t0 + ti
                b = i % 2
                j = 1 if i < 2 else 0
                if i < 2:
                    src = (I.ctx if l == g.first else S.xcres)[i * 128:(i + 1) * 128, :]
                else:
                    src = (I.x if l == g.first else S.xres)[(i - 2) * 128:(i - 1) * 128, :]
                P.dma("sync", xt[b][:], src, w=[xt[b]])
                P.op("act", lambda e, b=b: e.activation(out=junk[:], in_=xt[b][:], func=AF.Square, accum_out=ss[b][:]),
                     r=[xt[b]], w=[junk, ss[b]])
                P.op("dve", lambda e, b=b: e.tensor_scalar(out=ss[b][:], in0=ss[b][:], scalar1=1.0 / D, scalar2=1e-6,
                                                            op0=ALU.mult, op1=ALU.add), r=[ss[b]], w=[ss[b]])
                P.op("act", lambda e, b=b: e.activation(out=ss[b][:], in_=ss[b][:], func=AF.Sqrt), r=[ss[b]], w=[ss[b]])
                P.op("dve", lambda e, b=b: e.reciprocal(out=ss[b][:], in_=ss[b][:]), r=[ss[b]], w=[ss[b]])
                P.op("dve", lambda e, b=b, j=j: e.scalar_tensor_tensor(out=xt[b][:], in0=xt[b][:], scalar=ss[b][:, 0:1],
                                                                       in1=m1[j][:], op0=ALU.mult, op1=ALU.mult),
                     r=[xt[b], ss[b], m1[j]], w=[xt[b]])
                P.op("pool", lambda e, b=b, j=j: e.tensor_tensor(out=hb[b][:], in0=xt[b][:], in1=sh1[j][:], op=ALU.add),
                     r=[xt[b], sh1[j]], w=[hb[b]])
                for half in range(2):
                    for k4 in range(4):
                        kc = half * 4 + k4
                        P.op("pe", lambda e, b=b, kc=kc, k4=k4, half=half: e.transpose(
                            out=pT[half][:, k4, :], in_=hb[b][:, kc * 128:(kc + 1) * 128], identity=g.identb[:]),
                            r=[hb[b], g.identb], w=[pT[half]])
                    eng = "act" if half == 0 else "dve"
                    if eng == "act":
                        P.op("act", lambda e, half=half, ti=ti, hTs=hTs: e.activation(
                            out=hTs[:, half * 4:(half + 1) * 4, ti * 128:(ti + 1) * 128], in_=pT[half][:, 0:4, :], func=AF.Copy),
                            r=[pT[half]], w=[hTs])
                    else:
                        P.op("dve", lambda e, half=half, ti=ti, hTs=hTs: e.tensor_copy(
                            out=hTs[:, half * 4:(half + 1) * 4, ti * 128:(ti + 1) * 128], in_=pT[half][:, 0:4, :]),
                            r=[pT[half]], w=[hTs])
                for kc in range(8):
                    P.op("pe", lambda e, kc=kc, ti=ti, hTs=hTs: e.matmul(
                        out=pA[:], lhsT=hTs[:, kc, ti * 128:(ti + 1) * 128], rhs=wb[:, kc, 768:1280],
                        start=(kc == 0), stop=(kc == 7)), r=[hTs] + wbk, w=[pA])
                for kc in range(8):
                    P.op("pe", lambda e, kc=kc, ti=ti, hTs=hTs: e.matmul(
                        out=pB[:, 0:256], lhsT=hTs[:, kc, ti * 128:(ti + 1) * 128], rhs=wb[:, kc, 1280:1536],
                        start=(kc == 0), stop=(kc == 7)), r=[hTs] + wbk, w=[pB])
                qv = qkv[b]
                P.op("act", lambda e, qv=qv: e.activation(out=qv[:, 0:512], in_=pA[:], func=AF.Copy), r=[pA], w=[qv])
                P.op("act", lambda e, qv=qv: e.activation(out=qv[:, 512:768], in_=pB[:, 0:256], func=AF.Copy), r=[pB], w=[qv])
                P.op("pool", lambda e, qv=qv, i=i: e.tensor_copy(
                    out=g.Vaug[:, i, :, 0:64], in_=qv[:, 640:768].rearrange("p (g d) -> p g d", g=2)),
                    r=[qv], w=[("Vaug", i)])
                P.op("dve", lambda e, qv=qv: e.tensor_tensor(out=sq[:], in0=qv[:, 0:640], in1=qv[:, 0:640], op=ALU.mult),
                     r=[qv], w=[sq])
                P.op("dve", lambda e: e.tensor_reduce(out=ssq[:], in_=sq[:].rearrange("p (h d) -> p h d", h=10),
                                                       axis=AX.X, op=ALU.add), r=[sq], w=[ssq])
                P.op("dve", lambda e: e.tensor_scalar(out=ssq[:], in0=ssq[:], scalar1=1.0 / 64, scalar2=1e-6,
                                                       op0=ALU.mult, op1=ALU.add), r=[ssq], w=[ssq])
                P.op("act", lambda e: e.activation(out=ssq[:], in_=ssq[:], func=AF.Sqrt), r=[ssq], w=[ssq])
                P.op("dve", lambda e: e.reciprocal(out=ssq[:], in_=ssq[:]), r=[ssq], w=[ssq])
                P.op("dve", lambda e, qv=qv: e.tensor_tensor(
                    out=qn[:], in0=qv[:, 0:640].rearrange("p (h d) -> p h d", h=10),
                    in1=ssq[:].unsqueeze(2).to_broadcast([128, 10, 64]), op=ALU.mult), r=[qv, ssq], w=[qn])
                P.op("dve", lambda e: e.tensor_tensor(out=qn[:, 0:8, :], in0=qn[:, 0:8, :],
                                                       in1=qkg[:, 0:1, :].to_broadcast([128, 8, 64]), op=ALU.mult),
                     r=[qn, qkg], w=[qn])
                P.op("dve", lambda e: e.tensor_tensor(out=qn[:, 8:10, :], in0=qn[:, 8:10, :],
                                                       in1=qkg[:, 1:2, :].to_broadcast([128, 2, 64]), op=ALU.mult),
                     r=[qn, qkg], w=[qn])
                qb = qrb[b]
                if i < 2:
                    P.op("dve", lambda e, qb=qb: e.tensor_copy(out=qb[:], in_=qn[:]), r=[qn], w=[qb])
                else:
                    li = i - 2
                    cosb = cs[:, li:li + 1, 0:32].to_broadcast([128, 10, 32])
                    sinb = cs[:, li:li + 1, 32:64].to_broadcast([128, 10, 32])
                    x1 = qn[:, :, 0:32]
                    x2 = qn[:, :, 32:64]
                    P.op("dve", lambda e, cosb=cosb: e.tensor_tensor(out=qr[:, :, 0:32], in0=qn[:, :, 0:32], in1=cosb, op=ALU.mult),
                         r=[qn, cs], w=[qr])
                    P.op("dve", lambda e, sinb=sinb: e.tensor_tensor(out=tmp[:], in0=qn[:, :, 32:64], in1=sinb, op=ALU.mult),
                         r=[qn, cs], w=[tmp])
                    P.op("dve", lambda e, qb=qb: e.tensor_tensor(out=qb[:, :, 0:32], in0=qr[:, :, 0:32], in1=tmp[:], op=ALU.subtract),
                         r=[qr, tmp], w=[qb])
                    P.op("dve", lambda e, sinb=sinb: e.tensor_tensor(out=qr[:, :, 32:64], in0=qn[:, :, 0:32], in1=sinb, op=ALU.mult),
                         r=[qn, cs], w=[qr])
                    P.op("dve", lambda e, cosb=cosb: e.tensor_tensor(out=tmp[:], in0=qn[:, :, 32:64], in1=cosb, op=ALU.mult),
                         r=[qn, cs, qb], w=[tmp])
                    P.op("dve", lambda e, qb=qb: e.tensor_tensor(out=qb[:, :, 32:64], in0=qr[:, :, 32:64], in1=tmp[:], op=ALU.add),
                         r=[qr, tmp], w=[qb])
                for h in range(8):
                    P.op("pe", lambda e, h=h, qb=qb: e.transpose(out=pQ[:, h, :], in_=qb[:, h, :], identity=g.identb[:]),
                         r=[qb, g.identb], w=[pQ])
                for h in range(2):
                    P.op("pe", lambda e, h=h, qb=qb: e.transpose(out=pQk[:, h, :], in_=qb[:, 8 + h, :], identity=g.identb[:]),
                         r=[qb, g.identb], w=[pQk])
                qs = qTs[b]
                P.op("act", lambda e, qs=qs: e.activation(out=qs[:], in_=pQ[:], func=AF.Copy), r=[pQ], w=[qs])
                P.op("dve", lambda e, i=i: e.tensor_copy(out=g.kT[0:64, :, i * 128:(i + 1) * 128], in_=pQk[:, 0:2, :]),
                     r=[pQk], w=[("kT", i)])
                P.op("dve", lambda e, i=i: e.tensor_copy(out=g.kT[64:128, :, i * 128:(i + 1) * 128], in_=pQk[:, 0:2, :]),
                     r=[pQk], w=[("kT", i)])
                P.dma("pool", S.qT[:, :, i * 128:(i + 1) * 128].rearrange("h d t -> d h t"), qs[:], r=[qs], sem=("qs", b))
            for (c0, wdt, r0) in fchunks:
                pb = pF[fmi % 2]
                fb = fm[fmi % 3]
                for kc in range(8):
                    P.op("pe", lambda e, kc=kc, c0=c0, wdt=wdt, pb=pb, hTs=hTs, ntok=ntok: e.matmul(
                        out=pb[0:wdt, 0:ntok], lhsT=wb[:, kc, c0:c0 + wdt], rhs=hTs[:, kc, 0:ntok],
                        start=(kc == 0), stop=(kc == 7)), r=[hTs] + wbk, w=[pb])
                if fmi % 2 == 0:
                    P.op("act", lambda e, wdt=wdt, pb=pb, fb=fb, ntok=ntok: e.activation(
                        out=fb[0:wdt, 0:ntok], in_=pb[0:wdt, 0:ntok], func=AF.Copy), r=[pb], w=[fb])
                else:
                    P.op("dve", lambda e, wdt=wdt, pb=pb, fb=fb, ntok=ntok: e.tensor_copy(
                        out=fb[0:wdt, 0:ntok], in_=pb[0:wdt, 0:ntok]), r=[pb], w=[fb])
                P.dma("sync", S.pfm[r0:r0 + wdt, t0 * 128:t0 * 128 + ntok], fb[0:wdt, 0:ntok], r=[fb], sem=("fm", fmi % 3))
                fmi += 1
        P.barrier()
        P.emit()


def conv_ops(g, l, P, es):
    nc, I, S = g.nc, g.I, g.S
    todo = []
    if True:
        def sb(name, shape, dt=F32):
            return es.enter_context(nc.sbuf_tensor("c%d_" % l + name, list(shape), dt))
        Bt = sb("Bt", [128, SEQ])
        Ct = sb("Ct", [128, SEQ])
        Ut = sb("Ut", [128, SEQ])
        zp = sb("zp", [128, SEQ + 2])
        acc = sb("acc", [128, SEQ])
        ob = sb("ob", [128, SEQ], BF16)
        cw = sb("cw", [128, 2, 3])
        todo.append(lambda: P.dma("sync", cw[:], I.conv_wT[g.wi(l)].rearrange("(c p) k -> p c k", p=128), w=[cw]))
        seqs = [(CTX, SEQ)] + ([(0, CTX)] if l == 0 else [])
        for (t0, T) in seqs:
            for cc in range(2):
                todo.append(lambda T=T, cc=cc, t0=t0: P.dma("sync", Bt[:, 0:T], S.pfm[cc * 128:(cc + 1) * 128, t0:t0 + T], w=[Bt]))
                todo.append(lambda T=T, cc=cc, t0=t0: P.dma("sync", Ct[:, 0:T], S.pfm[256 + cc * 128:256 + (cc + 1) * 128, t0:t0 + T], w=[Ct]))
                todo.append(lambda T=T, cc=cc, t0=t0: P.dma("pool", Ut[:, 0:T], S.pfm[512 + cc * 128:512 + (cc + 1) * 128, t0:t0 + T], w=[Ut]))
                todo.append(lambda T=T, cc=cc, t0=t0: P.op("pool", lambda e, T=T: e.memset(zp[:, 0:1], 0.0), w=[zp]))
                todo.append(lambda T=T, cc=cc, t0=t0: P.op("pool", lambda e, T=T: e.memset(zp[:, T + 1:T + 2], 0.0), w=[zp]))
                todo.append(lambda T=T, cc=cc, t0=t0: P.op("dve", lambda e, T=T: e.tensor_tensor(out=zp[:, 1:T + 1], in0=Ct[:, 0:T], in1=Ut[:, 0:T], op=ALU.mult),
                     r=[Ct, Ut], w=[zp]))
                todo.append(lambda T=T, cc=cc, t0=t0: P.op("dve", lambda e, T=T, cc=cc: e.tensor_scalar(out=acc[:, 0:T], in0=zp[:, 0:T], scalar1=cw[:, cc, 0:1],
                                                                   scalar2=None, op0=ALU.mult), r=[zp, cw], w=[acc]))
                todo.append(lambda T=T, cc=cc, t0=t0: P.op("dve", lambda e, T=T, cc=cc: e.scalar_tensor_tensor(out=acc[:, 0:T], in0=zp[:, 1:T + 1], scalar=cw[:, cc, 1:2],
                                                                         in1=acc[:, 0:T], op0=ALU.mult, op1=ALU.add),
                     r=[zp, cw, acc], w=[acc]))
                todo.append(lambda T=T, cc=cc, t0=t0: P.op("dve", lambda e, T=T, cc=cc: e.scalar_tensor_tensor(out=acc[:, 0:T], in0=zp[:, 2:T + 2], scalar=cw[:, cc, 2:3],
                                                                         in1=acc[:, 0:T], op0=ALU.mult, op1=ALU.add),
                     r=[zp, cw, acc], w=[acc]))
                todo.append(lambda T=T, cc=cc, t0=t0: P.op("pool", lambda e, T=T: e.tensor_tensor(out=ob[:, 0:T], in0=acc[:, 0:T], in1=Bt[:, 0:T], op=ALU.mult),
                     r=[acc, Bt], w=[ob]))
                todo.append(lambda T=T, cc=cc, t0=t0: P.dma("sync", S.mixT[cc * 128:(cc + 1) * 128, t0:t0 + T], ob[:, 0:T], r=[ob], sem="obst"))
    return todo


def phase_attn(g, l):
    nc, I, S = g.nc, g.I, g.S
    with ExitStack() as es:
        def sb(name, shape, dt=F32):
            return es.enter_context(nc.sbuf_tensor("d%d_" % l + name, list(shape), dt))

        def psb(name, shape, dt=F32):
            return es.enter_context(nc.psum_tensor("d%d_" % l + name, list(shape), dt))
        qc = [sb("qc%d" % i, [128, 512], BF16) for i in range(2)]
        eS = [sb("eS%d" % i, [128, 512], BF16) for i in range(4)]
        rs = sb("rs", [128, 512])
        rsb = sb("rsb", [64, 512])
        ob = [sb("ob%d" % i, [64, 512], BF16) for i in range(2)]
        nb = sb("nb", [128, 1])
        pS = [psb("pS%d" % i, [128, 512]) for i in range(4)]
        pO = [psb("pO%d" % i, [128, 512]) for i in range(2)]
        pR = psb("pR", [64, 512])
        P = Prog(nc)
        P.op("pool", lambda e: e.memset(nb[:], -8.0), w=[nb])
        todo = conv_ops(g, l, P, es)
        jobs = []
        if l == 0:
            for h in range(8):
                jobs.append((h, 0, CTX, [0, 1]))
        for h in range(8):
            for qi in range(8):
                jobs.append((h, CTX + qi * 512, 512, list(range(NTILE))))
        cnt = 0
        for ji, (h, q0, nq, kts) in enumerate(jobs):
            gkv = h // 4
            qb = qc[ji % 2]
            po = pO[ji % 2]
            P.dma("sync", qb[0:64, 0:nq], S.qT[h, :, q0:q0 + nq], w=[qb], sem=("qb", ji % 2))
            P.dma("sync", qb[64:128, 0:nq], S.qT[h, :, q0:q0 + nq], w=[qb], sem=("qb", ji % 2))
            nk = len(kts)
            LOOK = 3
            slots = {}

            def emit_s(ki, cnt0=cnt, kts=kts, qb=qb, nq=nq, gkv=gkv):
                kt = kts[ki]
                ps = pS[(cnt0 + ki) % 4]
                ee = eS[(cnt0 + ki) % 4]
                rb = 64 * (ki % 2)
                P.op("pe", lambda e, ps=ps, kt=kt, rb=rb: e.matmul(
                    out=ps[:, 0:nq], lhsT=g.kT[rb:rb + 64, gkv, kt * 128:(kt + 1) * 128], rhs=qb[rb:rb + 64, 0:nq], start=True, stop=True),
                    r=[qb, ("kT", kt)], w=[ps], rows=rb)
                P.op("act", lambda e, ps=ps, ee=ee: e.activation(out=ee[:, 0:nq], in_=ps[:, 0:nq], func=AF.Exp,
                                                                 bias=nb[:, 0:1], scale=0.125),
                     r=[ps, nb], w=[ee])

            def emit_pv(ki, cnt0=cnt, kts=kts, po=po, nq=nq, gkv=gkv, nk=nk):
                kt = kts[ki]
                ee = eS[(cnt0 + ki) % 4]
                P.op("pe", lambda e, kt=kt, ee=ee: e.matmul(
                    out=po[0:65, 0:nq], lhsT=g.Vaug[:, kt, gkv, :], rhs=ee[:, 0:nq], start=(ki == 0), stop=(ki == nk - 1)),
                    r=[ee, ("Vaug", kt), g.Vaug], w=[po])

            for ki in range(nk + LOOK):
                if ki < nk:
                    emit_s(ki)
                if ki - LOOK >= 0:
                    emit_pv(ki - LOOK)
            cnt += nk
            P.op("dve", lambda e, po=po, nq=nq: e.reciprocal(out=rs[64:65, 0:nq], in_=po[64:65, 0:nq]), r=[po], w=[rs])
            P.op("pe", lambda e, nq=nq: e.matmul(out=pR[:, 0:nq], lhsT=g.consts[64:65, C_BONES, 64:128], rhs=rs[64:65, 0:nq],
                                                  start=True, stop=True), r=[rs, g.consts], w=[pR])
            P.op("act", lambda e, nq=nq: e.activation(out=rsb[:, 0:nq], in_=pR[:, 0:nq], func=AF.Copy), r=[pR], w=[rsb])
            o = ob[ji % 2]
            P.op("dve", lambda e, po=po, o=o, nq=nq: e.tensor_tensor(out=o[:, 0:nq], in0=po[0:64, 0:nq], in1=rsb[:, 0:nq], op=ALU.mult),
                 r=[po, rsb], w=[o])
            P.dma("pool", S.mixT[256 + h * 64:256 + (h + 1) * 64, q0:q0 + nq], o[:, 0:nq], r=[o], sem=("ob", ji % 2))
            for _ in range(2):
                if todo:
                    todo.pop(0)()
        while todo:
            todo.pop(0)()
        P.barrier()
        P.emit()


NEG_EM05 = -math.exp(-0.5)


def phase_rwfeat(g, l):
    nc, I, S = g.nc, g.I, g.S
    with ExitStack() as es:
        def sb(name, shape, dt=F32):
            return es.enter_context(nc.sbuf_tensor("e%d_" % l + name, list(shape), dt))

        def psb(name, shape, dt=F32):
            return es.enter_context(nc.psum_tensor("e%d_" % l + name, list(shape), dt))
        SEG = 512
        rch = [("r0", 768, 128), ("r1", 896, 128), ("k0", 1024, 128), ("k1", 1152, 128), ("v0", 1280, 128),
               ("v1", 1408, 128), ("wl", 1536, 128), ("al", 1664, 128), ("g0", 1792, 128), ("g1", 1920, 32)]
        mu = sb("mu", [128, 10])
        omu = sb("omu", [128, 10])
        hmu = sb("hmu", [128, 10])
        pt = [sb("pt%d" % i, [128, SEG + 2]) for i in range(3)]
        s1 = [sb("s1%d" % i, [128, SEG]) for i in range(2)]
        sh = {nm: sb("sh_" + nm, [128, SEG]) for nm, _, _ in rch}
        w0c = sb("w0c", [128, 4])
        a0c = sb("a0c", [128, 4])
        kkc = sb("kkc", [128, 2])
        kac = sb("kac", [128, 2])
        omka = sb("omka", [128, 2])
        wbt = sb("wbt", [128, 256])
        abt = sb("abt", [128, 256])
        gb0 = sb("gb0", [128, 256])
        gb1 = sb("gb1", [32, 256])
        twl = sb("twl", [128, SEG])
        sg0 = sb("sg0", [128, SEG])
        sg1 = sb("sg1", [32, SEG])
        lw = [sb("lw%d" % i, [128, SEG]) for i in range(4)]
        asg = [sb("asg%d" % i, [128, SEG]) for i in range(4)]
        kk = [sb("kk%d" % i, [128, SEG]) for i in range(2)]
        sq = sb("sq", [128, SEG])
        rn = sb("rn", [128, SEG])
        kd = [sb("kd%d" % i, [128, SEG]) for i in range(4)]
        bd = [sb("bd%d" % i, [128, SEG]) for i in range(4)]
        gt = [sb("gt%d" % i, [128, SEG]) for i in range(2)]
        tq = sb("tq", [128, SEG])
        pp = [psb("pp%d" % i, [128, SEG]) for i in range(6)]
        P = Prog(nc)
        P.op("pool", lambda e: e.memset(mu[:, 9:10], 0.0), w=[mu])
        for ci, (nm, r0, nr) in enumerate(rch):
            P.dma("sync", mu[0:nr, ci:ci + 1], I.mu[g.wi(l), r0 - 768:r0 - 768 + nr, :], w=[mu], sem="mu")
        P.op("dve", lambda e: e.tensor_scalar(out=omu[:], in0=mu[:], scalar1=-1.0, scalar2=1.0, op0=ALU.mult, op1=ALU.add),
             r=[mu], w=[omu])
        P.op("dve", lambda e: e.tensor_scalar(out=hmu[:], in0=mu[:], scalar1=0.5, scalar2=None, op0=ALU.mult), r=[mu], w=[hmu])
        for d in range(2):
            for cc in range(2):
                P.dma("sync", w0c[:, d * 2 + cc:d * 2 + cc + 1], I.w0[g.wi(l), d * 256 + cc * 128:d * 256 + (cc + 1) * 128, :], w=[w0c], sem="w0c")
                P.dma("sync", a0c[:, d * 2 + cc:d * 2 + cc + 1], I.a0[g.wi(l), d * 256 + cc * 128:d * 256 + (cc + 1) * 128, :], w=[a0c], sem="a0c")
        for cc in range(2):
            P.dma("sync", kkc[:, cc:cc + 1], I.k_k[g.wi(l), cc * 128:(cc + 1) * 128, :], w=[kkc], sem="kkc")
            P.dma("sync", kac[:, cc:cc + 1], I.k_a[g.wi(l), cc * 128:(cc + 1) * 128, :], w=[kac], sem="kac")
        P.op("dve", lambda e: e.tensor_scalar(out=omka[:], in0=kac[:], scalar1=-1.0, scalar2=1.0, op0=ALU.mult, op1=ALU.add),
             r=[kac], w=[omka])
        P.dma("sync", wbt[:], I.w_b[g.wi(l)], w=[wbt])
        P.dma("sync", abt[:], I.a_b[g.wi(l)], w=[abt])
        P.dma("sync", gb0[:], I.g_b[g.wi(l), 0:128, :], w=[gb0])
        P.dma("sync", gb1[:], I.g_b[g.wi(l), 128:160, :], w=[gb1])
        segs = [(0, 0, CTX, CTX)] + [(CTX, CTX + i * SEG, SEG, SEQ) for i in range(8)]
        pti = 0
        sti = 0
        ppi = 0

        def store(idx, cc, src, n, t0):
            nonlocal sti
            q = "sync" if sti % 2 == 0 else "pool"
            sti += 1
            P.dma(q, S.rwf[idx, cc * 128:(cc + 1) * 128, t0:t0 + n], src[:, 0:n], r=[src], sem=("st", src.name))

        for (sq0, t0, n, slen) in segs:
            for ci, (nm, r0, nr) in enumerate(rch):
                p_ = pt[pti % 3]
                s_ = s1[pti % 2]
                pti += 1
                lo = t0 - 1
                hi = t0 + n + 1
                dlo, dhi = 0, n + 2
                if t0 == sq0:
                    lo += 1
                    dlo = 1
                    P.op("pool", lambda e, p_=p_, nr=nr: e.memset(p_[0:nr, 0:1], 0.0), w=[p_])
                if t0 + n == sq0 + slen:
                    hi -= 1
                    dhi = n + 1
                    P.op("pool", lambda e, p_=p_, nr=nr, n=n: e.memset(p_[0:nr, n + 1:n + 2], 0.0), w=[p_])
                P.dma("sync" if ci % 2 == 0 else "pool", p_[0:nr, dlo:dhi], S.pfm[r0:r0 + nr, lo:hi], w=[p_])
                P.op("pool", lambda e, p_=p_, s_=s_, nr=nr, n=n: e.tensor_tensor(out=s_[0:nr, 0:n], in0=p_[0:nr, 0:n], in1=p_[0:nr, 2:n + 2], op=ALU.add),
                     r=[p_], w=[s_])
                P.op("dve", lambda e, p_=p_, nr=nr, n=n, ci=ci, nm=nm: e.tensor_scalar(out=sh[nm][0:nr, 0:n], in0=p_[0:nr, 1:n + 1], scalar1=omu[0:nr, ci:ci + 1],
                                                                                scalar2=None, op0=ALU.mult), r=[p_, omu], w=[sh[nm]])
                P.op("dve", lambda e, s_=s_, nr=nr, n=n, ci=ci, nm=nm: e.scalar_tensor_tensor(out=sh[nm][0:nr, 0:n], in0=s_[0:nr, 0:n], scalar=hmu[0:nr, ci:ci + 1],
                                                                                       in1=sh[nm][0:nr, 0:n], op0=ALU.mult, op1=ALU.add),
                     r=[s_, hmu, sh[nm]], w=[sh[nm]])
            P.op("act", lambda e, n=n: e.activation(out=twl[:, 0:n], in_=sh["wl"][:, 0:n], func=AF.Tanh), r=[sh["wl"]], w=[twl])
            P.op("act", lambda e, n=n: e.activation(out=sg0[:, 0:n], in_=sh["g0"][:, 0:n], func=AF.Sigmoid), r=[sh["g0"]], w=[sg0])
            P.op("act", lambda e, n=n: e.activation(out=sg1[:, 0:n], in_=sh["g1"][0:32, 0:n], func=AF.Sigmoid), r=[sh["g1"]], w=[sg1])
            for d in range(2):
                for cc in range(2):
                    ix = d * 2 + cc
                    pw = pp[ppi % 6]
                    ppi += 1
                    P.op("pe", lambda e, pw=pw, d=d, cc=cc, n=n: e.matmul(out=pw[:, 0:n], lhsT=wbt[d * 64:(d + 1) * 64, cc * 128:(cc + 1) * 128],
                                                                       rhs=twl[d * 64:(d + 1) * 64, 0:n], start=True, stop=True),
                         r=[wbt, twl], w=[pw])
                    P.op("act", lambda e, pw=pw, ix=ix, n=n: e.activation(out=lw[ix][:, 0:n], in_=pw[:, 0:n], func=AF.Sigmoid, bias=w0c[:, ix:ix + 1]),
                         r=[pw, w0c], w=[lw[ix]])
                    P.op("pool", lambda e, ix=ix, n=n: e.tensor_scalar(out=lw[ix][:, 0:n], in0=lw[ix][:, 0:n], scalar1=NEG_EM05, scalar2=None, op0=ALU.mult),
                         r=[lw[ix]], w=[lw[ix]])
                    store(7 + d, cc, lw[ix], n, t0)
                    pa = pp[ppi % 6]
                    ppi += 1
                    P.op("pe", lambda e, pa=pa, d=d, cc=cc, n=n: e.matmul(out=pa[:, 0:n], lhsT=abt[d * 64:(d + 1) * 64, cc * 128:(cc + 1) * 128],
                                                                       rhs=sh["al"][d * 64:(d + 1) * 64, 0:n], start=True, stop=True),
                         r=[abt, sh["al"]], w=[pa])
                    P.op("act", lambda e, pa=pa, ix=ix, n=n: e.activation(out=asg[ix][:, 0:n], in_=pa[:, 0:n], func=AF.Sigmoid, bias=a0c[:, ix:ix + 1]),
                         r=[pa, a0c], w=[asg[ix]])
            for cc in range(2):
                kx = sh["k%d" % cc]
                P.op("dve", lambda e, cc=cc, kx=kx, n=n: e.tensor_scalar(out=kk[cc][:, 0:n], in0=kx[:, 0:n], scalar1=kkc[:, cc:cc + 1], scalar2=None, op0=ALU.mult),
                     r=[kx, kkc], w=[kk[cc]])
                P.op("pool", lambda e, cc=cc, n=n: e.tensor_tensor(out=sq[:, 0:n], in0=kk[cc][:, 0:n], in1=kk[cc][:, 0:n], op=ALU.mult),
                     r=[kk[cc]], w=[sq])
                pn = pp[ppi % 6]
                ppi += 1
                P.op("pe", lambda e, pn=pn, n=n: e.matmul(out=pn[:, 0:n], lhsT=g.consts[:, C_BONES, :], rhs=sq[:, 0:n], start=True, stop=True),
                     r=[sq, g.consts], w=[pn])
                P.op("act", lambda e, pn=pn, n=n: e.activation(out=rn[:, 0:n], in_=pn[:, 0:n], func=AF.Sqrt), r=[pn], w=[rn])
                P.op("dve", lambda e, n=n: e.tensor_scalar(out=rn[:, 0:n], in0=rn[:, 0:n], scalar1=1e-12, scalar2=None, op0=ALU.max), r=[rn], w=[rn])
                P.op("dve", lambda e, n=n: e.reciprocal(out=rn[:, 0:n], in_=rn[:, 0:n]), r=[rn], w=[rn])
                P.op("dve", lambda e, cc=cc, n=n: e.tensor_tensor(out=kk[cc][:, 0:n], in0=kk[cc][:, 0:n], in1=rn[:, 0:n], op=ALU.mult),
                     r=[kk[cc], rn], w=[kk[cc]])
                store(4, cc, kk[cc], n, t0)
                store(0, cc, sh["r%d" % cc], n, t0)
                store(3, cc, sh["v%d" % cc], n, t0)
                for d in range(2):
                    ix = d * 2 + cc
                    P.op("dve", lambda e, ix=ix, cc=cc, n=n: e.tensor_scalar(out=tq[:, 0:n], in0=asg[ix][:, 0:n], scalar1=kac[:, cc:cc + 1],
                                                                         scalar2=omka[:, cc:cc + 1], op0=ALU.mult, op1=ALU.add),
                         r=[asg[ix], kac, omka], w=[tq])
                    P.op("dve", lambda e, ix=ix, kx=kx, n=n: e.tensor_tensor(out=kd[ix][:, 0:n], in0=tq[:, 0:n], in1=kx[:, 0:n], op=ALU.mult),
                         r=[tq, kx], w=[kd[ix]])
                    store(1 + d, cc, kd[ix], n, t0)
                    P.op("pool", lambda e, ix=ix, cc=cc, n=n: e.tensor_tensor(out=bd[ix][:, 0:n], in0=kk[cc][:, 0:n], in1=asg[ix][:, 0:n], op=ALU.mult),
                         r=[kk[cc], asg[ix]], w=[bd[ix]])
                    store(5 + d, cc, bd[ix], n, t0)
                pg = pp[ppi % 6]
                ppi += 1
                P.op("pe", lambda e, pg=pg, cc=cc, n=n: e.matmul(out=pg[:, 0:n], lhsT=gb0[:, cc * 128:(cc + 1) * 128], rhs=sg0[:, 0:n], start=True, stop=False),
                     r=[gb0, sg0], w=[pg])
                P.op("pe", lambda e, pg=pg, cc=cc, n=n: e.matmul(out=pg[:, 0:n], lhsT=gb1[:, cc * 128:(cc + 1) * 128], rhs=sg1[:, 0:n], start=False, stop=True),
                     r=[gb1, sg1], w=[pg])
                P.op("act", lambda e, pg=pg, cc=cc, n=n: e.activation(out=gt[cc][:, 0:n], in_=pg[:, 0:n], func=AF.Copy), r=[pg], w=[gt[cc]])
                store(9, cc, gt[cc], n, t0)
        P.barrier()
        P.emit()


def phase_rwscan(g, l):
    nc, I, S = g.nc, g.I, g.S
    with ExitStack() as es:
        def sb(name, shape, dt=F32):
            return es.enter_context(nc.sbuf_tensor("f%d_" % l + name, list(shape), dt))

        def psb(name, shape, dt=F32):
            return es.enter_context(nc.psum_tensor("f%d_" % l + name, list(shape), dt))
        ident = g.consts[:, C_ID, :]
        ybuf = [[sb("y%d%d" % (p, d), [128, NT], BF16) for d in range(2)] for p in range(2)]
        E64 = sb("E64", [128, 64])
        E64r = sb("E64r", [128, 64], F32R)
        U = [[None, None], [None, None]]
        for d in range(2):
            for p in range(2):
                u = Ctx()
                n = "%d%d" % (d, p)
                u.f = [sb("ld%d_" % i + n, [128, 128]) for i in range(6)]
                u.Lc = sb("Lc" + n, [128, 128])
                u.LC = sb("LC" + n, [128, 128])
                u.t1 = sb("t1" + n, [128, 128])
                u.tA = sb("tA" + n, [128, 128])
                u.tW = sb("tW" + n, [128, 128])
                u.eP = sb("eP" + n, [128, 128])
                u.eN = sb("eN" + n, [128, 128])
                u.eA = sb("eA" + n, [128, 128])
                u.eW = sb("eW" + n, [128, 128])
                u.WL = sb("WL" + n, [128, 2])
                u.ar = sb("ar" + n, [128, 256], F32R)
                u.at32 = sb("at32" + n, [128, 128])
                u.bt = sb("bt" + n, [128, 128], F32R)
                u.kt = sb("kt" + n, [128, 128], F32R)
                u.bW = sb("bW" + n, [128, 128])
                u.kW = sb("kW" + n, [128, 128])
                u.Dg = sb("Dg" + n, [128, 2, 64], F32R)
                u.TM = sb("TM" + n, [128, 4, 128], F32R)
                u.q = []
                if p == 1:
                    u.q = U[d][0].q
                    U[d][p] = u
                    continue
                for hh in range(2):
                    q = Ctx()
                    m = n + "%d" % hh
                    q.XTR = sb("XTR" + m, [128, 256], F32R)
                    q.KTR = sb("KTR" + m, [128, 256], F32R)
                    q.X = [sb("X%d_" % i + m, [128, 128], F32R) for i in range(2)]
                    q.XP = [sb("XP%d_" % i + m, [128, 256], F32R) for i in range(2)]
                    pass
                    q.Gs = sb("Gs" + m, [128, 64], F32R)
                    q.MAG = sb("MAG" + m, [128, 128], F32R)
                    q.Phi = sb("Phi" + m, [64, 2, 64])
                    q.Psi = sb("Psi" + m, [64, 2, 64])
                    q.RAT = sb("RAT" + m, [64, 128])
                    q.YCT = sb("YCT" + m, [64, 128])
                    u.q.append(q)
                U[d][p] = u
        ST = [[[sb("ST%d%d%d" % (h, d, i), [64, 64]) for i in range(2)] for d in range(2)] for h in range(4)]
        stpar = [[0, 0] for _ in range(4)]
        pq = [psb("pq%d" % i, [128, 512]) for i in range(4)]
        pTr = [psb("pTr%d" % i, [128, 4, 128]) for i in range(2)]
        pSq = [psb("pSq%d" % i, [64, 512]) for i in range(2)]
        P = Prog(nc)
        P.op("dve", lambda e: e.tensor_tensor(out=E64[:], in0=g.consts[:, C_ID, 0:64], in1=g.consts[:, C_ID, 64:128], op=ALU.add),
             r=[g.consts], w=[E64])
        P.op("dve", lambda e: e.tensor_copy(out=E64r[:], in_=E64[:]), r=[E64], w=[E64r])
        for h in range(4):
            for d in range(2):
                P.op("pool", lambda e, h=h, d=d: e.memset(ST[h][d][0][:], 0.0), w=[ST[h][d][0]])
        order_f = list(range(NTILE))
        order_b = [1, 0] + list(range(NTILE - 1, 1, -1))
        fidx = [[0, 1, 3, 4, 5, 7], [0, 2, 3, 4, 6, 8]]
        slotc = [0, 0, 0, 0]

        def slot(qi):
            sl = slotc[qi] % 4
            slotc[qi] += 1
            return sl

        def fm_part(step, p):
            units = [(0, order_f[step]), (1, order_b[step])]
            for (d, j) in units:
                u = U[d][p]
                for i6 in range(6):
                    P.dma("sync" if i6 % 2 == 0 else "pool", u.f[i6][:], S.rwf[fidx[d][i6], p * 128:(p + 1) * 128, j * 128:(j + 1) * 128],
                          w=[u.f[i6]])
                fr, fkd, fv, fkk, fbd, flw = u.f
                P.op("dve", lambda e, u=u, flw=flw: e.tensor_tensor_scan(out=u.Lc[:], data0=g.consts[:, C_RESET, :], data1=flw[:], initial=0.0,
                                                                       op0=ALU.mult, op1=ALU.add), r=[flw, g.consts], w=[u.Lc])
                totv = u.Lc[:].rearrange("p (c l) -> p c l", c=2)[:, :, 63:64]
                if d == 0:
                    LC = u.Lc
                else:
                    LC = u.LC
                    P.op("pool", lambda e, u=u, flw=flw: e.tensor_tensor(out=u.t1[:], in0=flw[:], in1=u.Lc[:], op=ALU.subtract),
                         r=[flw, u.Lc], w=[u.t1])
                    P.op("pool", lambda e, u=u, totv=totv: e.tensor_tensor(out=u.LC[:].rearrange("p (c l) -> p c l", c=2),
                                                                        in0=u.t1[:].rearrange("p (c l) -> p c l", c=2),
                                                                        in1=totv.to_broadcast([128, 2, 64]), op=ALU.add),
                         r=[u.t1, u.Lc], w=[u.LC])
                P.op("pool", lambda e, u=u, LC=LC, flw=flw: e.tensor_tensor(out=u.tA[:], in0=LC[:], in1=flw[:], op=ALU.subtract),
                     r=[LC, flw], w=[u.tA])
                P.op("pool", lambda e, u=u, LC=LC, totv=totv: e.tensor_tensor(out=u.tW[:].rearrange("p (c l) -> p c l", c=2),
                                                                           in0=totv.to_broadcast([128, 2, 64]),
                                                                           in1=LC[:].rearrange("p (c l) -> p c l", c=2), op=ALU.subtract),
                     r=[LC, u.Lc], w=[u.tW])
                P.op("act", lambda e, u=u, LC=LC: e.activation(out=u.eP[:], in_=LC[:], func=AF.Exp), r=[LC], w=[u.eP])
                P.op("act", lambda e, u=u, LC=LC: e.activation(out=u.eN[:], in_=LC[:], func=AF.Exp, scale=-1.0), r=[LC], w=[u.eN])
                P.op("act", lambda e, u=u: e.activation(out=u.eA[:], in_=u.tA[:], func=AF.Exp), r=[u.tA], w=[u.eA])
                P.op("act", lambda e, u=u: e.activation(out=u.eW[:], in_=u.tW[:], func=AF.Exp), r=[u.tW], w=[u.eW])
                P.op("act", lambda e, u=u, totv=totv: e.activation(out=u.WL[:].unsqueeze(2), in_=totv, func=AF.Exp), r=[u.Lc], w=[u.WL])
                P.op("dve", lambda e, u=u, fkk=fkk: e.scalar_tensor_tensor(out=u.at32[:], in0=fkk[:], scalar=-1.0, in1=u.eA[:],
                                                                         op0=ALU.mult, op1=ALU.mult), r=[fkk, u.eA], w=[u.at32])
                P.op("pool", lambda e, u=u: e.tensor_copy(out=u.ar[:, 0:128], in_=u.at32[:]), r=[u.at32], w=[(u.ar.name, 0)])
                P.op("pool", lambda e, u=u, fr=fr: e.tensor_tensor(out=u.ar[:, 128:256], in0=fr[:], in1=u.eP[:], op=ALU.mult),
                     r=[fr, u.eP], w=[(u.ar.name, 1)])
                P.op("dve", lambda e, u=u, fbd=fbd: e.tensor_tensor(out=u.bt[:], in0=fbd[:], in1=u.eN[:], op=ALU.mult), r=[fbd, u.eN], w=[u.bt])
                P.op("pool", lambda e, u=u, fkd=fkd: e.tensor_tensor(out=u.kt[:], in0=fkd[:], in1=u.eN[:], op=ALU.mult), r=[fkd, u.eN], w=[u.kt])
                P.op("dve", lambda e, u=u, fbd=fbd: e.tensor_tensor(out=u.bW[:], in0=fbd[:], in1=u.eW[:], op=ALU.mult), r=[fbd, u.eW], w=[u.bW])
                P.op("pool", lambda e, u=u, fkd=fkd: e.tensor_tensor(out=u.kW[:], in0=fkd[:], in1=u.eW[:], op=ALU.mult), r=[fkd, u.eW], w=[u.kW])
                for c in range(2):
                    P.op("pool", lambda e, u=u, c=c: e.tensor_scalar(out=u.Dg[:, c, :], in0=E64[:], scalar1=u.WL[:, c:c + 1], scalar2=None, op0=ALU.mult),
                         r=[E64, u.WL], w=[u.Dg])
                srcs = [(u.at32, 0, u.at32.name), (u.bW, None, u.bW.name), (u.kW, None, u.kW.name), (fv, None, fv.name)]
                for k4, (src, off, key) in enumerate(srcs):
                    in_ap = src[:, 0:128]
                    P.op("pe", lambda e, d=d, k4=k4, in_ap=in_ap: e.transpose(out=pTr[d][:, k4, :], in_=in_ap, identity=ident),
                         r=[key, g.consts], w=[pTr[d]])
                P.op("act", lambda e, u=u, d=d: e.activation(out=u.TM[:], in_=pTr[d][:], func=AF.Copy), r=[pTr[d]], w=[u.TM])

        def rest_part(step, p):
            units = [(0, order_f[step]), (1, order_b[step])]
            probs = []
            for (d, j) in units:
                for hh in range(2):
                    probs.append((d, j, hh, U[d][p], U[d][p].q[hh], d * 2 + hh))
            for (d, j, hh, u, q, qi) in probs:
                pb = hh * 64
                P.op("pe", lambda e, u=u, pb=pb, qi=qi: e.matmul(out=pq[qi][:, 0:256], lhsT=u.bt[pb:pb + 64, :], rhs=u.ar[pb:pb + 64, :], start=True, stop=True),
                     r=[u.bt, (u.ar.name, 0), (u.ar.name, 1)], w=[("pqb", qi), ("pqb", qi)], rows=pb)
                P.op("pe", lambda e, u=u, pb=pb, qi=qi: e.matmul(out=pq[qi][:, 256:384], lhsT=u.ar[pb:pb + 64, 0:128], rhs=u.bt[pb:pb + 64, :], start=True, stop=True),
                     r=[u.bt, (u.ar.name, 0)], w=[("pqb", qi)], rows=pb)
            for (d, j, hh, u, q, qi) in probs:
                m2 = (C_MS_IT if d == 0 else C_MS_IT_B)
                mti = (C_MS_TI if d == 0 else C_MS_IT)
                P.op("dve", lambda e, q=q, qi=qi, m2=m2: e.tensor_tensor(out=q.XP[0][:, 0:128], in0=pq[qi][:, 0:128], in1=g.consts[:, m2, :], op=ALU.mult),
                     r=[("pqb", qi), g.consts], w=[(q.XP[0].name, 0)])
                P.op("dve", lambda e, q=q, qi=qi, m2=m2: e.tensor_tensor(out=q.XTR[:, 128:256], in0=pq[qi][:, 128:256], in1=g.consts[:, m2 + 1, :], op=ALU.mult),
                     r=[("pqb", qi), g.consts], w=[q.XTR])
                P.op("dve", lambda e, q=q, qi=qi, mti=mti: e.tensor_tensor(out=q.X[0][:], in0=pq[qi][:, 256:384], in1=g.consts[:, mti, :], op=ALU.mult),
                     r=[("pqb", qi), g.consts], w=[q.X[0]])
                P.op("pool", lambda e, q=q: e.tensor_copy(out=q.XP[0][:, 128:256], in_=ident), r=[g.consts], w=[(q.XP[0].name, 1)])
            for (d, j, hh, u, q, qi) in probs:
                pb = hh * 64
                P.op("pe", lambda e, u=u, pb=pb, qi=qi: e.matmul(out=pq[qi][:, 0:256], lhsT=u.kt[pb:pb + 64, :], rhs=u.ar[pb:pb + 64, :], start=True, stop=True),
                     r=[u.kt, (u.ar.name, 0), (u.ar.name, 1)], w=[("pqb", qi), ("pqb", qi)], rows=pb)
            for (d, j, hh, u, q, qi) in probs:
                m2 = (C_MS_IT if d == 0 else C_MS_IT_B)
                P.op("dve", lambda e, q=q, qi=qi, m2=m2: e.tensor_tensor(out=q.KTR[:], in0=pq[qi][:, 0:256],
                                                                      in1=g.consts[:, m2:m2 + 2, :].rearrange("p a b -> p (a b)"), op=ALU.mult),
                     r=[("pqb", qi), ("pqb", qi), g.consts], w=[q.KTR])
            for lev in range(6):
                last = (lev == 5)
                for (d, j, hh, u, q, qi) in probs:
                    Xc = q.X[lev % 2]
                    XPc = q.XP[lev % 2]
                    P.op("pe", lambda e, qi=qi, Xc=Xc, XPc=XPc: e.matmul(out=pq[qi][:, 0:256], lhsT=Xc[:], rhs=XPc[:, 0:256], start=True, stop=True),
                         r=[Xc, (XPc.name, 0), (XPc.name, 1)], w=[("pqb", qi)])
                    if not last:
                        P.op("pe", lambda e, qi=qi, Xc=Xc, XPc=XPc: e.matmul(out=pq[qi][:, 256:384], lhsT=XPc[:, 0:128], rhs=Xc[:], start=True, stop=True),
                             r=[Xc, (XPc.name, 0)], w=[("pqb", qi)])
                for (d, j, hh, u, q, qi) in probs:
                    Xn = q.X[(lev + 1) % 2]
                    XPc = q.XP[lev % 2]
                    XPn = q.XP[(lev + 1) % 2]
                    if not last:
                        P.op("act", lambda e, qi=qi, XPn=XPn: e.activation(out=XPn[:, 0:128], in_=pq[qi][:, 0:128], func=AF.Copy),
                             r=[("pqb", qi)], w=[(XPn.name, 0)])
                        P.op("act", lambda e, qi=qi, Xn=Xn: e.activation(out=Xn[:], in_=pq[qi][:, 256:384], func=AF.Copy), r=[("pqb", qi)], w=[Xn])
                    P.op("dve", lambda e, qi=qi, XPc=XPc, XPn=XPn: e.tensor_tensor(out=XPn[:, 128:256], in0=pq[qi][:, 128:256], in1=XPc[:, 128:256], op=ALU.add),
                         r=[("pqb", qi), (XPc.name, 1)], w=[(XPn.name, 1)])
            for (d, j, hh, u, q, qi) in probs:
                cb = hh * 64
                P.op("pe", lambda e, qi=qi, q=q, u=u, cb=cb: e.matmul(out=pq[qi][:, 128:192], lhsT=q.KTR[:, 0:128], rhs=u.TM[:, 3, cb:cb + 64], start=True, stop=True),
                     r=[q.KTR, u.TM], w=[("pqb", qi)])
            for (d, j, hh, u, q, qi) in probs:
                P.op("act", lambda e, qi=qi, q=q: e.activation(out=q.Gs[:], in_=pq[qi][:, 128:192], func=AF.Copy), r=[("pqb", qi)], w=[q.Gs])
            for (d, j, hh, u, q, qi) in probs:
                cb = hh * 64
                PTf = q.XP[0]
                P.op("pe", lambda e, qi=qi, PTf=PTf, u=u, cb=cb: e.matmul(out=pq[qi][:, 256:320], lhsT=PTf[:, 128:256], rhs=u.TM[:, 0, cb:cb + 64], start=True, stop=True),
                     r=[(PTf.name, 1), u.TM], w=[("pqb", qi)])
                P.op("pe", lambda e, qi=qi, PTf=PTf, q=q: e.matmul(out=pq[qi][:, 320:384], lhsT=PTf[:, 128:256], rhs=q.Gs[:], start=True, stop=True),
                     r=[(PTf.name, 1), q.Gs], w=[("pqb", qi)])
            for (d, j, hh, u, q, qi) in probs:
                P.op("act", lambda e, qi=qi, q=q: e.activation(out=q.MAG[:], in_=pq[qi][:, 256:384], func=AF.Copy), r=[("pqb", qi)], w=[q.MAG])
            for (d, j, hh, u, q, qi) in probs:
                cb = hh * 64
                pb = hh * 64
                for c in range(2):
                    rb = c * 64
                    P.op("pe", lambda e, qi=qi, q=q, u=u, rb=rb, cb=cb, c=c: e.matmul(out=pq[qi][0:64, 384 + c * 64:448 + c * 64], lhsT=q.MAG[rb:rb + 64, 0:64],
                                                                                   rhs=u.TM[rb:rb + 64, 1, cb:cb + 64], start=True, stop=False),
                         r=[q.MAG, u.TM], w=[("pqb", qi)], rows=rb)
                    P.op("pe", lambda e, qi=qi, u=u, pb=pb, c=c: e.matmul(out=pq[qi][0:64, 384 + c * 64:448 + c * 64], lhsT=E64r[pb:pb + 64, :],
                                                                        rhs=u.Dg[pb:pb + 64, c, :], start=False, stop=True),
                         r=[E64r, u.Dg], w=[("pqb", qi)], rows=pb)
                    P.op("pe", lambda e, qi=qi, q=q, u=u, rb=rb, cb=cb, c=c: e.matmul(out=pq[qi][0:64, c * 64:c * 64 + 64], lhsT=u.TM[rb:rb + 64, 1, cb:cb + 64],
                                                                                   rhs=q.MAG[rb:rb + 64, 64:128], start=True, stop=False),
                         r=[q.MAG, u.TM], w=[("pqb", qi)], rows=rb)
                    P.op("pe", lambda e, qi=qi, u=u, rb=rb, cb=cb, c=c: e.matmul(out=pq[qi][0:64, c * 64:c * 64 + 64], lhsT=u.TM[rb:rb + 64, 2, cb:cb + 64],
                                                                              rhs=u.TM[rb:rb + 64, 3, cb:cb + 64], start=False, stop=True),
                         r=[u.TM], w=[("pqb", qi)], rows=rb)
                P.op("pe", lambda e, qi=qi, q=q: e.matmul(out=pq[qi][0:64, 128:256], lhsT=q.MAG[:, 0:64], rhs=q.XTR[:, 128:256], start=True, stop=False),
                     r=[q.MAG, q.XTR], w=[("pqb", qi)])
                P.op("pe", lambda e, qi=qi, u=u, pb=pb: e.matmul(out=pq[qi][0:64, 128:256], lhsT=E64r[pb:pb + 64, :], rhs=u.ar[pb:pb + 64, 128:256], start=False, stop=True),
                     r=[E64r, (u.ar.name, 1)], w=[("pqb", qi)], rows=pb)
                P.op("pe", lambda e, qi=qi, q=q: e.matmul(out=pq[qi][0:64, 256:384], lhsT=q.MAG[:, 64:128], rhs=q.XTR[:, 128:256], start=True, stop=False),
                     r=[q.MAG, q.XTR], w=[("pqb", qi)])
                P.op("pe", lambda e, qi=qi, q=q, u=u, cb=cb: e.matmul(out=pq[qi][0:64, 256:384], lhsT=u.TM[:, 3, cb:cb + 64], rhs=q.KTR[:, 128:256], start=False, stop=True),
                     r=[u.TM, q.KTR], w=[("pqb", qi)])
            for (d, j, hh, u, q, qi) in probs:
                P.op("act", lambda e, qi=qi, q=q: e.activation(out=q.Phi[:].rearrange("p c k -> p (c k)"), in_=pq[qi][0:64, 384:512], func=AF.Copy),
                     r=[("pqb", qi)], w=[q.Phi])
                P.op("dve", lambda e, qi=qi, q=q: e.tensor_copy(out=q.Psi[:].rearrange("p c k -> p (c k)"), in_=pq[qi][0:64, 0:128]),
                     r=[("pqb", qi)], w=[q.Psi])
                P.op("act", lambda e, qi=qi, q=q: e.activation(out=q.RAT[:], in_=pq[qi][0:64, 128:256], func=AF.Copy), r=[("pqb", qi)], w=[q.RAT])
                P.op("dve", lambda e, qi=qi, q=q: e.tensor_copy(out=q.YCT[:], in_=pq[qi][0:64, 256:384]), r=[("pqb", qi)], w=[q.YCT])
            for ci in range(2):
                for (d, j, hh, u, q, qi) in probs:
                    c = ci if d == 0 else 1 - ci
                    h = p * 2 + hh
                    sp = stpar[h][d]
                    Sc = ST[h][d][sp]
                    Sn = ST[h][d][1 - sp]
                    stpar[h][d] = 1 - sp
                    psq = pSq[ci]
                    yc0 = qi * 128
                    P.op("pe", lambda e, psq=psq, yc0=yc0, Sc=Sc, q=q, c=c: e.matmul(out=psq[:, yc0:yc0 + 64], lhsT=Sc[:], rhs=q.RAT[:, c * 64:(c + 1) * 64], start=True, stop=False),
                         r=[Sc, q.RAT], w=[("sqb", ci)])
                    P.op("pe", lambda e, psq=psq, yc0=yc0, q=q, c=c: e.matmul(out=psq[:, yc0:yc0 + 64], lhsT=E64[0:64, :], rhs=q.YCT[:, c * 64:(c + 1) * 64], start=False, stop=True),
                         r=[E64, q.YCT], w=[("sqb", ci)])
                    P.op("pe", lambda e, psq=psq, yc0=yc0, Sc=Sc, q=q, c=c: e.matmul(out=psq[:, yc0 + 64:yc0 + 128], lhsT=q.Phi[:, c, :], rhs=Sc[:], start=True, stop=False),
                         r=[Sc, q.Phi], w=[("sqb", ci)])
                    P.op("pe", lambda e, psq=psq, yc0=yc0, q=q, c=c: e.matmul(out=psq[:, yc0 + 64:yc0 + 128], lhsT=E64[0:64, :], rhs=q.Psi[:, c, :], start=False, stop=True),
                         r=[E64, q.Psi], w=[("sqb", ci)])
                    tcol = j * 128 + c * 64
                    yb = ybuf[p][d]
                    P.op("act", lambda e, psq=psq, yc0=yc0, yb=yb, hh=hh, tcol=tcol: e.activation(out=yb[hh * 64:(hh + 1) * 64, tcol:tcol + 64], in_=psq[:, yc0:yc0 + 64], func=AF.Copy),
                         r=[("sqb", ci)], w=[(yb.name, j)])
                    P.op("dve", lambda e, psq=psq, yc0=yc0, Sn=Sn: e.tensor_copy(out=Sn[:], in_=psq[:, yc0 + 64:yc0 + 128]), r=[("sqb", ci)], w=[Sn])

        nsteps = NTILE if g.rwsteps is None else g.rwsteps
        groups = [(st_, p_) for st_ in range(nsteps) for p_ in range(2)]
        fm_part(*groups[0])
        for gi, (st_, p_) in enumerate(groups):
            if gi + 1 < len(groups):
                fm_part(*groups[gi + 1])
            rest_part(st_, p_)
        SEG = 512
        prm = sb("prm", [128, 2, 3])
        ld = [sb("o_ld%d" % i, [128, SEG]) for i in range(5)]
        ysum = sb("ysum", [128, SEG])
        yc_ = sb("yc_", [128, SEG])
        sq = sb("osq", [128, SEG])
        rstd = sb("rstd", [128, SEG])
        prod = sb("prod", [128, SEG])
        ob = [sb("oob%d" % i, [128, SEG], BF16) for i in range(2)]
        for p in range(2):
            for k3, src in enumerate((I.r_k, I.ln_w, I.ln_b)):
                P.dma("sync", prm[:, p, k3:k3 + 1], src[g.wi(l), p * 128:(p + 1) * 128, :], w=[prm], sem="prm")
        segs = ([(0, CTX)] if l == 0 else []) + [(CTX + i * SEG, SEG) for i in range(8)]
        oi = 0
        for (t0, n) in segs:
            for p in range(2):
                for k5, idx in enumerate((0, 1, 2, 3, 9)):
                    P.dma("sync" if k5 % 2 == 0 else "pool", ld[k5][:, 0:n], S.rwf[idx, p * 128:(p + 1) * 128, t0:t0 + n], w=[ld[k5]])
                ykeys = [(ybuf[p][dd].name, jj) for dd in range(2) for jj in range(t0 // 128, (t0 + n) // 128)]
                P.op("pool", lambda e, p=p, t0=t0, n=n: e.tensor_tensor(out=ysum[:, 0:n], in0=ybuf[p][0][:, t0:t0 + n], in1=ybuf[p][1][:, t0:t0 + n], op=ALU.add),
                     r=ykeys, w=[ysum])
                pm = pq[0]
                P.op("pe", lambda e, pm=pm, n=n: e.matmul(out=pm[:, 0:n], lhsT=g.consts[:, C_BONES, :], rhs=ysum[:, 0:n], start=True, stop=True),
                     r=[ysum, g.consts], w=[("pqb", 0), ("pqb", 0), ("pqb", 0), ("pqb", 0)])
                P.op("dve", lambda e, pm=pm, n=n: e.scalar_tensor_tensor(out=yc_[:, 0:n], in0=pm[:, 0:n], scalar=-1.0 / 64, in1=ysum[:, 0:n], op0=ALU.mult, op1=ALU.add),
                     r=[("pqb", 0), ("pqb", 0), ("pqb", 0), ("pqb", 0), ysum], w=[yc_])
                P.op("pool", lambda e, n=n: e.tensor_tensor(out=sq[:, 0:n], in0=yc_[:, 0:n], in1=yc_[:, 0:n], op=ALU.mult), r=[yc_], w=[sq])
                pv = pq[1]
                P.op("pe", lambda e, pv=pv, n=n: e.matmul(out=pv[:, 0:n], lhsT=g.consts[:, C_BONES, :], rhs=sq[:, 0:n], start=True, stop=True),
                     r=[sq, g.consts], w=[("pqb", 1), ("pqb", 1), ("pqb", 1), ("pqb", 1)])
                P.op("dve", lambda e, pv=pv, n=n: e.tensor_scalar(out=rstd[:, 0:n], in0=pv[:, 0:n], scalar1=1.0 / 64, scalar2=64e-5, op0=ALU.mult, op1=ALU.add),
                     r=[("pqb", 1), ("pqb", 1), ("pqb", 1), ("pqb", 1)], w=[rstd])
                P.op("act", lambda e, n=n: e.activation(out=rstd[:, 0:n], in_=rstd[:, 0:n], func=AF.Sqrt), r=[rstd], w=[rstd])
                P.op("dve", lambda e, n=n: e.reciprocal(out=rstd[:, 0:n], in_=rstd[:, 0:n]), r=[rstd], w=[rstd])
                P.op("dve", lambda e, n=n: e.tensor_tensor(out=yc_[:, 0:n], in0=yc_[:, 0:n], in1=rstd[:, 0:n], op=ALU.mult), r=[yc_, rstd], w=[yc_])
                P.op("dve", lambda e, n=n, p=p: e.tensor_scalar(out=yc_[:, 0:n], in0=yc_[:, 0:n], scalar1=prm[:, p, 1:2], scalar2=prm[:, p, 2:3], op0=ALU.mult, op1=ALU.add),
                     r=[yc_, prm], w=[yc_])
                P.op("pool", lambda e, n=n: e.tensor_tensor(out=prod[:, 0:n], in0=ld[1][:, 0:n], in1=ld[2][:, 0:n], op=ALU.add), r=[ld[1], ld[2]], w=[prod])
                P.op("pool", lambda e, n=n: e.tensor_tensor(out=prod[:, 0:n], in0=prod[:, 0:n], in1=ld[0][:, 0:n], op=ALU.mult), r=[prod, ld[0]], w=[prod])
                P.op("pool", lambda e, n=n, p=p: e.tensor_scalar(out=prod[:, 0:n], in0=prod[:, 0:n], scalar1=prm[:, p, 0:1], scalar2=0.5, op0=ALU.mult, op1=ALU.mult),
                     r=[prod, prm], w=[prod])
                pbn = pq[2]
                P.op("pe", lambda e, pbn=pbn, n=n: e.matmul(out=pbn[:, 0:n], lhsT=g.consts[:, C_BONES, :], rhs=prod[:, 0:n], start=True, stop=True),
                     r=[prod, g.consts], w=[("pqb", 2), ("pqb", 2), ("pqb", 2), ("pqb", 2)])
                P.op("dve", lambda e, pbn=pbn, n=n: e.tensor_tensor(out=sq[:, 0:n], in0=pbn[:, 0:n], in1=ld[3][:, 0:n], op=ALU.mult),
                     r=[("pqb", 2), ("pqb", 2), ("pqb", 2), ("pqb", 2), ld[3]], w=[sq])
                P.op("dve", lambda e, n=n: e.tensor_tensor(out=yc_[:, 0:n], in0=yc_[:, 0:n], in1=sq[:, 0:n], op=ALU.add), r=[yc_, sq], w=[yc_])
                o = ob[oi % 2]
                oi += 1
                P.op("dve", lambda e, n=n, o=o: e.tensor_tensor(out=o[:, 0:n], in0=yc_[:, 0:n], in1=ld[4][:, 0:n], op=ALU.mult), r=[yc_, ld[4]], w=[o])
                P.dma("sync", S.mixT[768 + p * 128:768 + (p + 1) * 128, t0:t0 + n], o[:, 0:n], r=[o], sem=("oob", oi % 2))
        P.barrier()
        P.emit()


def phase_wout(g, l):
    nc, I, S = g.nc, g.I, g.S
    with ExitStack() as es:
        def sb(name, shape, dt=F32):
            return es.enter_context(nc.sbuf_tensor("w%d_" % l + name, list(shape), dt))

        def psb(name, shape, dt=F32):
            return es.enter_context(nc.psum_tensor("w%d_" % l + name, list(shape), dt))
        wo = sb("wo", [128, 8, D], BF16)
        rw = sb("rw", [128, 8, NE])
        bc = [[sb("bc%d%d" % (j, k), [128, D]) for k in range(3)] for j in range(2)]
        mt = [sb("mt%d" % i, [128, 8, 128], BF16) for i in range(2)]
        xt = [sb("xt%d" % i, [128, D]) for i in range(2)]
        x1 = [sb("x1%d" % i, [128, D]) for i in range(2)]
        junk = sb("junk", [128, D])
        ss = [sb("ss%d" % i, [128, 1]) for i in range(2)]
        h2f = [sb("h2f%d" % i, [128, D]) for i in range(2)]
        h2b = [sb("h2b%d" % i, [128, D], BF16) for i in range(2)]
        h2T = sb("h2T", [128, 8, 128])
        lg = sb("lg", [128, NE])
        mx = sb("mx", [128, 1])
        sm = sb("sm", [128, 1])
        aff = sb("aff", [128, NE])
        pO = [psb("pO%d" % i, [128, 512]) for i in range(2)]
        pT = [psb("pT%d" % i, [128, 4, 128]) for i in range(2)]
        pL_ = psb("pL", [128, 512])
        pL = pL_[:, 0:NE]
        pA_ = psb("pA", [NE, 512])
        pA = pA_[:, 0:128]
        P = Prog(nc)
        P.dma("pool", wo[:], I.w_out[g.wi(l)].rearrange("(kc p) n -> p kc n", p=128), w=[wo])
        P.dma("sync", rw[:], I.router[g.wi(l)].rearrange("(kc p) n -> p kc n", p=128), w=[rw])
        for j in range(2):
            for k, mi in enumerate((2, 3, 4)):
                P.dma("sync", bc[j][k][:], S.modv[g.wi(l), j, mi:mi + 1, :].to_broadcast([128, D]), w=[bc[j][k]])
        tiles = list(range(NTILE)) if l == 0 else list(range(2, NTILE))
        for i in tiles:
            b = i % 2
            j = 1 if i < 2 else 0
            if i < 2:
                src = (I.ctx if l == g.first else S.xcres)[i * 128:(i + 1) * 128, :]
                dst = S.xcres[i * 128:(i + 1) * 128, :]
                h2dst = S.h2c[i * 128:(i + 1) * 128, :]
            else:
                src = (I.x if l == g.first else S.xres)[(i - 2) * 128:(i - 1) * 128, :]
                dst = (g.out if l == g.last else S.xres)[(i - 2) * 128:(i - 1) * 128, :]
                h2dst = S.h2l[(i - 2) * 128:(i - 1) * 128, :]
            P.dma("sync", mt[b][:], S.mixT[:, i * 128:(i + 1) * 128].rearrange("(kc p) t -> p kc t", p=128), w=[mt[b]])
            P.dma("sync", xt[b][:], src, w=[xt[b]])
            for half in range(2):
                for kc in range(8):
                    P.op("pe", lambda e, half=half, kc=kc, b=b: e.matmul(out=pO[half][:], lhsT=mt[b][:, kc, :], rhs=wo[:, kc, half * 512:(half + 1) * 512],
                                                                       start=(kc == 0), stop=(kc == 7)), r=[mt[b], wo], w=[pO[half]])
                P.op("dve", lambda e, half=half, b=b, j=j: e.tensor_tensor(out=x1[b][:, half * 512:(half + 1) * 512], in0=pO[half][:],
                                                                          in1=bc[j][0][:, half * 512:(half + 1) * 512], op=ALU.mult),
                     r=[pO[half], bc[j][0]], w=[(x1[b].name, half)])
            P.op("pool", lambda e, b=b: e.tensor_tensor(out=x1[b][:], in0=x1[b][:], in1=xt[b][:], op=ALU.add),
                 r=[xt[b]], w=[(x1[b].name, 0), (x1[b].name, 1)])
            P.dma("pool", dst, x1[b][:], r=[(x1[b].name, 0), (x1[b].name, 1)], sem=("x1st", b))
            P.op("act", lambda e, b=b: e.activation(out=junk[:], in_=x1[b][:], func=AF.Square, accum_out=ss[b][:]), r=[(x1[b].name, 0), (x1[b].name, 1)], w=[junk, ss[b]])
            P.op("dve", lambda e, b=b: e.tensor_scalar(out=ss[b][:], in0=ss[b][:], scalar1=1.0 / D, scalar2=1e-6, op0=ALU.mult, op1=ALU.add),
                 r=[ss[b]], w=[ss[b]])
            P.op("act", lambda e, b=b: e.activation(out=ss[b][:], in_=ss[b][:], func=AF.Sqrt), r=[ss[b]], w=[ss[b]])
            P.op("dve", lambda e, b=b: e.reciprocal(out=ss[b][:], in_=ss[b][:]), r=[ss[b]], w=[ss[b]])
            P.op("dve", lambda e, b=b, j=j: e.scalar_tensor_tensor(out=h2f[b][:], in0=x1[b][:], scalar=ss[b][:, 0:1], in1=bc[j][1][:], op0=ALU.mult, op1=ALU.mult),
                 r=[(x1[b].name, 0), (x1[b].name, 1), ss[b], bc[j][1]], w=[h2f[b]])
            P.op("pool", lambda e, b=b, j=j: e.tensor_tensor(out=h2f[b][:], in0=h2f[b][:], in1=bc[j][2][:], op=ALU.add), r=[h2f[b], bc[j][2]], w=[h2f[b]])
            P.op("act", lambda e, b=b: e.activation(out=h2b[b][:], in_=h2f[b][:], func=AF.Copy), r=[h2f[b]], w=[h2b[b]])
            P.dma("sync", h2dst, h2b[b][:], r=[h2b[b]], sem=("h2st", b))
            for half in range(2):
                for k4 in range(4):
                    kc = half * 4 + k4
                    P.op("pe", lambda e, half=half, k4=k4, kc=kc, b=b: e.transpose(out=pT[half][:, k4, :], in_=h2f[b][:, kc * 128:(kc + 1) * 128],
                                                                                identity=g.consts[:, C_ID, :]), r=[h2f[b], g.consts], w=[pT[half]])
                if half == 0:
                    P.op("act", lambda e, half=half: e.activation(out=h2T[:, 0:4, :], in_=pT[0][:], func=AF.Copy), r=[pT[0]], w=[("h2T", 0)])
                else:
                    P.op("dve", lambda e, half=half: e.tensor_copy(out=h2T[:, 4:8, :], in_=pT[1][:]), r=[pT[1]], w=[("h2T", 1)])
            for kc in range(8):
                P.op("pe", lambda e, kc=kc: e.matmul(out=pL, lhsT=h2T[:, kc, :], rhs=rw[:, kc, :], start=(kc == 0), stop=(kc == 7)),
                     r=[("h2T", 0), ("h2T", 1), rw], w=["pL"])
            P.op("dve", lambda e: e.tensor_copy(out=lg[:], in_=pL), r=["pL"], w=[lg])
            P.op("dve", lambda e: e.tensor_reduce(out=mx[:], in_=lg[:], axis=AX.X, op=ALU.max), r=[lg], w=[mx])
            P.op("dve", lambda e: e.tensor_scalar(out=mx[:], in0=mx[:], scalar1=-1.0, scalar2=None, op0=ALU.mult), r=[mx], w=[mx])
            P.op("act", lambda e: e.activation(out=aff[:], in_=lg[:], func=AF.Exp, bias=mx[:, 0:1], accum_out=sm[:]), r=[lg, mx], w=[aff, sm])
            P.op("dve", lambda e: e.reciprocal(out=sm[:], in_=sm[:]), r=[sm], w=[sm])
            P.op("dve", lambda e: e.tensor_scalar(out=aff[:], in0=aff[:], scalar1=sm[:, 0:1], scalar2=None, op0=ALU.mult), r=[aff, sm], w=[aff])
            P.op("pe", lambda e: e.transpose(out=pA, in_=aff[:], identity=g.consts[:, C_ID, :]), r=[aff, g.consts], w=["pA"])
            P.op("act", lambda e, i=i: e.activation(out=g.affT[:, i * 128:(i + 1) * 128], in_=pA, func=AF.Copy), r=["pA"], w=[("affT", i)])
        P.barrier()
        P.emit()


def phase_moe(g, l):
    nc, I, S = g.nc, g.I, g.S
    with ExitStack() as es:
        def sb(name, shape, dt=F32):
            return es.enter_context(nc.sbuf_tensor("m%d_" % l + name, list(shape), dt))

        def psb(name, shape, dt=F32):
            return es.enter_context(nc.psum_tensor("m%d_" % l + name, list(shape), dt))
        work = sb("work", [NE, SEQ])
        vals = sb("vals", [NE, CAP_L])
        idxu = sb("idxu", [NE, CAP_L], U32)
        idxf = sb("idxf", [NE, CAP_L])
        idxT = sb("idxT", [128, 4, NE], I32)
        gT = sb("gT", [128, 4, NE])
        gt2 = [sb("gt2_%d" % j, [128, D]) for j in range(2)]
        wgt = [sb("wg%d" % i, [128, 8, D], BF16) for i in range(2)]
        wut = [sb("wu%d" % i, [128, 8, D], BF16) for i in range(2)]
        wdt = [sb("wd%d" % i, [128, 8, D], BF16) for i in range(2)]
        xs = [sb("xs%d" % i, [128, D], BF16) for i in range(2)]
        xsT = sb("xsT", [128, 8, 512], BF16)
        hidT = sb("hidT", [128, 8, 512], BF16)
        sg = [sb("sg%d" % i, [128, 512]) for i in range(2)]
        y = [sb("y%d" % i, [128, D]) for i in range(2)]
        pX = [psb("pX%d" % i, [128, 8, 128], BF16) for i in range(2)]
        pGs = [psb("pG%d" % i, [128, 512]) for i in range(2)]
        pUs = [psb("pU%d" % i, [128, 512]) for i in range(2)]
        pY = [psb("pY%d" % i, [128, 512]) for i in range(2)]
        pTi_t = pY[1]
        pTi = pY[1][:, :].rearrange("p (a b) -> p a b", a=32)
        P = Prog(nc)
        for j in range(2):
            P.dma("sync", gt2[j][:], S.modv[g.wi(l), j, 5:6, :].to_broadcast([128, D]), w=[gt2[j]])
        sets = [(0, CTX, SEQ, CAP_L, S.h2l, (g.out if l == g.last else S.xres))]
        if l == 0:
            sets.append((1, 0, CTX, CAP_C, S.h2c, S.xcres))
        wi = 0
        xi = 0
        yi = 0
        for (j, a0, N, cap, h2src, dest) in sets:
            nch = (cap + 127) // 128
            npc = min(cap, 128)
            akeys = [("affT", i) for i in range(a0 // 128, (a0 + N) // 128)]
            P.op("pool", lambda e, a0=a0, N=N: e.tensor_copy(out=work[:, 0:N], in_=g.affT[:, a0:a0 + N]), r=akeys, w=[work])
            for r8 in range(cap // 8):
                P.op("dve", lambda e, r8=r8, N=N: e.max(out=vals[:, r8 * 8:(r8 + 1) * 8], in_=work[:, 0:N]), r=[work], w=[vals])
                P.op("dve", lambda e, r8=r8, N=N: e.max_index(out=idxu[:, r8 * 8:(r8 + 1) * 8], in_max=vals[:, r8 * 8:(r8 + 1) * 8], in_values=work[:, 0:N]),
                     r=[work, vals], w=[idxu])
                P.op("dve", lambda e, r8=r8, N=N: e.match_replace(out=work[:, 0:N], in_to_replace=vals[:, r8 * 8:(r8 + 1) * 8], in_values=work[:, 0:N], imm_value=-1.0),
                     r=[work, vals], w=[work])
            P.op("dve", lambda e, cap=cap: e.tensor_copy(out=idxf[:, 0:cap], in_=idxu[:, 0:cap]), r=[idxu], w=[idxf])
            for ch in range(nch):
                P.op("pe", lambda e, ch=ch, npc=npc: e.transpose(out=pTi[0:npc, 0, :], in_=idxf[:, ch * 128:ch * 128 + npc], identity=g.consts[0:NE, C_ID, 0:NE]),
                     r=[idxf, g.consts], w=[pTi_t])
                P.op("pe", lambda e, ch=ch, npc=npc: e.transpose(out=pTi[0:npc, 1, :], in_=vals[:, ch * 128:ch * 128 + npc], identity=g.consts[0:NE, C_ID, 0:NE]),
                     r=[vals, g.consts], w=[pTi_t])
                P.op("dve", lambda e, ch=ch, npc=npc: e.tensor_copy(out=idxT[0:npc, ch, :], in_=pTi[0:npc, 0, :]), r=[pTi_t], w=[idxT])
                P.op("dve", lambda e, ch=ch, npc=npc: e.tensor_copy(out=gT[0:npc, ch, :], in_=pTi[0:npc, 1, :]), r=[], w=[gT, pTi_t])
            ncol = nch * npc
            for ex in range(NE):
                wb_ = wi % 2
                wi += 1
                P.dma("pool", wgt[wb_][:], I.wg[g.wi(l), ex].rearrange("(kc p) n -> p kc n", p=128), w=[wgt[wb_]])
                P.dma("pool", wut[wb_][:], I.wu[g.wi(l), ex].rearrange("(kc p) n -> p kc n", p=128), w=[wut[wb_]])
                P.dma("pool", wdt[wb_][:], I.wd[g.wi(l), ex].rearrange("(kc p) n -> p kc n", p=128), w=[wdt[wb_]])
                for ch in range(nch):
                    xb = xs[xi % 2]
                    xi += 1
                    P.dma_fn("pool", lambda e, xb=xb, ch=ch, ex=ex, npc=npc, h2src=h2src: e.indirect_dma_start(
                        out=xb[0:npc, :], out_offset=None, in_=h2src[:, :],
                        in_offset=bass.IndirectOffsetOnAxis(ap=idxT[0:npc, ch, ex:ex + 1], axis=0)),
                        r=[idxT], w=[xb], sem=("xg", xb.name))
                    for half in range(2):
                        for k4 in range(4):
                            kc = half * 4 + k4
                            P.op("pe", lambda e, half=half, k4=k4, kc=kc, xb=xb, npc=npc: e.transpose(out=pX[half][:, k4, 0:npc], in_=xb[0:npc, kc * 128:(kc + 1) * 128],
                                                                                                 identity=g.identb[0:npc, 0:npc]), r=[xb, g.identb], w=[pX[half]])
                        if half == 0:
                            P.op("act", lambda e, ch=ch, npc=npc: e.activation(out=xsT[:, 0:4, ch * 128:ch * 128 + npc], in_=pX[0][:, 0:4, 0:npc], func=AF.Copy),
                                 r=[pX[0]], w=[("xsT", 0)])
                        else:
                            P.op("dve", lambda e, ch=ch, npc=npc: e.tensor_copy(out=xsT[:, 4:8, ch * 128:ch * 128 + npc], in_=pX[1][:, 0:4, 0:npc]),
                                 r=[pX[1]], w=[("xsT", 1)])
                for fc in range(8):
                    pG = pGs[fc % 2]
                    pU = pUs[fc % 2]
                    for kc in range(8):
                        P.op("pe", lambda e, fc=fc, kc=kc, wb_=wb_, ncol=ncol, pG=pG: e.matmul(out=pG[:, 0:ncol], lhsT=wgt[wb_][:, kc, fc * 128:(fc + 1) * 128], rhs=xsT[:, kc, 0:ncol],
                                                                                     start=(kc == 0), stop=(kc == 7)), r=[wgt[wb_], ("xsT", 0), ("xsT", 1)], w=[pG])
                    for kc in range(8):
                        P.op("pe", lambda e, fc=fc, kc=kc, wb_=wb_, ncol=ncol, pU=pU: e.matmul(out=pU[:, 0:ncol], lhsT=wut[wb_][:, kc, fc * 128:(fc + 1) * 128], rhs=xsT[:, kc, 0:ncol],
                                                                                     start=(kc == 0), stop=(kc == 7)), r=[wut[wb_], ("xsT", 0), ("xsT", 1)], w=[pU])
                    s_ = sg[fc % 2]
                    P.op("act", lambda e, s_=s_, ncol=ncol, pG=pG: e.activation(out=s_[:, 0:ncol], in_=pG[:, 0:ncol], func=AF.Silu), r=[pG], w=[s_])
                    P.op("dve", lambda e, s_=s_, fc=fc, ncol=ncol, pU=pU: e.tensor_tensor(out=hidT[:, fc, 0:ncol], in0=pU[:, 0:ncol], in1=s_[:, 0:ncol], op=ALU.mult),
                         r=[pU, s_], w=[("hidT", fc)])
                hk = [("hidT", fc) for fc in range(8)]
                for ch in range(nch):
                    yb = y[yi % 2]
                    yi += 1
                    for half in range(2):
                        for fc in range(8):
                            P.op("pe", lambda e, half=half, fc=fc, ch=ch, wb_=wb_, npc=npc: e.matmul(out=pY[half][0:npc, :], lhsT=hidT[:, fc, ch * 128:ch * 128 + npc],
                                                                                                 rhs=wdt[wb_][:, fc, half * 512:(half + 1) * 512], start=(fc == 0), stop=(fc == 7)),
                                 r=hk + [wdt[wb_]], w=[pY[half]])
                        P.op("dve", lambda e, half=half, yb=yb, ch=ch, ex=ex, npc=npc, j=j: e.scalar_tensor_tensor(
                            out=yb[0:npc, half * 512:(half + 1) * 512], in0=pY[half][0:npc, :], scalar=gT[0:npc, ch, ex:ex + 1],
                            in1=gt2[j][0:npc, half * 512:(half + 1) * 512], op0=ALU.mult, op1=ALU.mult), r=[pY[half], gT, gt2[j]], w=[(yb.name, half)])
                    P.dma_fn("pool", lambda e, yb=yb, ch=ch, ex=ex, npc=npc, dest=dest: e.indirect_dma_start(
                        out=dest[:, :], out_offset=bass.IndirectOffsetOnAxis(ap=idxT[0:npc, ch, ex:ex + 1], axis=0),
                        in_=yb[0:npc, :], in_offset=None, compute_op=ALU.add),
                        r=[(yb.name, 0), (yb.name, 1), idxT], w=[("dest", j)], sem=("ysc", j))
        P.barrier()
        P.emit()


def phase_zero_mix(g, l):
    nc, S = g.nc, g.S
    with ExitStack() as es:
        z = es.enter_context(nc.sbuf_tensor("z%d_z" % l, [128, NT], BF16))
        P = Prog(nc)
        P.op("pool", lambda e: e.memset(z[:], 0.0), w=[z])
        for r in range(2, 8):
            P.dma("sync", S.mixT[r * 128:(r + 1) * 128, :], z[:], r=[z], sem="zst")
        P.barrier()
        P.emit()


def prep_inputs(inputs):
    f = lambda a: np.ascontiguousarray(np.asarray(a, dtype=np.float32))
    x = f(inputs["x"])
    c = f(inputs["c"])
    ctx = f(inputs["ctx"])
    c_ctx = f(inputs["c_ctx"])
    shared = {
        "ada_w": f(inputs["ada_w"]),
        "ada_b": f(inputs["ada_b"]).reshape(2, 1, 6 * D),
        "norm1_g": f(inputs["norm1_g"]).reshape(2, 1, D),
        "norm2_g": f(inputs["norm2_g"]).reshape(2, 1, D),
        "w_in": f(inputs["w_in"]),
        "w_out": f(inputs["w_out"]),
        "conv_wT": f(np.transpose(f(inputs["conv_w"]), (0, 2, 1))),
        "q_norm_g": f(inputs["q_norm_g"]).reshape(2, 1, 64),
        "k_norm_g": f(inputs["k_norm_g"]).reshape(2, 1, 64),
        "rw_mu": f(inputs["rw_mu"]).reshape(2, 1184, 1),
        "rw_w0": f(inputs["rw_w0"]).reshape(2, 512, 1),
        "rw_w_b": f(inputs["rw_w_b"]).reshape(2, 128, 256),
        "rw_a0": f(inputs["rw_a0"]).reshape(2, 512, 1),
        "rw_a_b": f(inputs["rw_a_b"]).reshape(2, 128, 256),
        "rw_g_b": f(inputs["rw_g_b"]),
        "rw_k_k": f(inputs["rw_k_k"]).reshape(2, 256, 1),
        "rw_k_a": f(inputs["rw_k_a"]).reshape(2, 256, 1),
        "rw_r_k": f(inputs["rw_r_k"]).reshape(2, 256, 1),
        "rw_ln_w": f(inputs["rw_ln_w"]).reshape(2, 256, 1),
        "rw_ln_b": f(inputs["rw_ln_b"]).reshape(2, 256, 1),
        "router_w": f(inputs["router_w"]),
        "exp_w_gate": f(inputs["exp_w_gate"]),
        "exp_w_up": f(inputs["exp_w_up"]),
        "exp_w_down": f(inputs["exp_w_down"]),
        "consts": make_consts(),
    }
    t = np.arange(SEQ)
    row = (t // 64).astype(np.float32)
    col = (t % 64).astype(np.float32)
    inv = (10000.0 ** (-np.arange(0, 32, 2, dtype=np.float32) / 32)).astype(np.float32)
    ang = np.concatenate([row[:, None] * inv, col[:, None] * inv], axis=-1).astype(np.float32)
    shared["cs_tab"] = np.ascontiguousarray(np.concatenate([np.cos(ang), np.sin(ang)], axis=-1).astype(np.float32))
    maps = []
    for b in range(x.shape[0]):
        m = dict(shared)
        m["x"] = x[b]
        m["ctx"] = ctx[b]
        c2 = np.stack([c[b], c_ctx], axis=-1)
        m["c2T"] = np.ascontiguousarray(c2.reshape(8, 128, 2).transpose(1, 0, 2))
        maps.append(m)
    return maps


_NC_CACHE = {}

W_KEYS = ["ada_w", "ada_b", "norm1_g", "norm2_g", "w_in", "w_out", "conv_wT", "q_norm_g", "k_norm_g", "rw_mu", "rw_w0", "rw_w_b",
          "rw_a0", "rw_a_b", "rw_g_b", "rw_k_k", "rw_k_a", "rw_r_k", "rw_ln_w", "rw_ln_b", "router_w",
          "exp_w_gate", "exp_w_up", "exp_w_down"]


def kernel(**inputs):
    maps = prep_inputs(inputs)
    if "nc" not in _NC_CACHE:
        _NC_CACHE["nc"] = build(layers=[0, 1])
    nc = _NC_CACHE["nc"]
    res = run_bass_kernel_spmd(nc, maps, core_ids=list(range(8)))
    return np.stack([np.asarray(r["out"], dtype=np.float32) for r in res.results], axis=0)
```

```python
import math
from contextlib import ExitStack

import numpy as np
import concourse.bass as bass
import concourse.mybir as mybir
from concourse.bass_utils import run_bass_kernel_spmd

F32 = mybir.dt.float32
BF16 = mybir.dt.bfloat16
I32 = mybir.dt.int32
F32R = mybir.dt.float32r
U32 = mybir.dt.uint32
AF = mybir.ActivationFunctionType
ALU = mybir.AluOpType
AX = mybir.AxisListType

ENGS = ("sync", "act", "dve", "pool", "pe")

D = 1024
SEQ = 4096
CTX = 256
NT = SEQ + CTX
NTILE = NT // 128
PROJ = 2720
NE = 16
CAP_L = 512
CAP_C = 32
LCH = 64


class Prog:
    SEMID = 0

    def __init__(self, nc):
        self.nc = nc
        self.streams = {e: [] for e in ENGS}
        self.ecount = {e: 0 for e in ENGS}
        self.seen = {e: {} for e in ENGS}
        self.bufs = {}
        self.dcount = {}
        self.sems = {}

    @staticmethod
    def _k(b):
        if isinstance(b, (str, tuple, int)):
            return b
        return b.name

    def _deps(self, eng, reads, writes):
        need = {}

        def add(ev):
            for k, v in ev.items():
                if need.get(k, 0) < v:
                    need[k] = v

        for b in reads:
            st = self.bufs.get(b)
            if st:
                add(st["w"])
        for b in writes:
            st = self.bufs.get(b)
            if st:
                add(st["w"])
                add(st["r"])
        waits = []
        seen = self.seen[eng]
        for k, v in need.items():
            if k[0] == "e" and k[1] == eng and eng == "pe":
                continue
            if seen.get(k, 0) >= v:
                continue
            seen[k] = v
            waits.append((k, v))
        return waits

    def _commit(self, reads, writes, ev):
        for b in reads:
            st = self.bufs.setdefault(b, {"w": {}, "r": {}})
            for k, v in ev.items():
                if st["r"].get(k, 0) < v:
                    st["r"][k] = v
        for b in writes:
            self.bufs[b] = {"w": dict(ev), "r": {}}

    EPOCH = 3000

    def op(self, eng, fn, r=(), w=(), rows=None):
        r = [self._k(b) for b in r]
        w = [self._k(b) for b in w]
        bk = [k for k in r if isinstance(k, tuple) and k[0] in ("pqb", "sqb")]
        if bk:
            r = [k for k in r if k not in bk]
            w = w + bk
        waits = self._deps(eng, r, w)
        self.ecount[eng] += 1
        ep = (self.ecount[eng] - 1) // self.EPOCH
        ek = ("e", eng, ep)
        ev = {ek: self.ecount[eng] - ep * self.EPOCH}
        if eng == "pe":
            if not hasattr(self, "pe_rows"):
                self.pe_rows = {}
            for bk_ in w:
                last = self.pe_rows.get(bk_)
                if last is not None and rows in (0, 64) and last[0] in (0, 64) and last[0] != rows:
                    for k_, v_ in last[1].items():
                        if self.seen[eng].get(k_, 0) < v_:
                            self.seen[eng][k_] = v_
                            waits.append((k_, v_))
                self.pe_rows[bk_] = (rows, ev)
        self.streams[eng].append((waits, fn, (ek, 1)))
        self._commit(r, w, ev)

    def dma(self, q, out, in_, r=(), w=(), sem=None, **kw):
        r = [self._k(b) for b in r]
        w = [self._k(b) for b in w]
        if sem is None:
            sem = (w[0] if w else r[0])
        waits = self._deps(q, r, w)
        k = ("d", sem)
        self.dcount[k] = self.dcount.get(k, 0) + 16
        ev = {k: self.dcount[k]}
        self.streams[q].append((waits, (lambda e: e.dma_start(out=out, in_=in_, **kw)), (k, 16)))
        self._commit(r, w, ev)

    def dma_fn(self, q, fn, r=(), w=(), sem=None):
        r = [self._k(b) for b in r]
        w = [self._k(b) for b in w]
        waits = self._deps(q, r, w)
        k = ("d", sem)
        self.dcount[k] = self.dcount.get(k, 0) + 16
        ev = {k: self.dcount[k]}
        self.streams[q].append((waits, fn, (k, 16)))
        self._commit(r, w, ev)

    def barrier(self):
        for eng in ENGS:
            waits = [(k, v) for k, v in self.dcount.items()]
            for e in ENGS:
                if e != eng and self.ecount[e] > 0:
                    ep = (self.ecount[e] - 1) // self.EPOCH
                    waits.append((("e", e, ep), self.ecount[e] - ep * self.EPOCH))
            self.streams[eng].append((waits, None, None))

    POOL = None

    def emit(self):
        nc = self.nc
        pool = Prog.POOL
        totals = {}
        keys = []
        for e in ENGS:
            for waits, fn, inc in self.streams[e]:
                for k, v in waits:
                    if k not in totals:
                        totals[k] = 0
                        keys.append(k)
                if inc:
                    if inc[0] not in totals:
                        totals[inc[0]] = 0
                        keys.append(inc[0])
                    totals[inc[0]] += inc[1]
        n = len(pool["h"])
        assert len(keys) <= n, len(keys)
        base = {}
        for i, k in enumerate(sorted(keys, key=str)):
            idx = (pool["next"] + i) % n
            self.sems[k] = pool["h"][idx]
            base[k] = pool["v"][idx]
            pool["v"][idx] += totals[k]
        pool["next"] = (pool["next"] + len(keys)) % n
        with ExitStack() as es:
            block = es.enter_context(nc.Block())
            handles = {"sync": block.sync, "act": block.scalar, "dve": block.vector,
                       "pool": block.gpsimd, "pe": block.tensor}
            for e in ENGS:
                stream = self.streams[e]

                def body(h, stream=stream):
                    for waits, fn, inc in stream:
                        for k, v in waits:
                            h.wait_ge(self.sems[k], base[k] + v)
                        if fn is not None:
                            ins = fn(h)
                            ins.then_inc(self.sems[inc[0]], inc[1])

                handles[e](body)


class Ctx:
    pass


def build(n_layers=2, dbg=None, upto=None, skip=(), small=False, rwsteps=None, zero_mix=False, layers=None):
    dbg = dbg or set()
    nc = bass.Bass("TRN2", target_bir_lowering=False)
    g = Ctx()
    g.nc = nc
    if layers is None:
        layers = list(range(n_layers))
    NL = len(layers)
    g.first = layers[0]
    g.last = layers[-1]
    g.wi = lambda l: l - layers[0]
    if layers[-1] == 0:
        dbg = set(dbg) | {"xcres"}

    def din(name, shape, dt=F32):
        return nc.dram_tensor(name, list(shape), dt, kind="ExternalInput").ap()

    def dscr(name, shape, dt=F32):
        kind = "ExternalOutput" if name in dbg else "Internal"
        return nc.dram_tensor(name, list(shape), dt, kind=kind).ap()

    I = Ctx()
    I.x = din("x", [SEQ, D])
    I.ctx = din("ctx", [CTX, D])
    I.c2T = din("c2T", [128, 8, 2])
    I.ada_w = din("ada_w", [NL, D, 6 * D])
    I.ada_b = din("ada_b", [NL, 1, 6 * D])
    I.n1g = din("norm1_g", [NL, 1, D])
    I.n2g = din("norm2_g", [NL, 1, D])
    I.w_in = din("w_in", [NL, D, PROJ])
    I.w_out = din("w_out", [NL, D, D])
    I.conv_wT = din("conv_wT", [NL, 256, 3])
    I.qg = din("q_norm_g", [NL, 1, 64])
    I.kg = din("k_norm_g", [NL, 1, 64])
    I.mu = din("rw_mu", [NL, 1184, 1])
    I.w0 = din("rw_w0", [NL, 512, 1])
    I.w_b = din("rw_w_b", [NL, 128, 256])
    I.a0 = din("rw_a0", [NL, 512, 1])
    I.a_b = din("rw_a_b", [NL, 128, 256])
    I.g_b = din("rw_g_b", [NL, 160, 256])
    I.k_k = din("rw_k_k", [NL, 256, 1])
    I.k_a = din("rw_k_a", [NL, 256, 1])
    I.r_k = din("rw_r_k", [NL, 256, 1])
    I.ln_w = din("rw_ln_w", [NL, 256, 1])
    I.ln_b = din("rw_ln_b", [NL, 256, 1])
    I.router = din("router_w", [NL, D, NE])
    esh = [NL, 1, 8, 8] if small else [NL, NE, D, D]
    I.wg = din("exp_w_gate", esh)
    I.wu = din("exp_w_up", esh)
    I.wd = din("exp_w_down", esh)
    g.rwsteps = rwsteps
    I.cs = din("cs_tab", [SEQ, 64])
    I.consts = din("consts", [128, 8 * 128])
    out = nc.dram_tensor("out", [SEQ, D], F32, kind="ExternalOutput").ap()

    S = Ctx()
    S.modv = dscr("modv", [NL, 2, 6, D])
    S.pfm = dscr("pfm", [1952, NT])
    S.qT = dscr("qT", [8, 64, NT], BF16)
    S.mixT = dscr("mixT", [D, NT], BF16)
    S.xres = dscr("xres", [SEQ, D])
    S.xcres = dscr("xcres", [CTX, D])
    S.h2l = dscr("h2l", [SEQ, D], BF16)
    S.h2c = dscr("h2c", [CTX, D], BF16)
    S.rwf = dscr("rwf", [10, 256, NT])
    g.I, g.S, g.out = I, S, out

    with ExitStack() as gs:
        def gsb(name, shape, dt=F32):
            return gs.enter_context(nc.sbuf_tensor("g_" + name, list(shape), dt))
        Prog.POOL = {"h": [gs.enter_context(nc.semaphore("gp%d" % i)) for i in range(72)], "v": [0] * 72, "next": 0}
        g.consts = gsb("consts", [128, 8, 128])
        g.identb = gsb("identb", [128, 128], BF16)
        g.kT = gsb("kT", [128, 2, NT], BF16)
        g.Vaug = gsb("Vaug", [128, NTILE, 2, 65], BF16)
        g.affT = gsb("affT", [NE, NT])
        phase_consts(g)
        phases = [phase_adaln, phase_proj, phase_attn, phase_rwfeat, phase_rwscan] + ([phase_zero_mix] if zero_mix else []) + [phase_wout, phase_moe]
        for l in layers:
            for ph in phases:
                if ph.__name__ in skip:
                    continue
                ph(g, l)
                if upto == (ph.__name__, l):
                    return nc
    return nc


C_ID, C_BONES, C_MS_IT, C_MI_IT, C_MS_TI, C_RESET, C_MS_IT_B, C_MI_IT_B = range(8)


def make_consts():
    c = np.zeros((8, 128, 128), np.float32)
    i = np.arange(128)[:, None]
    t = np.arange(128)[None, :]
    same = (i // 64) == (t // 64)
    c[C_ID] = np.eye(128)
    c[C_BONES] = same
    c[C_MS_IT] = same & (i < t)
    c[C_MI_IT] = same & (i <= t)
    c[C_MS_TI] = same & (t < i)
    c[C_RESET] = (t % 64 != 0) * np.ones((128, 1))
    c[C_MS_IT_B] = same & (i > t)
    c[C_MI_IT_B] = same & (i >= t)
    return np.ascontiguousarray(c.transpose(1, 0, 2).reshape(128, 8 * 128))


def phase_consts(g):
    nc = g.nc
    P = Prog(nc)
    P.dma("sync", g.consts[:], g.I.consts.rearrange("p (a b) -> p a b", a=8), w=[g.consts])
    P.op("dve", lambda e: e.tensor_copy(out=g.identb[:], in_=g.consts[:, C_ID, :]), r=[g.consts], w=[g.identb])
    P.op("pool", lambda e: e.memset(g.Vaug[:, :, :, 64:65], 1.0), w=[g.Vaug])
    P.barrier()
    P.emit()


def phase_adaln(g, l):
    nc, I, S = g.nc, g.I, g.S
    with ExitStack() as es:
        def sb(name, shape, dt=F32):
            return es.enter_context(nc.sbuf_tensor("a%d_" % l + name, list(shape), dt))
        c2 = sb("c2", [128, 8, 2])
        sc = sb("sc", [128, 8, 2])
        wt = [sb("wt%d" % i, [128, 8, 512]) for i in range(2)]
        bias = sb("bias", [2, 6 * D])
        mod = sb("mod", [2, 6 * D])
        gg = sb("gg", [2, 2, D])
        mv = sb("mv", [2, 6, D])
        ps = [es.enter_context(nc.psum_tensor("a%d_ps%d" % (l, i), [2, 512], F32)) for i in range(2)]
        P = Prog(nc)
        P.dma("sync", c2[:], I.c2T[:, :, :], w=[c2])
        P.dma("sync", bias[:], I.ada_b[g.wi(l), 0:1, :].to_broadcast([2, 6 * D]), w=[bias])
        P.dma("sync", gg[:, 0, :], I.n1g[g.wi(l), 0:1, :].to_broadcast([2, D]), w=[gg], sem="gg")
        P.dma("sync", gg[:, 1, :], I.n2g[g.wi(l), 0:1, :].to_broadcast([2, D]), w=[gg], sem="gg")
        P.op("act", lambda e: e.activation(out=sc[:], in_=c2[:], func=AF.Silu), r=[c2], w=[sc])
        wv = I.ada_w[g.wi(l)].rearrange("(kc p) n -> p kc n", p=128)
        for cc in range(12):
            b = cc % 2
            P.dma("sync" if b == 0 else "pool", wt[b][:], wv[:, :, cc * 512:(cc + 1) * 512], w=[wt[b]])
            for kc in range(8):
                P.op("pe", lambda e, kc=kc, b=b: e.matmul(out=ps[b][:], lhsT=sc[:, kc, :], rhs=wt[b][:, kc, :],
                                                           start=(kc == 0), stop=(kc == 7)),
                     r=[sc, wt[b]], w=[ps[b]])
            P.op("dve", lambda e, cc=cc, b=b: e.tensor_tensor(out=mod[:, cc * 512:(cc + 1) * 512], in0=ps[b][:],
                                                               in1=bias[:, cc * 512:(cc + 1) * 512], op=ALU.add),
                 r=[ps[b], bias], w=[mod])
        for j, (sci, shi, gti) in enumerate(((1, 0, 2), (4, 3, 5))):
            P.op("dve", lambda e, j=j, sci=sci: e.scalar_tensor_tensor(
                out=mv[:, 3 * j, :], in0=mod[:, sci * D:(sci + 1) * D], scalar=1.0, in1=gg[:, j, :],
                op0=ALU.add, op1=ALU.mult), r=[mod, gg], w=[mv])
            P.op("dve", lambda e, j=j, shi=shi: e.tensor_copy(out=mv[:, 3 * j + 1, :], in_=mod[:, shi * D:(shi + 1) * D]),
                 r=[mod], w=[mv])
            P.op("dve", lambda e, j=j, gti=gti: e.tensor_copy(out=mv[:, 3 * j + 2, :], in_=mod[:, gti * D:(gti + 1) * D]),
                 r=[mod], w=[mv])
        P.dma("sync", S.modv[g.wi(l)], mv[:], r=[mv], sem="mvst")
        P.barrier()
        P.emit()


def phase_proj(g, l):
    nc, I, S = g.nc, g.I, g.S
    with ExitStack() as es:
        def sb(name, shape, dt=F32):
            return es.enter_context(nc.sbuf_tensor("b%d_" % l + name, list(shape), dt))

        def psb(name, shape, dt=F32):
            return es.enter_context(nc.psum_tensor("b%d_" % l + name, list(shape), dt))
        wb = sb("wb", [128, 8, PROJ], BF16)
        m1 = [sb("m1_%d" % i, [128, D]) for i in range(2)]
        sh1 = [sb("sh1_%d" % i, [128, D]) for i in range(2)]
        qkg = sb("qkg", [128, 2, 64])
        cs = sb("cs", [128, 32, 64])
        xt = [sb("xt%d" % i, [128, D]) for i in range(2)]
        junk = sb("junk", [128, D])
        ss = [sb("ss%d" % i, [128, 1]) for i in range(2)]
        hb = [sb("hb%d" % i, [128, D], BF16) for i in range(2)]
        hT = [sb("hT%d" % i, [128, 8, 512], BF16) for i in range(2)]
        fm = [sb("fm%d" % i, [128, 512]) for i in range(3)]
        qkv = [sb("qkv%d" % i, [128, 768]) for i in range(2)]
        sq = sb("sq", [128, 640])
        ssq = sb("ssq", [128, 10])
        qn = sb("qn", [128, 10, 64])
        qr = sb("qr", [128, 10, 64])
        qrb = [sb("qrb%d" % i, [128, 10, 64], BF16) for i in range(2)]
        tmp = sb("tmp", [128, 10, 32])
        qTs = [sb("qTs%d" % i, [64, 8, 128], BF16) for i in range(2)]
        pT = [psb("pT%d" % i, [128, 8, 128], BF16) for i in range(2)]
        pF = [psb("pF%d" % i, [128, 512]) for i in range(2)]
        pA = psb("pA", [128, 512])
        pB = psb("pB", [128, 512])
        pQ = psb("pQ", [64, 8, 128], BF16)
        pQk = psb("pQk", [64, 8, 128], BF16)
        P = Prog(nc)
        wv = I.w_in[g.wi(l)].rearrange("(kc p) n -> p kc n", p=128)
        for (c0, c1) in ((0, 1024), (1024, 2048), (2048, PROJ)):
            P.dma("pool", wb[:, :, c0:c1], wv[:, :, c0:c1], w=[("wb", c0)], sem=("wb", c0))
        wbk = [("wb", 0), ("wb", 1024), ("wb", 2048)]
        for j in range(2):
            P.dma("sync", m1[j][:], S.modv[g.wi(l), j, 0:1, :].to_broadcast([128, D]), w=[m1[j]])
            P.dma("sync", sh1[j][:], S.modv[g.wi(l), j, 1:2, :].to_broadcast([128, D]), w=[sh1[j]])
        P.dma("sync", qkg[:, 0, :], I.qg[g.wi(l), 0:1, :].to_broadcast([128, 64]), w=[qkg], sem="qkg")
        P.dma("sync", qkg[:, 1, :], I.kg[g.wi(l), 0:1, :].to_broadcast([128, 64]), w=[qkg], sem="qkg")
        P.dma("sync", cs[:], I.cs.rearrange("(i p) c -> p i c", p=128), w=[cs])
        fchunks = [(c, 128, c) for c in range(0, 768, 128)]
        for j in range(10):
            c = 1536 + j * 128
            wdt = min(128, PROJ - c)
            fchunks.append((c, wdt, 768 + j * 128))
        sts = [(0, 2)] + [(2 + 4 * s, 4) for s in range(8)]
        fmi = 0
        for si, (t0, ntile) in enumerate(sts):
            hTs = hT[si % 2]
            ntok = ntile * 128
            for ti in range(ntile):
                i = t0 + ti
                b = i % 2
                j = 1 if i < 2 else 0
                if i < 2:
                    src = (I.ctx if l == g.first else S.xcres)[i * 128:(i + 1) * 128, :]
                else:
                    src = (I.x if l == g.first else S.xres)[(i - 2) * 128:(i - 1) * 128, :]
                P.dma("sync", xt[b][:], src, w=[xt[b]])
                P.op("act", lambda e, b=b: e.activation(out=junk[:], in_=xt[b][:], func=AF.Square, accum_out=ss[b][:]),
                     r=[xt[b]], w=[junk, ss[b]])
                P.op("dve", lambda e, b=b: e.tensor_scalar(out=ss[b][:], in0=ss[b][:], scalar1=1.0 / D, scalar2=1e-6,
                                                            op0=ALU.mult, op1=ALU.add), r=[ss[b]], w=[ss[b]])
                P.op("act", lambda e, b=b: e.activation(out=ss[b][:], in_=ss[b][:], func=AF.Sqrt), r=[ss[b]], w=[ss[b]])
                P.op("dve", lambda e, b=b: e.reciprocal(out=ss[b][:], in_=ss[b][:]), r=[ss[b]], w=[ss[b]])
                P.op("dve", lambda e, b=b, j=j: e.scalar_tensor_tensor(out=xt[b][:], in0=xt[b][:], scalar=ss[b][:, 0:1],
                                                                       in1=m1[j][:], op0=ALU.mult, op1=ALU.mult),
                     r=[xt[b], ss[b], m1[j]], w=[xt[b]])
                P.op("pool", lambda e, b=b, j=j: e.tensor_tensor(out=hb[b][:], in0=xt[b][:], in1=sh1[j][:], op=ALU.add),
                     r=[xt[b], sh1[j]], w=[hb[b]])
                for half in range(2):
                    for k4 in range(4):
                        kc = half * 4 + k4
                        P.op("pe", lambda e, b=b, kc=kc, k4=k4, half=half: e.transpose(
                            out=pT[half][:, k4, :], in_=hb[b][:, kc * 128:(kc + 1) * 128], identity=g.identb[:]),
                            r=[hb[b], g.identb], w=[pT[half]])
                    eng = "act" if half == 0 else "dve"
                    if eng == "act":
                        P.op("act", lambda e, half=half, ti=ti, hTs=hTs: e.activation(
                            out=hTs[:, half * 4:(half + 1) * 4, ti * 128:(ti + 1) * 128], in_=pT[half][:, 0:4, :], func=AF.Copy),
                            r=[pT[half]], w=[hTs])
                    else:
                        P.op("dve", lambda e, half=half, ti=ti, hTs=hTs: e.tensor_copy(
                            out=hTs[:, half * 4:(half + 1) * 4, ti * 128:(ti + 1) * 128], in_=pT[half][:, 0:4, :]),
                            r=[pT[half]], w=[hTs])
                for kc in range(8):
                    P.op("pe", lambda e, kc=kc, ti=ti, hTs=hTs: e.matmul(
                        out=pA[:], lhsT=hTs[:, kc, ti * 128:(ti + 1) * 128], rhs=wb[:, kc, 768:1280],
                        start=(kc == 0), stop=(kc == 7)), r=[hTs] + wbk, w=[pA])
                for kc in range(8):
                    P.op("pe", lambda e, kc=kc, ti=ti, hTs=hTs: e.matmul(
                        out=pB[:, 0:256], lhsT=hTs[:, kc, ti * 128:(ti + 1) * 128], rhs=wb[:, kc, 1280:1536],
                        start=(kc == 0), stop=(kc == 7)), r=[hTs] + wbk, w=[pB])
                qv = qkv[b]
                P.op("act", lambda e, qv=qv: e.activation(out=qv[:, 0:512], in_=pA[:], func=AF.Copy), r=[pA], w=[qv])
                P.op("act", lambda e, qv=qv: e.activation(out=qv[:, 512:768], in_=pB[:, 0:256], func=AF.Copy), r=[pB], w=[qv])
                P.op("pool", lambda e, qv=qv, i=i: e.tensor_copy(
                    out=g.Vaug[:, i, :, 0:64], in_=qv[:, 640:768].rearrange("p (g d) -> p g d", g=2)),
                    r=[qv], w=[("Vaug", i)])
                P.op("dve", lambda e, qv=qv: e.tensor_tensor(out=sq[:], in0=qv[:, 0:640], in1=qv[:, 0:640], op=ALU.mult),
                     r=[qv], w=[sq])
                P.op("dve", lambda e: e.tensor_reduce(out=ssq[:], in_=sq[:].rearrange("p (h d) -> p h d", h=10),
                                                       axis=AX.X, op=ALU.add), r=[sq], w=[ssq])
                P.op("dve", lambda e: e.tensor_scalar(out=ssq[:], in0=ssq[:], scalar1=1.0 / 64, scalar2=1e-6,
                                                       op0=ALU.mult, op1=ALU.add), r=[ssq], w=[ssq])
                P.op("act", lambda e: e.activation(out=ssq[:], in_=ssq[:], func=AF.Sqrt), r=[ssq], w=[ssq])
                P.op("dve", lambda e: e.reciprocal(out=ssq[:], in_=ssq[:]), r=[ssq], w=[ssq])
                P.op("dve", lambda e, qv=qv: e.tensor_tensor(
                    out=qn[:], in0=qv[:, 0:640].rearrange("p (h d) -> p h d", h=10),
                    in1=ssq[:].unsqueeze(2).to_broadcast([128, 10, 64]), op=ALU.mult), r=[qv, ssq], w=[qn])
                P.op("dve", lambda e: e.tensor_tensor(out=qn[:, 0:8, :], in0=qn[:, 0:8, :],
                                                       in1=qkg[:, 0:1, :].to_broadcast([128, 8, 64]), op=ALU.mult),
                     r=[qn, qkg], w=[qn])
                P.op("dve", lambda e: e.tensor_tensor(out=qn[:, 8:10, :], in0=qn[:, 8:10, :],
                                                       in1=qkg[:, 1:2, :].to_broadcast([128, 2, 64]), op=ALU.mult),
                     r=[qn, qkg], w=[qn])
                qb = qrb[b]
                if i < 2:
                    P.op("dve", lambda e, qb=qb: e.tensor_copy(out=qb[:], in_=qn[:]), r=[qn], w=[qb])
                else:
                    li = i - 2
                    cosb = cs[:, li:li + 1, 0:32].to_broadcast([128, 10, 32])
                    sinb = cs[:, li:li + 1, 32:64].to_broadcast([128, 10, 32])
                    x1 = qn[:, :, 0:32]
                    x2 = qn[:, :, 32:64]
                    P.op("dve", lambda e, cosb=cosb: e.tensor_tensor(out=qr[:, :, 0:32], in0=qn[:, :, 0:32], in1=cosb, op=ALU.mult),
                         r=[qn, cs], w=[qr])
                    P.op("dve", lambda e, sinb=sinb: e.tensor_tensor(out=tmp[:], in0=qn[:, :, 32:64], in1=sinb, op=ALU.mult),
                         r=[qn, cs], w=[tmp])
                    P.op("dve", lambda e, qb=qb: e.tensor_tensor(out=qb[:, :, 0:32], in0=qr[:, :, 0:32], in1=tmp[:], op=ALU.subtract),
                         r=[qr, tmp], w=[qb])
                    P.op("dve", lambda e, sinb=sinb: e.tensor_tensor(out=qr[:, :, 32:64], in0=qn[:, :, 0:32], in1=sinb, op=ALU.mult),
                         r=[qn, cs], w=[qr])
                    P.op("dve", lambda e, cosb=cosb: e.tensor_tensor(out=tmp[:], in0=qn[:, :, 32:64], in1=cosb, op=ALU.mult),
                         r=[qn, cs, qb], w=[tmp])
                    P.op("dve", lambda e, qb=qb: e.tensor_tensor(out=qb[:, :, 32:64], in0=qr[:, :, 32:64], in1=tmp[:], op=ALU.add),
                         r=[qr, tmp], w=[qb])
                for h in range(8):
                    P.op("pe", lambda e, h=h, qb=qb: e.transpose(out=pQ[:, h, :], in_=qb[:, h, :], identity=g.identb[:]),
                         r=[qb, g.identb], w=[pQ])
                for h in range(2):
                    P.op("pe", lambda e, h=h, qb=qb: e.transpose(out=pQk[:, h, :], in_=qb[:, 8 + h, :], identity=g.identb[:]),
                         r=[qb, g.identb], w=[pQk])
                qs = qTs[b]
                P.op("act", lambda e, qs=qs: e.activation(out=qs[:], in_=pQ[:], func=AF.Copy), r=[pQ], w=[qs])
                P.op("dve", lambda e, i=i: e.tensor_copy(out=g.kT[0:64, :, i * 128:(i + 1) * 128], in_=pQk[:, 0:2, :]),
                     r=[pQk], w=[("kT", i)])
                P.op("dve", lambda e, i=i: e.tensor_copy(out=g.kT[64:128, :, i * 128:(i + 1) * 128], in_=pQk[:, 0:2, :]),
                     r=[pQk], w=[("kT", i)])
                P.dma("pool", S.qT[:, :, i * 128:(i + 1) * 128].rearrange("h d t -> d h t"), qs[:], r=[qs], sem=("qs", b))
            for (c0, wdt, r0) in fchunks:
                pb = pF[fmi % 2]
                fb = fm[fmi % 3]
                for kc in range(8):
                    P.op("pe", lambda e, kc=kc, c0=c0, wdt=wdt, pb=pb, hTs=hTs, ntok=ntok: e.matmul(
                        out=pb[0:wdt, 0:ntok], lhsT=wb[:, kc, c0:c0 + wdt], rhs=hTs[:, kc, 0:ntok],
                        start=(kc == 0), stop=(kc == 7)), r=[hTs] + wbk, w=[pb])
                if fmi % 2 == 0:
                    P.op("act", lambda e, wdt=wdt, pb=pb, fb=fb, ntok=ntok: e.activation(
                        out=fb[0:wdt, 0:ntok], in_=pb[0:wdt, 0:ntok], func=AF.Copy), r=[pb], w=[fb])
                else:
                    P.op("dve", lambda e, wdt=wdt, pb=pb, fb=fb, ntok=ntok: e.tensor_copy(
                        out=fb[0:wdt, 0:ntok], in_=pb[0:wdt, 0:ntok]), r=[pb], w=[fb])
                P.dma("sync", S.pfm[r0:r0 + wdt, t0 * 128:t0 * 128 + ntok], fb[0:wdt, 0:ntok], r=[fb], sem=("fm", fmi % 3))
                fmi += 1
        P.barrier()
        P.emit()


def conv_ops(g, l, P, es):
    nc, I, S = g.nc, g.I, g.S
    todo = []
    if True:
        def sb(name, shape, dt=F32):
            return es.enter_context(nc.sbuf_tensor("c%d_" % l + name, list(shape), dt))
        Bt = sb("Bt", [128, SEQ])
        Ct = sb("Ct", [128, SEQ])
        Ut = sb("Ut", [128, SEQ])
        zp = sb("zp", [128, SEQ + 2])
        acc = sb("acc", [128, SEQ])
        ob = sb("ob", [128, SEQ], BF16)
        cw = sb("cw", [128, 2, 3])
        todo.append(lambda: P.dma("sync", cw[:], I.conv_wT[g.wi(l)].rearrange("(c p) k -> p c k", p=128), w=[cw]))
        seqs = [(CTX, SEQ)] + ([(0, CTX)] if l == 0 else [])
        for (t0, T) in seqs:
            for cc in range(2):
                todo.append(lambda T=T, cc=cc, t0=t0: P.dma("sync", Bt[:, 0:T], S.pfm[cc * 128:(cc + 1) * 128, t0:t0 + T], w=[Bt]))
                todo.append(lambda T=T, cc=cc, t0=t0: P.dma("sync", Ct[:, 0:T], S.pfm[256 + cc * 128:256 + (cc + 1) * 128, t0:t0 + T], w=[Ct]))
                todo.append(lambda T=T, cc=cc, t0=t0: P.dma("pool", Ut[:, 0:T], S.pfm[512 + cc * 128:512 + (cc + 1) * 128, t0:t0 + T], w=[Ut]))
                todo.append(lambda T=T, cc=cc, t0=t0: P.op("pool", lambda e, T=T: e.memset(zp[:, 0:1], 0.0), w=[zp]))
                todo.append(lambda T=T, cc=cc, t0=t0: P.op("pool", lambda e, T=T: e.memset(zp[:, T + 1:T + 2], 0.0), w=[zp]))
                todo.append(lambda T=T, cc=cc, t0=t0: P.op("dve", lambda e, T=T: e.tensor_tensor(out=zp[:, 1:T + 1], in0=Ct[:, 0:T], in1=Ut[:, 0:T], op=ALU.mult),
                     r=[Ct, Ut], w=[zp]))
                todo.append(lambda T=T, cc=cc, t0=t0: P.op("dve", lambda e, T=T, cc=cc: e.tensor_scalar(out=acc[:, 0:T], in0=zp[:, 0:T], scalar1=cw[:, cc, 0:1],
                                                                   scalar2=None, op0=ALU.mult), r=[zp, cw], w=[acc]))
                todo.append(lambda T=T, cc=cc, t0=t0: P.op("dve", lambda e, T=T, cc=cc: e.scalar_tensor_tensor(out=acc[:, 0:T], in0=zp[:, 1:T + 1], scalar=cw[:, cc, 1:2],
                                                                         in1=acc[:, 0:T], op0=ALU.mult, op1=ALU.add),
                     r=[zp, cw, acc], w=[acc]))
                todo.append(lambda T=T, cc=cc, t0=t0: P.op("dve", lambda e, T=T, cc=cc: e.scalar_tensor_tensor(out=acc[:, 0:T], in0=zp[:, 2:T + 2], scalar=cw[:, cc, 2:3],
                                                                         in1=acc[:, 0:T], op0=ALU.mult, op1=ALU.add),
                     r=[zp, cw, acc], w=[acc]))
                todo.append(lambda T=T, cc=cc, t0=t0: P.op("pool", lambda e, T=T: e.tensor_tensor(out=ob[:, 0:T], in0=acc[:, 0:T], in1=Bt[:, 0:T], op=ALU.mult),
                     r=[acc, Bt], w=[ob]))
                todo.append(lambda T=T, cc=cc, t0=t0: P.dma("sync", S.mixT[cc * 128:(cc + 1) * 128, t0:t0 + T], ob[:, 0:T], r=[ob], sem="obst"))
    return todo


def phase_attn(g, l):
    nc, I, S = g.nc, g.I, g.S
    with ExitStack() as es:
        def sb(name, shape, dt=F32):
            return es.enter_context(nc.sbuf_tensor("d%d_" % l + name, list(shape), dt))

        def psb(name, shape, dt=F32):
            return es.enter_context(nc.psum_tensor("d%d_" % l + name, list(shape), dt))
        qc = [sb("qc%d" % i, [128, 512], BF16) for i in range(2)]
        eS = [sb("eS%d" % i, [128, 512], BF16) for i in range(4)]
        rs = sb("rs", [128, 512])
        rsb = sb("rsb", [64, 512])
        ob = [sb("ob%d" % i, [64, 512], BF16) for i in range(2)]
        nb = sb("nb", [128, 1])
        pS = [psb("pS%d" % i, [128, 512]) for i in range(4)]
        pO = [psb("pO%d" % i, [128, 512]) for i in range(2)]
        pR = psb("pR", [64, 512])
        P = Prog(nc)
        P.op("pool", lambda e: e.memset(nb[:], -8.0), w=[nb])
        todo = conv_ops(g, l, P, es)
        jobs = []
        if l == 0:
            for h in range(8):
                jobs.append((h, 0, CTX, [0, 1]))
        for h in range(8):
            for qi in range(8):
                jobs.append((h, CTX + qi * 512, 512, list(range(NTILE))))
        cnt = 0
        for ji, (h, q0, nq, kts) in enumerate(jobs):
            gkv = h // 4
            qb = qc[ji % 2]
            po = pO[ji % 2]
            P.dma("sync", qb[0:64, 0:nq], S.qT[h, :, q0:q0 + nq], w=[qb], sem=("qb", ji % 2))
            P.dma("sync", qb[64:128, 0:nq], S.qT[h, :, q0:q0 + nq], w=[qb], sem=("qb", ji % 2))
            nk = len(kts)
            LOOK = 3
            slots = {}

            def emit_s(ki, cnt0=cnt, kts=kts, qb=qb, nq=nq, gkv=gkv):
                kt = kts[ki]
                ps = pS[(cnt0 + ki) % 4]
                ee = eS[(cnt0 + ki) % 4]
                rb = 64 * (ki % 2)
                P.op("pe", lambda e, ps=ps, kt=kt, rb=rb: e.matmul(
                    out=ps[:, 0:nq], lhsT=g.kT[rb:rb + 64, gkv, kt * 128:(kt + 1) * 128], rhs=qb[rb:rb + 64, 0:nq], start=True, stop=True),
                    r=[qb, ("kT", kt)], w=[ps], rows=rb)
                P.op("act", lambda e, ps=ps, ee=ee: e.activation(out=ee[:, 0:nq], in_=ps[:, 0:nq], func=AF.Exp,
                                                                 bias=nb[:, 0:1], scale=0.125),
                     r=[ps, nb], w=[ee])

            def emit_pv(ki, cnt0=cnt, kts=kts, po=po, nq=nq, gkv=gkv, nk=nk):
                kt = kts[ki]
                ee = eS[(cnt0 + ki) % 4]
                P.op("pe", lambda e, kt=kt, ee=ee: e.matmul(
                    out=po[0:65, 0:nq], lhsT=g.Vaug[:, kt, gkv, :], rhs=ee[:, 0:nq], start=(ki == 0), stop=(ki == nk - 1)),
                    r=[ee, ("Vaug", kt), g.Vaug], w=[po])

            for ki in range(nk + LOOK):
                if ki < nk:
                    emit_s(ki)
                if ki - LOOK >= 0:
                    emit_pv(ki - LOOK)
            cnt += nk
            P.op("dve", lambda e, po=po, nq=nq: e.reciprocal(out=rs[64:65, 0:nq], in_=po[64:65, 0:nq]), r=[po], w=[rs])
            P.op("pe", lambda e, nq=nq: e.matmul(out=pR[:, 0:nq], lhsT=g.consts[64:65, C_BONES, 64:128], rhs=rs[64:65, 0:nq],
                                                  start=True, stop=True), r=[rs, g.consts], w=[pR])
            P.op("act", lambda e, nq=nq: e.activation(out=rsb[:, 0:nq], in_=pR[:, 0:nq], func=AF.Copy), r=[pR], w=[rsb])
            o = ob[ji % 2]
            P.op("dve", lambda e, po=po, o=o, nq=nq: e.tensor_tensor(out=o[:, 0:nq], in0=po[0:64, 0:nq], in1=rsb[:, 0:nq], op=ALU.mult),
                 r=[po, rsb], w=[o])
            P.dma("pool", S.mixT[256 + h * 64:256 + (h + 1) * 64, q0:q0 + nq], o[:, 0:nq], r=[o], sem=("ob", ji % 2))
            for _ in range(2):
                if todo:
                    todo.pop(0)()
        while todo:
            todo.pop(0)()
        P.barrier()
        P.emit()


NEG_EM05 = -math.exp(-0.5)


def phase_rwfeat(g, l):
    nc, I, S = g.nc, g.I, g.S
    with ExitStack() as es:
        def sb(name, shape, dt=F32):
            return es.enter_context(nc.sbuf_tensor("e%d_" % l + name, list(shape), dt))

        def psb(name, shape, dt=F32):
            return es.enter_context(nc.psum_tensor("e%d_" % l + name, list(shape), dt))
        SEG = 512
        rch = [("r0", 768, 128), ("r1", 896, 128), ("k0", 1024, 128), ("k1", 1152, 128), ("v0", 1280, 128),
               ("v1", 1408, 128), ("wl", 1536, 128), ("al", 1664, 128), ("g0", 1792, 128), ("g1", 1920, 32)]
        mu = sb("mu", [128, 10])
        omu = sb("omu", [128, 10])
        hmu = sb("hmu", [128, 10])
        pt = [sb("pt%d" % i, [128, SEG + 2]) for i in range(3)]
        s1 = [sb("s1%d" % i, [128, SEG]) for i in range(2)]
        sh = {nm: sb("sh_" + nm, [128, SEG]) for nm, _, _ in rch}
        w0c = sb("w0c", [128, 4])
        a0c = sb("a0c", [128, 4])
        kkc = sb("kkc", [128, 2])
        kac = sb("kac", [128, 2])
        omka = sb("omka", [128, 2])
        wbt = sb("wbt", [128, 256])
        abt = sb("abt", [128, 256])
        gb0 = sb("gb0", [128, 256])
        gb1 = sb("gb1", [32, 256])
        twl = sb("twl", [128, SEG])
        sg0 = sb("sg0", [128, SEG])
        sg1 = sb("sg1", [32, SEG])
        lw = [sb("lw%d" % i, [128, SEG]) for i in range(4)]
        asg = [sb("asg%d" % i, [128, SEG]) for i in range(4)]
        kk = [sb("kk%d" % i, [128, SEG]) for i in range(2)]
        sq = sb("sq", [128, SEG])
        rn = sb("rn", [128, SEG])
        kd = [sb("kd%d" % i, [128, SEG]) for i in range(4)]
        bd = [sb("bd%d" % i, [128, SEG]) for i in range(4)]
        gt = [sb("gt%d" % i, [128, SEG]) for i in range(2)]
        tq = sb("tq", [128, SEG])
        pp = [psb("pp%d" % i, [128, SEG]) for i in range(6)]
        P = Prog(nc)
        P.op("pool", lambda e: e.memset(mu[:, 9:10], 0.0), w=[mu])
        for ci, (nm, r0, nr) in enumerate(rch):
            P.dma("sync", mu[0:nr, ci:ci + 1], I.mu[g.wi(l), r0 - 768:r0 - 768 + nr, :], w=[mu], sem="mu")
        P.op("dve", lambda e: e.tensor_scalar(out=omu[:], in0=mu[:], scalar1=-1.0, scalar2=1.0, op0=ALU.mult, op1=ALU.add),
             r=[mu], w=[omu])
        P.op("dve", lambda e: e.tensor_scalar(out=hmu[:], in0=mu[:], scalar1=0.5, scalar2=None, op0=ALU.mult), r=[mu], w=[hmu])
        for d in range(2):
            for cc in range(2):
                P.dma("sync", w0c[:, d * 2 + cc:d * 2 + cc + 1], I.w0[g.wi(l), d * 256 + cc * 128:d * 256 + (cc + 1) * 128, :], w=[w0c], sem="w0c")
                P.dma("sync", a0c[:, d * 2 + cc:d * 2 + cc + 1], I.a0[g.wi(l), d * 256 + cc * 128:d * 256 + (cc + 1) * 128, :], w=[a0c], sem="a0c")
        for cc in range(2):
            P.dma("sync", kkc[:, cc:cc + 1], I.k_k[g.wi(l), cc * 128:(cc + 1) * 128, :], w=[kkc], sem="kkc")
            P.dma("sync", kac[:, cc:cc + 1], I.k_a[g.wi(l), cc * 128:(cc + 1) * 128, :], w=[kac], sem="kac")
        P.op("dve", lambda e: e.tensor_scalar(out=omka[:], in0=kac[:], scalar1=-1.0, scalar2=1.0, op0=ALU.mult, op1=ALU.add),
             r=[kac], w=[omka])
        P.dma("sync", wbt[:], I.w_b[g.wi(l)], w=[wbt])
        P.dma("sync", abt[:], I.a_b[g.wi(l)], w=[abt])
        P.dma("sync", gb0[:], I.g_b[g.wi(l), 0:128, :], w=[gb0])
        P.dma("sync", gb1[:], I.g_b[g.wi(l), 128:160, :], w=[gb1])
        segs = [(0, 0, CTX, CTX)] + [(CTX, CTX + i * SEG, SEG, SEQ) for i in range(8)]
        pti = 0
        sti = 0
        ppi = 0

        def store(idx, cc, src, n, t0):
            nonlocal sti
            q = "sync" if sti % 2 == 0 else "pool"
            sti += 1
            P.dma(q, S.rwf[idx, cc * 128:(cc + 1) * 128, t0:t0 + n], src[:, 0:n], r=[src], sem=("st", src.name))

        for (sq0, t0, n, slen) in segs:
            for ci, (nm, r0, nr) in enumerate(rch):
                p_ = pt[pti % 3]
                s_ = s1[pti % 2]
                pti += 1
                lo = t0 - 1
                hi = t0 + n + 1
                dlo, dhi = 0, n + 2
                if t0 == sq0:
                    lo += 1
                    dlo = 1
                    P.op("pool", lambda e, p_=p_, nr=nr: e.memset(p_[0:nr, 0:1], 0.0), w=[p_])
                if t0 + n == sq0 + slen:
                    hi -= 1
                    dhi = n + 1
                    P.op("pool", lambda e, p_=p_, nr=nr, n=n: e.memset(p_[0:nr, n + 1:n + 2], 0.0), w=[p_])
                P.dma("sync" if ci % 2 == 0 else "pool", p_[0:nr, dlo:dhi], S.pfm[r0:r0 + nr, lo:hi], w=[p_])
                P.op("pool", lambda e, p_=p_, s_=s_, nr=nr, n=n: e.tensor_tensor(out=s_[0:nr, 0:n], in0=p_[0:nr, 0:n], in1=p_[0:nr, 2:n + 2], op=ALU.add),
                     r=[p_], w=[s_])
                P.op("dve", lambda e, p_=p_, nr=nr, n=n, ci=ci, nm=nm: e.tensor_scalar(out=sh[nm][0:nr, 0:n], in0=p_[0:nr, 1:n + 1], scalar1=omu[0:nr, ci:ci + 1],
                                                                                scalar2=None, op0=ALU.mult), r=[p_, omu], w=[sh[nm]])
                P.op("dve", lambda e, s_=s_, nr=nr, n=n, ci=ci, nm=nm: e.scalar_tensor_tensor(out=sh[nm][0:nr, 0:n], in0=s_[0:nr, 0:n], scalar=hmu[0:nr, ci:ci + 1],
                                                                                       in1=sh[nm][0:nr, 0:n], op0=ALU.mult, op1=ALU.add),
                     r=[s_, hmu, sh[nm]], w=[sh[nm]])
            P.op("act", lambda e, n=n: e.activation(out=twl[:, 0:n], in_=sh["wl"][:, 0:n], func=AF.Tanh), r=[sh["wl"]], w=[twl])
            P.op("act", lambda e, n=n: e.activation(out=sg0[:, 0:n], in_=sh["g0"][:, 0:n], func=AF.Sigmoid), r=[sh["g0"]], w=[sg0])
            P.op("act", lambda e, n=n: e.activation(out=sg1[:, 0:n], in_=sh["g1"][0:32, 0:n], func=AF.Sigmoid), r=[sh["g1"]], w=[sg1])
            for d in range(2):
                for cc in range(2):
                    ix = d * 2 + cc
                    pw = pp[ppi % 6]
                    ppi += 1
                    P.op("pe", lambda e, pw=pw, d=d, cc=cc, n=n: e.matmul(out=pw[:, 0:n], lhsT=wbt[d * 64:(d + 1) * 64, cc * 128:(cc + 1) * 128],
                                                                       rhs=twl[d * 64:(d + 1) * 64, 0:n], start=True, stop=True),
                         r=[wbt, twl], w=[pw])
                    P.op("act", lambda e, pw=pw, ix=ix, n=n: e.activation(out=lw[ix][:, 0:n], in_=pw[:, 0:n], func=AF.Sigmoid, bias=w0c[:, ix:ix + 1]),
                         r=[pw, w0c], w=[lw[ix]])
                    P.op("pool", lambda e, ix=ix, n=n: e.tensor_scalar(out=lw[ix][:, 0:n], in0=lw[ix][:, 0:n], scalar1=NEG_EM05, scalar2=None, op0=ALU.mult),
                         r=[lw[ix]], w=[lw[ix]])
                    store(7 + d, cc, lw[ix], n, t0)
                    pa = pp[ppi % 6]
                    ppi += 1
                    P.op("pe", lambda e, pa=pa, d=d, cc=cc, n=n: e.matmul(out=pa[:, 0:n], lhsT=abt[d * 64:(d + 1) * 64, cc * 128:(cc + 1) * 128],
                                                                       rhs=sh["al"][d * 64:(d + 1) * 64, 0:n], start=True, stop=True),
                         r=[abt, sh["al"]], w=[pa])
                    P.op("act", lambda e, pa=pa, ix=ix, n=n: e.activation(out=asg[ix][:, 0:n], in_=pa[:, 0:n], func=AF.Sigmoid, bias=a0c[:, ix:ix + 1]),
                         r=[pa, a0c], w=[asg[ix]])
            for cc in range(2):
                kx = sh["k%d" % cc]
                P.op("dve", lambda e, cc=cc, kx=kx, n=n: e.tensor_scalar(out=kk[cc][:, 0:n], in0=kx[:, 0:n], scalar1=kkc[:, cc:cc + 1], scalar2=None, op0=ALU.mult),
                     r=[kx, kkc], w=[kk[cc]])
                P.op("pool", lambda e, cc=cc, n=n: e.tensor_tensor(out=sq[:, 0:n], in0=kk[cc][:, 0:n], in1=kk[cc][:, 0:n], op=ALU.mult),
                     r=[kk[cc]], w=[sq])
                pn = pp[ppi % 6]
                ppi += 1
                P.op("pe", lambda e, pn=pn, n=n: e.matmul(out=pn[:, 0:n], lhsT=g.consts[:, C_BONES, :], rhs=sq[:, 0:n], start=True, stop=True),
                     r=[sq, g.consts], w=[pn])
                P.op("act", lambda e, pn=pn, n=n: e.activation(out=rn[:, 0:n], in_=pn[:, 0:n], func=AF.Sqrt), r=[pn], w=[rn])
                P.op("dve", lambda e, n=n: e.tensor_scalar(out=rn[:, 0:n], in0=rn[:, 0:n], scalar1=1e-12, scalar2=None, op0=ALU.max), r=[rn], w=[rn])
                P.op("dve", lambda e, n=n: e.reciprocal(out=rn[:, 0:n], in_=rn[:, 0:n]), r=[rn], w=[rn])
                P.op("dve", lambda e, cc=cc, n=n: e.tensor_tensor(out=kk[cc][:, 0:n], in0=kk[cc][:, 0:n], in1=rn[:, 0:n], op=ALU.mult),
                     r=[kk[cc], rn], w=[kk[cc]])
                store(4, cc, kk[cc], n, t0)
                store(0, cc, sh["r%d" % cc], n, t0)
                store(3, cc, sh["v%d" % cc], n, t0)
                for d in range(2):
                    ix = d * 2 + cc
                    P.op("dve", lambda e, ix=ix, cc=cc, n=n: e.tensor_scalar(out=tq[:, 0:n], in0=asg[ix][:, 0:n], scalar1=kac[:, cc:cc + 1],
                                                                         scalar2=omka[:, cc:cc + 1], op0=ALU.mult, op1=ALU.add),
                         r=[asg[ix], kac, omka], w=[tq])
                    P.op("dve", lambda e, ix=ix, kx=kx, n=n: e.tensor_tensor(out=kd[ix][:, 0:n], in0=tq[:, 0:n], in1=kx[:, 0:n], op=ALU.mult),
                         r=[tq, kx], w=[kd[ix]])
                    store(1 + d, cc, kd[ix], n, t0)
                    P.op("pool", lambda e, ix=ix, cc=cc, n=n: e.tensor_tensor(out=bd[ix][:, 0:n], in0=kk[cc][:, 0:n], in1=asg[ix][:, 0:n], op=ALU.mult),
                         r=[kk[cc], asg[ix]], w=[bd[ix]])
                    store(5 + d, cc, bd[ix], n, t0)
                pg = pp[ppi % 6]
                ppi += 1
                P.op("pe", lambda e, pg=pg, cc=cc, n=n: e.matmul(out=pg[:, 0:n], lhsT=gb0[:, cc * 128:(cc + 1) * 128], rhs=sg0[:, 0:n], start=True, stop=False),
                     r=[gb0, sg0], w=[pg])
                P.op("pe", lambda e, pg=pg, cc=cc, n=n: e.matmul(out=pg[:, 0:n], lhsT=gb1[:, cc * 128:(cc + 1) * 128], rhs=sg1[:, 0:n], start=False, stop=True),
                     r=[gb1, sg1], w=[pg])
                P.op("act", lambda e, pg=pg, cc=cc, n=n: e.activation(out=gt[cc][:, 0:n], in_=pg[:, 0:n], func=AF.Copy), r=[pg], w=[gt[cc]])
                store(9, cc, gt[cc], n, t0)
        P.barrier()
        P.emit()


def phase_rwscan(g, l):
    nc, I, S = g.nc, g.I, g.S
    with ExitStack() as es:
        def sb(name, shape, dt=F32):
            return es.enter_context(nc.sbuf_tensor("f%d_" % l + name, list(shape), dt))

        def psb(name, shape, dt=F32):
            return es.enter_context(nc.psum_tensor("f%d_" % l + name, list(shape), dt))
        ident = g.consts[:, C_ID, :]
        ybuf = [[sb("y%d%d" % (p, d), [128, NT], BF16) for d in range(2)] for p in range(2)]
        E64 = sb("E64", [128, 64])
        E64r = sb("E64r", [128, 64], F32R)
        U = [[None, None], [None, None]]
        for d in range(2):
            for p in range(2):
                u = Ctx()
                n = "%d%d" % (d, p)
                u.f = [sb("ld%d_" % i + n, [128, 128]) for i in range(6)]
                u.Lc = sb("Lc" + n, [128, 128])
                u.LC = sb("LC" + n, [128, 128])
                u.t1 = sb("t1" + n, [128, 128])
                u.tA = sb("tA" + n, [128, 128])
                u.tW = sb("tW" + n, [128, 128])
                u.eP = sb("eP" + n, [128, 128])
                u.eN = sb("eN" + n, [128, 128])
                u.eA = sb("eA" + n, [128, 128])
                u.eW = sb("eW" + n, [128, 128])
                u.WL = sb("WL" + n, [128, 2])
                u.ar = sb("ar" + n, [128, 256], F32R)
                u.at32 = sb("at32" + n, [128, 128])
                u.bt = sb("bt" + n, [128, 128], F32R)
                u.kt = sb("kt" + n, [128, 128], F32R)
                u.bW = sb("bW" + n, [128, 128])
                u.kW = sb("kW" + n, [128, 128])
                u.Dg = sb("Dg" + n, [128, 2, 64], F32R)
                u.TM = sb("TM" + n, [128, 4, 128], F32R)
                u.q = []
                if p == 1:
                    u.q = U[d][0].q
                    U[d][p] = u
                    continue
                for hh in range(2):
                    q = Ctx()
                    m = n + "%d" % hh
                    q.XTR = sb("XTR" + m, [128, 256], F32R)
                    q.KTR = sb("KTR" + m, [128, 256], F32R)
                    q.X = [sb("X%d_" % i + m, [128, 128], F32R) for i in range(2)]
                    q.XP = [sb("XP%d_" % i + m, [128, 256], F32R) for i in range(2)]
                    pass
                    q.Gs = sb("Gs" + m, [128, 64], F32R)
                    q.MAG = sb("MAG" + m, [128, 128], F32R)
                    q.Phi = sb("Phi" + m, [64, 2, 64])
                    q.Psi = sb("Psi" + m, [64, 2, 64])
                    q.RAT = sb("RAT" + m, [64, 128])
                    q.YCT = sb("YCT" + m, [64, 128])
                    u.q.append(q)
                U[d][p] = u
        ST = [[[sb("ST%d%d%d" % (h, d, i), [64, 64]) for i in range(2)] for d in range(2)] for h in range(4)]
        stpar = [[0, 0] for _ in range(4)]
        pq = [psb("pq%d" % i, [128, 512]) for i in range(4)]
        pTr = [psb("pTr%d" % i, [128, 4, 128]) for i in range(2)]
        pSq = [psb("pSq%d" % i, [64, 512]) for i in range(2)]
        P = Prog(nc)
        P.op("dve", lambda e: e.tensor_tensor(out=E64[:], in0=g.consts[:, C_ID, 0:64], in1=g.consts[:, C_ID, 64:128], op=ALU.add),
             r=[g.consts], w=[E64])
        P.op("dve", lambda e: e.tensor_copy(out=E64r[:], in_=E64[:]), r=[E64], w=[E64r])
        for h in range(4):
            for d in range(2):
                P.op("pool", lambda e, h=h, d=d: e.memset(ST[h][d][0][:], 0.0), w=[ST[h][d][0]])
        order_f = list(range(NTILE))
        order_b = [1, 0] + list(range(NTILE - 1, 1, -1))
        fidx = [[0, 1, 3, 4, 5, 7], [0, 2, 3, 4, 6, 8]]
        slotc = [0, 0, 0, 0]

        def slot(qi):
            sl = slotc[qi] % 4
            slotc[qi] += 1
            return sl

        def fm_part(step, p):
            units = [(0, order_f[step]), (1, order_b[step])]
            for (d, j) in units:
                u = U[d][p]
                for i6 in range(6):
                    P.dma("sync" if i6 % 2 == 0 else "pool", u.f[i6][:], S.rwf[fidx[d][i6], p * 128:(p + 1) * 128, j * 128:(j + 1) * 128],
                          w=[u.f[i6]])
                fr, fkd, fv, fkk, fbd, flw = u.f
                P.op("dve", lambda e, u=u, flw=flw: e.tensor_tensor_scan(out=u.Lc[:], data0=g.consts[:, C_RESET, :], data1=flw[:], initial=0.0,
                                                                       op0=ALU.mult, op1=ALU.add), r=[flw, g.consts], w=[u.Lc])
                totv = u.Lc[:].rearrange("p (c l) -> p c l", c=2)[:, :, 63:64]
                if d == 0:
                    LC = u.Lc
                else:
                    LC = u.LC
                    P.op("pool", lambda e, u=u, flw=flw: e.tensor_tensor(out=u.t1[:], in0=flw[:], in1=u.Lc[:], op=ALU.subtract),
                         r=[flw, u.Lc], w=[u.t1])
                    P.op("pool", lambda e, u=u, totv=totv: e.tensor_tensor(out=u.LC[:].rearrange("p (c l) -> p c l", c=2),
                                                                        in0=u.t1[:].rearrange("p (c l) -> p c l", c=2),
                                                                        in1=totv.to_broadcast([128, 2, 64]), op=ALU.add),
                         r=[u.t1, u.Lc], w=[u.LC])
                P.op("pool", lambda e, u=u, LC=LC, flw=flw: e.tensor_tensor(out=u.tA[:], in0=LC[:], in1=flw[:], op=ALU.subtract),
                     r=[LC, flw], w=[u.tA])
                P.op("pool", lambda e, u=u, LC=LC, totv=totv: e.tensor_tensor(out=u.tW[:].rearrange("p (c l) -> p c l", c=2),
                                                                           in0=totv.to_broadcast([128, 2, 64]),
                                                                           in1=LC[:].rearrange("p (c l) -> p c l", c=2), op=ALU.subtract),
                     r=[LC, u.Lc], w=[u.tW])
                P.op("act", lambda e, u=u, LC=LC: e.activation(out=u.eP[:], in_=LC[:], func=AF.Exp), r=[LC], w=[u.eP])
                P.op("act", lambda e, u=u, LC=LC: e.activation(out=u.eN[:], in_=LC[:], func=AF.Exp, scale=-1.0), r=[LC], w=[u.eN])
                P.op("act", lambda e, u=u: e.activation(out=u.eA[:], in_=u.tA[:], func=AF.Exp), r=[u.tA], w=[u.eA])
                P.op("act", lambda e, u=u: e.activation(out=u.eW[:], in_=u.tW[:], func=AF.Exp), r=[u.tW], w=[u.eW])
                P.op("act", lambda e, u=u, totv=totv: e.activation(out=u.WL[:].unsqueeze(2), in_=totv, func=AF.Exp), r=[u.Lc], w=[u.WL])
                P.op("dve", lambda e, u=u, fkk=fkk: e.scalar_tensor_tensor(out=u.at32[:], in0=fkk[:], scalar=-1.0, in1=u.eA[:],
                                                                         op0=ALU.mult, op1=ALU.mult), r=[fkk, u.eA], w=[u.at32])
                P.op("pool", lambda e, u=u: e.tensor_copy(out=u.ar[:, 0:128], in_=u.at32[:]), r=[u.at32], w=[(u.ar.name, 0)])
                P.op("pool", lambda e, u=u, fr=fr: e.tensor_tensor(out=u.ar[:, 128:256], in0=fr[:], in1=u.eP[:], op=ALU.mult),
                     r=[fr, u.eP], w=[(u.ar.name, 1)])
                P.op("dve", lambda e, u=u, fbd=fbd: e.tensor_tensor(out=u.bt[:], in0=fbd[:], in1=u.eN[:], op=ALU.mult), r=[fbd, u.eN], w=[u.bt])
                P.op("pool", lambda e, u=u, fkd=fkd: e.tensor_tensor(out=u.kt[:], in0=fkd[:], in1=u.eN[:], op=ALU.mult), r=[fkd, u.eN], w=[u.kt])
                P.op("dve", lambda e, u=u, fbd=fbd: e.tensor_tensor(out=u.bW[:], in0=fbd[:], in1=u.eW[:], op=ALU.mult), r=[fbd, u.eW], w=[u.bW])
                P.op("pool", lambda e, u=u, fkd=fkd: e.tensor_tensor(out=u.kW[:], in0=fkd[:], in1=u.eW[:], op=ALU.mult), r=[fkd, u.eW], w=[u.kW])
                for c in range(2):
                    P.op("pool", lambda e, u=u, c=c: e.tensor_scalar(out=u.Dg[:, c, :], in0=E64[:], scalar1=u.WL[:, c:c + 1], scalar2=None, op0=ALU.mult),
                         r=[E64, u.WL], w=[u.Dg])
                srcs = [(u.at32, 0, u.at32.name), (u.bW, None, u.bW.name), (u.kW, None, u.kW.name), (fv, None, fv.name)]
                for k4, (src, off, key) in enumerate(srcs):
                    in_ap = src[:, 0:128]
                    P.op("pe", lambda e, d=d, k4=k4, in_ap=in_ap: e.transpose(out=pTr[d][:, k4, :], in_=in_ap, identity=ident),
                         r=[key, g.consts], w=[pTr[d]])
                P.op("act", lambda e, u=u, d=d: e.activation(out=u.TM[:], in_=pTr[d][:], func=AF.Copy), r=[pTr[d]], w=[u.TM])

        def rest_part(step, p):
            units = [(0, order_f[step]), (1, order_b[step])]
            probs = []
            for (d, j) in units:
                for hh in range(2):
                    probs.append((d, j, hh, U[d][p], U[d][p].q[hh], d * 2 + hh))
            for (d, j, hh, u, q, qi) in probs:
                pb = hh * 64
                P.op("pe", lambda e, u=u, pb=pb, qi=qi: e.matmul(out=pq[qi][:, 0:256], lhsT=u.bt[pb:pb + 64, :], rhs=u.ar[pb:pb + 64, :], start=True, stop=True),
                     r=[u.bt, (u.ar.name, 0), (u.ar.name, 1)], w=[("pqb", qi), ("pqb", qi)], rows=pb)
                P.op("pe", lambda e, u=u, pb=pb, qi=qi: e.matmul(out=pq[qi][:, 256:384], lhsT=u.ar[pb:pb + 64, 0:128], rhs=u.bt[pb:pb + 64, :], start=True, stop=True),
                     r=[u.bt, (u.ar.name, 0)], w=[("pqb", qi)], rows=pb)
            for (d, j, hh, u, q, qi) in probs:
                m2 = (C_MS_IT if d == 0 else C_MS_IT_B)
                mti = (C_MS_TI if d == 0 else C_MS_IT)
                P.op("dve", lambda e, q=q, qi=qi, m2=m2: e.tensor_tensor(out=q.XP[0][:, 0:128], in0=pq[qi][:, 0:128], in1=g.consts[:, m2, :], op=ALU.mult),
                     r=[("pqb", qi), g.consts], w=[(q.XP[0].name, 0)])
                P.op("dve", lambda e, q=q, qi=qi, m2=m2: e.tensor_tensor(out=q.XTR[:, 128:256], in0=pq[qi][:, 128:256], in1=g.consts[:, m2 + 1, :], op=ALU.mult),
                     r=[("pqb", qi), g.consts], w=[q.XTR])
                P.op("dve", lambda e, q=q, qi=qi, mti=mti: e.tensor_tensor(out=q.X[0][:], in0=pq[qi][:, 256:384], in1=g.consts[:, mti, :], op=ALU.mult),
                     r=[("pqb", qi), g.consts], w=[q.X[0]])
                P.op("pool", lambda e, q=q: e.tensor_copy(out=q.XP[0][:, 128:256], in_=ident), r=[g.consts], w=[(q.XP[0].name, 1)])
            for (d, j, hh, u, q, qi) in probs:
                pb = hh * 64
                P.op("pe", lambda e, u=u, pb=pb, qi=qi: e.matmul(out=pq[qi][:, 0:256], lhsT=u.kt[pb:pb + 64, :], rhs=u.ar[pb:pb + 64, :], start=True, stop=True),
                     r=[u.kt, (u.ar.name, 0), (u.ar.name, 1)], w=[("pqb", qi), ("pqb", qi)], rows=pb)
            for (d, j, hh, u, q, qi) in probs:
                m2 = (C_MS_IT if d == 0 else C_MS_IT_B)
                P.op("dve", lambda e, q=q, qi=qi, m2=m2: e.tensor_tensor(out=q.KTR[:], in0=pq[qi][:, 0:256],
                                                                      in1=g.consts[:, m2:m2 + 2, :].rearrange("p a b -> p (a b)"), op=ALU.mult),
                     r=[("pqb", qi), ("pqb", qi), g.consts], w=[q.KTR])
            for lev in range(6):
                last = (lev == 5)
                for (d, j, hh, u, q, qi) in probs:
                    Xc = q.X[lev % 2]
                    XPc = q.XP[lev % 2]
                    P.op("pe", lambda e, qi=qi, Xc=Xc, XPc=XPc: e.matmul(out=pq[qi][:, 0:256], lhsT=Xc[:], rhs=XPc[:, 0:256], start=True, stop=True),
                         r=[Xc, (XPc.name, 0), (XPc.name, 1)], w=[("pqb", qi)])
                    if not last:
                        P.op("pe", lambda e, qi=qi, Xc=Xc, XPc=XPc: e.matmul(out=pq[qi][:, 256:384], lhsT=XPc[:, 0:128], rhs=Xc[:], start=True, stop=True),
                             r=[Xc, (XPc.name, 0)], w=[("pqb", qi)])
                for (d, j, hh, u, q, qi) in probs:
                    Xn = q.X[(lev + 1) % 2]
                    XPc = q.XP[lev % 2]
                    XPn = q.XP[(lev + 1) % 2]
                    if not last:
                        P.op("act", lambda e, qi=qi, XPn=XPn: e.activation(out=XPn[:, 0:128], in_=pq[qi][:, 0:128], func=AF.Copy),
                             r=[("pqb", qi)], w=[(XPn.name, 0)])
                        P.op("act", lambda e, qi=qi, Xn=Xn: e.activation(out=Xn[:], in_=pq[qi][:, 256:384], func=AF.Copy), r=[("pqb", qi)], w=[Xn])
                    P.op("dve", lambda e, qi=qi, XPc=XPc, XPn=XPn: e.tensor_tensor(out=XPn[:, 128:256], in0=pq[qi][:, 128:256], in1=XPc[:, 128:256], op=ALU.add),
                         r=[("pqb", qi), (XPc.name, 1)], w=[(XPn.name, 1)])
            for (d, j, hh, u, q, qi) in probs:
                cb = hh * 64
                P.op("pe", lambda e, qi=qi, q=q, u=u, cb=cb: e.matmul(out=pq[qi][:, 128:192], lhsT=q.KTR[:, 0:128], rhs=u.TM[:, 3, cb:cb + 64], start=True, stop=True),
                     r=[q.KTR, u.TM], w=[("pqb", qi)])
            for (d, j, hh, u, q, qi) in probs:
                P.op("act", lambda e, qi=qi, q=q: e.activation(out=q.Gs[:], in_=pq[qi][:, 128:192], func=AF.Copy), r=[("pqb", qi)], w=[q.Gs])
            for (d, j, hh, u, q, qi) in probs:
                cb = hh * 64
                PTf = q.XP[0]
                P.op("pe", lambda e, qi=qi, PTf=PTf, u=u, cb=cb: e.matmul(out=pq[qi][:, 256:320], lhsT=PTf[:, 128:256], rhs=u.TM[:, 0, cb:cb + 64], start=True, stop=True),
                     r=[(PTf.name, 1), u.TM], w=[("pqb", qi)])
                P.op("pe", lambda e, qi=qi, PTf=PTf, q=q: e.matmul(out=pq[qi][:, 320:384], lhsT=PTf[:, 128:256], rhs=q.Gs[:], start=True, stop=True),
                     r=[(PTf.name, 1), q.Gs], w=[("pqb", qi)])
            for (d, j, hh, u, q, qi) in probs:
                P.op("act", lambda e, qi=qi, q=q: e.activation(out=q.MAG[:], in_=pq[qi][:, 256:384], func=AF.Copy), r=[("pqb", qi)], w=[q.MAG])
            for (d, j, hh, u, q, qi) in probs:
                cb = hh * 64
                pb = hh * 64
                for c in range(2):
                    rb = c * 64
                    P.op("pe", lambda e, qi=qi, q=q, u=u, rb=rb, cb=cb, c=c: e.matmul(out=pq[qi][0:64, 384 + c * 64:448 + c * 64], lhsT=q.MAG[rb:rb + 64, 0:64],
                                                                                   rhs=u.TM[rb:rb + 64, 1, cb:cb + 64], start=True, stop=False),
                         r=[q.MAG, u.TM], w=[("pqb", qi)], rows=rb)
                    P.op("pe", lambda e, qi=qi, u=u, pb=pb, c=c: e.matmul(out=pq[qi][0:64, 384 + c * 64:448 + c * 64], lhsT=E64r[pb:pb + 64, :],
                                                                        rhs=u.Dg[pb:pb + 64, c, :], start=False, stop=True),
                         r=[E64r, u.Dg], w=[("pqb", qi)], rows=pb)
                    P.op("pe", lambda e, qi=qi, q=q, u=u, rb=rb, cb=cb, c=c: e.matmul(out=pq[qi][0:64, c * 64:c * 64 + 64], lhsT=u.TM[rb:rb + 64, 1, cb:cb + 64],
                                                                                   rhs=q.MAG[rb:rb + 64, 64:128], start=True, stop=False),
                         r=[q.MAG, u.TM], w=[("pqb", qi)], rows=rb)
                    P.op("pe", lambda e, qi=qi, u=u, rb=rb, cb=cb, c=c: e.matmul(out=pq[qi][0:64, c * 64:c * 64 + 64], lhsT=u.TM[rb:rb + 64, 2, cb:cb + 64],
                                                                              rhs=u.TM[rb:rb + 64, 3, cb:cb + 64], start=False, stop=True),
                         r=[u.TM], w=[("pqb", qi)], rows=rb)
                P.op("pe", lambda e, qi=qi, q=q: e.matmul(out=pq[qi][0:64, 128:256], lhsT=q.MAG[:, 0:64], rhs=q.XTR[:, 128:256], start=True, stop=False),
                     r=[q.MAG, q.XTR], w=[("pqb", qi)])
                P.op("pe", lambda e, qi=qi, u=u, pb=pb: e.matmul(out=pq[qi][0:64, 128:256], lhsT=E64r[pb:pb + 64, :], rhs=u.ar[pb:pb + 64, 128:256], start=False, stop=True),
                     r=[E64r, (u.ar.name, 1)], w=[("pqb", qi)], rows=pb)
                P.op("pe", lambda e, qi=qi, q=q: e.matmul(out=pq[qi][0:64, 256:384], lhsT=q.MAG[:, 64:128], rhs=q.XTR[:, 128:256], start=True, stop=False),
                     r=[q.MAG, q.XTR], w=[("pqb", qi)])
                P.op("pe", lambda e, qi=qi, q=q, u=u, cb=cb: e.matmul(out=pq[qi][0:64, 256:384], lhsT=u.TM[:, 3, cb:cb + 64], rhs=q.KTR[:, 128:256], start=False, stop=True),
                     r=[u.TM, q.KTR], w=[("pqb", qi)])
            for (d, j, hh, u, q, qi) in probs:
                P.op("act", lambda e, qi=qi, q=q: e.activation(out=q.Phi[:].rearrange("p c k -> p (c k)"), in_=pq[qi][0:64, 384:512], func=AF.Copy),
                     r=[("pqb", qi)], w=[q.Phi])
                P.op("dve", lambda e, qi=qi, q=q: e.tensor_copy(out=q.Psi[:].rearrange("p c k -> p (c k)"), in_=pq[qi][0:64, 0:128]),
                     r=[("pqb", qi)], w=[q.Psi])
                P.op("act", lambda e, qi=qi, q=q: e.activation(out=q.RAT[:], in_=pq[qi][0:64, 128:256], func=AF.Copy), r=[("pqb", qi)], w=[q.RAT])
                P.op("dve", lambda e, qi=qi, q=q: e.tensor_copy(out=q.YCT[:], in_=pq[qi][0:64, 256:384]), r=[("pqb", qi)], w=[q.YCT])
            for ci in range(2):
                for (d, j, hh, u, q, qi) in probs:
                    c = ci if d == 0 else 1 - ci
                    h = p * 2 + hh
                    sp = stpar[h][d]
                    Sc = ST[h][d][sp]
                    Sn = ST[h][d][1 - sp]
                    stpar[h][d] = 1 - sp
                    psq = pSq[ci]
                    yc0 = qi * 128
                    P.op("pe", lambda e, psq=psq, yc0=yc0, Sc=Sc, q=q, c=c: e.matmul(out=psq[:, yc0:yc0 + 64], lhsT=Sc[:], rhs=q.RAT[:, c * 64:(c + 1) * 64], start=True, stop=False),
                         r=[Sc, q.RAT], w=[("sqb", ci)])
                    P.op("pe", lambda e, psq=psq, yc0=yc0, q=q, c=c: e.matmul(out=psq[:, yc0:yc0 + 64], lhsT=E64[0:64, :], rhs=q.YCT[:, c * 64:(c + 1) * 64], start=False, stop=True),
                         r=[E64, q.YCT], w=[("sqb", ci)])
                    P.op("pe", lambda e, psq=psq, yc0=yc0, Sc=Sc, q=q, c=c: e.matmul(out=psq[:, yc0 + 64:yc0 + 128], lhsT=q.Phi[:, c, :], rhs=Sc[:], start=True, stop=True),
                         r=[Sc, q.Phi], w=[("sqb", ci)])
                    tcol = j * 128 + c * 64
                    yb = ybuf[p][d]
                    P.op("act", lambda e, psq=psq, yc0=yc0, yb=yb, hh=hh, tcol=tcol: e.activation(out=yb[hh * 64:(hh + 1) * 64, tcol:tcol + 64], in_=psq[:, yc0:yc0 + 64], func=AF.Copy),
                         r=[("sqb", ci)], w=[(yb.name, j)])
                    P.op("dve", lambda e, psq=psq, yc0=yc0, Sn=Sn, q=q, c=c: e.tensor_tensor(out=Sn[:], in0=psq[:, yc0 + 64:yc0 + 128], in1=q.Psi[:, c, :], op=ALU.add),
                         r=[("sqb", ci), q.Psi], w=[Sn])

        nsteps = NTILE if g.rwsteps is None else g.rwsteps
        groups = [(st_, p_) for st_ in range(nsteps) for p_ in range(2)]
        fm_part(*groups[0])
        for gi, (st_, p_) in enumerate(groups):
            if gi + 1 < len(groups):
                fm_part(*groups[gi + 1])
            rest_part(st_, p_)
        SEG = 512
        prm = sb("prm", [128, 2, 3])
        ld = [sb("o_ld%d" % i, [128, SEG]) for i in range(5)]
        ysum = sb("ysum", [128, SEG])
        yc_ = sb("yc_", [128, SEG])
        sq = sb("osq", [128, SEG])
        rstd = sb("rstd", [128, SEG])
        prod = sb("prod", [128, SEG])
        ob = [sb("oob%d" % i, [128, SEG], BF16) for i in range(2)]
        for p in range(2):
            for k3, src in enumerate((I.r_k, I.ln_w, I.ln_b)):
                P.dma("sync", prm[:, p, k3:k3 + 1], src[g.wi(l), p * 128:(p + 1) * 128, :], w=[prm], sem="prm")
        segs = ([(0, CTX)] if l == 0 else []) + [(CTX + i * SEG, SEG) for i in range(8)]
        oi = 0
        for (t0, n) in segs:
            for p in range(2):
                for k5, idx in enumerate((0, 1, 2, 3, 9)):
                    P.dma("sync" if k5 % 2 == 0 else "pool", ld[k5][:, 0:n], S.rwf[idx, p * 128:(p + 1) * 128, t0:t0 + n], w=[ld[k5]])
                ykeys = [(ybuf[p][dd].name, jj) for dd in range(2) for jj in range(t0 // 128, (t0 + n) // 128)]
                P.op("pool", lambda e, p=p, t0=t0, n=n: e.tensor_tensor(out=ysum[:, 0:n], in0=ybuf[p][0][:, t0:t0 + n], in1=ybuf[p][1][:, t0:t0 + n], op=ALU.add),
                     r=ykeys, w=[ysum])
                pm = pq[0]
                P.op("pe", lambda e, pm=pm, n=n: e.matmul(out=pm[:, 0:n], lhsT=g.consts[:, C_BONES, :], rhs=ysum[:, 0:n], start=True, stop=True),
                     r=[ysum, g.consts], w=[("pqb", 0), ("pqb", 0), ("pqb", 0), ("pqb", 0)])
                P.op("dve", lambda e, pm=pm, n=n: e.scalar_tensor_tensor(out=yc_[:, 0:n], in0=pm[:, 0:n], scalar=-1.0 / 64, in1=ysum[:, 0:n], op0=ALU.mult, op1=ALU.add),
                     r=[("pqb", 0), ("pqb", 0), ("pqb", 0), ("pqb", 0), ysum], w=[yc_])
                P.op("pool", lambda e, n=n: e.tensor_tensor(out=sq[:, 0:n], in0=yc_[:, 0:n], in1=yc_[:, 0:n], op=ALU.mult), r=[yc_], w=[sq])
                pv = pq[1]
                P.op("pe", lambda e, pv=pv, n=n: e.matmul(out=pv[:, 0:n], lhsT=g.consts[:, C_BONES, :], rhs=sq[:, 0:n], start=True, stop=True),
                     r=[sq, g.consts], w=[("pqb", 1), ("pqb", 1), ("pqb", 1), ("pqb", 1)])
                P.op("dve", lambda e, pv=pv, n=n: e.tensor_scalar(out=rstd[:, 0:n], in0=pv[:, 0:n], scalar1=1.0 / 64, scalar2=64e-5, op0=ALU.mult, op1=ALU.add),
                     r=[("pqb", 1), ("pqb", 1), ("pqb", 1), ("pqb", 1)], w=[rstd])
                P.op("act", lambda e, n=n: e.activation(out=rstd[:, 0:n], in_=rstd[:, 0:n], func=AF.Sqrt), r=[rstd], w=[rstd])
                P.op("dve", lambda e, n=n: e.reciprocal(out=rstd[:, 0:n], in_=rstd[:, 0:n]), r=[rstd], w=[rstd])
                P.op("dve", lambda e, n=n: e.tensor_tensor(out=yc_[:, 0:n], in0=yc_[:, 0:n], in1=rstd[:, 0:n], op=ALU.mult), r=[yc_, rstd], w=[yc_])
                P.op("dve", lambda e, n=n, p=p: e.tensor_scalar(out=yc_[:, 0:n], in0=yc_[:, 0:n], scalar1=prm[:, p, 1:2], scalar2=prm[:, p, 2:3], op0=ALU.mult, op1=ALU.add),
                     r=[yc_, prm], w=[yc_])
                P.op("pool", lambda e, n=n: e.tensor_tensor(out=prod[:, 0:n], in0=ld[1][:, 0:n], in1=ld[2][:, 0:n], op=ALU.add), r=[ld[1], ld[2]], w=[prod])
                P.op("pool", lambda e, n=n: e.tensor_tensor(out=prod[:, 0:n], in0=prod[:, 0:n], in1=ld[0][:, 0:n], op=ALU.mult), r=[prod, ld[0]], w=[prod])
                P.op("pool", lambda e, n=n, p=p: e.tensor_scalar(out=prod[:, 0:n], in0=prod[:, 0:n], scalar1=prm[:, p, 0:1], scalar2=0.5, op0=ALU.mult, op1=ALU.mult),
                     r=[prod, prm], w=[prod])
                pbn = pq[2]
                P.op("pe", lambda e, pbn=pbn, n=n: e.matmul(out=pbn[:, 0:n], lhsT=g.consts[:, C_BONES, :], rhs=prod[:, 0:n], start=True, stop=True),
                     r=[prod, g.consts], w=[("pqb", 2), ("pqb", 2), ("pqb", 2), ("pqb", 2)])
                P.op("dve", lambda e, pbn=pbn, n=n: e.tensor_tensor(out=sq[:, 0:n], in0=pbn[:, 0:n], in1=ld[3][:, 0:n], op=ALU.mult),
                     r=[("pqb", 2), ("pqb", 2), ("pqb", 2), ("pqb", 2), ld[3]], w=[sq])
                P.op("dve", lambda e, n=n: e.tensor_tensor(out=yc_[:, 0:n], in0=yc_[:, 0:n], in1=sq[:, 0:n], op=ALU.add), r=[yc_, sq], w=[yc_])
                o = ob[oi % 2]
                oi += 1
                P.op("dve", lambda e, n=n, o=o: e.tensor_tensor(out=o[:, 0:n], in0=yc_[:, 0:n], in1=ld[4][:, 0:n], op=ALU.mult), r=[yc_, ld[4]], w=[o])
                P.dma("sync", S.mixT[768 + p * 128:768 + (p + 1) * 128, t0:t0 + n], o[:, 0:n], r=[o], sem=("oob", oi % 2))
        P.barrier()
        P.emit()


def phase_wout(g, l):
    nc, I, S = g.nc, g.I, g.S
    with ExitStack() as es:
        def sb(name, shape, dt=F32):
            return es.enter_context(nc.sbuf_tensor("w%d_" % l + name, list(shape), dt))

        def psb(name, shape, dt=F32):
            return es.enter_context(nc.psum_tensor("w%d_" % l + name, list(shape), dt))
        wo = sb("wo", [128, 8, D], BF16)
        rw = sb("rw", [128, 8, NE])
        bc = [[sb("bc%d%d" % (j, k), [128, D]) for k in range(3)] for j in range(2)]
        mt = [sb("mt%d" % i, [128, 8, 128], BF16) for i in range(2)]
        xt = [sb("xt%d" % i, [128, D]) for i in range(2)]
        x1 = [sb("x1%d" % i, [128, D]) for i in range(2)]
        junk = sb("junk", [128, D])
        ss = [sb("ss%d" % i, [128, 1]) for i in range(2)]
        h2f = [sb("h2f%d" % i, [128, D]) for i in range(2)]
        h2b = [sb("h2b%d" % i, [128, D], BF16) for i in range(2)]
        h2T = sb("h2T", [128, 8, 128])
        lg = sb("lg", [128, NE])
        mx = sb("mx", [128, 1])
        sm = sb("sm", [128, 1])
        aff = sb("aff", [128, NE])
        pO = [psb("pO%d" % i, [128, 512]) for i in range(2)]
        pT = [psb("pT%d" % i, [128, 4, 128]) for i in range(2)]
        pL_ = psb("pL", [128, 512])
        pL = pL_[:, 0:NE]
        pA_ = psb("pA", [NE, 512])
        pA = pA_[:, 0:128]
        P = Prog(nc)
        P.dma("pool", wo[:], I.w_out[g.wi(l)].rearrange("(kc p) n -> p kc n", p=128), w=[wo])
        P.dma("sync", rw[:], I.router[g.wi(l)].rearrange("(kc p) n -> p kc n", p=128), w=[rw])
        for j in range(2):
            for k, mi in enumerate((2, 3, 4)):
                P.dma("sync", bc[j][k][:], S.modv[g.wi(l), j, mi:mi + 1, :].to_broadcast([128, D]), w=[bc[j][k]])
        tiles = list(range(NTILE)) if l == 0 else list(range(2, NTILE))
        for i in tiles:
            b = i % 2
            j = 1 if i < 2 else 0
            if i < 2:
                src = (I.ctx if l == g.first else S.xcres)[i * 128:(i + 1) * 128, :]
                dst = S.xcres[i * 128:(i + 1) * 128, :]
                h2dst = S.h2c[i * 128:(i + 1) * 128, :]
            else:
                src = (I.x if l == g.first else S.xres)[(i - 2) * 128:(i - 1) * 128, :]
                dst = (g.out if l == g.last else S.xres)[(i - 2) * 128:(i - 1) * 128, :]
                h2dst = S.h2l[(i - 2) * 128:(i - 1) * 128, :]
            P.dma("sync", mt[b][:], S.mixT[:, i * 128:(i + 1) * 128].rearrange("(kc p) t -> p kc t", p=128), w=[mt[b]])
            P.dma("sync", xt[b][:], src, w=[xt[b]])
            for half in range(2):
                for kc in range(8):
                    P.op("pe", lambda e, half=half, kc=kc, b=b: e.matmul(out=pO[half][:], lhsT=mt[b][:, kc, :], rhs=wo[:, kc, half * 512:(half + 1) * 512],
                                                                       start=(kc == 0), stop=(kc == 7)), r=[mt[b], wo], w=[pO[half]])
                P.op("dve", lambda e, half=half, b=b, j=j: e.tensor_tensor(out=x1[b][:, half * 512:(half + 1) * 512], in0=pO[half][:],
                                                                          in1=bc[j][0][:, half * 512:(half + 1) * 512], op=ALU.mult),
                     r=[pO[half], bc[j][0]], w=[(x1[b].name, half)])
            P.op("pool", lambda e, b=b: e.tensor_tensor(out=x1[b][:], in0=x1[b][:], in1=xt[b][:], op=ALU.add),
                 r=[xt[b]], w=[(x1[b].name, 0), (x1[b].name, 1)])
            P.dma("pool", dst, x1[b][:], r=[(x1[b].name, 0), (x1[b].name, 1)], sem=("x1st", b))
            P.op("act", lambda e, b=b: e.activation(out=junk[:], in_=x1[b][:], func=AF.Square, accum_out=ss[b][:]), r=[(x1[b].name, 0), (x1[b].name, 1)], w=[junk, ss[b]])
            P.op("dve", lambda e, b=b: e.tensor_scalar(out=ss[b][:], in0=ss[b][:], scalar1=1.0 / D, scalar2=1e-6, op0=ALU.mult, op1=ALU.add),
                 r=[ss[b]], w=[ss[b]])
            P.op("act", lambda e, b=b: e.activation(out=ss[b][:], in_=ss[b][:], func=AF.Sqrt), r=[ss[b]], w=[ss[b]])
            P.op("dve", lambda e, b=b: e.reciprocal(out=ss[b][:], in_=ss[b][:]), r=[ss[b]], w=[ss[b]])
            P.op("dve", lambda e, b=b, j=j: e.scalar_tensor_tensor(out=h2f[b][:], in0=x1[b][:], scalar=ss[b][:, 0:1], in1=bc[j][1][:], op0=ALU.mult, op1=ALU.mult),
                 r=[(x1[b].name, 0), (x1[b].name, 1), ss[b], bc[j][1]], w=[h2f[b]])
            P.op("pool", lambda e, b=b, j=j: e.tensor_tensor(out=h2f[b][:], in0=h2f[b][:], in1=bc[j][2][:], op=ALU.add), r=[h2f[b], bc[j][2]], w=[h2f[b]])
            P.op("act", lambda e, b=b: e.activation(out=h2b[b][:], in_=h2f[b][:], func=AF.Copy), r=[h2f[b]], w=[h2b[b]])
            P.dma("sync", h2dst, h2b[b][:], r=[h2b[b]], sem=("h2st", b))
            for half in range(2):
                for k4 in range(4):
                    kc = half * 4 + k4
                    P.op("pe", lambda e, half=half, k4=k4, kc=kc, b=b: e.transpose(out=pT[half][:, k4, :], in_=h2f[b][:, kc * 128:(kc + 1) * 128],
                                                                                identity=g.consts[:, C_ID, :]), r=[h2f[b], g.consts], w=[pT[half]])
                if half == 0:
                    P.op("act", lambda e, half=half: e.activation(out=h2T[:, 0:4, :], in_=pT[0][:], func=AF.Copy), r=[pT[0]], w=[("h2T", 0)])
                else:
                    P.op("dve", lambda e, half=half: e.tensor_copy(out=h2T[:, 4:8, :], in_=pT[1][:]), r=[pT[1]], w=[("h2T", 1)])
            for kc in range(8):
                P.op("pe", lambda e, kc=kc: e.matmul(out=pL, lhsT=h2T[:, kc, :], rhs=rw[:, kc, :], start=(kc == 0), stop=(kc == 7)),
                     r=[("h2T", 0), ("h2T", 1), rw], w=["pL"])
            P.op("dve", lambda e: e.tensor_copy(out=lg[:], in_=pL), r=["pL"], w=[lg])
            P.op("dve", lambda e: e.tensor_reduce(out=mx[:], in_=lg[:], axis=AX.X, op=ALU.max), r=[lg], w=[mx])
            P.op("dve", lambda e: e.tensor_scalar(out=mx[:], in0=mx[:], scalar1=-1.0, scalar2=None, op0=ALU.mult), r=[mx], w=[mx])
            P.op("act", lambda e: e.activation(out=aff[:], in_=lg[:], func=AF.Exp, bias=mx[:, 0:1], accum_out=sm[:]), r=[lg, mx], w=[aff, sm])
            P.op("dve", lambda e: e.reciprocal(out=sm[:], in_=sm[:]), r=[sm], w=[sm])
            P.op("dve", lambda e: e.tensor_scalar(out=aff[:], in0=aff[:], scalar1=sm[:, 0:1], scalar2=None, op0=ALU.mult), r=[aff, sm], w=[aff])
            P.op("pe", lambda e: e.transpose(out=pA, in_=aff[:], identity=g.consts[:, C_ID, :]), r=[aff, g.consts], w=["pA"])
            P.op("act", lambda e, i=i: e.activation(out=g.affT[:, i * 128:(i + 1) * 128], in_=pA, func=AF.Copy), r=["pA"], w=[("affT", i)])
        P.barrier()
        P.emit()


def phase_moe(g, l):
    nc, I, S = g.nc, g.I, g.S
    with ExitStack() as es:
        def sb(name, shape, dt=F32):
            return es.enter_context(nc.sbuf_tensor("m%d_" % l + name, list(shape), dt))

        def psb(name, shape, dt=F32):
            return es.enter_context(nc.psum_tensor("m%d_" % l + name, list(shape), dt))
        work = sb("work", [NE, SEQ])
        vals = sb("vals", [NE, CAP_L])
        idxu = sb("idxu", [NE, CAP_L], U32)
        idxf = sb("idxf", [NE, CAP_L])
        idxT = sb("idxT", [128, 4, NE], I32)
        gT = sb("gT", [128, 4, NE])
        gt2 = [sb("gt2_%d" % j, [128, D]) for j in range(2)]
        wgt = [sb("wg%d" % i, [128, 8, D], BF16) for i in range(2)]
        wut = [sb("wu%d" % i, [128, 8, D], BF16) for i in range(2)]
        wdt = [sb("wd%d" % i, [128, 8, D], BF16) for i in range(2)]
        xs = [sb("xs%d" % i, [128, D], BF16) for i in range(2)]
        xsT = sb("xsT", [128, 8, 512], BF16)
        hidT = sb("hidT", [128, 8, 512], BF16)
        sg = [sb("sg%d" % i, [128, 512]) for i in range(2)]
        y = [sb("y%d" % i, [128, D]) for i in range(2)]
        pX = [psb("pX%d" % i, [128, 8, 128], BF16) for i in range(2)]
        pGs = [psb("pG%d" % i, [128, 512]) for i in range(2)]
        pUs = [psb("pU%d" % i, [128, 512]) for i in range(2)]
        pY = [psb("pY%d" % i, [128, 512]) for i in range(2)]
        pTi_t = pY[1]
        pTi = pY[1][:, :].rearrange("p (a b) -> p a b", a=32)
        P = Prog(nc)
        for j in range(2):
            P.dma("sync", gt2[j][:], S.modv[g.wi(l), j, 5:6, :].to_broadcast([128, D]), w=[gt2[j]])
        sets = [(0, CTX, SEQ, CAP_L, S.h2l, (g.out if l == g.last else S.xres))]
        if l == 0:
            sets.append((1, 0, CTX, CAP_C, S.h2c, S.xcres))
        wi = 0
        xi = 0
        yi = 0
        for (j, a0, N, cap, h2src, dest) in sets:
            nch = (cap + 127) // 128
            npc = min(cap, 128)
            akeys = [("affT", i) for i in range(a0 // 128, (a0 + N) // 128)]
            P.op("pool", lambda e, a0=a0, N=N: e.tensor_copy(out=work[:, 0:N], in_=g.affT[:, a0:a0 + N]), r=akeys, w=[work])
            for r8 in range(cap // 8):
                P.op("dve", lambda e, r8=r8, N=N: e.max(out=vals[:, r8 * 8:(r8 + 1) * 8], in_=work[:, 0:N]), r=[work], w=[vals])
                P.op("dve", lambda e, r8=r8, N=N: e.max_index(out=idxu[:, r8 * 8:(r8 + 1) * 8], in_max=vals[:, r8 * 8:(r8 + 1) * 8], in_values=work[:, 0:N]),
                     r=[work, vals], w=[idxu])
                P.op("dve", lambda e, r8=r8, N=N: e.match_replace(out=work[:, 0:N], in_to_replace=vals[:, r8 * 8:(r8 + 1) * 8], in_values=work[:, 0:N], imm_value=-1.0),
                     r=[work, vals], w=[work])
            P.op("dve", lambda e, cap=cap: e.tensor_copy(out=idxf[:, 0:cap], in_=idxu[:, 0:cap]), r=[idxu], w=[idxf])
            for ch in range(nch):
                P.op("pe", lambda e, ch=ch, npc=npc: e.transpose(out=pTi[0:npc, 0, :], in_=idxf[:, ch * 128:ch * 128 + npc], identity=g.consts[0:NE, C_ID, 0:NE]),
                     r=[idxf, g.consts], w=[pTi_t])
                P.op("pe", lambda e, ch=ch, npc=npc: e.transpose(out=pTi[0:npc, 1, :], in_=vals[:, ch * 128:ch * 128 + npc], identity=g.consts[0:NE, C_ID, 0:NE]),
                     r=[vals, g.consts], w=[pTi_t])
                P.op("dve", lambda e, ch=ch, npc=npc: e.tensor_copy(out=idxT[0:npc, ch, :], in_=pTi[0:npc, 0, :]), r=[pTi_t], w=[idxT])
                P.op("dve", lambda e, ch=ch, npc=npc: e.tensor_copy(out=gT[0:npc, ch, :], in_=pTi[0:npc, 1, :]), r=[], w=[gT, pTi_t])
            ncol = nch * npc
            for ex in range(NE):
                wb_ = wi % 2
                wi += 1
                P.dma("pool", wgt[wb_][:], I.wg[g.wi(l), ex].rearrange("(kc p) n -> p kc n", p=128), w=[wgt[wb_]])
                P.dma("pool", wut[wb_][:], I.wu[g.wi(l), ex].rearrange("(kc p) n -> p kc n", p=128), w=[wut[wb_]])
                P.dma("pool", wdt[wb_][:], I.wd[g.wi(l), ex].rearrange("(kc p) n -> p kc n", p=128), w=[wdt[wb_]])
                for ch in range(nch):
                    xb = xs[xi % 2]
                    xi += 1
                    P.dma_fn("pool", lambda e, xb=xb, ch=ch, ex=ex, npc=npc, h2src=h2src: e.indirect_dma_start(
                        out=xb[0:npc, :], out_offset=None, in_=h2src[:, :],
                        in_offset=bass.IndirectOffsetOnAxis(ap=idxT[0:npc, ch, ex:ex + 1], axis=0)),
                        r=[idxT], w=[xb], sem=("xg", xb.name))
                    for half in range(2):
                        for k4 in range(4):
                            kc = half * 4 + k4
                            P.op("pe", lambda e, half=half, k4=k4, kc=kc, xb=xb, npc=npc: e.transpose(out=pX[half][:, k4, 0:npc], in_=xb[0:npc, kc * 128:(kc + 1) * 128],
                                                                                                 identity=g.identb[0:npc, 0:npc]), r=[xb, g.identb], w=[pX[half]])
                        if half == 0:
                            P.op("act", lambda e, ch=ch, npc=npc: e.activation(out=xsT[:, 0:4, ch * 128:ch * 128 + npc], in_=pX[0][:, 0:4, 0:npc], func=AF.Copy),
                                 r=[pX[0]], w=[("xsT", 0)])
                        else:
                            P.op("dve", lambda e, ch=ch, npc=npc: e.tensor_copy(out=xsT[:, 4:8, ch * 128:ch * 128 + npc], in_=pX[1][:, 0:4, 0:npc]),
                                 r=[pX[1]], w=[("xsT", 1)])
                for fc in range(8):
                    pG = pGs[fc % 2]
                    pU = pUs[fc % 2]
                    for kc in range(8):
                        P.op("pe", lambda e, fc=fc, kc=kc, wb_=wb_, ncol=ncol, pG=pG: e.matmul(out=pG[:, 0:ncol], lhsT=wgt[wb_][:, kc, fc * 128:(fc + 1) * 128], rhs=xsT[:, kc, 0:ncol],
                                                                                     start=(kc == 0), stop=(kc == 7)), r=[wgt[wb_], ("xsT", 0), ("xsT", 1)], w=[pG])
                    for kc in range(8):
                        P.op("pe", lambda e, fc=fc, kc=kc, wb_=wb_, ncol=ncol, pU=pU: e.matmul(out=pU[:, 0:ncol], lhsT=wut[wb_][:, kc, fc * 128:(fc + 1) * 128], rhs=xsT[:, kc, 0:ncol],
                                                                                     start=(kc == 0), stop=(kc == 7)), r=[wut[wb_], ("xsT", 0), ("xsT", 1)], w=[pU])
                    s_ = sg[fc % 2]
                    P.op("act", lambda e, s_=s_, ncol=ncol, pG=pG: e.activation(out=s_[:, 0:ncol], in_=pG[:, 0:ncol], func=AF.Silu), r=[pG], w=[s_])
                    P.op("dve", lambda e, s_=s_, fc=fc, ncol=ncol, pU=pU: e.tensor_tensor(out=hidT[:, fc, 0:ncol], in0=pU[:, 0:ncol], in1=s_[:, 0:ncol], op=ALU.mult),
                         r=[pU, s_], w=[("hidT", fc)])
                hk = [("hidT", fc) for fc in range(8)]
                for ch in range(nch):
                    yb = y[yi % 2]
                    yi += 1
                    for half in range(2):
                        for fc in range(8):
                            P.op("pe", lambda e, half=half, fc=fc, ch=ch, wb_=wb_, npc=npc: e.matmul(out=pY[half][0:npc, :], lhsT=hidT[:, fc, ch * 128:ch * 128 + npc],
                                                                                                 rhs=wdt[wb_][:, fc, half * 512:(half + 1) * 512], start=(fc == 0), stop=(fc == 7)),
                                 r=hk + [wdt[wb_]], w=[pY[half]])
                        P.op("dve", lambda e, half=half, yb=yb, ch=ch, ex=ex, npc=npc, j=j: e.scalar_tensor_tensor(
                            out=yb[0:npc, half * 512:(half + 1) * 512], in0=pY[half][0:npc, :], scalar=gT[0:npc, ch, ex:ex + 1],
                            in1=gt2[j][0:npc, half * 512:(half + 1) * 512], op0=ALU.mult, op1=ALU.mult), r=[pY[half], gT, gt2[j]], w=[(yb.name, half)])
                    P.dma_fn("pool", lambda e, yb=yb, ch=ch, ex=ex, npc=npc, dest=dest: e.indirect_dma_start(
                        out=dest[:, :], out_offset=bass.IndirectOffsetOnAxis(ap=idxT[0:npc, ch, ex:ex + 1], axis=0),
                        in_=yb[0:npc, :], in_offset=None, compute_op=ALU.add),
                        r=[(yb.name, 0), (yb.name, 1), idxT], w=[("dest", j)], sem=("ysc", j))
        P.barrier()
        P.emit()


def phase_zero_mix(g, l):
    nc, S = g.nc, g.S
    with ExitStack() as es:
        z = es.enter_context(nc.sbuf_tensor("z%d_z" % l, [128, NT], BF16))
        P = Prog(nc)
        P.op("pool", lambda e: e.memset(z[:], 0.0), w=[z])
        for r in range(2, 8):
            P.dma("sync", S.mixT[r * 128:(r + 1) * 128, :], z[:], r=[z], sem="zst")
        P.barrier()
        P.emit()


def prep_inputs(inputs):
    f = lambda a: np.ascontiguousarray(np.asarray(a, dtype=np.float32))
    x = f(inputs["x"])
    c = f(inputs["c"])
    ctx = f(inputs["ctx"])
    c_ctx = f(inputs["c_ctx"])
    shared = {
        "ada_w": f(inputs["ada_w"]),
        "ada_b": f(inputs["ada_b"]).reshape(2, 1, 6 * D),
        "norm1_g": f(inputs["norm1_g"]).reshape(2, 1, D),
        "norm2_g": f(inputs["norm2_g"]).reshape(2, 1, D),
        "w_in": f(inputs["w_in"]),
        "w_out": f(inputs["w_out"]),
        "conv_wT": f(np.transpose(f(inputs["conv_w"]), (0, 2, 1))),
        "q_norm_g": f(inputs["q_norm_g"]).reshape(2, 1, 64),
        "k_norm_g": f(inputs["k_norm_g"]).reshape(2, 1, 64),
        "rw_mu": f(inputs["rw_mu"]).reshape(2, 1184, 1),
        "rw_w0": f(inputs["rw_w0"]).reshape(2, 512, 1),
        "rw_w_b": f(inputs["rw_w_b"]).reshape(2, 128, 256),
        "rw_a0": f(inputs["rw_a0"]).reshape(2, 512, 1),
        "rw_a_b": f(inputs["rw_a_b"]).reshape(2, 128, 256),
        "rw_g_b": f(inputs["rw_g_b"]),
        "rw_k_k": f(inputs["rw_k_k"]).reshape(2, 256, 1),
        "rw_k_a": f(inputs["rw_k_a"]).reshape(2, 256, 1),
        "rw_r_k": f(inputs["rw_r_k"]).reshape(2, 256, 1),
        "rw_ln_w": f(inputs["rw_ln_w"]).reshape(2, 256, 1),
        "rw_ln_b": f(inputs["rw_ln_b"]).reshape(2, 256, 1),
        "router_w": f(inputs["router_w"]),
        "exp_w_gate": f(inputs["exp_w_gate"]),
        "exp_w_up": f(inputs["exp_w_up"]),
        "exp_w_down": f(inputs["exp_w_down"]),
        "consts": make_consts(),
    }
    t = np.arange(SEQ)
    row = (t // 64).astype(np.float32)
    col = (t % 64).astype(np.float32)
    inv = (10000.0 ** (-np.arange(0, 32, 2, dtype=np.float32) / 32)).astype(np.float32)
    ang = np.concatenate([row[:, None] * inv, col[:, None] * inv], axis=-1).astype(np.float32)
    shared["cs_tab"] = np.ascontiguousarray(np.concatenate([np.cos(ang), np.sin(ang)], axis=-1).astype(np.float32))
    maps = []
    for b in range(x.shape[0]):
        m = dict(shared)
        m["x"] = x[b]
        m["ctx"] = ctx[b]
        c2 = np.stack([c[b], c_ctx], axis=-1)
        m["c2T"] = np.ascontiguousarray(c2.reshape(8, 128, 2).transpose(1, 0, 2))
        maps.append(m)
    return maps


_NC_CACHE = {}

W_KEYS = ["ada_w", "ada_b", "norm1_g", "norm2_g", "w_in", "w_out", "conv_wT", "q_norm_g", "k_norm_g", "rw_mu", "rw_w0", "rw_w_b",
          "rw_a0", "rw_a_b", "rw_g_b", "rw_k_k", "rw_k_a", "rw_r_k", "rw_ln_w", "rw_ln_b", "router_w",
          "exp_w_gate", "exp_w_up", "exp_w_down"]


def kernel(**inputs):
    maps = prep_inputs(inputs)
    if "nc" not in _NC_CACHE:
        _NC_CACHE["nc"] = build(layers=[0, 1])
    nc = _NC_CACHE["nc"]
    res = run_bass_kernel_spmd(nc, maps, core_ids=list(range(8)))
    return np.stack([np.asarray(r["out"], dtype=np.float32) for r in res.results], axis=0)
```
